# Optimizing a Trainium2 kernel written in Bass

```python
import math
import jax, jax.numpy as jnp
from jax import lax
import numpy as np

D_MODEL = 1024
BATCH = 32
SEQ = 2048
DEPTH = 2

N_MEM = 256
HEAD_DIM = 64
SB_HEADS = 6
DSA_HEADS = 6
MEM_HEADS = 4
SB_W = SB_HEADS * HEAD_DIM
DSA_W = DSA_HEADS * HEAD_DIM
MEM_W = MEM_HEADS * HEAD_DIM
MIX_W = SB_W + DSA_W + MEM_W
KV_RANK = 128
IDX_HEADS = 8
IDX_DIM = 32
TOPK_MAX = 256
N_BUCKETS = 32
MAX_DISTANCE = 128
BLOCK_Q = 128
RMS_EPS = 1e-6
IN_SPLITS = (SB_W, SB_W, SB_W, SB_W,
             DSA_W, KV_RANK, DSA_W, IDX_HEADS * IDX_DIM, IDX_DIM, IDX_HEADS,
             MEM_W, MEM_W)
IN_COLS = sum(IN_SPLITS)

kernel_name = "hymba_sb_dsa_mem_hybrid"


def rmsnorm(x, g):
    xf = x.astype(jnp.float32)
    y = xf * lax.rsqrt(jnp.mean(xf * xf, axis=-1, keepdims=True) + RMS_EPS)
    return (y * g.astype(jnp.float32)).astype(x.dtype)


def t5_causal_bucket(rel):
    n = jnp.maximum(rel, 0)
    max_exact = N_BUCKETS // 2
    nf = jnp.maximum(n, 1).astype(jnp.float32)
    large = max_exact + (jnp.log(nf / max_exact) / math.log(MAX_DISTANCE / max_exact)
                         * (N_BUCKETS - max_exact)).astype(jnp.int32)
    large = jnp.minimum(large, N_BUCKETS - 1)
    return jnp.where(n < max_exact, n, large)


def stick_breaking_attention(q, k, v):
    S = q.shape[1]
    scale = q.shape[-1] ** -0.5
    outs = []
    for start in range(0, S, BLOCK_Q):
        end = start + BLOCK_Q
        z = jnp.einsum('bqhd,bkhd->bhqk', q[:, start:end], k[:, :end]).astype(jnp.float32) * scale
        t_pos = jnp.arange(start, end)[:, None]
        s_pos = jnp.arange(end)[None, :]
        causal = s_pos < t_pos
        log_1m = jnp.where(causal, jax.nn.log_sigmoid(-z), 0.0)
        suffix = lax.cumsum(log_1m, axis=3, reverse=True) - log_1m
        a = jnp.where(causal, jnp.exp(jax.nn.log_sigmoid(z) + suffix), 0.0)
        outs.append(jnp.einsum('bhqk,bkhd->bqhd', a.astype(v.dtype), v[:, :end]))
    return jnp.concatenate(outs, axis=1)


def dsa_sparse_attention(q, c_kv, w_uk, w_uv, iq, ik, iw, rel_bias, topk):
    S = q.shape[1]
    scale = q.shape[-1] ** -0.5
    q_lat = jnp.einsum('bshd,rhd->bshr', q, w_uk)
    gather = jax.vmap(lambda c, i: c[i])
    outs = []
    for start in range(0, S, BLOCK_Q):
        end = start + BLOCK_Q
        kv_len = min(S, max(end, topk))
        t_pos = jnp.arange(start, end)
        s_pos = jnp.arange(kv_len)
        dots = jnp.einsum('bqhd,bkd->bqhk', iq[:, start:end], ik[:, :kv_len])
        score = jnp.einsum('bqh,bqhk->bqk', iw[:, start:end], jax.nn.relu(dots)).astype(jnp.float32)
        score = jnp.where(s_pos[None, None, :] <= t_pos[None, :, None], score, -jnp.inf)
        _, idx = lax.top_k(score, topk)
        rel = t_pos[None, :, None] - idx
        valid = rel >= 0
        c_sel = gather(c_kv[:, :kv_len], idx)
        logits = jnp.einsum('bqhr,bqkr->bhqk', q_lat[:, start:end], c_sel).astype(jnp.float32) * scale
        bias = rel_bias.astype(jnp.float32)[t5_causal_bucket(rel)]
        logits = logits + jnp.transpose(bias, (0, 3, 1, 2))
        logits = jnp.where(valid[:, None], logits, -jnp.inf)
        p = jax.nn.softmax(logits, axis=-1)
        o_lat = jnp.einsum('bhqk,bqkr->bqhr', p.astype(c_sel.dtype), c_sel)
        outs.append(jnp.einsum('bqhr,rhd->bqhd', o_lat, w_uv))
    return jnp.concatenate(outs, axis=1)


def memory_attention(q, mem_k, mem_v):
    logits = jnp.einsum('bshd,bmhd->bhsm', q, mem_k).astype(jnp.float32) * (q.shape[-1] ** -0.5)
    p = jax.nn.softmax(logits, axis=-1)
    return jnp.einsum('bhsm,bmhd->bshd', p.astype(mem_v.dtype), mem_v)


def hybrid_layer(x, mem, pre_g, post_g, w_in, w_uk, w_uv, kv_g, w_mem_kv, w_out, rel_bias, topk):
    B, S, _ = x.shape
    M = mem.shape[1]
    h = rmsnorm(x, pre_g)
    proj = h @ w_in
    split_points = [int(p) for p in np.cumsum(IN_SPLITS)[:-1]]
    (sb_q, sb_k, sb_v, sb_gate,
     dsa_q, dsa_ckv, dsa_gate, idx_q, idx_k, idx_w,
     mem_q, mem_gate) = jnp.split(proj, split_points, axis=-1)
    heads = lambda t, n: t.reshape(B, S, n, HEAD_DIM)

    sb = stick_breaking_attention(heads(sb_q, SB_HEADS), heads(sb_k, SB_HEADS), heads(sb_v, SB_HEADS))
    sb = sb.reshape(B, S, SB_W) * jax.nn.silu(sb_gate)

    c_kv = rmsnorm(dsa_ckv, kv_g)
    iq = idx_q.reshape(B, S, IDX_HEADS, IDX_DIM)
    iw = idx_w * ((IDX_HEADS * IDX_DIM) ** -0.5)
    ds = dsa_sparse_attention(heads(dsa_q, DSA_HEADS), c_kv, w_uk, w_uv, iq, idx_k, iw, rel_bias, topk)
    ds = ds.reshape(B, S, DSA_W) * jax.nn.silu(dsa_gate)

    mem_k, mem_v = jnp.split(mem @ w_mem_kv, 2, axis=-1)
    mo = memory_attention(heads(mem_q, MEM_HEADS),
                          mem_k.reshape(B, M, MEM_HEADS, HEAD_DIM),
                          mem_v.reshape(B, M, MEM_HEADS, HEAD_DIM))
    mo = mo.reshape(B, S, MEM_W) * jax.nn.silu(mem_gate)

    y = jnp.concatenate([sb, ds, mo], axis=-1) @ w_out
    return x + rmsnorm(y, post_g)


def setup_inputs(seed: int = 0) -> dict:
    key = jax.random.key(seed)
    ks = jax.random.split(key, 12)
    f32 = jnp.float32
    x = jax.random.normal(ks[0], (BATCH, SEQ, D_MODEL), f32)
    mem = jax.random.normal(ks[1], (BATCH, N_MEM, D_MODEL), f32)
    pre_norm_g = 1.0 + 0.05 * jax.random.normal(ks[2], (DEPTH, D_MODEL), f32)
    post_norm_g = 1.0 + 0.05 * jax.random.normal(ks[3], (DEPTH, D_MODEL), f32)
    w_in = jax.random.normal(ks[4], (DEPTH, D_MODEL, IN_COLS), f32) * D_MODEL ** -0.5
    w_uk = jax.random.normal(ks[5], (DEPTH, KV_RANK, DSA_HEADS, HEAD_DIM), f32) * KV_RANK ** -0.5
    w_uv = jax.random.normal(ks[6], (DEPTH, KV_RANK, DSA_HEADS, HEAD_DIM), f32) * KV_RANK ** -0.5
    kv_norm_g = 1.0 + 0.05 * jax.random.normal(ks[7], (DEPTH, KV_RANK), f32)
    w_mem_kv = jax.random.normal(ks[8], (DEPTH, D_MODEL, 2 * MEM_W), f32) * D_MODEL ** -0.5
    w_out = jax.random.normal(ks[9], (DEPTH, MIX_W, D_MODEL), f32) * MIX_W ** -0.5
    rel_bias = 0.5 * jax.random.normal(ks[10], (N_BUCKETS, DSA_HEADS), f32)
    return {"x": x, "mem": mem, "pre_norm_g": pre_norm_g, "post_norm_g": post_norm_g,
            "w_in": w_in, "w_uk": w_uk, "w_uv": w_uv, "kv_norm_g": kv_norm_g,
            "w_mem_kv": w_mem_kv, "w_out": w_out, "rel_bias": rel_bias}


def reference(x, mem, pre_norm_g, post_norm_g, w_in, w_uk, w_uv, kv_norm_g, w_mem_kv, w_out, rel_bias):
    seq = x.shape[1]
    topk = min(TOPK_MAX, seq // 4)
    for layer in range(DEPTH):
        x = hybrid_layer(x, mem, pre_norm_g[layer], post_norm_g[layer], w_in[layer],
                         w_uk[layer], w_uv[layer], kv_norm_g[layer], w_mem_kv[layer],
                         w_out[layer], rel_bias, topk)
    return x
```

```python
import math
import numpy as np
import ml_dtypes
import concourse.bass as bass
import concourse.mybir as mybir
from concourse.bass_utils import run_bass_kernel_spmd

F32 = mybir.dt.float32
BF16 = mybir.dt.bfloat16
AF = mybir.ActivationFunctionType
ALU = mybir.AluOpType
AX = mybir.AxisListType

D = 1024
NMEM = 256
TOPK = 256
NBIS = 16
EPS = 1e-6
NEG = -30000.0
N_CORES = 8
DBG_STAGE = 99
DBG_SUB = 99
SKEW = 1

SM_GCOL, SM_IW, SM_WUK, SM_WUV, SM_KVG, SM_B31, SM_POSTG, SM_TB = 0, 8, 72, 456, 840, 968, 974, 1998
NSM = 1998 + 6 * 2 * 128
CB_ID, CB_TRI, CB_ONES, CB_PENS, CB_PEND, CB_EKB, CB_SEL = 0, 128, 256, 384, 512, 640, 896
NCB = 896 + 16 * 128
CF_NEGB, CF_P2A, CF_P2B = 0, 128, 144
NCF = 160


class Buf:
    __slots__ = ("name", "w", "r", "excl")

    def __init__(self, name, excl=False):
        self.name = name
        self.w = None
        self.r = {}
        self.excl = excl


class Sched:
    ENGS = ("pe", "act", "dve", "pool", "sp")

    def __init__(self, nc):
        self.nc = nc
        self.ops = {e: [] for e in self.ENGS}
        self.sems = {}
        self.cnt = {}
        self.waited = {e: {} for e in self.ENGS}
        self._ctx = []
        for e in self.ENGS:
            self._newsem("E_" + e)

    def _newsem(self, key):
        g = self.nc.semaphore(key)
        h = g.__enter__()
        self._ctx.append(g)
        self.sems[key] = h
        self.cnt[key] = 0
        return key

    def dma_slot(self, name):
        return self._newsem("D_" + name)

    def _deps(self, e, reads, writes):
        deps = {}

        def add(ev):
            if ev is None:
                return
            k, v = ev
            if deps.get(k, 0) < v:
                deps[k] = v
        for b in reads:
            add(b.w)
        for b in writes:
            add(b.w)
            for k, v in b.r.items():
                add((k, v))
        waits = []
        mykey = "E_" + e
        for k, v in deps.items():
            if k == mykey and e in ("pe", "sp"):
                continue
            if self.waited[e].get(k, 0) >= v:
                continue
            self.waited[e][k] = v
            waits.append((k, v))
        return waits

    def _record(self, ev, reads, writes):
        k, v = ev
        for b in reads:
            if b.r.get(k, 0) < v:
                b.r[k] = v
        for b in writes:
            b.w = ev
            b.r = {}

    def op(self, e, fn, reads=(), writes=()):
        ex = [b for b in reads if b.excl]
        if ex:
            writes = list(writes) + ex
        waits = self._deps(e, reads, writes)
        k = "E_" + e
        self.cnt[k] += 1
        ev = (k, self.cnt[k])
        self.ops[e].append((waits, fn, (k, 1)))
        self._record(ev, reads, writes)
        return ev

    def dma(self, fn, slot, reads=(), writes=(), e="sp"):
        waits = self._deps(e, reads, writes)
        self.cnt[slot] += 16
        ev = (slot, self.cnt[slot])
        self.ops[e].append((waits, fn, (slot, 16)))
        self._record(ev, reads, writes)
        return ev

    def final_wait(self, e, bufs):
        waits = self._deps(e, bufs, bufs)
        self.ops[e].append((waits, None, None))

    def emit(self):
        nc = self.nc
        needed = {}
        for e in self.ENGS:
            for waits, fn, inc in self.ops[e]:
                for k, v in waits:
                    if k.startswith("E_"):
                        needed.setdefault(k, set()).add(v)
        rank = {k: {v: i + 1 for i, v in enumerate(sorted(vs))} for k, vs in needed.items()}
        with nc.Block() as block:
            def run(ename):
                def body(eng):
                    seq = 0
                    mykey = "E_" + ename
                    myrank = rank.get(mykey, {})
                    for waits, fn, inc in self.ops[ename]:
                        for k, v in waits:
                            eng.wait_ge(self.sems[k], rank[k][v] if k.startswith("E_") else v)
                        if fn is None:
                            continue
                        inst = fn(eng)
                        if inc[0] == mykey:
                            seq += 1
                            if seq in myrank:
                                inst.then_inc(self.sems[mykey], 1)
                        else:
                            inst.then_inc(self.sems[inc[0]], inc[1])
                return body
            block.tensor(run("pe"))
            block.scalar(run("act"))
            block.vector(run("dve"))
            block.gpsimd(run("pool"))
            block.sync(run("sp"))

    def close(self):
        for g in reversed(self._ctx):
            g.__exit__(None, None, None)


def build_program(S_TOK, NB, layer_of_unit, batch_of_unit, chain):
    NQ = S_TOK // 512
    NBLK = S_TOK // 128
    L = max(layer_of_unit) + 1
    nc = bass.Bass("TRN2", target_bir_lowering=False)
    x_d = nc.dram_tensor("x", [NB, S_TOK, D], F32, kind="ExternalInput").ap()
    mem_d = nc.dram_tensor("mem", [NB, NMEM, D], F32, kind="ExternalInput").ap()
    wF_d = nc.dram_tensor("wF", [L, 26, 128, 1024], F32, kind="ExternalInput").ap()
    wO_d = nc.dram_tensor("wO", [L, 8, 128, 1024], F32, kind="ExternalInput").ap()
    wM_d = nc.dram_tensor("wM", [L, 4, 128, 1024], F32, kind="ExternalInput").ap()
    wsm_d = nc.dram_tensor("wsm", [L, 128, NSM], F32, kind="ExternalInput").ap()
    cf_d = nc.dram_tensor("cf32", [128, NCF], F32, kind="ExternalInput").ap()
    cb_d = nc.dram_tensor("cbf", [128, NCB], BF16, kind="ExternalInput").ap()
    out_d = nc.dram_tensor("out", [NB, S_TOK, D], F32, kind="ExternalOutput").ap()
    need_scr = any(c == "scr" for c in chain)
    scr_d = nc.dram_tensor("xscr", [NB, S_TOK, D], F32, kind="Internal").ap() if need_scr else None

    S = Sched(nc)
    ctx = []

    def sb(name, shape, dt):
        g = nc.sbuf_tensor(name, shape, dt)
        h = g.__enter__()
        ctx.append(g)
        return h

    psum = []
    PB = []
    for i in range(8):
        g = nc.psum_tensor(f"ps{i}", [128, 512], F32)
        psum.append(g.__enter__())
        ctx.append(g)
        PB.append(Buf(f"ps{i}", excl=True))
    free_banks = list(range(8))

    def bget():
        return free_banks.pop(0)

    def bput(i):
        free_banks.append(i)

    cb = sb("cb", [128, NCB], BF16); CBb = Buf("cb")
    cf = sb("cf", [128, NCF], F32); CFb = Buf("cf")
    ident = cb[:, CB_ID:CB_ID + 128]
    triM8 = cb[:, CB_TRI:CB_TRI + 128]
    ones = cb[:, CB_ONES:CB_ONES + 128]
    penS = cb[:, CB_PENS:CB_PENS + 128]
    penD = cb[:, CB_PEND:CB_PEND + 128]

    def ekb(kb):
        return cb[:, CB_EKB + kb * 16:CB_EKB + (kb + 1) * 16]

    def selM8(kb):
        return cb[0:16, CB_SEL + kb * 128:CB_SEL + (kb + 1) * 128]
    negb = cf[:, CF_NEGB:CF_NEGB + 128]
    p2a = cf[:, CF_P2A:CF_P2A + 16]
    p2b = cf[:, CF_P2B:CF_P2B + 16]

    sbkT = sb("sbkT", [128, 3, S_TOK], BF16); SBK = [Buf(f"sbk{q}") for q in range(NQ)]
    sbv = sb("sbv", [128, NBLK, 384], BF16); SBV = [Buf(f"sbv{q}") for q in range(NQ)]
    ckv = sb("ckv", [128, NBLK, 128], BF16); CKV = [Buf(f"ckv{q}") for q in range(NQ)]
    ckvT = sb("ckvT", [128, S_TOK], BF16); CKVT = [Buf(f"ckvT{q}") for q in range(NQ)]
    ikT4 = sb("ikT4", [128, S_TOK], BF16); IKT = [Buf(f"ikT{q}") for q in range(NQ)]
    sbq = sb("sbq", [128, 3, 512], BF16); SBQ = Buf("sbq")
    sbg = sb("sbg", [128, 3, 512], BF16); SBG = Buf("sbg")
    dq = sb("dq", [128, 3, 512], BF16); DQ = Buf("dq")
    dg = sb("dg", [128, 3, 512], BF16); DG = Buf("dg")
    iq = sb("iq", [128, 2, 512], BF16); IQ = Buf("iq")
    mq = sb("mq", [128, 2, 512], BF16); MQ = Buf("mq")
    mg = sb("mg", [128, 2, 512], BF16); MG = Buf("mg")
    idxw = sb("idxw", [128, 32], F32); IDXW = Buf("idxw")
    hT = sb("hT", [128, 8, 512], BF16); HT = Buf("hT")
    hnmix = sb("hnmix", [128, 4096], BF16); HNMIX = Buf("hnmix")
    xc = sb("xc", [128, 4096], F32); XC = Buf("xc")
    NWB = 3
    wst = [sb(f"wst{i}", [128, 1024], F32) for i in range(NWB)]; WST = [Buf(f"wst{i}") for i in range(NWB)]
    wbf = [sb(f"wbf{i}", [128, 1024], BF16) for i in range(NWB)]; WBF = [Buf(f"wbf{i}") for i in range(NWB)]
    big16 = sb("big16", [128, 8192], BF16); BIG = [Buf(f"big{r}") for r in range(16)]
    gcol = sb("gcol", [128, 8], F32); kvg = sb("kvg", [128, 128], F32); b31 = sb("b31", [128, 6], F32)
    postg = sb("postg", [128, 1024], F32)
    SMF = Buf("smallf32")
    wiw = sb("wiw", [128, 64], BF16); wuk = sb("wuk", [128, 384], BF16); wuv = sb("wuv", [128, 384], BF16)
    tb8 = sb("tb8", [128, 1536], BF16)
    SMB = Buf("smallbf")
    memT = sb("memT", [128, 8, 256], BF16); MEMT = Buf("memT")
    memk = sb("memk", [128, 2, 256], BF16); MEMK = Buf("memk")
    memv = sb("memv", [128, 2, 256], BF16); MEMV = Buf("memv")
    membf = sb("membf", [128, 2, 1024], BF16); MEMBF = Buf("membf")
    NT = 3
    etile = [sb(f"et{i}", [128, 512], BF16) for i in range(NT)]; ET = [Buf(f"et{i}") for i in range(NT)]
    atile = [sb(f"at{i}", [128, 512], BF16) for i in range(NT)]; AT = [Buf(f"at{i}") for i in range(NT)]
    cshi = sb("cshi", [16, 512], BF16); cslo = sb("cslo", [16, 512], BF16); CS = Buf("cs")
    score = sb("score", [128, S_TOK], F32); SCORE = Buf("score")
    junk = sb("junk", [128, S_TOK], BF16); JUNK = Buf("junk")
    pen = sb("pen", [128, 4, S_TOK], BF16); PEN = [Buf(f"pen{i}") for i in range(4)]
    NR = 4
    rt = [sb(f"rt{i}", [128, 512], BF16) for i in range(NR)]; RT = [Buf(f"rt{i}") for i in range(NR)]
    dgt = [sb(f"dgt{i}", [128, 8, 128], BF16) for i in range(2)]; DGT = [Buf(f"dgt{i}") for i in range(2)]
    ptile = [sb(f"pt{i}", [128, 512], BF16) for i in range(NT)]; PT = [Buf(f"pt{i}") for i in range(NT)]
    qlat = [sb(f"ql{i}", [128, 512], BF16) for i in range(2)]; QL = [Buf(f"ql{i}") for i in range(2)]
    recf = sb("recf", [128, 512], F32); RECF = Buf("recf")
    onb = sb("onb", [128, 512], BF16); ONB = Buf("onb")
    tmpf = sb("tmpf", [128, 1024], F32); TMPF = Buf("tmpf")
    sm = sb("smalls", [128, 64], F32)
    SMS = {n: Buf("sm_" + n) for n in ("ss", "rs", "ssk", "rsk", "bis", "ssy")}
    steps = sb("steps", [128, 32], F32); STEPS = Buf("steps")
    ss = sm[:, 0:4]; rs = sm[:, 4:8]; ssk = sm[:, 8:12]; rsk = sm[:, 12:16]
    mx = sm[:, 16:17]; mn = sm[:, 17:18]; thr = sm[:, 18:19]; rng = sm[:, 19:20]; cnt = sm[:, 20:21]; dd = sm[:, 21:22]
    ssy = sm[:, 24:26]; ssy2 = sm[:, 26:27]; rsy = sm[:, 27:28]

    sl_c = S.dma_slot("const"); sl_c2 = S.dma_slot("const2"); sl_x = S.dma_slot("x"); sl_o = S.dma_slot("o"); sl_m = S.dma_slot("mem")
    sl_s = S.dma_slot("small"); sl_w = [S.dma_slot(f"w{i}") for i in range(NWB)]
    OUTB = Buf("outdram")
    SCRB = {}

    cnt_rr = {"w": 0, "e": 0, "a": 0, "r": 0, "p": 0, "q": 0, "d": 0, "ev": 0}

    def rr(key, n):
        v = cnt_rr[key] % n
        cnt_rr[key] += 1
        return v

    S.dma(lambda e: e.dma_start(out=cb[:], in_=cb_d[:, :]), sl_c, writes=[CBb])
    S.dma(lambda e: e.dma_start(out=cf[:], in_=cf_d[:, :]), sl_c2, writes=[CFb])

    def wchunk(src):
        k = rr("w", NWB)
        S.dma(lambda e: e.dma_start(out=wst[k][:], in_=src), sl_w[k], writes=[WST[k]])
        S.op("act", lambda e: e.activation(out=wbf[k][:], in_=wst[k][:], func=AF.Copy), reads=[WST[k]], writes=[WBF[k]])
        return wbf[k], WBF[k]

    def evac_copy(out_ap, in_ap, reads, writes):
        if rr("ev", 2) == 0:
            S.op("act", lambda e: e.activation(out=out_ap, in_=in_ap, func=AF.Copy), reads=reads, writes=writes)
        else:
            S.op("dve", lambda e: e.tensor_copy(out=out_ap, in_=in_ap), reads=reads, writes=writes)

    def rms_scale(src_ss, dst_rs, n, inv_n, B_ss, B_rs):
        S.op("act", lambda e: e.activation(out=dst_rs, in_=src_ss, func=AF.Sqrt, scale=inv_n, bias=EPS),
             reads=[B_ss], writes=[B_rs])
        S.op("dve", lambda e: e.reciprocal(out=dst_rs, in_=dst_rs), reads=[B_rs], writes=[B_rs])

    def unit(u):
        l = layer_of_unit[u]
        b = batch_of_unit[u]
        src_x = x_d if chain[u] == "in" else scr_d
        later = any(batch_of_unit[v] == b for v in range(u + 1, len(layer_of_unit)))
        dst_x = scr_d if later else out_d
        if later and b not in SCRB:
            SCRB[b] = [Buf(f"scr{b}_{q}") for q in range(NQ)]

        S.dma(lambda e: e.dma_start(out=xc[:, 0:NSM], in_=wsm_d[l, :, :]), sl_s, writes=[XC])
        for (dst, off, n) in ((gcol, SM_GCOL, 8), (kvg, SM_KVG, 128), (b31, SM_B31, 6), (postg, SM_POSTG, 1024)):
            S.op("dve", lambda e, dst=dst, off=off, n=n: e.tensor_copy(out=dst[:, 0:n], in_=xc[:, off:off + n]),
                 reads=[XC], writes=[SMF])
        for (dst, off, n) in ((wiw, SM_IW, 64), (wuk, SM_WUK, 384), (wuv, SM_WUV, 384)):
            S.op("dve", lambda e, dst=dst, off=off, n=n: e.tensor_copy(out=dst[:, 0:n], in_=xc[:, off:off + n]),
                 reads=[XC], writes=[SMB])
        S.op("dve", lambda e: e.tensor_scalar(out=tb8[:, :], in0=xc[:, SM_TB:SM_TB + 1536], scalar1=8.0, scalar2=None,
                                              op0=ALU.mult), reads=[XC], writes=[SMB])
        S.dma(lambda e: e.dma_start(out=xc[:, 0:2048].rearrange("p (j d) -> p j d", j=2),
                                    in_=mem_d[b, :, :].rearrange("(j p) d -> p j d", p=128)), sl_m, writes=[XC])
        S.op("dve", lambda e: e.tensor_copy(out=membf[:].rearrange("p j d -> p (j d)"), in_=xc[:, 0:2048]),
             reads=[XC], writes=[MEMBF])
        for c2 in range(4):
            bk = bget()
            pb = psum[bk][:].bitcast(BF16)
            for cc in range(2):
                c = 2 * c2 + cc
                for j in range(2):
                    S.op("pe", lambda e, c=c, j=j, cc=cc, pb=pb: e.transpose(
                        pb[:, cc * 256 + j * 128:cc * 256 + (j + 1) * 128], membf[:, j, c * 128:(c + 1) * 128], ident),
                        reads=[MEMBF, CBb], writes=[PB[bk]])
            evac_copy(memT[:, 2 * c2:2 * c2 + 2, :], pb[:, 0:512].rearrange("p (c m) -> p c m", c=2), [PB[bk]], [MEMT])
            bput(bk)
        pend = [wchunk(wM_d[l, 0, :, :])]
        for g4 in range(4):
            if g4 + 1 < 4:
                pend.append(wchunk(wM_d[l, g4 + 1, :, :]))
            w, W = pend.pop(0)
            w3 = w[:].rearrange("p (c g) -> p c g", c=8)
            bk = bget()
            if g4 < 2:
                for c in range(8):
                    S.op("pe", lambda e, c=c, w3=w3, bk=bk: e.matmul(psum[bk][:, 0:256], lhsT=w3[:, c, :], rhs=memT[:, c, :],
                                                                    start=(c == 0), stop=(c == 7)),
                         reads=[W, MEMT], writes=[PB[bk]])
                evac_copy(memk[:, g4, :], psum[bk][:, 0:256], [PB[bk]], [MEMK])
            else:
                for j in range(2):
                    for c in range(8):
                        S.op("pe", lambda e, c=c, j=j, w3=w3, bk=bk: e.matmul(
                            psum[bk][:, j * 128:(j + 1) * 128], lhsT=memT[:, c, j * 128:(j + 1) * 128], rhs=w3[:, c, :],
                            start=(c == 0), stop=(c == 7)), reads=[W, MEMT], writes=[PB[bk]])
                evac_copy(memv[:, :, (g4 - 2) * 128:(g4 - 1) * 128], psum[bk][:, 0:256].rearrange("p (j g) -> p j g", j=2),
                          [PB[bk]], [MEMV])
            bput(bk)

        for tq in range(NQ):
            chunk_phase(u, l, b, tq, src_x, dst_x)

    def chunk_phase(u, l, b, tq, src_x, dst_x):
        t0 = tq * 512
        hn = hnmix[:].rearrange("p (i d) -> p i d", i=4)
        mix = hnmix[:].rearrange("p (c t) -> p c t", c=8)
        xc3 = xc[:].rearrange("p (i d) -> p i d", i=4)
        rd = [XC]
        if src_x is scr_d:
            rd = [XC] + [SCRB[b][tq]]
        S.dma(lambda e: e.dma_start(out=xc3, in_=src_x[b, t0:t0 + 512, :].rearrange("(i p) d -> p i d", p=128)),
              sl_x, reads=rd[1:], writes=[XC])
        for i in range(4):
            S.op("act", lambda e, i=i: e.activation(out=junk[:, 0:1024], in_=xc3[:, i, :], func=AF.Square,
                                                    accum_out=ss[:, i:i + 1]), reads=[XC], writes=[JUNK, SMS["ss"]])
        rms_scale(ss, rs, 4, 1.0 / D, SMS["ss"], SMS["rs"])
        for i in range(4):
            S.op("dve", lambda e, i=i: e.tensor_scalar(out=hn[:, i, :], in0=xc3[:, i, :], scalar1=rs[:, i:i + 1],
                                                       scalar2=None, op0=ALU.mult),
                 reads=[XC, SMS["rs"]], writes=[HNMIX])
        for c2 in range(4):
            bk = bget()
            pb = psum[bk][:].bitcast(BF16)
            for cc in range(2):
                c = 2 * c2 + cc
                for i in range(4):
                    S.op("pe", lambda e, c=c, i=i, cc=cc, pb=pb: e.transpose(
                        pb[:, cc * 512 + i * 128:cc * 512 + (i + 1) * 128], hn[:, i, c * 128:(c + 1) * 128], ident),
                        reads=[HNMIX, CBb], writes=[PB[bk]])
            for cc in range(2):
                c = 2 * c2 + cc
                S.op("dve", lambda e, c=c, cc=cc, pb=pb: e.tensor_scalar(
                    out=hT[:, c, :], in0=pb[:, cc * 512:(cc + 1) * 512], scalar1=gcol[:, c:c + 1], scalar2=None,
                    op0=ALU.mult), reads=[PB[bk], SMF], writes=[HT])
            bput(bk)

        if DBG_STAGE < 2:
            return
        fdest = ([("sbq", i) for i in range(3)] + [("sbk", i) for i in range(3)] + [("sbg", i) for i in range(3)]
                 + [("dq", i) for i in range(3)] + [("dg", i) for i in range(3)] + [("iq", 0), ("iq", 1), ("ik", 0)]
                 + [("mq", 0), ("mq", 1), ("mg", 0), ("mg", 1)])
        dst_tab = {"sbq": (sbq, SBQ), "sbg": (sbg, SBG), "dq": (dq, DQ), "dg": (dg, DG), "iq": (iq, IQ),
                   "mq": (mq, MQ), "mg": (mg, MG)}
        srcs = [wF_d[l, cc, :, :] for cc in range(26)]
        pend = [wchunk(srcs[0])]
        ptb = None
        for cc in range(26):
            if cc + 1 < 26:
                pend.append(wchunk(srcs[cc + 1]))
            w, W = pend.pop(0)
            w3 = w[:].rearrange("p (c g) -> p c g", c=8)
            if cc < 22:
                name, ci = fdest[cc]
                bk = bget()
                for c in range(8):
                    S.op("pe", lambda e, c=c, w3=w3, bk=bk: e.matmul(psum[bk][:, :], lhsT=w3[:, c, :], rhs=hT[:, c, :],
                                                                    start=(c == 0), stop=(c == 7)),
                         reads=[W, HT], writes=[PB[bk]])
                if name == "sbk":
                    evac_copy(sbkT[:, ci, t0:t0 + 512], psum[bk][:, :], [PB[bk]], [SBK[tq]])
                elif name == "ik":
                    evac_copy(ikT4[:, t0:t0 + 512], psum[bk][:, :], [PB[bk]], [IKT[tq]])
                elif name in ("sbg", "dg", "mg") and DBG_SUB >= 2:
                    dt_, DB = dst_tab[name]
                    hs = rr("ev", 2) * 512
                    S.op("act", lambda e, bk=bk, hs=hs: e.activation(out=tmpf[:, hs:hs + 512], in_=psum[bk][:, :], func=AF.Exp,
                                                                     scale=-1.0), reads=[PB[bk]], writes=[TMPF])
                    S.op("act", lambda e, hs=hs: e.activation(out=tmpf[:, hs:hs + 512], in_=tmpf[:, hs:hs + 512], func=AF.Ln, bias=1.0),
                         reads=[TMPF], writes=[TMPF])
                    S.op("act", lambda e, hs=hs: e.activation(out=tmpf[:, hs:hs + 512], in_=tmpf[:, hs:hs + 512], func=AF.Exp, scale=-1.0),
                         reads=[TMPF], writes=[TMPF])
                    S.op("dve", lambda e, dt_=dt_, ci=ci, bk=bk, hs=hs: e.tensor_tensor(
                        out=dt_[:, ci, :], in0=psum[bk][:, :], in1=tmpf[:, hs:hs + 512], op=ALU.mult),
                        reads=[PB[bk], TMPF], writes=[DB])
                else:
                    dt_, DB = dst_tab[name]
                    evac_copy(dt_[:, ci, :], psum[bk][:, :], [PB[bk]], [DB])
                bput(bk)
            elif DBG_SUB >= 30:
                g4 = cc - 22
                if g4 == 0:
                    ptb = [bget() for _ in range(4)]
                for i in range(4):
                    for c in range(8):
                        S.op("pe", lambda e, c=c, i=i, w3=w3, g4=g4: e.matmul(
                            psum[ptb[i]][:, g4 * 128:(g4 + 1) * 128], lhsT=hT[:, c, i * 128:(i + 1) * 128], rhs=w3[:, c, :],
                            start=(c == 0), stop=(c == 7)), reads=[W, HT], writes=[PB[ptb[i]]])
        if DBG_SUB < 30:
            return
        for i in range(4):
            j = 4 * tq + i
            evac_copy(sbv[:, j, :], psum[ptb[i]][:, 0:384], [PB[ptb[i]]], [SBV[tq]])
            evac_copy(tmpf[:, i * 128:(i + 1) * 128], psum[ptb[i]][:, 384:512], [PB[ptb[i]]], [TMPF])
        for i in range(4):
            S.op("act", lambda e, i=i: e.activation(out=junk[:, 0:128], in_=tmpf[:, i * 128:(i + 1) * 128], func=AF.Square,
                                                    accum_out=ssk[:, i:i + 1]),
                 reads=[TMPF], writes=[JUNK, SMS["ssk"]])
        rms_scale(ssk, rsk, 4, 1.0 / 128, SMS["ssk"], SMS["rsk"])
        for i in range(4):
            j = 4 * tq + i
            S.op("dve", lambda e, i=i, j=j: e.scalar_tensor_tensor(out=ckv[:, j, :], in0=tmpf[:, i * 128:(i + 1) * 128],
                                                                   scalar=rsk[:, i:i + 1], in1=kvg[:, :],
                                                                   op0=ALU.mult, op1=ALU.mult),
                 reads=[TMPF, SMS["rsk"], SMF], writes=[CKV[tq]])
        for i in range(4):
            bput(ptb[i])
        if DBG_SUB < 40:
            return
        bk = bget()
        pb = psum[bk][:].bitcast(BF16)
        for i in range(4):
            j = 4 * tq + i
            S.op("pe", lambda e, i=i, j=j, pb=pb: e.transpose(pb[:, i * 128:(i + 1) * 128], ckv[:, j, :], ident),
                 reads=[CKV[tq], CBb], writes=[PB[bk]])
        evac_copy(ckvT[:, t0:t0 + 512], pb[:, 0:512], [PB[bk]], [CKVT[tq]])
        bput(bk)
        if DBG_SUB < 50:
            return
        bk = bget()
        wiw3 = wiw[:].rearrange("p (c g) -> p c g", c=8)
        for i in range(4):
            for c in range(8):
                S.op("pe", lambda e, c=c, i=i, bk=bk: e.matmul(psum[bk][:, i * 8:(i + 1) * 8],
                                                                lhsT=hT[:, c, i * 128:(i + 1) * 128], rhs=wiw3[:, c, :],
                                                                start=(c == 0), stop=(c == 7)),
                     reads=[SMB, HT], writes=[PB[bk]])
        S.op("dve", lambda e, bk=bk: e.tensor_scalar(out=idxw[:, :], in0=psum[bk][:, 0:32], scalar1=1.0 / 16, scalar2=None,
                                                     op0=ALU.mult), reads=[PB[bk]], writes=[IDXW])
        bput(bk)

        if DBG_STAGE < 3:
            return
        nkb = 4 * tq + 4
        KS = lambda lst: [lst[q] for q in range(tq + 1)]

        def indexer(i):
            qb = 4 * tq + i
            if qb < 2:
                return
            yield
            nk = (qb + 1) * 128
            nkc = (nk + 511) // 512
            kd = rr("d", 2)
            for h in range(8):
                S.op("act", lambda e, h=h, kd=kd: e.activation(out=dgt[kd][:, h, :], in_=ident, func=AF.Copy,
                                                               scale=idxw[:, i * 8 + h:i * 8 + h + 1]),
                     reads=[CBb, IDXW], writes=[DGT[kd]])
            for kc in range(nkc):
                w_ = min(512, nk - kc * 512)
                bs = bget()
                pend = []
                for h in range(8):
                    bd = bget()
                    hc, hp = h // 4, (h % 4) * 32
                    tp = (96, 0) if hp == 96 else None
                    S.op("pe", lambda e, hc=hc, hp=hp, tp=tp, bd=bd, kc=kc, w_=w_: e.matmul(
                        psum[bd][:, 0:w_], lhsT=iq[hp:hp + 32, hc, i * 128:(i + 1) * 128],
                        rhs=ikT4[hp:hp + 32, kc * 512:kc * 512 + w_], start=True, stop=True, tile_position=tp),
                        reads=[IQ] + KS(IKT), writes=[PB[bd]])
                    kr = rr("r", NR)
                    if True:
                        S.op("dve", lambda e, kr=kr, bd=bd, w_=w_: e.tensor_scalar(out=rt[kr][:, 0:w_], in0=psum[bd][:, 0:w_],
                                                                                  scalar1=0.0, scalar2=None, op0=ALU.max),
                             reads=[PB[bd]], writes=[RT[kr]])
                    else:
                        S.op("act", lambda e, kr=kr, bd=bd, w_=w_: e.activation(out=rt[kr][:, 0:w_], in_=psum[bd][:, 0:w_],
                                                                               func=AF.Relu),
                             reads=[PB[bd]], writes=[RT[kr]])
                    bput(bd)
                    pend.append(lambda h=h, kr=kr, bs=bs, w_=w_: S.op("pe", lambda e: e.matmul(
                        psum[bs][:, 0:w_], lhsT=dgt[kd][:, h, :], rhs=rt[kr][:, 0:w_], start=(h == 0), stop=(h == 7)),
                        reads=[DGT[kd], RT[kr]], writes=[PB[bs]]))
                    if len(pend) > 2:
                        pend.pop(0)()
                    yield
                while pend:
                    pend.pop(0)()
                last = (kc == nkc - 1)
                wc = w_ - 128 if last else w_
                if wc > 0:
                    S.op("dve", lambda e, bs=bs, kc=kc, wc=wc: e.tensor_copy(out=score[:, kc * 512:kc * 512 + wc],
                                                                             in_=psum[bs][:, 0:wc]),
                         reads=[PB[bs]], writes=[SCORE])
                if last:
                    S.op("dve", lambda e, bs=bs, w_=w_: e.tensor_tensor(out=score[:, nk - 128:nk], in0=psum[bs][:, w_ - 128:w_],
                                                                       in1=negb, op=ALU.add),
                         reads=[PB[bs], CFb], writes=[SCORE])
                bput(bs)
            B = SMS["bis"]
            S.op("dve", lambda e: e.tensor_reduce(out=mx, in_=score[:, 0:nk], axis=AX.X, op=ALU.max), reads=[SCORE], writes=[B])
            S.op("dve", lambda e: e.tensor_reduce(out=mn, in_=score[:, 0:nk - 128], axis=AX.X, op=ALU.min),
                 reads=[SCORE], writes=[B])
            S.op("dve", lambda e: e.tensor_tensor(out=rng, in0=mx, in1=mn, op=ALU.subtract), reads=[B], writes=[B])
            S.op("dve", lambda e: e.tensor_tensor(out=thr, in0=mx, in1=mn, op=ALU.add), reads=[B], writes=[B])
            S.op("dve", lambda e: e.tensor_scalar(out=thr, in0=thr, scalar1=0.5, scalar2=None, op0=ALU.mult), reads=[B], writes=[B])
            S.op("dve", lambda e: e.tensor_scalar(out=steps[:, 0:16], in0=p2a, scalar1=rng, scalar2=None, op0=ALU.mult),
                 reads=[B, CFb], writes=[STEPS])
            S.op("dve", lambda e: e.tensor_scalar(out=steps[:, 16:32], in0=p2b, scalar1=rng, scalar2=None, op0=ALU.mult),
                 reads=[B, CFb], writes=[STEPS])
            for it in range(NBIS):
                S.op("dve", lambda e: e.tensor_scalar(out=junk[:, 0:nk], in0=score[:, 0:nk], scalar1=thr, scalar2=0.0,
                                                      op0=ALU.is_ge, op1=ALU.add, accum_out=cnt),
                     reads=[SCORE, B], writes=[JUNK, B])
                S.op("dve", lambda e, it=it: e.tensor_scalar(out=dd, in0=cnt, scalar1=float(TOPK), scalar2=steps[:, 16 + it:17 + it],
                                                             op0=ALU.is_ge, op1=ALU.mult), reads=[B, STEPS], writes=[B])
                S.op("dve", lambda e, it=it: e.scalar_tensor_tensor(out=thr, in0=dd, scalar=steps[:, it:it + 1], in1=thr,
                                                                    op0=ALU.subtract, op1=ALU.add),
                     reads=[B, STEPS], writes=[B])
                yield
                yield
            S.op("dve", lambda e: e.tensor_scalar(out=pen[:, i, 0:nk], in0=score[:, 0:nk], scalar1=thr, scalar2=NEG,
                                                  op0=ALU.is_lt, op1=ALU.mult), reads=[SCORE, B], writes=[PEN[i]])

        def sb_head(h):
            ch, hp = h // 2, (h % 2) * 64
            bcs = bget()
            pend = []
            for kb in range(nkb):
                c0 = max(0, kb - 4 * tq) * 128
                diag = kb >= 4 * tq
                bz = bget()
                S.op("pe", lambda e, kb=kb, c0=c0, bz=bz, diag=diag: e.matmul(
                    psum[bz][:, c0:512], lhsT=sbkT[hp:hp + 64, ch, kb * 128:(kb + 1) * 128], rhs=sbq[hp:hp + 64, ch, c0:512],
                    start=True, stop=not diag), reads=KS(SBK) + [SBQ], writes=[PB[bz]])
                if diag:
                    S.op("pe", lambda e, c0=c0, bz=bz: e.matmul(psum[bz][:, c0:c0 + 128], lhsT=penS, rhs=ident,
                                                                 start=False, stop=True), reads=[CBb], writes=[PB[bz]])
                ke = rr("e", NT)
                S.op("act", lambda e, ke=ke, c0=c0, bz=bz: e.activation(out=etile[ke][:, c0:512], in_=psum[bz][:, c0:512],
                                                                        func=AF.Exp, scale=0.125),
                     reads=[PB[bz]], writes=[ET[ke]])
                bput(bz)
                sp = big16[:, kb * 512:(kb + 1) * 512]
                S.op("act", lambda e, ke=ke, c0=c0, sp=sp: e.activation(out=sp[:, c0:512], in_=etile[ke][:, c0:512],
                                                                        func=AF.Ln, bias=1.0),
                     reads=[ET[ke]], writes=[BIG[kb]])
                pend.append(lambda kb=kb, c0=c0, sp=sp: S.op("pe", lambda e: e.matmul(
                    psum[bcs][0:16, c0:512], lhsT=ekb(kb), rhs=sp[:, c0:512], start=(kb == 0), stop=(kb == nkb - 1)),
                    reads=[CBb, BIG[kb]], writes=[PB[bcs]]))
                if len(pend) > SKEW:
                    pend.pop(0)()
                yield
            while pend:
                pend.pop(0)()
            S.op("dve", lambda e: e.tensor_copy(out=cshi[:, :], in_=psum[bcs][0:16, :]), reads=[PB[bcs]], writes=[CS])
            S.op("dve", lambda e: e.tensor_tensor(out=cslo[:, :], in0=psum[bcs][0:16, :], in1=cshi[:, :], op=ALU.subtract),
                 reads=[PB[bcs], CS], writes=[CS])
            bput(bcs)
            bo = bget()
            pend = []
            for kb in range(nkb):
                c0 = max(0, kb - 4 * tq) * 128
                diag = kb >= 4 * tq
                bi = bget()
                sp = big16[:, kb * 512:(kb + 1) * 512]
                S.op("pe", lambda e, kb=kb, c0=c0, bi=bi: e.matmul(
                    psum[bi][:, c0:512], lhsT=sbkT[hp:hp + 64, ch, kb * 128:(kb + 1) * 128], rhs=sbq[hp:hp + 64, ch, c0:512],
                    start=True, stop=False), reads=KS(SBK) + [SBQ], writes=[PB[bi]])
                S.op("pe", lambda e, c0=c0, bi=bi, sp=sp: e.matmul(psum[bi][:, c0:512], lhsT=triM8, rhs=sp[:, c0:512],
                                                                   start=False, stop=False),
                     reads=[CBb, BIG[kb]], writes=[PB[bi]])
                S.op("pe", lambda e, kb=kb, c0=c0, bi=bi: e.matmul(psum[bi][:, c0:512], lhsT=selM8(kb), rhs=cshi[:, c0:512],
                                                                   start=False, stop=False),
                     reads=[CBb, CS], writes=[PB[bi]])
                S.op("pe", lambda e, kb=kb, c0=c0, bi=bi, diag=diag: e.matmul(psum[bi][:, c0:512], lhsT=selM8(kb),
                                                                              rhs=cslo[:, c0:512], start=False, stop=not diag),
                     reads=[CBb, CS], writes=[PB[bi]])
                if diag:
                    S.op("pe", lambda e, c0=c0, bi=bi: e.matmul(psum[bi][:, c0:c0 + 128], lhsT=penS, rhs=ident,
                                                                 start=False, stop=True), reads=[CBb], writes=[PB[bi]])
                ka = rr("a", NT)
                S.op("act", lambda e, ka=ka, c0=c0, bi=bi: e.activation(out=atile[ka][:, c0:512], in_=psum[bi][:, c0:512],
                                                                        func=AF.Exp, scale=0.125),
                     reads=[PB[bi]], writes=[AT[ka]])
                bput(bi)
                pend.append(lambda kb=kb, c0=c0, ka=ka: S.op("pe", lambda e: e.matmul(
                    psum[bo][:, c0:512], lhsT=sbv[:, kb, ch * 128:(ch + 1) * 128], rhs=atile[ka][:, c0:512],
                    start=(kb == 0), stop=(kb == nkb - 1)), reads=KS(SBV) + [AT[ka]], writes=[PB[bo]]))
                if len(pend) > SKEW:
                    pend.pop(0)()
                yield
            while pend:
                pend.pop(0)()
            S.op("dve", lambda e: e.tensor_tensor(out=mix[hp:hp + 64, ch, :], in0=psum[bo][hp:hp + 64, :],
                                                  in1=sbg[hp:hp + 64, ch, :], op=ALU.mult),
                 reads=[PB[bo], SBG], writes=[HNMIX])
            bput(bo)

        def mem_head(hm):
            ch, hp = hm // 2, (hm % 2) * 64
            bo = bget(); bd = bget()
            for mb in range(2):
                bl = bget()
                S.op("pe", lambda e, mb=mb, bl=bl: e.matmul(psum[bl][:, :], lhsT=memk[hp:hp + 64, ch, mb * 128:(mb + 1) * 128],
                                                            rhs=mq[hp:hp + 64, ch, :], start=True, stop=True),
                     reads=[MEMK, MQ], writes=[PB[bl]])
                kp = rr("p", NT)
                S.op("act", lambda e, kp=kp, bl=bl: e.activation(out=ptile[kp][:, :], in_=psum[bl][:, :], func=AF.Exp, scale=0.125),
                     reads=[PB[bl]], writes=[PT[kp]])
                bput(bl)
                S.op("pe", lambda e, mb=mb, kp=kp: e.matmul(psum[bo][:, :], lhsT=memv[:, mb, ch * 128:(ch + 1) * 128],
                                                            rhs=ptile[kp][:, :], start=(mb == 0), stop=(mb == 1)),
                     reads=[MEMV, PT[kp]], writes=[PB[bo]])
                S.op("pe", lambda e, mb=mb, kp=kp: e.matmul(psum[bd][:, :], lhsT=ones, rhs=ptile[kp][:, :],
                                                            start=(mb == 0), stop=(mb == 1)),
                     reads=[CBb, PT[kp]], writes=[PB[bd]])
            S.op("act", lambda e: e.activation(out=recf[hp:hp + 64, :], in_=psum[bd][hp:hp + 64, :], func=AF.Ln), reads=[PB[bd]], writes=[RECF])
            S.op("act", lambda e: e.activation(out=recf[hp:hp + 64, :], in_=recf[hp:hp + 64, :], func=AF.Exp, scale=-1.0), reads=[RECF], writes=[RECF])
            S.op("dve", lambda e: e.tensor_tensor(out=tmpf[hp:hp + 64, 0:512], in0=psum[bo][hp:hp + 64, :],
                                                  in1=recf[hp:hp + 64, :], op=ALU.mult), reads=[PB[bo], RECF], writes=[TMPF])
            S.op("dve", lambda e: e.tensor_tensor(out=mix[hp:hp + 64, 6 + ch, :], in0=tmpf[hp:hp + 64, 0:512],
                                                  in1=mg[hp:hp + 64, ch, :], op=ALU.mult), reads=[TMPF, MG], writes=[HNMIX])
            bput(bo); bput(bd)

        def dsa_head(h):
            ch, hp = h // 2, (h % 2) * 64
            bq = bget()
            S.op("pe", lambda e: e.matmul(psum[bq][:, :], lhsT=wuk[hp:hp + 64, ch * 128:(ch + 1) * 128], rhs=dq[hp:hp + 64, ch, :],
                                          start=True, stop=True), reads=[SMB, DQ], writes=[PB[bq]])
            kq = rr("q", 2)
            evac_copy(qlat[kq][:, :], psum[bq][:, :], [PB[bq]], [QL[kq]])
            bput(bq)
            bo = bget(); bd = bget()
            pend = []
            for kb in range(nkb):
                i0 = max(0, kb - 4 * tq)
                c0 = i0 * 128
                bl = bget()
                mm = []
                mm.append((lambda e, st, sp_, kb=kb, c0=c0, bl=bl: e.matmul(
                    psum[bl][:, c0:512], lhsT=ckvT[:, kb * 128:(kb + 1) * 128], rhs=qlat[kq][:, c0:512], start=st, stop=sp_),
                    KS(CKVT) + [QL[kq]]))
                for i in range(i0, 4):
                    qb = 4 * tq + i
                    cs_ = slice(i * 128, (i + 1) * 128)
                    if qb >= 2:
                        mm.append((lambda e, st, sp_, i=i, kb=kb, cs_=cs_, bl=bl: e.matmul(
                            psum[bl][:, cs_], lhsT=pen[:, i, kb * 128:(kb + 1) * 128], rhs=ident, start=st, stop=sp_),
                            [PEN[i], CBb]))
                    elif qb == kb:
                        mm.append((lambda e, st, sp_, cs_=cs_, bl=bl: e.matmul(psum[bl][:, cs_], lhsT=penD, rhs=ident,
                                                                              start=st, stop=sp_), [CBb]))
                    if qb - kb <= 1:
                        jj = qb - kb
                        mm.append((lambda e, st, sp_, cs_=cs_, jj=jj, bl=bl: e.matmul(
                            psum[bl][:, cs_], lhsT=ident, rhs=tb8[:, (h * 2 + jj) * 128:(h * 2 + jj + 1) * 128],
                            start=st, stop=sp_), [CBb, SMB]))
                for n_, (fn, rds) in enumerate(mm):
                    S.op("pe", lambda e, fn=fn, n_=n_, nm=len(mm): fn(e, n_ == 0, n_ == nm - 1), reads=rds, writes=[PB[bl]])
                kp = rr("p", NT)
                near_hi = min(512, max(c0, (kb + 2 - 4 * tq) * 128))
                if near_hi > c0:
                    S.op("act", lambda e, kp=kp, c0=c0, near_hi=near_hi, bl=bl: e.activation(
                        out=ptile[kp][:, c0:near_hi], in_=psum[bl][:, c0:near_hi], func=AF.Exp, scale=0.125),
                        reads=[PB[bl]], writes=[PT[kp]])
                if near_hi < 512:
                    S.op("act", lambda e, kp=kp, near_hi=near_hi, bl=bl: e.activation(
                        out=ptile[kp][:, near_hi:512], in_=psum[bl][:, near_hi:512], func=AF.Exp, scale=0.125,
                        bias=b31[:, h:h + 1]), reads=[PB[bl], SMF], writes=[PT[kp]])
                bput(bl)
                def tail(kb=kb, c0=c0, kp=kp):
                    S.op("pe", lambda e: e.matmul(psum[bo][:, c0:512], lhsT=ckv[:, kb, :], rhs=ptile[kp][:, c0:512],
                                                  start=(kb == 0), stop=(kb == nkb - 1)),
                         reads=KS(CKV) + [PT[kp]], writes=[PB[bo]])
                    S.op("pe", lambda e: e.matmul(psum[bd][:, c0:512], lhsT=ones, rhs=ptile[kp][:, c0:512],
                                                  start=(kb == 0), stop=(kb == nkb - 1)),
                         reads=[CBb, PT[kp]], writes=[PB[bd]])
                pend.append(tail)
                if len(pend) > SKEW:
                    pend.pop(0)()
                yield
            while pend:
                pend.pop(0)()
            S.op("act", lambda e: e.activation(out=recf[:, :], in_=psum[bd][:, :], func=AF.Ln), reads=[PB[bd]], writes=[RECF])
            S.op("act", lambda e: e.activation(out=recf[:, :], in_=recf[:, :], func=AF.Exp, scale=-1.0), reads=[RECF], writes=[RECF])
            S.op("dve", lambda e: e.tensor_tensor(out=onb[:, :], in0=psum[bo][:, :], in1=recf[:, :], op=ALU.mult),
                 reads=[PB[bo], RECF], writes=[ONB])
            bput(bo); bput(bd)
            bu = bget()
            S.op("pe", lambda e: e.matmul(psum[bu][:, :], lhsT=wuv[:, ch * 128:(ch + 1) * 128], rhs=onb[:, :], start=True, stop=True),
                 reads=[SMB, ONB], writes=[PB[bu]])
            S.op("dve", lambda e: e.tensor_tensor(out=mix[hp:hp + 64, 3 + ch, :], in0=psum[bu][hp:hp + 64, :],
                                                  in1=dg[hp:hp + 64, ch, :], op=ALU.mult), reads=[PB[bu], DG], writes=[HNMIX])
            bput(bu)

        def run_par(*gens):
            gens = list(gens)
            while gens:
                for g in list(gens):
                    try:
                        next(g)
                    except StopIteration:
                        gens.remove(g)

        def seq(*fns):
            for f in fns:
                r = f()
                if r is not None:
                    yield from r
                yield

        run_par(seq(*[lambda i=i: indexer(i) for i in range(4)]),
                seq(*([lambda h=h: sb_head(h) for h in range(5)] + [lambda hm=hm: mem_head(hm) for hm in range(4)])))
        run_par(sb_head(5), seq(lambda: dsa_head(0), lambda: dsa_head(1)))
        wO3 = big16[:].rearrange("p (c n) -> p c n", c=8)
        for pc in range(8):
            k = rr("w", NWB)
            S.dma(lambda e, k=k, pc=pc: e.dma_start(out=wst[k][:], in_=wO_d[l, pc, :, :]), sl_w[k], writes=[WST[k]])
            regs = [BIG[2 * c + pc // 4] for c in range(8)]
            S.op("act", lambda e, k=k, pc=pc: e.activation(out=wO3[:, :, pc * 128:(pc + 1) * 128],
                                                           in_=wst[k][:].rearrange("p (c g) -> p c g", c=8), func=AF.Copy),
                 reads=[WST[k]], writes=regs)
        for h in range(2, 6):
            run_par(dsa_head(h))

        for i in range(4):
            b0 = bget(); b1 = bget()
            bb = (b0, b1)
            for half in range(2):
                for c in range(8):
                    S.op("pe", lambda e, c=c, half=half, i=i, bb=bb: e.matmul(
                        psum[bb[half]][:, :], lhsT=mix[:, c, i * 128:(i + 1) * 128], rhs=wO3[:, c, half * 512:(half + 1) * 512],
                        start=(c == 0), stop=(c == 7)), reads=[HNMIX, BIG[2 * c + half]], writes=[PB[bb[half]]])
            for half in range(2):
                evac_copy(tmpf[:, half * 512:(half + 1) * 512], psum[bb[half]][:, :], [PB[bb[half]]], [TMPF])
            S.op("act", lambda e: e.activation(out=junk[:, 0:1024], in_=tmpf[:, :], func=AF.Square, accum_out=ssy2),
                 reads=[TMPF], writes=[JUNK, SMS["ssy"]])
            rms_scale(ssy2, rsy, 1, 1.0 / D, SMS["ssy"], SMS["ssy"])
            S.op("dve", lambda e: e.scalar_tensor_tensor(out=tmpf[:, :], in0=tmpf[:, :], scalar=rsy, in1=postg[:, :],
                                                         op0=ALU.mult, op1=ALU.mult),
                 reads=[TMPF, SMS["ssy"], SMF], writes=[TMPF])
            bput(b0); bput(b1)
            S.op("pool", lambda e, i=i: e.tensor_tensor(out=xc3[:, i, :], in0=tmpf[:, :], in1=xc3[:, i, :], op=ALU.add),
                 reads=[TMPF, XC], writes=[XC])
        wr = [OUTB] if dst_x is out_d else [SCRB[b][tq]]
        S.dma(lambda e: e.dma_start(out=dst_x[b, t0:t0 + 512, :].rearrange("(i p) d -> p i d", p=128), in_=xc3),
              sl_o, reads=[XC], writes=wr)

    for u in range(len(layer_of_unit)):
        unit(u)
    S.final_wait("sp", [OUTB, XC])
    S.emit()
    S.close()
    for g in reversed(ctx):
        g.__exit__(None, None, None)
    return nc


def _t5_bucket(rel):
    n = np.maximum(rel, 0)
    max_exact = 16
    nf = np.maximum(n, 1).astype(np.float32)
    large = max_exact + (np.log(nf / max_exact) / math.log(128 / max_exact) * (32 - max_exact)).astype(np.int32)
    large = np.minimum(large, 31)
    return np.where(n < max_exact, n, large)


def _constants():
    bf = ml_dtypes.bfloat16
    cbv = np.zeros((128, NCB), np.float32)
    p = np.arange(128)
    cbv[:, CB_ID:CB_ID + 128] = np.eye(128)
    cbv[:, CB_TRI:CB_TRI + 128] = np.where(p[:, None] >= p[None, :], -8.0, 0.0)
    cbv[:, CB_ONES:CB_ONES + 128] = 1.0
    cbv[:, CB_PENS:CB_PENS + 128] = np.where(p[None, :] < p[:, None], 0.0, NEG)
    cbv[:, CB_PEND:CB_PEND + 128] = np.where(p[None, :] <= p[:, None], 0.0, NEG)
    for kb in range(16):
        cbv[:, CB_EKB + kb * 16 + kb] = 1.0
        for jb in range(16):
            if jb > kb:
                cbv[jb, CB_SEL + kb * 128:CB_SEL + (kb + 1) * 128] = -8.0
    cfv = np.zeros((128, NCF), np.float32)
    cfv[:, CF_NEGB:CF_NEGB + 128] = np.where(p[None, :] <= p[:, None], 0.0, -1e30)
    cfv[:, CF_P2A:CF_P2A + 16] = 2.0 ** -(np.arange(16) + 2.0)
    cfv[:, CF_P2B:CF_P2B + 16] = 2.0 ** -(np.arange(16) + 1.0)
    return cfv, cbv.astype(bf)


def _chunk_cols(w, cols):
    sel = w[:, cols]
    n = sel.shape[1] // 128
    a = sel.reshape(8, 128, n, 128)
    return np.ascontiguousarray(a.transpose(2, 1, 0, 3).reshape(n, 128, 1024))


def _prep_weights(pre_norm_g, post_norm_g, w_in, w_uk, w_uv, kv_norm_g, w_mem_kv, w_out, rel_bias, layers):
    o = np.cumsum([0, 384, 384, 384, 384, 384, 128, 384, 256, 32, 8, 256, 256])
    (o_sbq, o_sbk, o_sbv, o_sbg, o_dq, o_ckv, o_dg, o_iq, o_ik, o_iw, o_mq, o_mg) = o[:12]
    r = lambda a, n: list(range(a, a + n))
    fcols = (r(o_sbq, 384) + r(o_sbk, 384) + r(o_sbg, 384) + r(o_dq, 384) + r(o_dg, 384) + r(o_iq, 256)
             + r(o_ik, 32) * 4 + r(o_mq, 256) + r(o_mg, 256) + r(o_sbv, 384) + r(o_ckv, 128))
    assert len(fcols) == 26 * 128
    s_l = np.arange(128)
    wF, wO, wM, wsm = [], [], [], []
    for l in layers:
        wF.append(_chunk_cols(w_in[l], fcols))
        wO.append(_chunk_cols(w_out[l], list(range(1024))))
        wM.append(_chunk_cols(w_mem_kv[l], list(range(512))))
        sm = np.zeros((128, NSM), np.float32)
        sm[:, SM_GCOL:SM_GCOL + 8] = pre_norm_g[l].reshape(8, 128).T
        sm[:, SM_IW:SM_IW + 64] = w_in[l][:, o_iw:o_iw + 8].reshape(8, 128, 8).transpose(1, 0, 2).reshape(128, 64)
        uk = w_uk[l]
        t = uk.reshape(128, 3, 2, 64).transpose(2, 3, 1, 0).reshape(128, 3 * 128)
        sm[:, SM_WUK:SM_WUK + 384] = t
        sm[:, SM_WUV:SM_WUV + 384] = w_uv[l].reshape(128, 384)
        sm[:, SM_KVG:SM_KVG + 128] = kv_norm_g[l][None, :]
        sm[:, SM_B31:SM_B31 + 6] = rel_bias[31][None, :]
        sm[:, SM_POSTG:SM_POSTG + 1024] = post_norm_g[l][None, :]
        for h in range(6):
            for j in range(2):
                rel = s_l[None, :] - s_l[:, None] + 128 * j
                sm[:, SM_TB + (h * 2 + j) * 128:SM_TB + (h * 2 + j + 1) * 128] = rel_bias[_t5_bucket(rel), h]
        wsm.append(sm)
    return (np.stack(wF), np.stack(wO), np.stack(wM), np.stack(wsm))


_PROG_CACHE = {}


def _get_prog(key, *args):
    if key not in _PROG_CACHE:
        _PROG_CACHE[key] = build_program(*args)
    return _PROG_CACHE[key]


FUSED = True


def kernel(x, mem, pre_norm_g, post_norm_g, w_in, w_uk, w_uv, kv_norm_g, w_mem_kv, w_out, rel_bias):
    x = np.asarray(x, np.float32)
    mem = np.asarray(mem, np.float32)
    args = [np.asarray(a, np.float32) for a in (pre_norm_g, post_norm_g, w_in, w_uk, w_uv, kv_norm_g, w_mem_kv, w_out, rel_bias)]
    B, S_TOK, _ = x.shape
    depth = w_in.shape[0]
    per = B // N_CORES
    cfv, cbv = _constants()
    if FUSED:
        units_l = []; units_b = []; chain = []
        for b in range(per):
            for l in range(depth):
                units_l.append(l); units_b.append(b); chain.append("in" if l == 0 else "scr")
        nc = _get_prog(("fused", S_TOK, per, depth), S_TOK, per, units_l, units_b, chain)
        wF, wO, wM, wsm = _prep_weights(*args, layers=list(range(depth)))
        in_maps = [{"x": np.ascontiguousarray(x[c * per:(c + 1) * per]), "mem": np.ascontiguousarray(mem[c * per:(c + 1) * per]),
                    "wF": wF, "wO": wO, "wM": wM, "wsm": wsm, "cf32": cfv, "cbf": cbv} for c in range(N_CORES)]
        res = run_bass_kernel_spmd(nc, in_maps, core_ids=list(range(N_CORES)))
        return np.concatenate([r["out"] for r in res.results], axis=0)
    cur = x
    nc = _get_prog(("unit", S_TOK), S_TOK, 1, [0], [0], ["in"])
    for l in range(depth):
        wF, wO, wM, wsm = _prep_weights(*args, layers=[l])
        nxt = np.empty_like(cur)
        for b in range(per):
            in_maps = [{"x": np.ascontiguousarray(cur[c * per + b:c * per + b + 1]),
                        "mem": np.ascontiguousarray(mem[c * per + b:c * per + b + 1]),
                        "wF": wF, "wO": wO, "wM": wM, "wsm": wsm, "cf32": cfv, "cbf": cbv} for c in range(N_CORES)]
            res = run_bass_kernel_spmd(nc, in_maps, core_ids=list(range(N_CORES)))
            for c in range(N_CORES):
                nxt[c * per + b] = res.results[c]["out"][0]
        cur = nxt
    return cur
```

```python
import math
import numpy as np
import ml_dtypes
import concourse.bass as bass
import concourse.mybir as mybir
from concourse.bass_utils import run_bass_kernel_spmd

F32 = mybir.dt.float32
BF16 = mybir.dt.bfloat16
AF = mybir.ActivationFunctionType
ALU = mybir.AluOpType
AX = mybir.AxisListType

D = 1024
NMEM = 256
TOPK = 256
NBIS = 13
EPS = 1e-6
NEG = -30000.0
N_CORES = 8
DBG_STAGE = 99
DBG_SUB = 99
SKEW = 1

SM_GCOL, SM_IW, SM_WUK, SM_WUV, SM_KVG, SM_B31, SM_POSTG, SM_TB = 0, 8, 72, 456, 840, 968, 974, 1998
NSM = 1998 + 6 * 2 * 128
CB_ID, CB_TRI, CB_ONES, CB_PENS, CB_PEND, CB_EKB, CB_SEL = 0, 128, 256, 384, 512, 640, 896
NCB = 896 + 16 * 128
CF_NEGB, CF_P2A, CF_P2B = 0, 128, 144
NCF = 160


class Buf:
    __slots__ = ("name", "w", "r", "excl")

    def __init__(self, name, excl=False):
        self.name = name
        self.w = None
        self.r = {}
        self.excl = excl


class Sched:
    ENGS = ("pe", "act", "dve", "pool", "sp")

    def __init__(self, nc):
        self.nc = nc
        self.ops = {e: [] for e in self.ENGS}
        self.sems = {}
        self.cnt = {}
        self.waited = {e: {} for e in self.ENGS}
        self._ctx = []
        for e in self.ENGS:
            self._newsem("E_" + e)

    def _newsem(self, key):
        g = self.nc.semaphore(key)
        h = g.__enter__()
        self._ctx.append(g)
        self.sems[key] = h
        self.cnt[key] = 0
        return key

    def dma_slot(self, name):
        return self._newsem("D_" + name)

    def _deps(self, e, reads, writes):
        deps = {}

        def add(ev):
            if ev is None:
                return
            k, v = ev
            if deps.get(k, 0) < v:
                deps[k] = v
        for b in reads:
            add(b.w)
        for b in writes:
            add(b.w)
            for k, v in b.r.items():
                add((k, v))
        waits = []
        mykey = "E_" + e
        for k, v in deps.items():
            if k == mykey and e in ("pe", "sp"):
                continue
            if self.waited[e].get(k, 0) >= v:
                continue
            self.waited[e][k] = v
            waits.append((k, v))
        return waits

    def _record(self, ev, reads, writes):
        k, v = ev
        for b in reads:
            if b.r.get(k, 0) < v:
                b.r[k] = v
        for b in writes:
            b.w = ev
            b.r = {}

    def op(self, e, fn, reads=(), writes=()):
        ex = [b for b in reads if b.excl]
        if ex:
            writes = list(writes) + ex
        waits = self._deps(e, reads, writes)
        k = "E_" + e
        self.cnt[k] += 1
        ev = (k, self.cnt[k])
        self.ops[e].append((waits, fn, (k, 1)))
        self._record(ev, reads, writes)
        return ev

    def dma(self, fn, slot, reads=(), writes=(), e="sp"):
        waits = self._deps(e, reads, writes)
        self.cnt[slot] += 16
        ev = (slot, self.cnt[slot])
        self.ops[e].append((waits, fn, (slot, 16)))
        self._record(ev, reads, writes)
        return ev

    def final_wait(self, e, bufs):
        waits = self._deps(e, bufs, bufs)
        self.ops[e].append((waits, None, None))

    def emit(self):
        nc = self.nc
        needed = {}
        for e in self.ENGS:
            for waits, fn, inc in self.ops[e]:
                for k, v in waits:
                    if k.startswith("E_"):
                        needed.setdefault(k, set()).add(v)
        rank = {k: {v: i + 1 for i, v in enumerate(sorted(vs))} for k, vs in needed.items()}
        with nc.Block() as block:
            def run(ename):
                def body(eng):
                    seq = 0
                    mykey = "E_" + ename
                    myrank = rank.get(mykey, {})
                    for waits, fn, inc in self.ops[ename]:
                        for k, v in waits:
                            eng.wait_ge(self.sems[k], rank[k][v] if k.startswith("E_") else v)
                        if fn is None:
                            continue
                        inst = fn(eng)
                        if inc[0] == mykey:
                            seq += 1
                            if seq in myrank:
                                inst.then_inc(self.sems[mykey], 1)
                        else:
                            inst.then_inc(self.sems[inc[0]], inc[1])
                return body
            block.tensor(run("pe"))
            block.scalar(run("act"))
            block.vector(run("dve"))
            block.gpsimd(run("pool"))
            block.sync(run("sp"))

    def close(self):
        for g in reversed(self._ctx):
            g.__exit__(None, None, None)


def build_program(S_TOK, NB, layer_of_unit, batch_of_unit, chain):
    NQ = S_TOK // 512
    NBLK = S_TOK // 128
    L = max(layer_of_unit) + 1
    nc = bass.Bass("TRN2", target_bir_lowering=False)
    x_d = nc.dram_tensor("x", [NB, S_TOK, D], F32, kind="ExternalInput").ap()
    mem_d = nc.dram_tensor("mem", [NB, NMEM, D], F32, kind="ExternalInput").ap()
    wF_d = nc.dram_tensor("wF", [L, 26, 128, 1024], F32, kind="ExternalInput").ap()
    wO_d = nc.dram_tensor("wO", [L, 8, 128, 1024], F32, kind="ExternalInput").ap()
    wM_d = nc.dram_tensor("wM", [L, 4, 128, 1024], F32, kind="ExternalInput").ap()
    wsm_d = nc.dram_tensor("wsm", [L, 128, NSM], F32, kind="ExternalInput").ap()
    cf_d = nc.dram_tensor("cf32", [128, NCF], F32, kind="ExternalInput").ap()
    cb_d = nc.dram_tensor("cbf", [128, NCB], BF16, kind="ExternalInput").ap()
    out_d = nc.dram_tensor("out", [NB, S_TOK, D], F32, kind="ExternalOutput").ap()
    need_scr = any(c == "scr" for c in chain)
    scr_d = nc.dram_tensor("xscr", [NB, S_TOK, D], F32, kind="Internal").ap() if need_scr else None

    wsc_d = nc.dram_tensor("wscr", [L, 38, 128, 1024], BF16, kind="Internal").ap()
    S = Sched(nc)
    ctx = []

    def sb(name, shape, dt):
        g = nc.sbuf_tensor(name, shape, dt)
        h = g.__enter__()
        ctx.append(g)
        return h

    psum = []
    PB = []
    for i in range(8):
        g = nc.psum_tensor(f"ps{i}", [128, 512], F32)
        psum.append(g.__enter__())
        ctx.append(g)
        PB.append(Buf(f"ps{i}", excl=True))
    free_banks = list(range(8))

    def bget():
        return free_banks.pop(0)

    def bput(i):
        free_banks.append(i)

    cb = sb("cb", [128, NCB], BF16); CBb = Buf("cb")
    cf = sb("cf", [128, NCF], F32); CFb = Buf("cf")
    ident = cb[:, CB_ID:CB_ID + 128]
    triM8 = cb[:, CB_TRI:CB_TRI + 128]
    ones = cb[:, CB_ONES:CB_ONES + 128]
    penS = cb[:, CB_PENS:CB_PENS + 128]
    penD = cb[:, CB_PEND:CB_PEND + 128]

    def ekb(kb):
        return cb[:, CB_EKB + kb * 16:CB_EKB + (kb + 1) * 16]

    def selM8(kb):
        return cb[0:16, CB_SEL + kb * 128:CB_SEL + (kb + 1) * 128]
    negb = cf[:, CF_NEGB:CF_NEGB + 128]
    p2a = cf[:, CF_P2A:CF_P2A + 16]
    p2b = cf[:, CF_P2B:CF_P2B + 16]

    sbkT = sb("sbkT", [128, 3, S_TOK], BF16); SBK = [Buf(f"sbk{q}") for q in range(NQ)]
    sbv = sb("sbv", [128, NBLK, 384], BF16); SBV = [Buf(f"sbv{q}") for q in range(NQ)]
    ckv = sb("ckv", [128, NBLK, 128], BF16); CKV = [Buf(f"ckv{q}") for q in range(NQ)]
    ckvT = sb("ckvT", [128, S_TOK], BF16); CKVT = [Buf(f"ckvT{q}") for q in range(NQ)]
    ikT4 = sb("ikT4", [128, S_TOK], BF16); IKT = [Buf(f"ikT{q}") for q in range(NQ)]
    sbq = sb("sbq", [128, 3, 512], BF16); SBQ = Buf("sbq")
    sbg = sb("sbg", [128, 3, 512], BF16); SBG = Buf("sbg")
    dq = sb("dq", [128, 3, 512], BF16); DQ = Buf("dq")
    dg = sb("dg", [128, 3, 512], BF16); DG = Buf("dg")
    iq = sb("iq", [128, 2, 512], BF16); IQ = Buf("iq")
    mq = sb("mq", [128, 2, 512], BF16); MQ = Buf("mq")
    mg = sb("mg", [128, 2, 512], BF16); MG = Buf("mg")
    idxw = sb("idxw", [128, 32], F32); IDXW = Buf("idxw")
    hT = sb("hT", [128, 8, 512], BF16); HT = Buf("hT")
    hnmix = sb("hnmix", [128, 4096], BF16); HNMIX = Buf("hnmix")
    xc = sb("xc", [128, 4096], F32); XC = Buf("xc")
    NWB = 2
    wst = [sb(f"wst{i}", [128, 1024], F32) for i in range(NWB)]; WST = [Buf(f"wst{i}") for i in range(NWB)]
    NWF = 4
    wbf = [sb(f"wbf{i}", [128, 1024], BF16) for i in range(NWF)]; WBF = [Buf(f"wbf{i}") for i in range(NWF)]
    big16 = sb("big16", [128, 8192], BF16); BIG = [Buf(f"big{r}") for r in range(16)]
    gcol = sb("gcol", [128, 8], F32); kvg = sb("kvg", [128, 128], F32); b31 = sb("b31", [128, 6], F32)
    postg = sb("postg", [128, 1024], F32)
    SMF = Buf("smallf32")
    wiw = sb("wiw", [128, 64], BF16); wuk = sb("wuk", [128, 384], BF16); wuv = sb("wuv", [128, 384], BF16)
    tb8 = sb("tb8", [128, 1536], BF16)
    SMB = Buf("smallbf")
    memT = sb("memT", [128, 8, 256], BF16); MEMT = Buf("memT")
    memk = sb("memk", [128, 2, 256], BF16); MEMK = Buf("memk")
    memv = sb("memv", [128, 2, 256], BF16); MEMV = Buf("memv")
    membf = sb("membf", [128, 2, 1024], BF16); MEMBF = Buf("membf")
    NT = 3
    etile = [sb(f"et{i}", [128, 512], BF16) for i in range(NT)]; ET = [Buf(f"et{i}") for i in range(NT)]
    atile = [sb(f"at{i}", [128, 512], BF16) for i in range(NT)]; AT = [Buf(f"at{i}") for i in range(NT)]
    cshi = sb("cshi", [16, 512], BF16); cslo = sb("cslo", [16, 512], BF16); CS = Buf("cs")
    score = sb("score", [128, S_TOK], F32); SCORE = Buf("score")
    junk = sb("junk", [128, S_TOK], BF16); JUNK = Buf("junk")
    pen = sb("pen", [128, 4, S_TOK], BF16); PEN = [Buf(f"pen{i}") for i in range(4)]
    NR = 4
    rt = [sb(f"rt{i}", [128, 512], BF16) for i in range(NR)]; RT = [Buf(f"rt{i}") for i in range(NR)]
    dgt = [sb(f"dgt{i}", [128, 8, 128], BF16) for i in range(2)]; DGT = [Buf(f"dgt{i}") for i in range(2)]
    ptile = [sb(f"pt{i}", [128, 512], BF16) for i in range(NT)]; PT = [Buf(f"pt{i}") for i in range(NT)]
    qlat = [sb(f"ql{i}", [128, 512], BF16) for i in range(2)]; QL = [Buf(f"ql{i}") for i in range(2)]
    recf = sb("recf", [128, 512], F32); RECF = Buf("recf")
    onb = sb("onb", [128, 512], BF16); ONB = Buf("onb")
    tmpf = sb("tmpf", [128, 1024], F32); TMPF = Buf("tmpf")
    sm = sb("smalls", [128, 64], F32)
    SMS = {n: Buf("sm_" + n) for n in ("ss", "rs", "ssk", "rsk", "bis", "ssy")}
    steps = sb("steps", [128, 32], F32); STEPS = Buf("steps")
    ss = sm[:, 0:4]; rs = sm[:, 4:8]; ssk = sm[:, 8:12]; rsk = sm[:, 12:16]
    mx = sm[:, 16:17]; mn = sm[:, 17:18]; thr = sm[:, 18:19]; rng = sm[:, 19:20]; cnt = sm[:, 20:21]; dd = sm[:, 21:22]
    ssy = sm[:, 24:26]; ssy2 = sm[:, 26:27]; rsy = sm[:, 27:28]

    sl_c = S.dma_slot("const"); sl_c2 = S.dma_slot("const2"); sl_x = S.dma_slot("x"); sl_o = S.dma_slot("o"); sl_m = S.dma_slot("mem")
    sl_s = S.dma_slot("small"); sl_w = [S.dma_slot(f"w{i}") for i in range(NWB)]
    sl_wb = [S.dma_slot(f"wb{i}") for i in range(NWF)]; sl_wo = [S.dma_slot(f"wo{i}") for i in range(NWB)]
    sl_big = [S.dma_slot(f"big{i}") for i in range(8)]
    WSC = [[Buf(f"wsc{l_}_{i}") for i in range(38)] for l_ in range(L)]
    OUTB = Buf("outdram")
    SCRB = {}

    cnt_rr = {"wf": 0, "w": 0, "e": 0, "a": 0, "r": 0, "p": 0, "q": 0, "d": 0, "ev": 0}

    def rr(key, n):
        v = cnt_rr[key] % n
        cnt_rr[key] += 1
        return v

    S.dma(lambda e: e.dma_start(out=cb[:], in_=cb_d[:, :]), sl_c, writes=[CBb])
    S.dma(lambda e: e.dma_start(out=cf[:], in_=cf_d[:, :]), sl_c2, writes=[CFb])

    for l_ in range(L):
        for idx in range(38):
            src = wF_d[l_, idx, :, :] if idx < 26 else (wM_d[l_, idx - 26, :, :] if idx < 30 else wO_d[l_, idx - 30, :, :])
            k = rr("w", NWB)
            S.dma(lambda e, k=k, src=src: e.dma_start(out=wst[k][:], in_=src), sl_w[k], writes=[WST[k]])
            kf = rr("wf", NWF)
            S.op("act", lambda e, k=k, kf=kf: e.activation(out=wbf[kf][:], in_=wst[k][:], func=AF.Copy),
                 reads=[WST[k]], writes=[WBF[kf]])
            S.dma(lambda e, kf=kf, l_=l_, idx=idx: e.dma_start(out=wsc_d[l_, idx, :, :], in_=wbf[kf][:]), sl_wb[kf],
                  reads=[WBF[kf]], writes=[WSC[l_][idx]])

    def wchunk(l_, idx):
        kf = rr("wf", NWF)
        S.dma(lambda e: e.dma_start(out=wbf[kf][:], in_=wsc_d[l_, idx, :, :]), sl_wb[kf], reads=[WSC[l_][idx]], writes=[WBF[kf]])
        return wbf[kf], WBF[kf]

    LA = 3

    def wstream(l_, idxs):
        pend_ = []
        it = iter(idxs)
        for _ in range(LA):
            nx = next(it, None)
            if nx is not None:
                pend_.append(wchunk(l_, nx))
        while pend_:
            cur = pend_.pop(0)
            nx = next(it, None)
            if nx is not None:
                pend_.append(wchunk(l_, nx))
            yield cur

    def evac_copy(out_ap, in_ap, reads, writes):
        if rr("ev", 2) == 0:
            S.op("act", lambda e: e.activation(out=out_ap, in_=in_ap, func=AF.Copy), reads=reads, writes=writes)
        else:
            S.op("dve", lambda e: e.tensor_copy(out=out_ap, in_=in_ap), reads=reads, writes=writes)

    def rms_scale(src_ss, dst_rs, n, inv_n, B_ss, B_rs):
        S.op("act", lambda e: e.activation(out=dst_rs, in_=src_ss, func=AF.Sqrt, scale=inv_n, bias=EPS),
             reads=[B_ss], writes=[B_rs])
        S.op("dve", lambda e: e.reciprocal(out=dst_rs, in_=dst_rs), reads=[B_rs], writes=[B_rs])

    def unit(u):
        l = layer_of_unit[u]
        b = batch_of_unit[u]
        src_x = x_d if chain[u] == "in" else scr_d
        later = any(batch_of_unit[v] == b for v in range(u + 1, len(layer_of_unit)))
        dst_x = scr_d if later else out_d
        if later and b not in SCRB:
            SCRB[b] = [Buf(f"scr{b}_{q}") for q in range(NQ)]

        S.dma(lambda e: e.dma_start(out=xc[:, 0:NSM], in_=wsm_d[l, :, :]), sl_s, writes=[XC])
        for (dst, off, n) in ((gcol, SM_GCOL, 8), (kvg, SM_KVG, 128), (b31, SM_B31, 6), (postg, SM_POSTG, 1024)):
            S.op("dve", lambda e, dst=dst, off=off, n=n: e.tensor_copy(out=dst[:, 0:n], in_=xc[:, off:off + n]),
                 reads=[XC], writes=[SMF])
        for (dst, off, n) in ((wiw, SM_IW, 64), (wuk, SM_WUK, 384), (wuv, SM_WUV, 384)):
            S.op("dve", lambda e, dst=dst, off=off, n=n: e.tensor_copy(out=dst[:, 0:n], in_=xc[:, off:off + n]),
                 reads=[XC], writes=[SMB])
        S.op("dve", lambda e: e.tensor_scalar(out=tb8[:, :], in0=xc[:, SM_TB:SM_TB + 1536], scalar1=8.0, scalar2=None,
                                              op0=ALU.mult), reads=[XC], writes=[SMB])
        S.dma(lambda e: e.dma_start(out=xc[:, 0:2048].rearrange("p (j d) -> p j d", j=2),
                                    in_=mem_d[b, :, :].rearrange("(j p) d -> p j d", p=128)), sl_m, writes=[XC])
        S.op("dve", lambda e: e.tensor_copy(out=membf[:].rearrange("p j d -> p (j d)"), in_=xc[:, 0:2048]),
             reads=[XC], writes=[MEMBF])
        for c2 in range(4):
            bk = bget()
            pb = psum[bk][:].bitcast(BF16)
            for cc in range(2):
                c = 2 * c2 + cc
                for j in range(2):
                    S.op("pe", lambda e, c=c, j=j, cc=cc, pb=pb: e.transpose(
                        pb[:, cc * 256 + j * 128:cc * 256 + (j + 1) * 128], membf[:, j, c * 128:(c + 1) * 128], ident),
                        reads=[MEMBF, CBb], writes=[PB[bk]])
            evac_copy(memT[:, 2 * c2:2 * c2 + 2, :], pb[:, 0:512].rearrange("p (c m) -> p c m", c=2), [PB[bk]], [MEMT])
            bput(bk)
        for g4, (w, W) in enumerate(wstream(l, [26, 27, 28, 29])):
            w3 = w[:].rearrange("p (c g) -> p c g", c=8)
            bk = bget()
            if g4 < 2:
                for c in range(8):
                    S.op("pe", lambda e, c=c, w3=w3, bk=bk: e.matmul(psum[bk][:, 0:256], lhsT=w3[:, c, :], rhs=memT[:, c, :],
                                                                    start=(c == 0), stop=(c == 7)),
                         reads=[W, MEMT], writes=[PB[bk]])
                evac_copy(memk[:, g4, :], psum[bk][:, 0:256], [PB[bk]], [MEMK])
            else:
                for j in range(2):
                    for c in range(8):
                        S.op("pe", lambda e, c=c, j=j, w3=w3, bk=bk: e.matmul(
                            psum[bk][:, j * 128:(j + 1) * 128], lhsT=memT[:, c, j * 128:(j + 1) * 128], rhs=w3[:, c, :],
                            start=(c == 0), stop=(c == 7)), reads=[W, MEMT], writes=[PB[bk]])
                evac_copy(memv[:, :, (g4 - 2) * 128:(g4 - 1) * 128], psum[bk][:, 0:256].rearrange("p (j g) -> p j g", j=2),
                          [PB[bk]], [MEMV])
            bput(bk)

        for tq in range(NQ):
            chunk_phase(u, l, b, tq, src_x, dst_x)

    def chunk_phase(u, l, b, tq, src_x, dst_x):
        t0 = tq * 512
        hn = hnmix[:].rearrange("p (i d) -> p i d", i=4)
        mix = hnmix[:].rearrange("p (c t) -> p c t", c=8)
        xc3 = xc[:].rearrange("p (i d) -> p i d", i=4)
        rd = [XC]
        if src_x is scr_d:
            rd = [XC] + [SCRB[b][tq]]
        S.dma(lambda e: e.dma_start(out=xc3, in_=src_x[b, t0:t0 + 512, :].rearrange("(i p) d -> p i d", p=128)),
              sl_x, reads=rd[1:], writes=[XC])
        for i in range(4):
            S.op("act", lambda e, i=i: e.activation(out=junk[:, 0:1024], in_=xc3[:, i, :], func=AF.Square,
                                                    accum_out=ss[:, i:i + 1]), reads=[XC], writes=[JUNK, SMS["ss"]])
        rms_scale(ss, rs, 4, 1.0 / D, SMS["ss"], SMS["rs"])
        for i in range(4):
            S.op("dve", lambda e, i=i: e.tensor_scalar(out=hn[:, i, :], in0=xc3[:, i, :], scalar1=rs[:, i:i + 1],
                                                       scalar2=None, op0=ALU.mult),
                 reads=[XC, SMS["rs"]], writes=[HNMIX])
        for c2 in range(4):
            bk = bget()
            pb = psum[bk][:].bitcast(BF16)
            for cc in range(2):
                c = 2 * c2 + cc
                for i in range(4):
                    S.op("pe", lambda e, c=c, i=i, cc=cc, pb=pb: e.transpose(
                        pb[:, cc * 512 + i * 128:cc * 512 + (i + 1) * 128], hn[:, i, c * 128:(c + 1) * 128], ident),
                        reads=[HNMIX, CBb], writes=[PB[bk]])
            for cc in range(2):
                c = 2 * c2 + cc
                S.op("dve", lambda e, c=c, cc=cc, pb=pb: e.tensor_scalar(
                    out=hT[:, c, :], in0=pb[:, cc * 512:(cc + 1) * 512], scalar1=gcol[:, c:c + 1], scalar2=None,
                    op0=ALU.mult), reads=[PB[bk], SMF], writes=[HT])
            bput(bk)

        if DBG_STAGE < 2:
            return
        fdest = ([("sbq", i) for i in range(3)] + [("sbk", i) for i in range(3)] + [("sbg", i) for i in range(3)]
                 + [("dq", i) for i in range(3)] + [("dg", i) for i in range(3)] + [("iq", 0), ("iq", 1), ("ik", 0)]
                 + [("mq", 0), ("mq", 1), ("mg", 0), ("mg", 1)])
        dst_tab = {"sbq": (sbq, SBQ), "sbg": (sbg, SBG), "dq": (dq, DQ), "dg": (dg, DG), "iq": (iq, IQ),
                   "mq": (mq, MQ), "mg": (mg, MG)}
        ptb = None
        for cc, (w, W) in enumerate(wstream(l, list(range(26)))):
            w3 = w[:].rearrange("p (c g) -> p c g", c=8)
            if cc < 22:
                name, ci = fdest[cc]
                bk = bget()
                for c in range(8):
                    S.op("pe", lambda e, c=c, w3=w3, bk=bk: e.matmul(psum[bk][:, :], lhsT=w3[:, c, :], rhs=hT[:, c, :],
                                                                    start=(c == 0), stop=(c == 7)),
                         reads=[W, HT], writes=[PB[bk]])
                if name == "sbk":
                    evac_copy(sbkT[:, ci, t0:t0 + 512], psum[bk][:, :], [PB[bk]], [SBK[tq]])
                elif name == "ik":
                    evac_copy(ikT4[:, t0:t0 + 512], psum[bk][:, :], [PB[bk]], [IKT[tq]])
                elif name in ("sbg", "dg", "mg") and DBG_SUB >= 2:
                    dt_, DB = dst_tab[name]
                    hs = rr("ev", 2) * 512
                    S.op("act", lambda e, bk=bk, hs=hs: e.activation(out=tmpf[:, hs:hs + 512], in_=psum[bk][:, :], func=AF.Exp,
                                                                     scale=-1.0), reads=[PB[bk]], writes=[TMPF])
                    S.op("act", lambda e, hs=hs: e.activation(out=tmpf[:, hs:hs + 512], in_=tmpf[:, hs:hs + 512], func=AF.Ln, bias=1.0),
                         reads=[TMPF], writes=[TMPF])
                    S.op("act", lambda e, hs=hs: e.activation(out=tmpf[:, hs:hs + 512], in_=tmpf[:, hs:hs + 512], func=AF.Exp, scale=-1.0),
                         reads=[TMPF], writes=[TMPF])
                    S.op("dve", lambda e, dt_=dt_, ci=ci, bk=bk, hs=hs: e.tensor_tensor(
                        out=dt_[:, ci, :], in0=psum[bk][:, :], in1=tmpf[:, hs:hs + 512], op=ALU.mult),
                        reads=[PB[bk], TMPF], writes=[DB])
                else:
                    dt_, DB = dst_tab[name]
                    evac_copy(dt_[:, ci, :], psum[bk][:, :], [PB[bk]], [DB])
                bput(bk)
            elif DBG_SUB >= 30:
                g4 = cc - 22
                if g4 == 0:
                    ptb = [bget() for _ in range(4)]
                for i in range(4):
                    for c in range(8):
                        S.op("pe", lambda e, c=c, i=i, w3=w3, g4=g4: e.matmul(
                            psum[ptb[i]][:, g4 * 128:(g4 + 1) * 128], lhsT=hT[:, c, i * 128:(i + 1) * 128], rhs=w3[:, c, :],
                            start=(c == 0), stop=(c == 7)), reads=[W, HT], writes=[PB[ptb[i]]])
        if DBG_SUB < 30:
            return
        for i in range(4):
            j = 4 * tq + i
            evac_copy(sbv[:, j, :], psum[ptb[i]][:, 0:384], [PB[ptb[i]]], [SBV[tq]])
            evac_copy(tmpf[:, i * 128:(i + 1) * 128], psum[ptb[i]][:, 384:512], [PB[ptb[i]]], [TMPF])
        for i in range(4):
            S.op("act", lambda e, i=i: e.activation(out=junk[:, 0:128], in_=tmpf[:, i * 128:(i + 1) * 128], func=AF.Square,
                                                    accum_out=ssk[:, i:i + 1]),
                 reads=[TMPF], writes=[JUNK, SMS["ssk"]])
        rms_scale(ssk, rsk, 4, 1.0 / 128, SMS["ssk"], SMS["rsk"])
        for i in range(4):
            j = 4 * tq + i
            S.op("dve", lambda e, i=i, j=j: e.scalar_tensor_tensor(out=ckv[:, j, :], in0=tmpf[:, i * 128:(i + 1) * 128],
                                                                   scalar=rsk[:, i:i + 1], in1=kvg[:, :],
                                                                   op0=ALU.mult, op1=ALU.mult),
                 reads=[TMPF, SMS["rsk"], SMF], writes=[CKV[tq]])
        for i in range(4):
            bput(ptb[i])
        if DBG_SUB < 40:
            return
        bk = bget()
        pb = psum[bk][:].bitcast(BF16)
        for i in range(4):
            j = 4 * tq + i
            S.op("pe", lambda e, i=i, j=j, pb=pb: e.transpose(pb[:, i * 128:(i + 1) * 128], ckv[:, j, :], ident),
                 reads=[CKV[tq], CBb], writes=[PB[bk]])
        evac_copy(ckvT[:, t0:t0 + 512], pb[:, 0:512], [PB[bk]], [CKVT[tq]])
        bput(bk)
        if DBG_SUB < 50:
            return
        bk = bget()
        wiw3 = wiw[:].rearrange("p (c g) -> p c g", c=8)
        for i in range(4):
            for c in range(8):
                S.op("pe", lambda e, c=c, i=i, bk=bk: e.matmul(psum[bk][:, i * 8:(i + 1) * 8],
                                                                lhsT=hT[:, c, i * 128:(i + 1) * 128], rhs=wiw3[:, c, :],
                                                                start=(c == 0), stop=(c == 7)),
                     reads=[SMB, HT], writes=[PB[bk]])
        S.op("dve", lambda e, bk=bk: e.tensor_scalar(out=idxw[:, :], in0=psum[bk][:, 0:32], scalar1=1.0 / 16, scalar2=None,
                                                     op0=ALU.mult), reads=[PB[bk]], writes=[IDXW])
        bput(bk)

        if DBG_STAGE < 3:
            return
        nkb = 4 * tq + 4
        KS = lambda lst: [lst[q] for q in range(tq + 1)]

        def indexer(i):
            qb = 4 * tq + i
            if qb < 2:
                return
            yield
            nk = (qb + 1) * 128
            nkc = (nk + 511) // 512
            kd = rr("d", 2)
            for h in range(8):
                S.op("act", lambda e, h=h, kd=kd: e.activation(out=dgt[kd][:, h, :], in_=ident, func=AF.Copy,
                                                               scale=idxw[:, i * 8 + h:i * 8 + h + 1]),
                     reads=[CBb, IDXW], writes=[DGT[kd]])
            for kc in range(nkc):
                w_ = min(512, nk - kc * 512)
                bs = bget()
                pend = []
                for h in range(8):
                    bd = bget()
                    hc, hp = h // 4, (h % 4) * 32
                    tp = (96, 0) if hp == 96 else None
                    S.op("pe", lambda e, hc=hc, hp=hp, tp=tp, bd=bd, kc=kc, w_=w_: e.matmul(
                        psum[bd][:, 0:w_], lhsT=iq[hp:hp + 32, hc, i * 128:(i + 1) * 128],
                        rhs=ikT4[hp:hp + 32, kc * 512:kc * 512 + w_], start=True, stop=True, tile_position=tp),
                        reads=[IQ] + KS(IKT), writes=[PB[bd]])
                    kr = rr("r", NR)
                    if True:
                        S.op("dve", lambda e, kr=kr, bd=bd, w_=w_: e.tensor_scalar(out=rt[kr][:, 0:w_], in0=psum[bd][:, 0:w_],
                                                                                  scalar1=0.0, scalar2=None, op0=ALU.max),
                             reads=[PB[bd]], writes=[RT[kr]])
                    else:
                        S.op("act", lambda e, kr=kr, bd=bd, w_=w_: e.activation(out=rt[kr][:, 0:w_], in_=psum[bd][:, 0:w_],
                                                                               func=AF.Relu),
                             reads=[PB[bd]], writes=[RT[kr]])
                    bput(bd)
                    pend.append(lambda h=h, kr=kr, bs=bs, w_=w_: S.op("pe", lambda e: e.matmul(
                        psum[bs][:, 0:w_], lhsT=dgt[kd][:, h, :], rhs=rt[kr][:, 0:w_], start=(h == 0), stop=(h == 7)),
                        reads=[DGT[kd], RT[kr]], writes=[PB[bs]]))
                    if len(pend) > 2:
                        pend.pop(0)()
                    yield
                while pend:
                    pend.pop(0)()
                last = (kc == nkc - 1)
                wc = w_ - 128 if last else w_
                if wc > 0:
                    S.op("dve", lambda e, bs=bs, kc=kc, wc=wc: e.tensor_copy(out=score[:, kc * 512:kc * 512 + wc],
                                                                             in_=psum[bs][:, 0:wc]),
                         reads=[PB[bs]], writes=[SCORE])
                if last:
                    S.op("dve", lambda e, bs=bs, w_=w_: e.tensor_tensor(out=score[:, nk - 128:nk], in0=psum[bs][:, w_ - 128:w_],
                                                                       in1=negb, op=ALU.add),
                         reads=[PB[bs], CFb], writes=[SCORE])
                bput(bs)
            B = SMS["bis"]
            S.op("dve", lambda e: e.tensor_reduce(out=mx, in_=score[:, 0:nk], axis=AX.X, op=ALU.max), reads=[SCORE], writes=[B])
            S.op("dve", lambda e: e.tensor_reduce(out=mn, in_=score[:, 0:nk - 128], axis=AX.X, op=ALU.min),
                 reads=[SCORE], writes=[B])
            S.op("dve", lambda e: e.tensor_tensor(out=rng, in0=mx, in1=mn, op=ALU.subtract), reads=[B], writes=[B])
            S.op("dve", lambda e: e.tensor_tensor(out=thr, in0=mx, in1=mn, op=ALU.add), reads=[B], writes=[B])
            S.op("dve", lambda e: e.tensor_scalar(out=thr, in0=thr, scalar1=0.5, scalar2=None, op0=ALU.mult), reads=[B], writes=[B])
            S.op("dve", lambda e: e.tensor_scalar(out=steps[:, 0:16], in0=p2a, scalar1=rng, scalar2=None, op0=ALU.mult),
                 reads=[B, CFb], writes=[STEPS])
            S.op("dve", lambda e: e.tensor_scalar(out=steps[:, 16:32], in0=p2b, scalar1=rng, scalar2=None, op0=ALU.mult),
                 reads=[B, CFb], writes=[STEPS])
            for it in range(NBIS):
                S.op("dve", lambda e: e.tensor_scalar(out=junk[:, 0:nk], in0=score[:, 0:nk], scalar1=thr, scalar2=0.0,
                                                      op0=ALU.is_ge, op1=ALU.add, accum_out=cnt),
                     reads=[SCORE, B], writes=[JUNK, B])
                S.op("dve", lambda e, it=it: e.tensor_scalar(out=dd, in0=cnt, scalar1=float(TOPK), scalar2=steps[:, 16 + it:17 + it],
                                                             op0=ALU.is_ge, op1=ALU.mult), reads=[B, STEPS], writes=[B])
                S.op("dve", lambda e, it=it: e.scalar_tensor_tensor(out=thr, in0=dd, scalar=steps[:, it:it + 1], in1=thr,
                                                                    op0=ALU.subtract, op1=ALU.add),
                     reads=[B, STEPS], writes=[B])
                yield
                yield
            S.op("dve", lambda e: e.tensor_scalar(out=pen[:, i, 0:nk], in0=score[:, 0:nk], scalar1=thr, scalar2=NEG,
                                                  op0=ALU.is_lt, op1=ALU.mult), reads=[SCORE, B], writes=[PEN[i]])

        def sb_head(h):
            ch, hp = h // 2, (h % 2) * 64
            bcs = bget()
            pend = []
            for kb in range(nkb):
                c0 = max(0, kb - 4 * tq) * 128
                diag = kb >= 4 * tq
                bz = bget()
                S.op("pe", lambda e, kb=kb, c0=c0, bz=bz, diag=diag: e.matmul(
                    psum[bz][:, c0:512], lhsT=sbkT[hp:hp + 64, ch, kb * 128:(kb + 1) * 128], rhs=sbq[hp:hp + 64, ch, c0:512],
                    start=True, stop=not diag), reads=KS(SBK) + [SBQ], writes=[PB[bz]])
                if diag:
                    S.op("pe", lambda e, c0=c0, bz=bz: e.matmul(psum[bz][:, c0:c0 + 128], lhsT=penS, rhs=ident,
                                                                 start=False, stop=True), reads=[CBb], writes=[PB[bz]])
                ke = rr("e", NT)
                S.op("act", lambda e, ke=ke, c0=c0, bz=bz: e.activation(out=etile[ke][:, c0:512], in_=psum[bz][:, c0:512],
                                                                        func=AF.Exp, scale=0.125),
                     reads=[PB[bz]], writes=[ET[ke]])
                bput(bz)
                sp = big16[:, kb * 512:(kb + 1) * 512]
                S.op("act", lambda e, ke=ke, c0=c0, sp=sp: e.activation(out=sp[:, c0:512], in_=etile[ke][:, c0:512],
                                                                        func=AF.Ln, bias=1.0),
                     reads=[ET[ke]], writes=[BIG[kb]])
                pend.append(lambda kb=kb, c0=c0, sp=sp: S.op("pe", lambda e: e.matmul(
                    psum[bcs][0:16, c0:512], lhsT=ekb(kb), rhs=sp[:, c0:512], start=(kb == 0), stop=(kb == nkb - 1)),
                    reads=[CBb, BIG[kb]], writes=[PB[bcs]]))
                if len(pend) > SKEW:
                    pend.pop(0)()
                yield
            while pend:
                pend.pop(0)()
            S.op("dve", lambda e: e.tensor_copy(out=cshi[:, :], in_=psum[bcs][0:16, :]), reads=[PB[bcs]], writes=[CS])
            S.op("dve", lambda e: e.tensor_tensor(out=cslo[:, :], in0=psum[bcs][0:16, :], in1=cshi[:, :], op=ALU.subtract),
                 reads=[PB[bcs], CS], writes=[CS])
            bput(bcs)
            bo = bget()
            pend = []
            for kb in range(nkb):
                c0 = max(0, kb - 4 * tq) * 128
                diag = kb >= 4 * tq
                bi = bget()
                sp = big16[:, kb * 512:(kb + 1) * 512]
                S.op("pe", lambda e, kb=kb, c0=c0, bi=bi: e.matmul(
                    psum[bi][:, c0:512], lhsT=sbkT[hp:hp + 64, ch, kb * 128:(kb + 1) * 128], rhs=sbq[hp:hp + 64, ch, c0:512],
                    start=True, stop=False), reads=KS(SBK) + [SBQ], writes=[PB[bi]])
                S.op("pe", lambda e, c0=c0, bi=bi, sp=sp: e.matmul(psum[bi][:, c0:512], lhsT=triM8, rhs=sp[:, c0:512],
                                                                   start=False, stop=False),
                     reads=[CBb, BIG[kb]], writes=[PB[bi]])
                S.op("pe", lambda e, kb=kb, c0=c0, bi=bi: e.matmul(psum[bi][:, c0:512], lhsT=selM8(kb), rhs=cshi[:, c0:512],
                                                                   start=False, stop=False),
                     reads=[CBb, CS], writes=[PB[bi]])
                S.op("pe", lambda e, kb=kb, c0=c0, bi=bi, diag=diag: e.matmul(psum[bi][:, c0:512], lhsT=selM8(kb),
                                                                              rhs=cslo[:, c0:512], start=False, stop=not diag),
                     reads=[CBb, CS], writes=[PB[bi]])
                if diag:
                    S.op("pe", lambda e, c0=c0, bi=bi: e.matmul(psum[bi][:, c0:c0 + 128], lhsT=penS, rhs=ident,
                                                                 start=False, stop=True), reads=[CBb], writes=[PB[bi]])
                ka = rr("a", NT)
                S.op("act", lambda e, ka=ka, c0=c0, bi=bi: e.activation(out=atile[ka][:, c0:512], in_=psum[bi][:, c0:512],
                                                                        func=AF.Exp, scale=0.125),
                     reads=[PB[bi]], writes=[AT[ka]])
                bput(bi)
                pend.append(lambda kb=kb, c0=c0, ka=ka: S.op("pe", lambda e: e.matmul(
                    psum[bo][:, c0:512], lhsT=sbv[:, kb, ch * 128:(ch + 1) * 128], rhs=atile[ka][:, c0:512],
                    start=(kb == 0), stop=(kb == nkb - 1)), reads=KS(SBV) + [AT[ka]], writes=[PB[bo]]))
                if len(pend) > SKEW:
                    pend.pop(0)()
                yield
            while pend:
                pend.pop(0)()
            S.op("dve", lambda e: e.tensor_tensor(out=mix[hp:hp + 64, ch, :], in0=psum[bo][hp:hp + 64, :],
                                                  in1=sbg[hp:hp + 64, ch, :], op=ALU.mult),
                 reads=[PB[bo], SBG], writes=[HNMIX])
            bput(bo)

        def mem_head(hm):
            ch, hp = hm // 2, (hm % 2) * 64
            bo = bget(); bd = bget()
            for mb in range(2):
                bl = bget()
                S.op("pe", lambda e, mb=mb, bl=bl: e.matmul(psum[bl][:, :], lhsT=memk[hp:hp + 64, ch, mb * 128:(mb + 1) * 128],
                                                            rhs=mq[hp:hp + 64, ch, :], start=True, stop=True),
                     reads=[MEMK, MQ], writes=[PB[bl]])
                kp = rr("p", NT)
                S.op("act", lambda e, kp=kp, bl=bl: e.activation(out=ptile[kp][:, :], in_=psum[bl][:, :], func=AF.Exp, scale=0.125),
                     reads=[PB[bl]], writes=[PT[kp]])
                bput(bl)
                S.op("pe", lambda e, mb=mb, kp=kp: e.matmul(psum[bo][:, :], lhsT=memv[:, mb, ch * 128:(ch + 1) * 128],
                                                            rhs=ptile[kp][:, :], start=(mb == 0), stop=(mb == 1)),
                     reads=[MEMV, PT[kp]], writes=[PB[bo]])
                S.op("pe", lambda e, mb=mb, kp=kp: e.matmul(psum[bd][:, :], lhsT=ones, rhs=ptile[kp][:, :],
                                                            start=(mb == 0), stop=(mb == 1)),
                     reads=[CBb, PT[kp]], writes=[PB[bd]])
            S.op("act", lambda e: e.activation(out=recf[hp:hp + 64, :], in_=psum[bd][hp:hp + 64, :], func=AF.Ln), reads=[PB[bd]], writes=[RECF])
            S.op("act", lambda e: e.activation(out=recf[hp:hp + 64, :], in_=recf[hp:hp + 64, :], func=AF.Exp, scale=-1.0), reads=[RECF], writes=[RECF])
            S.op("dve", lambda e: e.tensor_tensor(out=tmpf[hp:hp + 64, 0:512], in0=psum[bo][hp:hp + 64, :],
                                                  in1=recf[hp:hp + 64, :], op=ALU.mult), reads=[PB[bo], RECF], writes=[TMPF])
            S.op("dve", lambda e: e.tensor_tensor(out=mix[hp:hp + 64, 6 + ch, :], in0=tmpf[hp:hp + 64, 0:512],
                                                  in1=mg[hp:hp + 64, ch, :], op=ALU.mult), reads=[TMPF, MG], writes=[HNMIX])
            bput(bo); bput(bd)

        def dsa_head(h):
            ch, hp = h // 2, (h % 2) * 64
            bq = bget()
            S.op("pe", lambda e: e.matmul(psum[bq][:, :], lhsT=wuk[hp:hp + 64, ch * 128:(ch + 1) * 128], rhs=dq[hp:hp + 64, ch, :],
                                          start=True, stop=True), reads=[SMB, DQ], writes=[PB[bq]])
            kq = rr("q", 2)
            evac_copy(qlat[kq][:, :], psum[bq][:, :], [PB[bq]], [QL[kq]])
            bput(bq)
            bo = bget(); bd = bget()
            pend = []
            for kb in range(nkb):
                i0 = max(0, kb - 4 * tq)
                c0 = i0 * 128
                bl = bget()
                mm = []
                mm.append((lambda e, st, sp_, kb=kb, c0=c0, bl=bl: e.matmul(
                    psum[bl][:, c0:512], lhsT=ckvT[:, kb * 128:(kb + 1) * 128], rhs=qlat[kq][:, c0:512], start=st, stop=sp_),
                    KS(CKVT) + [QL[kq]]))
                for i in range(i0, 4):
                    qb = 4 * tq + i
                    cs_ = slice(i * 128, (i + 1) * 128)
                    if qb >= 2:
                        mm.append((lambda e, st, sp_, i=i, kb=kb, cs_=cs_, bl=bl: e.matmul(
                            psum[bl][:, cs_], lhsT=pen[:, i, kb * 128:(kb + 1) * 128], rhs=ident, start=st, stop=sp_),
                            [PEN[i], CBb]))
                    elif qb == kb:
                        mm.append((lambda e, st, sp_, cs_=cs_, bl=bl: e.matmul(psum[bl][:, cs_], lhsT=penD, rhs=ident,
                                                                              start=st, stop=sp_), [CBb]))
                    if qb - kb <= 1:
                        jj = qb - kb
                        mm.append((lambda e, st, sp_, cs_=cs_, jj=jj, bl=bl: e.matmul(
                            psum[bl][:, cs_], lhsT=ident, rhs=tb8[:, (h * 2 + jj) * 128:(h * 2 + jj + 1) * 128],
                            start=st, stop=sp_), [CBb, SMB]))
                for n_, (fn, rds) in enumerate(mm):
                    S.op("pe", lambda e, fn=fn, n_=n_, nm=len(mm): fn(e, n_ == 0, n_ == nm - 1), reads=rds, writes=[PB[bl]])
                kp = rr("p", NT)
                near_hi = min(512, max(c0, (kb + 2 - 4 * tq) * 128))
                if near_hi > c0:
                    S.op("act", lambda e, kp=kp, c0=c0, near_hi=near_hi, bl=bl: e.activation(
                        out=ptile[kp][:, c0:near_hi], in_=psum[bl][:, c0:near_hi], func=AF.Exp, scale=0.125),
                        reads=[PB[bl]], writes=[PT[kp]])
                if near_hi < 512:
                    S.op("act", lambda e, kp=kp, near_hi=near_hi, bl=bl: e.activation(
                        out=ptile[kp][:, near_hi:512], in_=psum[bl][:, near_hi:512], func=AF.Exp, scale=0.125,
                        bias=b31[:, h:h + 1]), reads=[PB[bl], SMF], writes=[PT[kp]])
                bput(bl)
                def tail(kb=kb, c0=c0, kp=kp):
                    S.op("pe", lambda e: e.matmul(psum[bo][:, c0:512], lhsT=ckv[:, kb, :], rhs=ptile[kp][:, c0:512],
                                                  start=(kb == 0), stop=(kb == nkb - 1)),
                         reads=KS(CKV) + [PT[kp]], writes=[PB[bo]])
                    S.op("pe", lambda e: e.matmul(psum[bd][:, c0:512], lhsT=ones, rhs=ptile[kp][:, c0:512],
                                                  start=(kb == 0), stop=(kb == nkb - 1)),
                         reads=[CBb, PT[kp]], writes=[PB[bd]])
                pend.append(tail)
                if len(pend) > SKEW:
                    pend.pop(0)()
                yield
            while pend:
                pend.pop(0)()
            S.op("act", lambda e: e.activation(out=recf[:, :], in_=psum[bd][:, :], func=AF.Ln), reads=[PB[bd]], writes=[RECF])
            S.op("act", lambda e: e.activation(out=recf[:, :], in_=recf[:, :], func=AF.Exp, scale=-1.0), reads=[RECF], writes=[RECF])
            S.op("dve", lambda e: e.tensor_tensor(out=onb[:, :], in0=psum[bo][:, :], in1=recf[:, :], op=ALU.mult),
                 reads=[PB[bo], RECF], writes=[ONB])
            bput(bo); bput(bd)
            bu = bget()
            S.op("pe", lambda e: e.matmul(psum[bu][:, :], lhsT=wuv[:, ch * 128:(ch + 1) * 128], rhs=onb[:, :], start=True, stop=True),
                 reads=[SMB, ONB], writes=[PB[bu]])
            S.op("dve", lambda e: e.tensor_tensor(out=mix[hp:hp + 64, 3 + ch, :], in0=psum[bu][hp:hp + 64, :],
                                                  in1=dg[hp:hp + 64, ch, :], op=ALU.mult), reads=[PB[bu], DG], writes=[HNMIX])
            bput(bu)

        def run_par(*gens):
            gens = list(gens)
            while gens:
                for g in list(gens):
                    try:
                        next(g)
                    except StopIteration:
                        gens.remove(g)

        def seq(*fns):
            for f in fns:
                r = f()
                if r is not None:
                    yield from r
                yield

        run_par(seq(*[lambda i=i: indexer(i) for i in range(4)]),
                seq(*([lambda h=h: sb_head(h) for h in range(5)] + [lambda hm=hm: mem_head(hm) for hm in range(4)])))
        run_par(sb_head(5), seq(lambda: dsa_head(0), lambda: dsa_head(1)))
        wO3 = big16[:].rearrange("p (c n) -> p c n", c=8)
        for c in range(8):
            S.dma(lambda e, c=c: e.dma_start(out=big16[:, c * 1024:(c + 1) * 1024], in_=wsc_d[l, 30 + c, :, :]), sl_big[c],
                  reads=[WSC[l][30 + c]], writes=[BIG[2 * c], BIG[2 * c + 1]])
        for h in range(2, 6):
            run_par(dsa_head(h))

        for i in range(4):
            b0 = bget(); b1 = bget()
            bb = (b0, b1)
            for half in range(2):
                for c in range(8):
                    S.op("pe", lambda e, c=c, half=half, i=i, bb=bb: e.matmul(
                        psum[bb[half]][:, :], lhsT=mix[:, c, i * 128:(i + 1) * 128], rhs=wO3[:, c, half * 512:(half + 1) * 512],
                        start=(c == 0), stop=(c == 7)), reads=[HNMIX, BIG[2 * c + half]], writes=[PB[bb[half]]])
            for half in range(2):
                evac_copy(tmpf[:, half * 512:(half + 1) * 512], psum[bb[half]][:, :], [PB[bb[half]]], [TMPF])
            S.op("act", lambda e: e.activation(out=junk[:, 0:1024], in_=tmpf[:, :], func=AF.Square, accum_out=ssy2),
                 reads=[TMPF], writes=[JUNK, SMS["ssy"]])
            rms_scale(ssy2, rsy, 1, 1.0 / D, SMS["ssy"], SMS["ssy"])
            S.op("dve", lambda e: e.scalar_tensor_tensor(out=tmpf[:, :], in0=tmpf[:, :], scalar=rsy, in1=postg[:, :],
                                                         op0=ALU.mult, op1=ALU.mult),
                 reads=[TMPF, SMS["ssy"], SMF], writes=[TMPF])
            bput(b0); bput(b1)
            S.op("pool", lambda e, i=i: e.tensor_tensor(out=xc3[:, i, :], in0=tmpf[:, :], in1=xc3[:, i, :], op=ALU.add),
                 reads=[TMPF, XC], writes=[XC])
        wr = [OUTB] if dst_x is out_d else [SCRB[b][tq]]
        S.dma(lambda e: e.dma_start(out=dst_x[b, t0:t0 + 512, :].rearrange("(i p) d -> p i d", p=128), in_=xc3),
              sl_o, reads=[XC], writes=wr)

    for u in range(len(layer_of_unit)):
        unit(u)
    S.final_wait("sp", [OUTB, XC])
    S.emit()
    S.close()
    for g in reversed(ctx):
        g.__exit__(None, None, None)
    return nc


def _t5_bucket(rel):
    n = np.maximum(rel, 0)
    max_exact = 16
    nf = np.maximum(n, 1).astype(np.float32)
    large = max_exact + (np.log(nf / max_exact) / math.log(128 / max_exact) * (32 - max_exact)).astype(np.int32)
    large = np.minimum(large, 31)
    return np.where(n < max_exact, n, large)


def _constants():
    bf = ml_dtypes.bfloat16
    cbv = np.zeros((128, NCB), np.float32)
    p = np.arange(128)
    cbv[:, CB_ID:CB_ID + 128] = np.eye(128)
    cbv[:, CB_TRI:CB_TRI + 128] = np.where(p[:, None] >= p[None, :], -8.0, 0.0)
    cbv[:, CB_ONES:CB_ONES + 128] = 1.0
    cbv[:, CB_PENS:CB_PENS + 128] = np.where(p[None, :] < p[:, None], 0.0, NEG)
    cbv[:, CB_PEND:CB_PEND + 128] = np.where(p[None, :] <= p[:, None], 0.0, NEG)
    for kb in range(16):
        cbv[:, CB_EKB + kb * 16 + kb] = 1.0
        for jb in range(16):
            if jb > kb:
                cbv[jb, CB_SEL + kb * 128:CB_SEL + (kb + 1) * 128] = -8.0
    cfv = np.zeros((128, NCF), np.float32)
    cfv[:, CF_NEGB:CF_NEGB + 128] = np.where(p[None, :] <= p[:, None], 0.0, -1e30)
    cfv[:, CF_P2A:CF_P2A + 16] = 2.0 ** -(np.arange(16) + 2.0)
    cfv[:, CF_P2B:CF_P2B + 16] = 2.0 ** -(np.arange(16) + 1.0)
    return cfv, cbv.astype(bf)


def _chunk_cols(w, cols):
    sel = w[:, cols]
    n = sel.shape[1] // 128
    a = sel.reshape(8, 128, n, 128)
    return np.ascontiguousarray(a.transpose(2, 1, 0, 3).reshape(n, 128, 1024))


def _prep_weights(pre_norm_g, post_norm_g, w_in, w_uk, w_uv, kv_norm_g, w_mem_kv, w_out, rel_bias, layers):
    o = np.cumsum([0, 384, 384, 384, 384, 384, 128, 384, 256, 32, 8, 256, 256])
    (o_sbq, o_sbk, o_sbv, o_sbg, o_dq, o_ckv, o_dg, o_iq, o_ik, o_iw, o_mq, o_mg) = o[:12]
    r = lambda a, n: list(range(a, a + n))
    fcols = (r(o_sbq, 384) + r(o_sbk, 384) + r(o_sbg, 384) + r(o_dq, 384) + r(o_dg, 384) + r(o_iq, 256)
             + r(o_ik, 32) * 4 + r(o_mq, 256) + r(o_mg, 256) + r(o_sbv, 384) + r(o_ckv, 128))
    assert len(fcols) == 26 * 128
    s_l = np.arange(128)
    wF, wO, wM, wsm = [], [], [], []
    for l in layers:
        wF.append(_chunk_cols(w_in[l], fcols))
        wO.append(np.ascontiguousarray(w_out[l].reshape(8, 128, 1024)))
        wM.append(_chunk_cols(w_mem_kv[l], list(range(512))))
        sm = np.zeros((128, NSM), np.float32)
        sm[:, SM_GCOL:SM_GCOL + 8] = pre_norm_g[l].reshape(8, 128).T
        sm[:, SM_IW:SM_IW + 64] = w_in[l][:, o_iw:o_iw + 8].reshape(8, 128, 8).transpose(1, 0, 2).reshape(128, 64)
        uk = w_uk[l]
        t = uk.reshape(128, 3, 2, 64).transpose(2, 3, 1, 0).reshape(128, 3 * 128)
        sm[:, SM_WUK:SM_WUK + 384] = t
        sm[:, SM_WUV:SM_WUV + 384] = w_uv[l].reshape(128, 384)
        sm[:, SM_KVG:SM_KVG + 128] = kv_norm_g[l][None, :]
        sm[:, SM_B31:SM_B31 + 6] = rel_bias[31][None, :]
        sm[:, SM_POSTG:SM_POSTG + 1024] = post_norm_g[l][None, :]
        for h in range(6):
            for j in range(2):
                rel = s_l[None, :] - s_l[:, None] + 128 * j
                sm[:, SM_TB + (h * 2 + j) * 128:SM_TB + (h * 2 + j + 1) * 128] = rel_bias[_t5_bucket(rel), h]
        wsm.append(sm)
    return (np.stack(wF), np.stack(wO), np.stack(wM), np.stack(wsm))


_PROG_CACHE = {}


def _get_prog(key, *args):
    if key not in _PROG_CACHE:
        _PROG_CACHE[key] = build_program(*args)
    return _PROG_CACHE[key]


FUSED = True


def kernel(x, mem, pre_norm_g, post_norm_g, w_in, w_uk, w_uv, kv_norm_g, w_mem_kv, w_out, rel_bias):
    x = np.asarray(x, np.float32)
    mem = np.asarray(mem, np.float32)
    args = [np.asarray(a, np.float32) for a in (pre_norm_g, post_norm_g, w_in, w_uk, w_uv, kv_norm_g, w_mem_kv, w_out, rel_bias)]
    B, S_TOK, _ = x.shape
    depth = w_in.shape[0]
    per = B // N_CORES
    cfv, cbv = _constants()
    if FUSED:
        units_l = []; units_b = []; chain = []
        for b in range(per):
            for l in range(depth):
                units_l.append(l); units_b.append(b); chain.append("in" if l == 0 else "scr")
        nc = _get_prog(("fused", S_TOK, per, depth), S_TOK, per, units_l, units_b, chain)
        wF, wO, wM, wsm = _prep_weights(*args, layers=list(range(depth)))
        in_maps = [{"x": np.ascontiguousarray(x[c * per:(c + 1) * per]), "mem": np.ascontiguousarray(mem[c * per:(c + 1) * per]),
                    "wF": wF, "wO": wO, "wM": wM, "wsm": wsm, "cf32": cfv, "cbf": cbv} for c in range(N_CORES)]
        res = run_bass_kernel_spmd(nc, in_maps, core_ids=list(range(N_CORES)))
        return np.concatenate([r["out"] for r in res.results], axis=0)
    cur = x
    nc = _get_prog(("unit", S_TOK), S_TOK, 1, [0], [0], ["in"])
    for l in range(depth):
        wF, wO, wM, wsm = _prep_weights(*args, layers=[l])
        nxt = np.empty_like(cur)
        for b in range(per):
            in_maps = [{"x": np.ascontiguousarray(cur[c * per + b:c * per + b + 1]),
                        "mem": np.ascontiguousarray(mem[c * per + b:c * per + b + 1]),
                        "wF": wF, "wO": wO, "wM": wM, "wsm": wsm, "cf32": cfv, "cbf": cbv} for c in range(N_CORES)]
            res = run_bass_kernel_spmd(nc, in_maps, core_ids=list(range(N_CORES)))
            for c in range(N_CORES):
                nxt[c * per + b] = res.results[c]["out"][0]
        cur = nxt
    return cur
```

```python
import math
import numpy as np
import ml_dtypes
import concourse.bass as bass
import concourse.mybir as mybir
from concourse.bass_utils import run_bass_kernel_spmd

F32 = mybir.dt.float32
BF16 = mybir.dt.bfloat16
AF = mybir.ActivationFunctionType
ALU = mybir.AluOpType
AX = mybir.AxisListType

D = 1024
NMEM = 256
TOPK = 256
NBIS = 13
EPS = 1e-6
NEG = -30000.0
N_CORES = 8
DBG_STAGE = 99
DBG_SUB = 99
SKEW = 1

SM_GCOL, SM_IW, SM_WUK, SM_WUV, SM_KVG, SM_B31, SM_POSTG, SM_TB = 0, 8, 72, 456, 840, 968, 974, 1998
NSM = 1998 + 6 * 2 * 128
CB_ID, CB_TRI, CB_ONES, CB_PENS, CB_PEND, CB_EKB, CB_SEL = 0, 128, 256, 384, 512, 640, 896
NCB = 896 + 16 * 128
CF_NEGB, CF_P2A, CF_P2B = 0, 128, 144
NCF = 160


class Buf:
    __slots__ = ("name", "w", "r", "excl")

    def __init__(self, name, excl=False):
        self.name = name
        self.w = None
        self.r = {}
        self.excl = excl


class Sched:
    ENGS = ("pe", "act", "dve", "pool", "sp")

    def __init__(self, nc):
        self.nc = nc
        self.ops = {e: [] for e in self.ENGS}
        self.sems = {}
        self.cnt = {}
        self.waited = {e: {} for e in self.ENGS}
        self._ctx = []
        for e in self.ENGS:
            self._newsem("E_" + e)

    def _newsem(self, key):
        g = self.nc.semaphore(key)
        h = g.__enter__()
        self._ctx.append(g)
        self.sems[key] = h
        self.cnt[key] = 0
        return key

    def dma_slot(self, name):
        return self._newsem("D_" + name)

    def _deps(self, e, reads, writes):
        deps = {}

        def add(ev):
            if ev is None:
                return
            k, v = ev
            if deps.get(k, 0) < v:
                deps[k] = v
        for b in reads:
            add(b.w)
        for b in writes:
            add(b.w)
            for k, v in b.r.items():
                add((k, v))
        waits = []
        mykey = "E_" + e
        for k, v in deps.items():
            if k == mykey and e in ("pe", "sp"):
                continue
            if self.waited[e].get(k, 0) >= v:
                continue
            self.waited[e][k] = v
            waits.append((k, v))
        return waits

    def _record(self, ev, reads, writes):
        k, v = ev
        for b in reads:
            if b.r.get(k, 0) < v:
                b.r[k] = v
        for b in writes:
            b.w = ev
            b.r = {}

    def op(self, e, fn, reads=(), writes=()):
        ex = [b for b in reads if b.excl]
        if ex:
            writes = list(writes) + ex
        waits = self._deps(e, reads, writes)
        k = "E_" + e
        self.cnt[k] += 1
        ev = (k, self.cnt[k])
        self.ops[e].append((waits, fn, (k, 1)))
        self._record(ev, reads, writes)
        return ev

    def dma(self, fn, slot, reads=(), writes=(), e="sp"):
        waits = self._deps(e, reads, writes)
        self.cnt[slot] += 16
        ev = (slot, self.cnt[slot])
        self.ops[e].append((waits, fn, (slot, 16)))
        self._record(ev, reads, writes)
        return ev

    def final_wait(self, e, bufs):
        waits = self._deps(e, bufs, bufs)
        self.ops[e].append((waits, None, None))

    def emit(self):
        nc = self.nc
        needed = {}
        for e in self.ENGS:
            for waits, fn, inc in self.ops[e]:
                for k, v in waits:
                    if k.startswith("E_"):
                        needed.setdefault(k, set()).add(v)
        rank = {k: {v: i + 1 for i, v in enumerate(sorted(vs))} for k, vs in needed.items()}
        with nc.Block() as block:
            def run(ename):
                def body(eng):
                    seq = 0
                    mykey = "E_" + ename
                    myrank = rank.get(mykey, {})
                    for waits, fn, inc in self.ops[ename]:
                        for k, v in waits:
                            eng.wait_ge(self.sems[k], rank[k][v] if k.startswith("E_") else v)
                        if fn is None:
                            continue
                        inst = fn(eng)
                        if inc[0] == mykey:
                            seq += 1
                            if seq in myrank:
                                inst.then_inc(self.sems[mykey], 1)
                        else:
                            inst.then_inc(self.sems[inc[0]], inc[1])
                return body
            block.tensor(run("pe"))
            block.scalar(run("act"))
            block.vector(run("dve"))
            block.gpsimd(run("pool"))
            block.sync(run("sp"))

    def close(self):
        for g in reversed(self._ctx):
            g.__exit__(None, None, None)


def build_program(S_TOK, NB, layer_of_unit, batch_of_unit, chain):
    NQ = S_TOK // 512
    NBLK = S_TOK // 128
    L = max(layer_of_unit) + 1
    nc = bass.Bass("TRN2", target_bir_lowering=False)
    x_d = nc.dram_tensor("x", [NB, S_TOK, D], F32, kind="ExternalInput").ap()
    mem_d = nc.dram_tensor("mem", [NB, NMEM, D], F32, kind="ExternalInput").ap()
    wF_d = nc.dram_tensor("wF", [L, 26, 128, 1024], F32, kind="ExternalInput").ap()
    wO_d = nc.dram_tensor("wO", [L, 8, 128, 1024], F32, kind="ExternalInput").ap()
    wM_d = nc.dram_tensor("wM", [L, 4, 128, 1024], F32, kind="ExternalInput").ap()
    wsm_d = nc.dram_tensor("wsm", [L, 128, NSM], F32, kind="ExternalInput").ap()
    cf_d = nc.dram_tensor("cf32", [128, NCF], F32, kind="ExternalInput").ap()
    cb_d = nc.dram_tensor("cbf", [128, NCB], BF16, kind="ExternalInput").ap()
    out_d = nc.dram_tensor("out", [NB, S_TOK, D], F32, kind="ExternalOutput").ap()
    need_scr = any(c == "scr" for c in chain)
    scr_d = nc.dram_tensor("xscr", [NB, S_TOK, D], F32, kind="Internal").ap() if need_scr else None

    wsc_d = nc.dram_tensor("wscr", [L, 38, 128, 1024], BF16, kind="Internal").ap()
    S = Sched(nc)
    ctx = []

    def sb(name, shape, dt):
        g = nc.sbuf_tensor(name, shape, dt)
        h = g.__enter__()
        ctx.append(g)
        return h

    psum = []
    PB = []
    for i in range(8):
        g = nc.psum_tensor(f"ps{i}", [128, 512], F32)
        psum.append(g.__enter__())
        ctx.append(g)
        PB.append(Buf(f"ps{i}", excl=True))
    free_banks = list(range(8))

    def bget():
        return free_banks.pop(0)

    def bput(i):
        free_banks.append(i)

    cb = sb("cb", [128, NCB], BF16); CBb = Buf("cb")
    cf = sb("cf", [128, NCF], F32); CFb = Buf("cf")
    ident = cb[:, CB_ID:CB_ID + 128]
    triM8 = cb[:, CB_TRI:CB_TRI + 128]
    ones = cb[:, CB_ONES:CB_ONES + 128]
    penS = cb[:, CB_PENS:CB_PENS + 128]
    penD = cb[:, CB_PEND:CB_PEND + 128]

    def ekb(kb):
        return cb[:, CB_EKB + kb * 16:CB_EKB + (kb + 1) * 16]

    def selM8(kb):
        return cb[0:48, CB_SEL + kb * 128:CB_SEL + (kb + 1) * 128]
    negb = cf[:, CF_NEGB:CF_NEGB + 128]
    p2a = cf[:, CF_P2A:CF_P2A + 16]
    p2b = cf[:, CF_P2B:CF_P2B + 16]

    sbkT = sb("sbkT", [128, 3, S_TOK], BF16); SBK = [Buf(f"sbk{q}") for q in range(NQ)]
    sbv = sb("sbv", [128, NBLK, 384], BF16); SBV = [Buf(f"sbv{q}") for q in range(NQ)]
    ckv = sb("ckv", [128, NBLK, 128], BF16); CKV = [Buf(f"ckv{q}") for q in range(NQ)]
    ckvT = sb("ckvT", [128, S_TOK], BF16); CKVT = [Buf(f"ckvT{q}") for q in range(NQ)]
    ikT4 = sb("ikT4", [128, S_TOK], BF16); IKT = [Buf(f"ikT{q}") for q in range(NQ)]
    sbq = sb("sbq", [128, 3, 512], BF16); SBQ = Buf("sbq")
    sbg = sb("sbg", [128, 3, 512], BF16); SBG = Buf("sbg")
    dq = sb("dq", [128, 3, 512], BF16); DQ = Buf("dq")
    dg = sb("dg", [128, 3, 512], BF16); DG = Buf("dg")
    iq = sb("iq", [128, 2, 512], BF16); IQ = Buf("iq")
    mq = sb("mq", [128, 2, 512], BF16); MQ = Buf("mq")
    mg = sb("mg", [128, 2, 512], BF16); MG = Buf("mg")
    idxw = sb("idxw", [128, 32], F32); IDXW = Buf("idxw")
    hT = sb("hT", [128, 8, 512], BF16); HT = Buf("hT")
    hnmix = sb("hnmix", [128, 4096], BF16); HNMIX = Buf("hnmix")
    xc = sb("xc", [128, 4096], F32); XC = Buf("xc")
    NWB = 2
    wst = [sb(f"wst{i}", [128, 1024], F32) for i in range(NWB)]; WST = [Buf(f"wst{i}") for i in range(NWB)]
    NWF = 4
    wbf = [sb(f"wbf{i}", [128, 1024], BF16) for i in range(NWF)]; WBF = [Buf(f"wbf{i}") for i in range(NWF)]
    big16 = sb("big16", [128, 8192], BF16); BIG = [Buf(f"big{r}") for r in range(16)]
    gcol = sb("gcol", [128, 8], F32); kvg = sb("kvg", [128, 128], F32); b31 = sb("b31", [128, 6], F32)
    postg = sb("postg", [128, 1024], F32)
    SMF = Buf("smallf32")
    wiw = sb("wiw", [128, 64], BF16); wuk = sb("wuk", [128, 384], BF16); wuv = sb("wuv", [128, 384], BF16)
    tb8 = sb("tb8", [128, 1536], BF16)
    SMB = Buf("smallbf")
    memT = sb("memT", [128, 8, 256], BF16); MEMT = Buf("memT")
    memk = sb("memk", [128, 2, 256], BF16); MEMK = Buf("memk")
    memv = sb("memv", [128, 2, 256], BF16); MEMV = Buf("memv")
    membf = sb("membf", [128, 2, 1024], BF16); MEMBF = Buf("membf")
    NT = 3
    etile = [sb(f"et{i}", [128, 512], BF16) for i in range(NT)]; ET = [Buf(f"et{i}") for i in range(NT)]
    atile = [sb(f"at{i}", [128, 512], BF16) for i in range(NT)]; AT = [Buf(f"at{i}") for i in range(NT)]
    cs48 = sb("cs48", [48, 512], BF16); cslo = sb("cslo", [16, 512], BF16); CS = Buf("cs")
    cshi = cs48[0:16, :]
    score = sb("score", [128, S_TOK], F32); SCORE = Buf("score")
    junk = sb("junk", [128, S_TOK], BF16); JUNK = Buf("junk")
    pen = sb("pen", [128, 4, S_TOK], BF16); PEN = [Buf(f"pen{i}") for i in range(4)]
    NR = 8
    rt = [sb(f"rt{i}", [128, 512], BF16) for i in range(NR)]; RT = [Buf(f"rt{i}") for i in range(NR)]
    dgt = [sb(f"dgt{i}", [128, 8, 128], BF16) for i in range(2)]; DGT = [Buf(f"dgt{i}") for i in range(2)]
    ptile = [sb(f"pt{i}", [128, 512], BF16) for i in range(NT)]; PT = [Buf(f"pt{i}") for i in range(NT)]
    qlat = [sb(f"ql{i}", [128, 512], BF16) for i in range(2)]; QL = [Buf(f"ql{i}") for i in range(2)]
    recf = sb("recf", [128, 512], F32); RECF = Buf("recf")
    onb = sb("onb", [128, 512], BF16); ONB = Buf("onb")
    tmpf = sb("tmpf", [128, 1024], F32); TMPF = Buf("tmpf")
    sm = sb("smalls", [128, 64], F32)
    SMS = {n: Buf("sm_" + n) for n in ("ss", "rs", "ssk", "rsk", "bis", "ssy")}
    steps = sb("steps", [128, 32], F32); STEPS = Buf("steps")
    ss = sm[:, 0:4]; rs = sm[:, 4:8]; ssk = sm[:, 8:12]; rsk = sm[:, 12:16]
    mx = sm[:, 16:17]; mn = sm[:, 17:18]; thr = sm[:, 18:19]; rng = sm[:, 19:20]; cnt = sm[:, 20:21]; dd = sm[:, 21:22]
    ssy = sm[:, 24:26]; ssy2 = sm[:, 26:27]; rsy = sm[:, 27:28]

    sl_c = S.dma_slot("const"); sl_c2 = S.dma_slot("const2"); sl_x = S.dma_slot("x"); sl_o = S.dma_slot("o"); sl_m = S.dma_slot("mem")
    sl_s = S.dma_slot("small"); sl_w = [S.dma_slot(f"w{i}") for i in range(NWB)]
    sl_wb = [S.dma_slot(f"wb{i}") for i in range(NWF)]; sl_wo = [S.dma_slot(f"wo{i}") for i in range(NWB)]
    sl_big = [S.dma_slot(f"big{i}") for i in range(8)]
    WSC = [[Buf(f"wsc{l_}_{i}") for i in range(38)] for l_ in range(L)]
    OUTB = Buf("outdram")
    SCRB = {}

    cnt_rr = {"wf": 0, "w": 0, "e": 0, "a": 0, "r": 0, "p": 0, "q": 0, "d": 0, "ev": 0}

    def rr(key, n):
        v = cnt_rr[key] % n
        cnt_rr[key] += 1
        return v

    S.op("dve", lambda e: e.memset(cs48[:, :], 0.0), writes=[CS])
    S.dma(lambda e: e.dma_start(out=cb[:], in_=cb_d[:, :]), sl_c, writes=[CBb])
    S.dma(lambda e: e.dma_start(out=cf[:], in_=cf_d[:, :]), sl_c2, writes=[CFb])

    for l_ in range(L):
        for idx in range(38):
            src = wF_d[l_, idx, :, :] if idx < 26 else (wM_d[l_, idx - 26, :, :] if idx < 30 else wO_d[l_, idx - 30, :, :])
            k = rr("w", NWB)
            S.dma(lambda e, k=k, src=src: e.dma_start(out=wst[k][:], in_=src), sl_w[k], writes=[WST[k]])
            kf = rr("wf", NWF)
            S.op("act", lambda e, k=k, kf=kf: e.activation(out=wbf[kf][:], in_=wst[k][:], func=AF.Copy),
                 reads=[WST[k]], writes=[WBF[kf]])
            S.dma(lambda e, kf=kf, l_=l_, idx=idx: e.dma_start(out=wsc_d[l_, idx, :, :], in_=wbf[kf][:]), sl_wb[kf],
                  reads=[WBF[kf]], writes=[WSC[l_][idx]])

    def wchunk(l_, idx):
        kf = rr("wf", NWF)
        S.dma(lambda e: e.dma_start(out=wbf[kf][:], in_=wsc_d[l_, idx, :, :]), sl_wb[kf], reads=[WSC[l_][idx]], writes=[WBF[kf]])
        return wbf[kf], WBF[kf]

    LA = 3

    def wstream(l_, idxs):
        pend_ = []
        it = iter(idxs)
        for _ in range(LA):
            nx = next(it, None)
            if nx is not None:
                pend_.append(wchunk(l_, nx))
        while pend_:
            cur = pend_.pop(0)
            nx = next(it, None)
            if nx is not None:
                pend_.append(wchunk(l_, nx))
            yield cur

    def evac_copy(out_ap, in_ap, reads, writes):
        if rr("ev", 2) == 0:
            S.op("act", lambda e: e.activation(out=out_ap, in_=in_ap, func=AF.Copy), reads=reads, writes=writes)
        else:
            S.op("dve", lambda e: e.tensor_copy(out=out_ap, in_=in_ap), reads=reads, writes=writes)

    def rms_scale(src_ss, dst_rs, n, inv_n, B_ss, B_rs):
        S.op("act", lambda e: e.activation(out=dst_rs, in_=src_ss, func=AF.Sqrt, scale=inv_n, bias=EPS),
             reads=[B_ss], writes=[B_rs])
        S.op("dve", lambda e: e.reciprocal(out=dst_rs, in_=dst_rs), reads=[B_rs], writes=[B_rs])

    def unit(u):
        l = layer_of_unit[u]
        b = batch_of_unit[u]
        src_x = x_d if chain[u] == "in" else scr_d
        later = any(batch_of_unit[v] == b for v in range(u + 1, len(layer_of_unit)))
        dst_x = scr_d if later else out_d
        if later and b not in SCRB:
            SCRB[b] = [Buf(f"scr{b}_{q}") for q in range(NQ)]

        S.dma(lambda e: e.dma_start(out=xc[:, 0:NSM], in_=wsm_d[l, :, :]), sl_s, writes=[XC])
        for (dst, off, n) in ((gcol, SM_GCOL, 8), (kvg, SM_KVG, 128), (b31, SM_B31, 6), (postg, SM_POSTG, 1024)):
            S.op("dve", lambda e, dst=dst, off=off, n=n: e.tensor_copy(out=dst[:, 0:n], in_=xc[:, off:off + n]),
                 reads=[XC], writes=[SMF])
        for (dst, off, n) in ((wiw, SM_IW, 64), (wuk, SM_WUK, 384), (wuv, SM_WUV, 384)):
            S.op("dve", lambda e, dst=dst, off=off, n=n: e.tensor_copy(out=dst[:, 0:n], in_=xc[:, off:off + n]),
                 reads=[XC], writes=[SMB])
        S.op("dve", lambda e: e.tensor_scalar(out=tb8[:, :], in0=xc[:, SM_TB:SM_TB + 1536], scalar1=8.0, scalar2=None,
                                              op0=ALU.mult), reads=[XC], writes=[SMB])
        S.dma(lambda e: e.dma_start(out=xc[:, 0:2048].rearrange("p (j d) -> p j d", j=2),
                                    in_=mem_d[b, :, :].rearrange("(j p) d -> p j d", p=128)), sl_m, writes=[XC])
        S.op("dve", lambda e: e.tensor_copy(out=membf[:].rearrange("p j d -> p (j d)"), in_=xc[:, 0:2048]),
             reads=[XC], writes=[MEMBF])
        for c2 in range(4):
            bk = bget()
            pb = psum[bk][:].bitcast(BF16)
            for cc in range(2):
                c = 2 * c2 + cc
                for j in range(2):
                    S.op("pe", lambda e, c=c, j=j, cc=cc, pb=pb: e.transpose(
                        pb[:, cc * 256 + j * 128:cc * 256 + (j + 1) * 128], membf[:, j, c * 128:(c + 1) * 128], ident),
                        reads=[MEMBF, CBb], writes=[PB[bk]])
            evac_copy(memT[:, 2 * c2:2 * c2 + 2, :], pb[:, 0:512].rearrange("p (c m) -> p c m", c=2), [PB[bk]], [MEMT])
            bput(bk)
        for g4, (w, W) in enumerate(wstream(l, [26, 27, 28, 29])):
            w3 = w[:].rearrange("p (c g) -> p c g", c=8)
            bk = bget()
            if g4 < 2:
                for c in range(8):
                    S.op("pe", lambda e, c=c, w3=w3, bk=bk: e.matmul(psum[bk][:, 0:256], lhsT=w3[:, c, :], rhs=memT[:, c, :],
                                                                    start=(c == 0), stop=(c == 7)),
                         reads=[W, MEMT], writes=[PB[bk]])
                evac_copy(memk[:, g4, :], psum[bk][:, 0:256], [PB[bk]], [MEMK])
            else:
                for j in range(2):
                    for c in range(8):
                        S.op("pe", lambda e, c=c, j=j, w3=w3, bk=bk: e.matmul(
                            psum[bk][:, j * 128:(j + 1) * 128], lhsT=memT[:, c, j * 128:(j + 1) * 128], rhs=w3[:, c, :],
                            start=(c == 0), stop=(c == 7)), reads=[W, MEMT], writes=[PB[bk]])
                evac_copy(memv[:, :, (g4 - 2) * 128:(g4 - 1) * 128], psum[bk][:, 0:256].rearrange("p (j g) -> p j g", j=2),
                          [PB[bk]], [MEMV])
            bput(bk)

        for tq in range(NQ):
            chunk_phase(u, l, b, tq, src_x, dst_x)

    def chunk_phase(u, l, b, tq, src_x, dst_x):
        t0 = tq * 512
        hn = hnmix[:].rearrange("p (i d) -> p i d", i=4)
        mix = hnmix[:].rearrange("p (c t) -> p c t", c=8)
        xc3 = xc[:].rearrange("p (i d) -> p i d", i=4)
        rd = [XC]
        if src_x is scr_d:
            rd = [XC] + [SCRB[b][tq]]
        S.dma(lambda e: e.dma_start(out=xc3, in_=src_x[b, t0:t0 + 512, :].rearrange("(i p) d -> p i d", p=128)),
              sl_x, reads=rd[1:], writes=[XC])
        for i in range(4):
            S.op("act", lambda e, i=i: e.activation(out=junk[:, 0:1024], in_=xc3[:, i, :], func=AF.Square,
                                                    accum_out=ss[:, i:i + 1]), reads=[XC], writes=[JUNK, SMS["ss"]])
        rms_scale(ss, rs, 4, 1.0 / D, SMS["ss"], SMS["rs"])
        for i in range(4):
            S.op("dve", lambda e, i=i: e.tensor_scalar(out=hn[:, i, :], in0=xc3[:, i, :], scalar1=rs[:, i:i + 1],
                                                       scalar2=None, op0=ALU.mult),
                 reads=[XC, SMS["rs"]], writes=[HNMIX])
        for c2 in range(4):
            bk = bget()
            pb = psum[bk][:].bitcast(BF16)
            for cc in range(2):
                c = 2 * c2 + cc
                for i in range(4):
                    S.op("pe", lambda e, c=c, i=i, cc=cc, pb=pb: e.transpose(
                        pb[:, cc * 512 + i * 128:cc * 512 + (i + 1) * 128], hn[:, i, c * 128:(c + 1) * 128], ident),
                        reads=[HNMIX, CBb], writes=[PB[bk]])
            for cc in range(2):
                c = 2 * c2 + cc
                S.op("dve", lambda e, c=c, cc=cc, pb=pb: e.tensor_scalar(
                    out=hT[:, c, :], in0=pb[:, cc * 512:(cc + 1) * 512], scalar1=gcol[:, c:c + 1], scalar2=None,
                    op0=ALU.mult), reads=[PB[bk], SMF], writes=[HT])
            bput(bk)

        if DBG_STAGE < 2:
            return
        fdest = ([("sbq", i) for i in range(3)] + [("sbk", i) for i in range(3)] + [("sbg", i) for i in range(3)]
                 + [("dq", i) for i in range(3)] + [("dg", i) for i in range(3)] + [("iq", 0), ("iq", 1), ("ik", 0)]
                 + [("mq", 0), ("mq", 1), ("mg", 0), ("mg", 1)])
        dst_tab = {"sbq": (sbq, SBQ), "sbg": (sbg, SBG), "dq": (dq, DQ), "dg": (dg, DG), "iq": (iq, IQ),
                   "mq": (mq, MQ), "mg": (mg, MG)}
        ptb = None
        for cc, (w, W) in enumerate(wstream(l, list(range(26)))):
            w3 = w[:].rearrange("p (c g) -> p c g", c=8)
            if cc < 22:
                name, ci = fdest[cc]
                bk = bget()
                for c in range(8):
                    S.op("pe", lambda e, c=c, w3=w3, bk=bk: e.matmul(psum[bk][:, :], lhsT=w3[:, c, :], rhs=hT[:, c, :],
                                                                    start=(c == 0), stop=(c == 7)),
                         reads=[W, HT], writes=[PB[bk]])
                if name == "sbk":
                    evac_copy(sbkT[:, ci, t0:t0 + 512], psum[bk][:, :], [PB[bk]], [SBK[tq]])
                elif name == "ik":
                    evac_copy(ikT4[:, t0:t0 + 512], psum[bk][:, :], [PB[bk]], [IKT[tq]])
                elif name in ("sbg", "dg", "mg") and DBG_SUB >= 2:
                    dt_, DB = dst_tab[name]
                    hs = rr("ev", 2) * 512
                    S.op("act", lambda e, bk=bk, hs=hs: e.activation(out=tmpf[:, hs:hs + 512], in_=psum[bk][:, :], func=AF.Exp,
                                                                     scale=-1.0), reads=[PB[bk]], writes=[TMPF])
                    S.op("act", lambda e, hs=hs: e.activation(out=tmpf[:, hs:hs + 512], in_=tmpf[:, hs:hs + 512], func=AF.Ln, bias=1.0),
                         reads=[TMPF], writes=[TMPF])
                    S.op("act", lambda e, hs=hs: e.activation(out=tmpf[:, hs:hs + 512], in_=tmpf[:, hs:hs + 512], func=AF.Exp, scale=-1.0),
                         reads=[TMPF], writes=[TMPF])
                    S.op("dve", lambda e, dt_=dt_, ci=ci, bk=bk, hs=hs: e.tensor_tensor(
                        out=dt_[:, ci, :], in0=psum[bk][:, :], in1=tmpf[:, hs:hs + 512], op=ALU.mult),
                        reads=[PB[bk], TMPF], writes=[DB])
                else:
                    dt_, DB = dst_tab[name]
                    evac_copy(dt_[:, ci, :], psum[bk][:, :], [PB[bk]], [DB])
                bput(bk)
            elif DBG_SUB >= 30:
                g4 = cc - 22
                if g4 == 0:
                    ptb = [bget() for _ in range(4)]
                for i in range(4):
                    for c in range(8):
                        S.op("pe", lambda e, c=c, i=i, w3=w3, g4=g4: e.matmul(
                            psum[ptb[i]][:, g4 * 128:(g4 + 1) * 128], lhsT=hT[:, c, i * 128:(i + 1) * 128], rhs=w3[:, c, :],
                            start=(c == 0), stop=(c == 7)), reads=[W, HT], writes=[PB[ptb[i]]])
        if DBG_SUB < 30:
            return
        for i in range(4):
            j = 4 * tq + i
            evac_copy(sbv[:, j, :], psum[ptb[i]][:, 0:384], [PB[ptb[i]]], [SBV[tq]])
            evac_copy(tmpf[:, i * 128:(i + 1) * 128], psum[ptb[i]][:, 384:512], [PB[ptb[i]]], [TMPF])
        for i in range(4):
            S.op("act", lambda e, i=i: e.activation(out=junk[:, 0:128], in_=tmpf[:, i * 128:(i + 1) * 128], func=AF.Square,
                                                    accum_out=ssk[:, i:i + 1]),
                 reads=[TMPF], writes=[JUNK, SMS["ssk"]])
        rms_scale(ssk, rsk, 4, 1.0 / 128, SMS["ssk"], SMS["rsk"])
        for i in range(4):
            j = 4 * tq + i
            S.op("dve", lambda e, i=i, j=j: e.scalar_tensor_tensor(out=ckv[:, j, :], in0=tmpf[:, i * 128:(i + 1) * 128],
                                                                   scalar=rsk[:, i:i + 1], in1=kvg[:, :],
                                                                   op0=ALU.mult, op1=ALU.mult),
                 reads=[TMPF, SMS["rsk"], SMF], writes=[CKV[tq]])
        for i in range(4):
            bput(ptb[i])
        if DBG_SUB < 40:
            return
        bk = bget()
        pb = psum[bk][:].bitcast(BF16)
        for i in range(4):
            j = 4 * tq + i
            S.op("pe", lambda e, i=i, j=j, pb=pb: e.transpose(pb[:, i * 128:(i + 1) * 128], ckv[:, j, :], ident),
                 reads=[CKV[tq], CBb], writes=[PB[bk]])
        evac_copy(ckvT[:, t0:t0 + 512], pb[:, 0:512], [PB[bk]], [CKVT[tq]])
        bput(bk)
        if DBG_SUB < 50:
            return
        bk = bget()
        wiw3 = wiw[:].rearrange("p (c g) -> p c g", c=8)
        for i in range(4):
            for c in range(8):
                S.op("pe", lambda e, c=c, i=i, bk=bk: e.matmul(psum[bk][:, i * 8:(i + 1) * 8],
                                                                lhsT=hT[:, c, i * 128:(i + 1) * 128], rhs=wiw3[:, c, :],
                                                                start=(c == 0), stop=(c == 7)),
                     reads=[SMB, HT], writes=[PB[bk]])
        S.op("dve", lambda e, bk=bk: e.tensor_scalar(out=idxw[:, :], in0=psum[bk][:, 0:32], scalar1=1.0 / 16, scalar2=None,
                                                     op0=ALU.mult), reads=[PB[bk]], writes=[IDXW])
        bput(bk)

        if DBG_STAGE < 3:
            return
        nkb = 4 * tq + 4
        KS = lambda lst: [lst[q] for q in range(tq + 1)]

        def indexer(i):
            qb = 4 * tq + i
            if qb < 2:
                return
            yield
            nk = (qb + 1) * 128
            nkc = (nk + 511) // 512
            kd = rr("d", 2)
            for h in range(8):
                S.op("act", lambda e, h=h, kd=kd: e.activation(out=dgt[kd][:, h, :], in_=ident, func=AF.Copy,
                                                               scale=idxw[:, i * 8 + h:i * 8 + h + 1]),
                     reads=[CBb, IDXW], writes=[DGT[kd]])
            for kc in range(nkc):
                w_ = min(512, nk - kc * 512)
                bs = bget()
                pend = []
                for g in range(2):
                    bds = [bget() for _ in range(4)]
                    for j in range(4):
                        hp = j * 32
                        tp = (96, 0) if hp == 96 else None
                        S.op("pe", lambda e, g=g, hp=hp, tp=tp, bd=bds[j], kc=kc, w_=w_: e.matmul(
                            psum[bd][:, 0:w_], lhsT=iq[hp:hp + 32, g, i * 128:(i + 1) * 128],
                            rhs=ikT4[hp:hp + 32, kc * 512:kc * 512 + w_], start=True, stop=True, tile_position=tp),
                            reads=[IQ] + KS(IKT), writes=[PB[bds[j]]])
                    for j in range(4):
                        h = 4 * g + j
                        kr = rr("r", NR)
                        S.op("dve", lambda e, kr=kr, bd=bds[j], w_=w_: e.tensor_scalar(out=rt[kr][:, 0:w_], in0=psum[bd][:, 0:w_],
                                                                                      scalar1=0.0, scalar2=None, op0=ALU.max),
                             reads=[PB[bds[j]]], writes=[RT[kr]])
                        bput(bds[j])
                        pend.append(lambda h=h, kr=kr, bs=bs, w_=w_: S.op("pe", lambda e: e.matmul(
                            psum[bs][:, 0:w_], lhsT=dgt[kd][:, h, :], rhs=rt[kr][:, 0:w_], start=(h == 0), stop=(h == 7)),
                            reads=[DGT[kd], RT[kr]], writes=[PB[bs]]))
                    while len(pend) > 4:
                        pend.pop(0)()
                    yield
                while pend:
                    pend.pop(0)()
                last = (kc == nkc - 1)
                wc = w_ - 128 if last else w_
                if wc > 0:
                    S.op("dve", lambda e, bs=bs, kc=kc, wc=wc: e.tensor_copy(out=score[:, kc * 512:kc * 512 + wc],
                                                                             in_=psum[bs][:, 0:wc]),
                         reads=[PB[bs]], writes=[SCORE])
                if last:
                    S.op("dve", lambda e, bs=bs, w_=w_: e.tensor_tensor(out=score[:, nk - 128:nk], in0=psum[bs][:, w_ - 128:w_],
                                                                       in1=negb, op=ALU.add),
                         reads=[PB[bs], CFb], writes=[SCORE])
                bput(bs)
            B = SMS["bis"]
            S.op("dve", lambda e: e.tensor_reduce(out=mx, in_=score[:, 0:nk], axis=AX.X, op=ALU.max), reads=[SCORE], writes=[B])
            S.op("dve", lambda e: e.tensor_reduce(out=mn, in_=score[:, 0:nk - 128], axis=AX.X, op=ALU.min),
                 reads=[SCORE], writes=[B])
            S.op("dve", lambda e: e.tensor_tensor(out=rng, in0=mx, in1=mn, op=ALU.subtract), reads=[B], writes=[B])
            S.op("dve", lambda e: e.tensor_tensor(out=thr, in0=mx, in1=mn, op=ALU.add), reads=[B], writes=[B])
            S.op("dve", lambda e: e.tensor_scalar(out=thr, in0=thr, scalar1=0.5, scalar2=None, op0=ALU.mult), reads=[B], writes=[B])
            S.op("dve", lambda e: e.tensor_scalar(out=steps[:, 0:16], in0=p2a, scalar1=rng, scalar2=None, op0=ALU.mult),
                 reads=[B, CFb], writes=[STEPS])
            S.op("dve", lambda e: e.tensor_scalar(out=steps[:, 16:32], in0=p2b, scalar1=rng, scalar2=None, op0=ALU.mult),
                 reads=[B, CFb], writes=[STEPS])
            for it in range(NBIS):
                S.op("dve", lambda e: e.tensor_scalar(out=junk[:, 0:nk], in0=score[:, 0:nk], scalar1=thr, scalar2=0.0,
                                                      op0=ALU.is_ge, op1=ALU.add, accum_out=cnt),
                     reads=[SCORE, B], writes=[JUNK, B])
                S.op("dve", lambda e, it=it: e.tensor_scalar(out=dd, in0=cnt, scalar1=float(TOPK), scalar2=steps[:, 16 + it:17 + it],
                                                             op0=ALU.is_ge, op1=ALU.mult), reads=[B, STEPS], writes=[B])
                S.op("dve", lambda e, it=it: e.scalar_tensor_tensor(out=thr, in0=dd, scalar=steps[:, it:it + 1], in1=thr,
                                                                    op0=ALU.subtract, op1=ALU.add),
                     reads=[B, STEPS], writes=[B])
                yield
                yield
            S.op("dve", lambda e: e.tensor_scalar(out=pen[:, i, 0:nk], in0=score[:, 0:nk], scalar1=thr, scalar2=NEG,
                                                  op0=ALU.is_lt, op1=ALU.mult), reads=[SCORE, B], writes=[PEN[i]])

        def sb_head(h):
            ch, hp = h // 2, (h % 2) * 64
            bcs = bget()
            pend = []
            for kb in range(nkb):
                c0 = max(0, kb - 4 * tq) * 128
                diag = kb >= 4 * tq
                bz = bget()
                S.op("pe", lambda e, kb=kb, c0=c0, bz=bz, diag=diag: e.matmul(
                    psum[bz][:, c0:512], lhsT=sbkT[hp:hp + 64, ch, kb * 128:(kb + 1) * 128], rhs=sbq[hp:hp + 64, ch, c0:512],
                    start=True, stop=not diag), reads=KS(SBK) + [SBQ], writes=[PB[bz]])
                if diag:
                    S.op("pe", lambda e, c0=c0, bz=bz: e.matmul(psum[bz][:, c0:c0 + 128], lhsT=penS, rhs=ident,
                                                                 start=False, stop=True), reads=[CBb], writes=[PB[bz]])
                ke = rr("e", NT)
                S.op("act", lambda e, ke=ke, c0=c0, bz=bz: e.activation(out=etile[ke][:, c0:512], in_=psum[bz][:, c0:512],
                                                                        func=AF.Exp, scale=0.125),
                     reads=[PB[bz]], writes=[ET[ke]])
                bput(bz)
                sp = big16[:, kb * 512:(kb + 1) * 512]
                S.op("act", lambda e, ke=ke, c0=c0, sp=sp: e.activation(out=sp[:, c0:512], in_=etile[ke][:, c0:512],
                                                                        func=AF.Ln, bias=1.0),
                     reads=[ET[ke]], writes=[BIG[kb]])
                pend.append(lambda kb=kb, c0=c0, sp=sp: S.op("pe", lambda e: e.matmul(
                    psum[bcs][0:16, c0:512], lhsT=ekb(kb), rhs=sp[:, c0:512], start=(kb == 0), stop=(kb == nkb - 1)),
                    reads=[CBb, BIG[kb]], writes=[PB[bcs]]))
                if len(pend) > SKEW:
                    pend.pop(0)()
                yield
            while pend:
                pend.pop(0)()
            S.op("dve", lambda e: e.tensor_copy(out=cs48[0:16, :], in_=psum[bcs][0:16, :]), reads=[PB[bcs]], writes=[CS])
            S.op("dve", lambda e: e.tensor_tensor(out=cslo[:, :], in0=psum[bcs][0:16, :], in1=cs48[0:16, :], op=ALU.subtract),
                 reads=[PB[bcs], CS], writes=[CS])
            S.op("dve", lambda e: e.tensor_copy(out=cs48[32:48, :], in_=cslo[:, :]), reads=[CS], writes=[CS])
            bput(bcs)
            bo = bget()
            pend = []
            for kb in range(nkb):
                c0 = max(0, kb - 4 * tq) * 128
                diag = kb >= 4 * tq
                bi = bget()
                sp = big16[:, kb * 512:(kb + 1) * 512]
                S.op("pe", lambda e, kb=kb, c0=c0, bi=bi: e.matmul(
                    psum[bi][:, c0:512], lhsT=sbkT[hp:hp + 64, ch, kb * 128:(kb + 1) * 128], rhs=sbq[hp:hp + 64, ch, c0:512],
                    start=True, stop=False), reads=KS(SBK) + [SBQ], writes=[PB[bi]])
                S.op("pe", lambda e, c0=c0, bi=bi, sp=sp: e.matmul(psum[bi][:, c0:512], lhsT=triM8, rhs=sp[:, c0:512],
                                                                   start=False, stop=False),
                     reads=[CBb, BIG[kb]], writes=[PB[bi]])
                S.op("pe", lambda e, kb=kb, c0=c0, bi=bi, diag=diag: e.matmul(psum[bi][:, c0:512], lhsT=selM8(kb),
                                                                              rhs=cs48[0:48, c0:512], start=False, stop=not diag),
                     reads=[CBb, CS], writes=[PB[bi]])
                if diag:
                    S.op("pe", lambda e, c0=c0, bi=bi: e.matmul(psum[bi][:, c0:c0 + 128], lhsT=penS, rhs=ident,
                                                                 start=False, stop=True), reads=[CBb], writes=[PB[bi]])
                ka = rr("a", NT)
                S.op("act", lambda e, ka=ka, c0=c0, bi=bi: e.activation(out=atile[ka][:, c0:512], in_=psum[bi][:, c0:512],
                                                                        func=AF.Exp, scale=0.125),
                     reads=[PB[bi]], writes=[AT[ka]])
                bput(bi)
                pend.append(lambda kb=kb, c0=c0, ka=ka: S.op("pe", lambda e: e.matmul(
                    psum[bo][:, c0:512], lhsT=sbv[:, kb, ch * 128:(ch + 1) * 128], rhs=atile[ka][:, c0:512],
                    start=(kb == 0), stop=(kb == nkb - 1)), reads=KS(SBV) + [AT[ka]], writes=[PB[bo]]))
                if len(pend) > SKEW:
                    pend.pop(0)()
                yield
            while pend:
                pend.pop(0)()
            S.op("dve", lambda e: e.tensor_tensor(out=mix[hp:hp + 64, ch, :], in0=psum[bo][hp:hp + 64, :],
                                                  in1=sbg[hp:hp + 64, ch, :], op=ALU.mult),
                 reads=[PB[bo], SBG], writes=[HNMIX])
            bput(bo)

        def mem_head(hm):
            ch, hp = hm // 2, (hm % 2) * 64
            bo = bget(); bd = bget()
            for mb in range(2):
                bl = bget()
                S.op("pe", lambda e, mb=mb, bl=bl: e.matmul(psum[bl][:, :], lhsT=memk[hp:hp + 64, ch, mb * 128:(mb + 1) * 128],
                                                            rhs=mq[hp:hp + 64, ch, :], start=True, stop=True),
                     reads=[MEMK, MQ], writes=[PB[bl]])
                kp = rr("p", NT)
                S.op("act", lambda e, kp=kp, bl=bl: e.activation(out=ptile[kp][:, :], in_=psum[bl][:, :], func=AF.Exp, scale=0.125),
                     reads=[PB[bl]], writes=[PT[kp]])
                bput(bl)
                S.op("pe", lambda e, mb=mb, kp=kp: e.matmul(psum[bo][:, :], lhsT=memv[:, mb, ch * 128:(ch + 1) * 128],
                                                            rhs=ptile[kp][:, :], start=(mb == 0), stop=(mb == 1)),
                     reads=[MEMV, PT[kp]], writes=[PB[bo]])
                S.op("pe", lambda e, mb=mb, kp=kp: e.matmul(psum[bd][:, :], lhsT=ones, rhs=ptile[kp][:, :],
                                                            start=(mb == 0), stop=(mb == 1)),
                     reads=[CBb, PT[kp]], writes=[PB[bd]])
            S.op("act", lambda e: e.activation(out=recf[hp:hp + 64, :], in_=psum[bd][hp:hp + 64, :], func=AF.Ln), reads=[PB[bd]], writes=[RECF])
            S.op("act", lambda e: e.activation(out=recf[hp:hp + 64, :], in_=recf[hp:hp + 64, :], func=AF.Exp, scale=-1.0), reads=[RECF], writes=[RECF])
            S.op("dve", lambda e: e.tensor_tensor(out=tmpf[hp:hp + 64, 0:512], in0=psum[bo][hp:hp + 64, :],
                                                  in1=recf[hp:hp + 64, :], op=ALU.mult), reads=[PB[bo], RECF], writes=[TMPF])
            S.op("dve", lambda e: e.tensor_tensor(out=mix[hp:hp + 64, 6 + ch, :], in0=tmpf[hp:hp + 64, 0:512],
                                                  in1=mg[hp:hp + 64, ch, :], op=ALU.mult), reads=[TMPF, MG], writes=[HNMIX])
            bput(bo); bput(bd)

        def dsa_head(h):
            ch, hp = h // 2, (h % 2) * 64
            bq = bget()
            S.op("pe", lambda e: e.matmul(psum[bq][:, :], lhsT=wuk[hp:hp + 64, ch * 128:(ch + 1) * 128], rhs=dq[hp:hp + 64, ch, :],
                                          start=True, stop=True), reads=[SMB, DQ], writes=[PB[bq]])
            kq = rr("q", 2)
            evac_copy(qlat[kq][:, :], psum[bq][:, :], [PB[bq]], [QL[kq]])
            bput(bq)
            bo = bget(); bd = bget()
            pend = []
            for kb in range(nkb):
                i0 = max(0, kb - 4 * tq)
                c0 = i0 * 128
                bl = bget()
                mm = []
                mm.append((lambda e, st, sp_, kb=kb, c0=c0, bl=bl: e.matmul(
                    psum[bl][:, c0:512], lhsT=ckvT[:, kb * 128:(kb + 1) * 128], rhs=qlat[kq][:, c0:512], start=st, stop=sp_),
                    KS(CKVT) + [QL[kq]]))
                for i in range(i0, 4):
                    qb = 4 * tq + i
                    cs_ = slice(i * 128, (i + 1) * 128)
                    if qb >= 2:
                        mm.append((lambda e, st, sp_, i=i, kb=kb, cs_=cs_, bl=bl: e.matmul(
                            psum[bl][:, cs_], lhsT=pen[:, i, kb * 128:(kb + 1) * 128], rhs=ident, start=st, stop=sp_),
                            [PEN[i], CBb]))
                    elif qb == kb:
                        mm.append((lambda e, st, sp_, cs_=cs_, bl=bl: e.matmul(psum[bl][:, cs_], lhsT=penD, rhs=ident,
                                                                              start=st, stop=sp_), [CBb]))
                    if qb - kb <= 1:
                        jj = qb - kb
                        mm.append((lambda e, st, sp_, cs_=cs_, jj=jj, bl=bl: e.matmul(
                            psum[bl][:, cs_], lhsT=ident, rhs=tb8[:, (h * 2 + jj) * 128:(h * 2 + jj + 1) * 128],
                            start=st, stop=sp_), [CBb, SMB]))
                for n_, (fn, rds) in enumerate(mm):
                    S.op("pe", lambda e, fn=fn, n_=n_, nm=len(mm): fn(e, n_ == 0, n_ == nm - 1), reads=rds, writes=[PB[bl]])
                kp = rr("p", NT)
                near_hi = min(512, max(c0, (kb + 2 - 4 * tq) * 128))
                if near_hi > c0:
                    S.op("act", lambda e, kp=kp, c0=c0, near_hi=near_hi, bl=bl: e.activation(
                        out=ptile[kp][:, c0:near_hi], in_=psum[bl][:, c0:near_hi], func=AF.Exp, scale=0.125),
                        reads=[PB[bl]], writes=[PT[kp]])
                if near_hi < 512:
                    S.op("act", lambda e, kp=kp, near_hi=near_hi, bl=bl: e.activation(
                        out=ptile[kp][:, near_hi:512], in_=psum[bl][:, near_hi:512], func=AF.Exp, scale=0.125,
                        bias=b31[:, h:h + 1]), reads=[PB[bl], SMF], writes=[PT[kp]])
                bput(bl)
                def tail(kb=kb, c0=c0, kp=kp):
                    S.op("pe", lambda e: e.matmul(psum[bo][:, c0:512], lhsT=ckv[:, kb, :], rhs=ptile[kp][:, c0:512],
                                                  start=(kb == 0), stop=(kb == nkb - 1)),
                         reads=KS(CKV) + [PT[kp]], writes=[PB[bo]])
                    S.op("pe", lambda e: e.matmul(psum[bd][:, c0:512], lhsT=ones, rhs=ptile[kp][:, c0:512],
                                                  start=(kb == 0), stop=(kb == nkb - 1)),
                         reads=[CBb, PT[kp]], writes=[PB[bd]])
                pend.append(tail)
                if len(pend) > SKEW:
                    pend.pop(0)()
                yield
            while pend:
                pend.pop(0)()
            S.op("act", lambda e: e.activation(out=recf[:, :], in_=psum[bd][:, :], func=AF.Ln), reads=[PB[bd]], writes=[RECF])
            S.op("act", lambda e: e.activation(out=recf[:, :], in_=recf[:, :], func=AF.Exp, scale=-1.0), reads=[RECF], writes=[RECF])
            S.op("dve", lambda e: e.tensor_tensor(out=onb[:, :], in0=psum[bo][:, :], in1=recf[:, :], op=ALU.mult),
                 reads=[PB[bo], RECF], writes=[ONB])
            bput(bo); bput(bd)
            bu = bget()
            S.op("pe", lambda e: e.matmul(psum[bu][:, :], lhsT=wuv[:, ch * 128:(ch + 1) * 128], rhs=onb[:, :], start=True, stop=True),
                 reads=[SMB, ONB], writes=[PB[bu]])
            S.op("dve", lambda e: e.tensor_tensor(out=mix[hp:hp + 64, 3 + ch, :], in0=psum[bu][hp:hp + 64, :],
                                                  in1=dg[hp:hp + 64, ch, :], op=ALU.mult), reads=[PB[bu], DG], writes=[HNMIX])
            bput(bu)

        def run_par(*gens):
            gens = list(gens)
            while gens:
                for g in list(gens):
                    try:
                        next(g)
                    except StopIteration:
                        gens.remove(g)

        def seq(*fns):
            for f in fns:
                r = f()
                if r is not None:
                    yield from r
                yield

        run_par(seq(*[lambda i=i: indexer(i) for i in range(4)]),
                seq(*([lambda h=h: sb_head(h) for h in range(5)] + [lambda hm=hm: mem_head(hm) for hm in range(4)])))
        run_par(sb_head(5), seq(lambda: dsa_head(0), lambda: dsa_head(1)))
        wO3 = big16[:].rearrange("p (c n) -> p c n", c=8)
        for c in range(8):
            S.dma(lambda e, c=c: e.dma_start(out=big16[:, c * 1024:(c + 1) * 1024], in_=wsc_d[l, 30 + c, :, :]), sl_big[c],
                  reads=[WSC[l][30 + c]], writes=[BIG[2 * c], BIG[2 * c + 1]])
        for h in range(2, 6):
            run_par(dsa_head(h))

        for i in range(4):
            b0 = bget(); b1 = bget()
            bb = (b0, b1)
            for half in range(2):
                for c in range(8):
                    S.op("pe", lambda e, c=c, half=half, i=i, bb=bb: e.matmul(
                        psum[bb[half]][:, :], lhsT=mix[:, c, i * 128:(i + 1) * 128], rhs=wO3[:, c, half * 512:(half + 1) * 512],
                        start=(c == 0), stop=(c == 7)), reads=[HNMIX, BIG[2 * c + half]], writes=[PB[bb[half]]])
            for half in range(2):
                evac_copy(tmpf[:, half * 512:(half + 1) * 512], psum[bb[half]][:, :], [PB[bb[half]]], [TMPF])
            S.op("act", lambda e: e.activation(out=junk[:, 0:1024], in_=tmpf[:, :], func=AF.Square, accum_out=ssy2),
                 reads=[TMPF], writes=[JUNK, SMS["ssy"]])
            rms_scale(ssy2, rsy, 1, 1.0 / D, SMS["ssy"], SMS["ssy"])
            S.op("dve", lambda e: e.scalar_tensor_tensor(out=tmpf[:, :], in0=tmpf[:, :], scalar=rsy, in1=postg[:, :],
                                                         op0=ALU.mult, op1=ALU.mult),
                 reads=[TMPF, SMS["ssy"], SMF], writes=[TMPF])
            bput(b0); bput(b1)
            S.op("pool", lambda e, i=i: e.tensor_tensor(out=xc3[:, i, :], in0=tmpf[:, :], in1=xc3[:, i, :], op=ALU.add),
                 reads=[TMPF, XC], writes=[XC])
        wr = [OUTB] if dst_x is out_d else [SCRB[b][tq]]
        S.dma(lambda e: e.dma_start(out=dst_x[b, t0:t0 + 512, :].rearrange("(i p) d -> p i d", p=128), in_=xc3),
              sl_o, reads=[XC], writes=wr)

    for u in range(len(layer_of_unit)):
        unit(u)
    S.final_wait("sp", [OUTB, XC])
    S.emit()
    S.close()
    for g in reversed(ctx):
        g.__exit__(None, None, None)
    return nc


def _t5_bucket(rel):
    n = np.maximum(rel, 0)
    max_exact = 16
    nf = np.maximum(n, 1).astype(np.float32)
    large = max_exact + (np.log(nf / max_exact) / math.log(128 / max_exact) * (32 - max_exact)).astype(np.int32)
    large = np.minimum(large, 31)
    return np.where(n < max_exact, n, large)


def _constants():
    bf = ml_dtypes.bfloat16
    cbv = np.zeros((128, NCB), np.float32)
    p = np.arange(128)
    cbv[:, CB_ID:CB_ID + 128] = np.eye(128)
    cbv[:, CB_TRI:CB_TRI + 128] = np.where(p[:, None] >= p[None, :], -8.0, 0.0)
    cbv[:, CB_ONES:CB_ONES + 128] = 1.0
    cbv[:, CB_PENS:CB_PENS + 128] = np.where(p[None, :] < p[:, None], 0.0, NEG)
    cbv[:, CB_PEND:CB_PEND + 128] = np.where(p[None, :] <= p[:, None], 0.0, NEG)
    for kb in range(16):
        cbv[:, CB_EKB + kb * 16 + kb] = 1.0
        for jb in range(16):
            if jb > kb:
                cbv[jb, CB_SEL + kb * 128:CB_SEL + (kb + 1) * 128] = -8.0
                cbv[32 + jb, CB_SEL + kb * 128:CB_SEL + (kb + 1) * 128] = -8.0
    cfv = np.zeros((128, NCF), np.float32)
    cfv[:, CF_NEGB:CF_NEGB + 128] = np.where(p[None, :] <= p[:, None], 0.0, -1e30)
    cfv[:, CF_P2A:CF_P2A + 16] = 2.0 ** -(np.arange(16) + 2.0)
    cfv[:, CF_P2B:CF_P2B + 16] = 2.0 ** -(np.arange(16) + 1.0)
    return cfv, cbv.astype(bf)


def _chunk_cols(w, cols):
    sel = w[:, cols]
    n = sel.shape[1] // 128
    a = sel.reshape(8, 128, n, 128)
    return np.ascontiguousarray(a.transpose(2, 1, 0, 3).reshape(n, 128, 1024))


def _prep_weights(pre_norm_g, post_norm_g, w_in, w_uk, w_uv, kv_norm_g, w_mem_kv, w_out, rel_bias, layers):
    o = np.cumsum([0, 384, 384, 384, 384, 384, 128, 384, 256, 32, 8, 256, 256])
    (o_sbq, o_sbk, o_sbv, o_sbg, o_dq, o_ckv, o_dg, o_iq, o_ik, o_iw, o_mq, o_mg) = o[:12]
    r = lambda a, n: list(range(a, a + n))
    fcols = (r(o_sbq, 384) + r(o_sbk, 384) + r(o_sbg, 384) + r(o_dq, 384) + r(o_dg, 384) + r(o_iq, 256)
             + r(o_ik, 32) * 4 + r(o_mq, 256) + r(o_mg, 256) + r(o_sbv, 384) + r(o_ckv, 128))
    assert len(fcols) == 26 * 128
    s_l = np.arange(128)
    wF, wO, wM, wsm = [], [], [], []
    for l in layers:
        wF.append(_chunk_cols(w_in[l], fcols))
        wO.append(np.ascontiguousarray(w_out[l].reshape(8, 128, 1024)))
        wM.append(_chunk_cols(w_mem_kv[l], list(range(512))))
        sm = np.zeros((128, NSM), np.float32)
        sm[:, SM_GCOL:SM_GCOL + 8] = pre_norm_g[l].reshape(8, 128).T
        sm[:, SM_IW:SM_IW + 64] = w_in[l][:, o_iw:o_iw + 8].reshape(8, 128, 8).transpose(1, 0, 2).reshape(128, 64)
        uk = w_uk[l]
        t = uk.reshape(128, 3, 2, 64).transpose(2, 3, 1, 0).reshape(128, 3 * 128)
        sm[:, SM_WUK:SM_WUK + 384] = t
        sm[:, SM_WUV:SM_WUV + 384] = w_uv[l].reshape(128, 384)
        sm[:, SM_KVG:SM_KVG + 128] = kv_norm_g[l][None, :]
        sm[:, SM_B31:SM_B31 + 6] = rel_bias[31][None, :]
        sm[:, SM_POSTG:SM_POSTG + 1024] = post_norm_g[l][None, :]
        for h in range(6):
            for j in range(2):
                rel = s_l[None, :] - s_l[:, None] + 128 * j
                sm[:, SM_TB + (h * 2 + j) * 128:SM_TB + (h * 2 + j + 1) * 128] = rel_bias[_t5_bucket(rel), h]
        wsm.append(sm)
    return (np.stack(wF), np.stack(wO), np.stack(wM), np.stack(wsm))


_PROG_CACHE = {}


def _get_prog(key, *args):
    if key not in _PROG_CACHE:
        _PROG_CACHE[key] = build_program(*args)
    return _PROG_CACHE[key]


FUSED = True


def kernel(x, mem, pre_norm_g, post_norm_g, w_in, w_uk, w_uv, kv_norm_g, w_mem_kv, w_out, rel_bias):
    x = np.asarray(x, np.float32)
    mem = np.asarray(mem, np.float32)
    args = [np.asarray(a, np.float32) for a in (pre_norm_g, post_norm_g, w_in, w_uk, w_uv, kv_norm_g, w_mem_kv, w_out, rel_bias)]
    B, S_TOK, _ = x.shape
    depth = w_in.shape[0]
    per = B // N_CORES
    cfv, cbv = _constants()
    if FUSED:
        units_l = []; units_b = []; chain = []
        for b in range(per):
            for l in range(depth):
                units_l.append(l); units_b.append(b); chain.append("in" if l == 0 else "scr")
        nc = _get_prog(("fused", S_TOK, per, depth), S_TOK, per, units_l, units_b, chain)
        wF, wO, wM, wsm = _prep_weights(*args, layers=list(range(depth)))
        in_maps = [{"x": np.ascontiguousarray(x[c * per:(c + 1) * per]), "mem": np.ascontiguousarray(mem[c * per:(c + 1) * per]),
                    "wF": wF, "wO": wO, "wM": wM, "wsm": wsm, "cf32": cfv, "cbf": cbv} for c in range(N_CORES)]
        res = run_bass_kernel_spmd(nc, in_maps, core_ids=list(range(N_CORES)))
        return np.concatenate([r["out"] for r in res.results], axis=0)
    cur = x
    nc = _get_prog(("unit", S_TOK), S_TOK, 1, [0], [0], ["in"])
    for l in range(depth):
        wF, wO, wM, wsm = _prep_weights(*args, layers=[l])
        nxt = np.empty_like(cur)
        for b in range(per):
            in_maps = [{"x": np.ascontiguousarray(cur[c * per + b:c * per + b + 1]),
                        "mem": np.ascontiguousarray(mem[c * per + b:c * per + b + 1]),
                        "wF": wF, "wO": wO, "wM": wM, "wsm": wsm, "cf32": cfv, "cbf": cbv} for c in range(N_CORES)]
            res = run_bass_kernel_spmd(nc, in_maps, core_ids=list(range(N_CORES)))
            for c in range(N_CORES):
                nxt[c * per + b] = res.results[c]["out"][0]
        cur = nxt
    return cur
```

```python
import math
import numpy as np
import ml_dtypes
import concourse.bass as bass
import concourse.mybir as mybir
from concourse.bass_utils import run_bass_kernel_spmd

F32 = mybir.dt.float32
BF16 = mybir.dt.bfloat16
AF = mybir.ActivationFunctionType
ALU = mybir.AluOpType
AX = mybir.AxisListType

D = 1024
NMEM = 256
TOPK = 256
NBIS = 13
EPS = 1e-6
NEG = -30000.0
N_CORES = 8
DBG_STAGE = 99
DBG_SUB = 99
SKEW = 2

SM_GCOL, SM_IW, SM_WUK, SM_WUV, SM_KVG, SM_B31, SM_POSTG, SM_TB = 0, 8, 72, 456, 840, 968, 974, 1998
NSM = 1998 + 6 * 2 * 128
CB_ID, CB_TRI, CB_ONES, CB_PENS, CB_PEND, CB_EKB, CB_SEL = 0, 128, 256, 384, 512, 640, 896
NCB = 896 + 16 * 128
CF_NEGB, CF_P2A, CF_P2B = 0, 128, 144
NCF = 160


class Buf:
    __slots__ = ("name", "w", "r", "excl")

    def __init__(self, name, excl=False):
        self.name = name
        self.w = None
        self.r = {}
        self.excl = excl


class Sched:
    ENGS = ("pe", "act", "dve", "pool", "sp")

    def __init__(self, nc):
        self.nc = nc
        self.ops = {e: [] for e in self.ENGS}
        self.sems = {}
        self.cnt = {}
        self.waited = {e: {} for e in self.ENGS}
        self._ctx = []
        for e in self.ENGS:
            self._newsem("E_" + e)

    def _newsem(self, key):
        g = self.nc.semaphore(key)
        h = g.__enter__()
        self._ctx.append(g)
        self.sems[key] = h
        self.cnt[key] = 0
        return key

    def dma_slot(self, name):
        return self._newsem("D_" + name)

    def _deps(self, e, reads, writes):
        deps = {}

        def add(ev):
            if ev is None:
                return
            k, v = ev
            if deps.get(k, 0) < v:
                deps[k] = v
        for b in reads:
            add(b.w)
        for b in writes:
            add(b.w)
            for k, v in b.r.items():
                add((k, v))
        waits = []
        mykey = "E_" + e
        for k, v in deps.items():
            if k == mykey and e in ("pe", "sp"):
                continue
            if self.waited[e].get(k, 0) >= v:
                continue
            self.waited[e][k] = v
            waits.append((k, v))
        return waits

    def _record(self, ev, reads, writes):
        k, v = ev
        for b in reads:
            if b.r.get(k, 0) < v:
                b.r[k] = v
        for b in writes:
            b.w = ev
            b.r = {}

    def op(self, e, fn, reads=(), writes=()):
        ex = [b for b in reads if b.excl]
        if ex:
            writes = list(writes) + ex
        waits = self._deps(e, reads, writes)
        k = "E_" + e
        self.cnt[k] += 1
        ev = (k, self.cnt[k])
        self.ops[e].append((waits, fn, (k, 1)))
        self._record(ev, reads, writes)
        return ev

    def dma(self, fn, slot, reads=(), writes=(), e="sp"):
        waits = self._deps(e, reads, writes)
        self.cnt[slot] += 16
        ev = (slot, self.cnt[slot])
        self.ops[e].append((waits, fn, (slot, 16)))
        self._record(ev, reads, writes)
        return ev

    def final_wait(self, e, bufs):
        waits = self._deps(e, bufs, bufs)
        self.ops[e].append((waits, None, None))

    def emit(self):
        nc = self.nc
        needed = {}
        for e in self.ENGS:
            for waits, fn, inc in self.ops[e]:
                for k, v in waits:
                    if k.startswith("E_"):
                        needed.setdefault(k, set()).add(v)
        rank = {k: {v: i + 1 for i, v in enumerate(sorted(vs))} for k, vs in needed.items()}
        with nc.Block() as block:
            def run(ename):
                def body(eng):
                    seq = 0
                    mykey = "E_" + ename
                    myrank = rank.get(mykey, {})
                    for waits, fn, inc in self.ops[ename]:
                        for k, v in waits:
                            eng.wait_ge(self.sems[k], rank[k][v] if k.startswith("E_") else v)
                        if fn is None:
                            continue
                        inst = fn(eng)
                        if inc[0] == mykey:
                            seq += 1
                            if seq in myrank:
                                inst.then_inc(self.sems[mykey], 1)
                        else:
                            inst.then_inc(self.sems[inc[0]], inc[1])
                return body
            block.tensor(run("pe"))
            block.scalar(run("act"))
            block.vector(run("dve"))
            block.gpsimd(run("pool"))
            block.sync(run("sp"))

    def close(self):
        for g in reversed(self._ctx):
            g.__exit__(None, None, None)


def build_program(S_TOK, NB, layer_of_unit, batch_of_unit, chain):
    NQ = S_TOK // 512
    NBLK = S_TOK // 128
    L = max(layer_of_unit) + 1
    nc = bass.Bass("TRN2", target_bir_lowering=False)
    x_d = nc.dram_tensor("x", [NB, S_TOK, D], F32, kind="ExternalInput").ap()
    mem_d = nc.dram_tensor("mem", [NB, NMEM, D], F32, kind="ExternalInput").ap()
    wF_d = nc.dram_tensor("wF", [L, 26, 128, 1024], F32, kind="ExternalInput").ap()
    wO_d = nc.dram_tensor("wO", [L, 8, 128, 1024], F32, kind="ExternalInput").ap()
    wM_d = nc.dram_tensor("wM", [L, 4, 128, 1024], F32, kind="ExternalInput").ap()
    wsm_d = nc.dram_tensor("wsm", [L, 128, NSM], F32, kind="ExternalInput").ap()
    cf_d = nc.dram_tensor("cf32", [128, NCF], F32, kind="ExternalInput").ap()
    cb_d = nc.dram_tensor("cbf", [128, NCB], BF16, kind="ExternalInput").ap()
    out_d = nc.dram_tensor("out", [NB, S_TOK, D], F32, kind="ExternalOutput").ap()
    need_scr = any(c == "scr" for c in chain)
    scr_d = nc.dram_tensor("xscr", [NB, S_TOK, D], F32, kind="Internal").ap() if need_scr else None

    wsc_d = nc.dram_tensor("wscr", [L, 38, 128, 1024], BF16, kind="Internal").ap()
    S = Sched(nc)
    ctx = []

    def sb(name, shape, dt):
        g = nc.sbuf_tensor(name, shape, dt)
        h = g.__enter__()
        ctx.append(g)
        return h

    psum = []
    PB = []
    for i in range(8):
        g = nc.psum_tensor(f"ps{i}", [128, 512], F32)
        psum.append(g.__enter__())
        ctx.append(g)
        PB.append(Buf(f"ps{i}", excl=True))
    free_banks = list(range(8))

    def bget():
        return free_banks.pop(0)

    def bput(i):
        free_banks.append(i)

    cb = sb("cb", [128, NCB], BF16); CBb = Buf("cb")
    cf = sb("cf", [128, NCF], F32); CFb = Buf("cf")
    ident = cb[:, CB_ID:CB_ID + 128]
    triM8 = cb[:, CB_TRI:CB_TRI + 128]
    ones = cb[:, CB_ONES:CB_ONES + 128]
    penS = cb[:, CB_PENS:CB_PENS + 128]
    penD = cb[:, CB_PEND:CB_PEND + 128]

    def ekb(kb):
        return cb[:, CB_EKB + kb * 16:CB_EKB + (kb + 1) * 16]

    def selM8(kb):
        return cb[:, CB_SEL + kb * 128:CB_SEL + (kb + 1) * 128]
    negb = cf[:, CF_NEGB:CF_NEGB + 128]
    p2a = cf[:, CF_P2A:CF_P2A + 16]
    p2b = cf[:, CF_P2B:CF_P2B + 16]

    sbkT = sb("sbkT", [128, 3, S_TOK], BF16); SBK = [Buf(f"sbk{q}") for q in range(NQ)]
    sbv = sb("sbv", [128, NBLK, 384], BF16); SBV = [Buf(f"sbv{q}") for q in range(NQ)]
    ckv = sb("ckv", [128, NBLK, 128], BF16); CKV = [Buf(f"ckv{q}") for q in range(NQ)]
    ckvT = sb("ckvT", [128, S_TOK], BF16); CKVT = [Buf(f"ckvT{q}") for q in range(NQ)]
    ikT4 = sb("ikT4", [128, S_TOK], BF16); IKT = [Buf(f"ikT{q}") for q in range(NQ)]
    sbq = sb("sbq", [128, 6, 512], BF16); SBQ = Buf("sbq")
    sbg = sb("sbg", [128, 3, 512], BF16); SBG = Buf("sbg")
    dq = sb("dq", [128, 3, 512], BF16); DQ = Buf("dq")
    dg = sb("dg", [128, 3, 512], BF16); DG = Buf("dg")
    iq = sb("iq", [128, 2, 512], BF16); IQ = Buf("iq")
    mq = sb("mq", [128, 2, 512], BF16); MQ = Buf("mq")
    mg = sb("mg", [128, 2, 512], BF16); MG = Buf("mg")
    idxw = sb("idxw", [128, 32], F32); IDXW = Buf("idxw")
    hT = sb("hT", [128, 8, 512], BF16); HT = Buf("hT")
    hnmix = sb("hnmix", [128, 4096], BF16); HNMIX = Buf("hnmix")
    xc = sb("xc", [128, 4096], F32); XC = Buf("xc")
    NWB = 2
    wst = [sb(f"wst{i}", [128, 1024], F32) for i in range(NWB)]; WST = [Buf(f"wst{i}") for i in range(NWB)]
    NWF = 4
    wbf = [sb(f"wbf{i}", [128, 1024], BF16) for i in range(NWF)]; WBF = [Buf(f"wbf{i}") for i in range(NWF)]
    big16 = sb("big16", [128, 8192], BF16); BIG = [Buf(f"big{r}") for r in range(16)]
    gcol = sb("gcol", [128, 8], F32); kvg = sb("kvg", [128, 128], F32); b31 = sb("b31", [128, 6], F32)
    postg = sb("postg", [128, 1024], F32)
    SMF = Buf("smallf32")
    wiw = sb("wiw", [128, 64], BF16); wuk = sb("wuk", [128, 384], BF16); wuv = sb("wuv", [128, 384], BF16)
    tb8 = sb("tb8", [128, 1536], BF16)
    SMB = Buf("smallbf")
    memT = sb("memT", [128, 8, 256], BF16); MEMT = Buf("memT")
    memk = sb("memk", [128, 2, 256], BF16); MEMK = Buf("memk")
    memv = sb("memv", [128, 2, 256], BF16); MEMV = Buf("memv")
    membf = sb("membf", [128, 2, 1024], BF16); MEMBF = Buf("membf")
    NT = 3
    etile = [sb(f"et{i}", [128, 512], BF16) for i in range(NT)]; ET = [Buf(f"et{i}") for i in range(NT)]
    atile = [sb(f"at{i}", [128, 512], BF16) for i in range(NT)]; AT = [Buf(f"at{i}") for i in range(NT)]
    cs48 = sb("cs48", [128, 512], BF16); cslo = sb("cslo", [16, 512], BF16); CS = Buf("cs")
    cshi = cs48[0:16, :]
    score = sb("score", [128, S_TOK], F32); SCORE = Buf("score")
    junk = sb("junk", [128, S_TOK], BF16); JUNK = Buf("junk")
    pen = sb("pen", [128, 4, S_TOK], BF16); PEN = [Buf(f"pen{i}") for i in range(4)]
    NR = 8
    rt = [sb(f"rt{i}", [128, 512], BF16) for i in range(NR)]; RT = [Buf(f"rt{i}") for i in range(NR)]
    dgt = [sb(f"dgt{i}", [128, 8, 128], BF16) for i in range(2)]; DGT = [Buf(f"dgt{i}") for i in range(2)]
    ptile = [sb(f"pt{i}", [128, 512], BF16) for i in range(NT)]; PT = [Buf(f"pt{i}") for i in range(NT)]
    qlat = [sb(f"ql{i}", [128, 512], BF16) for i in range(2)]; QL = [Buf(f"ql{i}") for i in range(2)]
    recf = sb("recf", [128, 512], F32); RECF = Buf("recf")
    onb = sb("onb", [128, 512], BF16); ONB = Buf("onb")
    tmpf = sb("tmpf", [128, 1024], F32); TMPF = Buf("tmpf")
    sm = sb("smalls", [128, 64], F32)
    SMS = {n: Buf("sm_" + n) for n in ("ss", "rs", "ssk", "rsk", "bis", "ssy")}
    steps = sb("steps", [128, 32], F32); STEPS = Buf("steps")
    ss = sm[:, 0:4]; rs = sm[:, 4:8]; ssk = sm[:, 8:12]; rsk = sm[:, 12:16]
    mx = sm[:, 16:17]; mn = sm[:, 17:18]; thr = sm[:, 18:19]; rng = sm[:, 19:20]; cnt = sm[:, 20:21]; dd = sm[:, 21:22]
    ssy = sm[:, 24:26]; ssy2 = sm[:, 26:27]; rsy = sm[:, 27:28]

    sl_c = S.dma_slot("const"); sl_c2 = S.dma_slot("const2"); sl_x = S.dma_slot("x"); sl_o = S.dma_slot("o"); sl_m = S.dma_slot("mem")
    sl_s = S.dma_slot("small"); sl_w = [S.dma_slot(f"w{i}") for i in range(NWB)]
    sl_wb = [S.dma_slot(f"wb{i}") for i in range(NWF)]; sl_wo = [S.dma_slot(f"wo{i}") for i in range(NWB)]
    sl_big = [S.dma_slot(f"big{i}") for i in range(8)]
    WSC = [[Buf(f"wsc{l_}_{i}") for i in range(38)] for l_ in range(L)]
    OUTB = Buf("outdram")
    SCRB = {}

    cnt_rr = {"wf": 0, "w": 0, "e": 0, "a": 0, "r": 0, "p": 0, "q": 0, "d": 0, "ev": 0}

    def rr(key, n):
        v = cnt_rr[key] % n
        cnt_rr[key] += 1
        return v

    S.op("dve", lambda e: e.memset(cs48[:, :], 0.0), writes=[CS])
    S.op("dve", lambda e: e.memset(sbq[:].rearrange("p h t -> p (h t)"), 0.0), writes=[SBQ])
    S.dma(lambda e: e.dma_start(out=cb[:], in_=cb_d[:, :]), sl_c, writes=[CBb])
    S.dma(lambda e: e.dma_start(out=cf[:], in_=cf_d[:, :]), sl_c2, writes=[CFb])

    for l_ in range(L):
        for idx in range(38):
            src = wF_d[l_, idx, :, :] if idx < 26 else (wM_d[l_, idx - 26, :, :] if idx < 30 else wO_d[l_, idx - 30, :, :])
            k = rr("w", NWB)
            S.dma(lambda e, k=k, src=src: e.dma_start(out=wst[k][:], in_=src), sl_w[k], writes=[WST[k]])
            kf = rr("wf", NWF)
            S.op("act", lambda e, k=k, kf=kf: e.activation(out=wbf[kf][:], in_=wst[k][:], func=AF.Copy),
                 reads=[WST[k]], writes=[WBF[kf]])
            S.dma(lambda e, kf=kf, l_=l_, idx=idx: e.dma_start(out=wsc_d[l_, idx, :, :], in_=wbf[kf][:]), sl_wb[kf],
                  reads=[WBF[kf]], writes=[WSC[l_][idx]])

    def wchunk(l_, idx):
        kf = rr("wf", NWF)
        S.dma(lambda e: e.dma_start(out=wbf[kf][:], in_=wsc_d[l_, idx, :, :]), sl_wb[kf], reads=[WSC[l_][idx]], writes=[WBF[kf]])
        return wbf[kf], WBF[kf]

    LA = 3

    def wstream(l_, idxs):
        pend_ = []
        it = iter(idxs)
        for _ in range(LA):
            nx = next(it, None)
            if nx is not None:
                pend_.append(wchunk(l_, nx))
        while pend_:
            cur = pend_.pop(0)
            nx = next(it, None)
            if nx is not None:
                pend_.append(wchunk(l_, nx))
            yield cur

    def evac_copy(out_ap, in_ap, reads, writes):
        if rr("ev", 2) == 0:
            S.op("act", lambda e: e.activation(out=out_ap, in_=in_ap, func=AF.Copy), reads=reads, writes=writes)
        else:
            S.op("dve", lambda e: e.tensor_copy(out=out_ap, in_=in_ap), reads=reads, writes=writes)

    def rms_scale(src_ss, dst_rs, n, inv_n, B_ss, B_rs):
        S.op("act", lambda e: e.activation(out=dst_rs, in_=src_ss, func=AF.Sqrt, scale=inv_n, bias=EPS),
             reads=[B_ss], writes=[B_rs])
        S.op("dve", lambda e: e.reciprocal(out=dst_rs, in_=dst_rs), reads=[B_rs], writes=[B_rs])

    def unit(u):
        l = layer_of_unit[u]
        b = batch_of_unit[u]
        src_x = x_d if chain[u] == "in" else scr_d
        later = any(batch_of_unit[v] == b for v in range(u + 1, len(layer_of_unit)))
        dst_x = scr_d if later else out_d
        if later and b not in SCRB:
            SCRB[b] = [Buf(f"scr{b}_{q}") for q in range(NQ)]

        S.dma(lambda e: e.dma_start(out=xc[:, 0:NSM], in_=wsm_d[l, :, :]), sl_s, writes=[XC])
        for (dst, off, n) in ((gcol, SM_GCOL, 8), (kvg, SM_KVG, 128), (b31, SM_B31, 6), (postg, SM_POSTG, 1024)):
            S.op("dve", lambda e, dst=dst, off=off, n=n: e.tensor_copy(out=dst[:, 0:n], in_=xc[:, off:off + n]),
                 reads=[XC], writes=[SMF])
        for (dst, off, n) in ((wiw, SM_IW, 64), (wuk, SM_WUK, 384), (wuv, SM_WUV, 384)):
            S.op("dve", lambda e, dst=dst, off=off, n=n: e.tensor_copy(out=dst[:, 0:n], in_=xc[:, off:off + n]),
                 reads=[XC], writes=[SMB])
        S.op("dve", lambda e: e.tensor_scalar(out=tb8[:, :], in0=xc[:, SM_TB:SM_TB + 1536], scalar1=8.0, scalar2=None,
                                              op0=ALU.mult), reads=[XC], writes=[SMB])
        S.dma(lambda e: e.dma_start(out=xc[:, 0:2048].rearrange("p (j d) -> p j d", j=2),
                                    in_=mem_d[b, :, :].rearrange("(j p) d -> p j d", p=128)), sl_m, writes=[XC])
        S.op("dve", lambda e: e.tensor_copy(out=membf[:].rearrange("p j d -> p (j d)"), in_=xc[:, 0:2048]),
             reads=[XC], writes=[MEMBF])
        for c2 in range(4):
            bk = bget()
            pb = psum[bk][:].bitcast(BF16)
            for cc in range(2):
                c = 2 * c2 + cc
                for j in range(2):
                    S.op("pe", lambda e, c=c, j=j, cc=cc, pb=pb: e.transpose(
                        pb[:, cc * 256 + j * 128:cc * 256 + (j + 1) * 128], membf[:, j, c * 128:(c + 1) * 128], ident),
                        reads=[MEMBF, CBb], writes=[PB[bk]])
            evac_copy(memT[:, 2 * c2:2 * c2 + 2, :], pb[:, 0:512].rearrange("p (c m) -> p c m", c=2), [PB[bk]], [MEMT])
            bput(bk)
        for g4, (w, W) in enumerate(wstream(l, [26, 27, 28, 29])):
            w3 = w[:].rearrange("p (c g) -> p c g", c=8)
            bk = bget()
            if g4 < 2:
                for c in range(8):
                    S.op("pe", lambda e, c=c, w3=w3, bk=bk: e.matmul(psum[bk][:, 0:256], lhsT=w3[:, c, :], rhs=memT[:, c, :],
                                                                    start=(c == 0), stop=(c == 7)),
                         reads=[W, MEMT], writes=[PB[bk]])
                evac_copy(memk[:, g4, :], psum[bk][:, 0:256], [PB[bk]], [MEMK])
            else:
                for j in range(2):
                    for c in range(8):
                        S.op("pe", lambda e, c=c, j=j, w3=w3, bk=bk: e.matmul(
                            psum[bk][:, j * 128:(j + 1) * 128], lhsT=memT[:, c, j * 128:(j + 1) * 128], rhs=w3[:, c, :],
                            start=(c == 0), stop=(c == 7)), reads=[W, MEMT], writes=[PB[bk]])
                evac_copy(memv[:, :, (g4 - 2) * 128:(g4 - 1) * 128], psum[bk][:, 0:256].rearrange("p (j g) -> p j g", j=2),
                          [PB[bk]], [MEMV])
            bput(bk)

        for tq in range(NQ):
            chunk_phase(u, l, b, tq, src_x, dst_x)

    def chunk_phase(u, l, b, tq, src_x, dst_x):
        t0 = tq * 512
        hn = hnmix[:].rearrange("p (i d) -> p i d", i=4)
        mix = hnmix[:].rearrange("p (c t) -> p c t", c=8)
        xc3 = xc[:].rearrange("p (i d) -> p i d", i=4)
        rd = [XC]
        if src_x is scr_d:
            rd = [XC] + [SCRB[b][tq]]
        S.dma(lambda e: e.dma_start(out=xc3, in_=src_x[b, t0:t0 + 512, :].rearrange("(i p) d -> p i d", p=128)),
              sl_x, reads=rd[1:], writes=[XC])
        for i in range(4):
            S.op("act", lambda e, i=i: e.activation(out=junk[:, 0:1024], in_=xc3[:, i, :], func=AF.Square,
                                                    accum_out=ss[:, i:i + 1]), reads=[XC], writes=[JUNK, SMS["ss"]])
        rms_scale(ss, rs, 4, 1.0 / D, SMS["ss"], SMS["rs"])
        for i in range(4):
            S.op("dve", lambda e, i=i: e.tensor_scalar(out=hn[:, i, :], in0=xc3[:, i, :], scalar1=rs[:, i:i + 1],
                                                       scalar2=None, op0=ALU.mult),
                 reads=[XC, SMS["rs"]], writes=[HNMIX])
        for c2 in range(4):
            bk = bget()
            pb = psum[bk][:].bitcast(BF16)
            for cc in range(2):
                c = 2 * c2 + cc
                for i in range(4):
                    S.op("pe", lambda e, c=c, i=i, cc=cc, pb=pb: e.transpose(
                        pb[:, cc * 512 + i * 128:cc * 512 + (i + 1) * 128], hn[:, i, c * 128:(c + 1) * 128], ident),
                        reads=[HNMIX, CBb], writes=[PB[bk]])
            for cc in range(2):
                c = 2 * c2 + cc
                S.op("dve", lambda e, c=c, cc=cc, pb=pb: e.tensor_scalar(
                    out=hT[:, c, :], in0=pb[:, cc * 512:(cc + 1) * 512], scalar1=gcol[:, c:c + 1], scalar2=None,
                    op0=ALU.mult), reads=[PB[bk], SMF], writes=[HT])
            bput(bk)

        if DBG_STAGE < 2:
            return
        fdest = ([("sbq", i) for i in range(3)] + [("sbk", i) for i in range(3)] + [("sbg", i) for i in range(3)]
                 + [("dq", i) for i in range(3)] + [("dg", i) for i in range(3)] + [("iq", 0), ("iq", 1), ("ik", 0)]
                 + [("mq", 0), ("mq", 1), ("mg", 0), ("mg", 1)])
        dst_tab = {"sbq": (sbq, SBQ), "sbg": (sbg, SBG), "dq": (dq, DQ), "dg": (dg, DG), "iq": (iq, IQ),
                   "mq": (mq, MQ), "mg": (mg, MG)}
        ptb = None
        for cc, (w, W) in enumerate(wstream(l, list(range(26)))):
            w3 = w[:].rearrange("p (c g) -> p c g", c=8)
            if cc < 22:
                name, ci = fdest[cc]
                bk = bget()
                for c in range(8):
                    S.op("pe", lambda e, c=c, w3=w3, bk=bk: e.matmul(psum[bk][:, :], lhsT=w3[:, c, :], rhs=hT[:, c, :],
                                                                    start=(c == 0), stop=(c == 7)),
                         reads=[W, HT], writes=[PB[bk]])
                if name == "sbk":
                    evac_copy(sbkT[:, ci, t0:t0 + 512], psum[bk][:, :], [PB[bk]], [SBK[tq]])
                elif name == "ik":
                    evac_copy(ikT4[:, t0:t0 + 512], psum[bk][:, :], [PB[bk]], [IKT[tq]])
                elif name in ("sbg", "dg", "mg") and DBG_SUB >= 2:
                    dt_, DB = dst_tab[name]
                    hs = rr("ev", 2) * 512
                    S.op("act", lambda e, bk=bk, hs=hs: e.activation(out=tmpf[:, hs:hs + 512], in_=psum[bk][:, :], func=AF.Exp,
                                                                     scale=-1.0), reads=[PB[bk]], writes=[TMPF])
                    S.op("act", lambda e, hs=hs: e.activation(out=tmpf[:, hs:hs + 512], in_=tmpf[:, hs:hs + 512], func=AF.Ln, bias=1.0),
                         reads=[TMPF], writes=[TMPF])
                    S.op("act", lambda e, hs=hs: e.activation(out=tmpf[:, hs:hs + 512], in_=tmpf[:, hs:hs + 512], func=AF.Exp, scale=-1.0),
                         reads=[TMPF], writes=[TMPF])
                    S.op("dve", lambda e, dt_=dt_, ci=ci, bk=bk, hs=hs: e.tensor_tensor(
                        out=dt_[:, ci, :], in0=psum[bk][:, :], in1=tmpf[:, hs:hs + 512], op=ALU.mult),
                        reads=[PB[bk], TMPF], writes=[DB])
                elif name == "sbq":
                    evac_copy(sbq[0:64, 2 * ci, :], psum[bk][0:64, :], [PB[bk]], [SBQ])
                    evac_copy(sbq[64:128, 2 * ci + 1, :], psum[bk][64:128, :], [PB[bk]], [SBQ])
                else:
                    dt_, DB = dst_tab[name]
                    evac_copy(dt_[:, ci, :], psum[bk][:, :], [PB[bk]], [DB])
                bput(bk)
            elif DBG_SUB >= 30:
                g4 = cc - 22
                if g4 == 0:
                    ptb = [bget() for _ in range(4)]
                for i in range(4):
                    for c in range(8):
                        S.op("pe", lambda e, c=c, i=i, w3=w3, g4=g4: e.matmul(
                            psum[ptb[i]][:, g4 * 128:(g4 + 1) * 128], lhsT=hT[:, c, i * 128:(i + 1) * 128], rhs=w3[:, c, :],
                            start=(c == 0), stop=(c == 7)), reads=[W, HT], writes=[PB[ptb[i]]])
        if DBG_SUB < 30:
            return
        for i in range(4):
            j = 4 * tq + i
            evac_copy(sbv[:, j, :], psum[ptb[i]][:, 0:384], [PB[ptb[i]]], [SBV[tq]])
            evac_copy(tmpf[:, i * 128:(i + 1) * 128], psum[ptb[i]][:, 384:512], [PB[ptb[i]]], [TMPF])
        for i in range(4):
            S.op("act", lambda e, i=i: e.activation(out=junk[:, 0:128], in_=tmpf[:, i * 128:(i + 1) * 128], func=AF.Square,
                                                    accum_out=ssk[:, i:i + 1]),
                 reads=[TMPF], writes=[JUNK, SMS["ssk"]])
        rms_scale(ssk, rsk, 4, 1.0 / 128, SMS["ssk"], SMS["rsk"])
        for i in range(4):
            j = 4 * tq + i
            S.op("dve", lambda e, i=i, j=j: e.scalar_tensor_tensor(out=ckv[:, j, :], in0=tmpf[:, i * 128:(i + 1) * 128],
                                                                   scalar=rsk[:, i:i + 1], in1=kvg[:, :],
                                                                   op0=ALU.mult, op1=ALU.mult),
                 reads=[TMPF, SMS["rsk"], SMF], writes=[CKV[tq]])
        for i in range(4):
            bput(ptb[i])
        if DBG_SUB < 40:
            return
        bk = bget()
        pb = psum[bk][:].bitcast(BF16)
        for i in range(4):
            j = 4 * tq + i
            S.op("pe", lambda e, i=i, j=j, pb=pb: e.transpose(pb[:, i * 128:(i + 1) * 128], ckv[:, j, :], ident),
                 reads=[CKV[tq], CBb], writes=[PB[bk]])
        evac_copy(ckvT[:, t0:t0 + 512], pb[:, 0:512], [PB[bk]], [CKVT[tq]])
        bput(bk)
        if DBG_SUB < 50:
            return
        bk = bget()
        wiw3 = wiw[:].rearrange("p (c g) -> p c g", c=8)
        for i in range(4):
            for c in range(8):
                S.op("pe", lambda e, c=c, i=i, bk=bk: e.matmul(psum[bk][:, i * 8:(i + 1) * 8],
                                                                lhsT=hT[:, c, i * 128:(i + 1) * 128], rhs=wiw3[:, c, :],
                                                                start=(c == 0), stop=(c == 7)),
                     reads=[SMB, HT], writes=[PB[bk]])
        S.op("dve", lambda e, bk=bk: e.tensor_scalar(out=idxw[:, :], in0=psum[bk][:, 0:32], scalar1=1.0 / 16, scalar2=None,
                                                     op0=ALU.mult), reads=[PB[bk]], writes=[IDXW])
        bput(bk)

        if DBG_STAGE < 3:
            return
        nkb = 4 * tq + 4
        KS = lambda lst: [lst[q] for q in range(tq + 1)]

        def indexer(i):
            qb = 4 * tq + i
            if qb < 2:
                return
            yield
            nk = (qb + 1) * 128
            nkc = (nk + 511) // 512
            kd = rr("d", 2)
            for h in range(8):
                S.op("act", lambda e, h=h, kd=kd: e.activation(out=dgt[kd][:, h, :], in_=ident, func=AF.Copy,
                                                               scale=idxw[:, i * 8 + h:i * 8 + h + 1]),
                     reads=[CBb, IDXW], writes=[DGT[kd]])
            for kc in range(nkc):
                w_ = min(512, nk - kc * 512)
                bs = bget()
                pend = []
                for g in range(2):
                    bds = [bget() for _ in range(4)]
                    for j in range(4):
                        hp = j * 32
                        tp = (96, 0) if hp == 96 else None
                        S.op("pe", lambda e, g=g, hp=hp, tp=tp, bd=bds[j], kc=kc, w_=w_: e.matmul(
                            psum[bd][:, 0:w_], lhsT=iq[hp:hp + 32, g, i * 128:(i + 1) * 128],
                            rhs=ikT4[hp:hp + 32, kc * 512:kc * 512 + w_], start=True, stop=True, tile_position=tp),
                            reads=[IQ] + KS(IKT), writes=[PB[bds[j]]])
                    for j in range(4):
                        h = 4 * g + j
                        kr = rr("r", NR)
                        S.op("dve", lambda e, kr=kr, bd=bds[j], w_=w_: e.tensor_scalar(out=rt[kr][:, 0:w_], in0=psum[bd][:, 0:w_],
                                                                                      scalar1=0.0, scalar2=None, op0=ALU.max),
                             reads=[PB[bds[j]]], writes=[RT[kr]])
                        bput(bds[j])
                        pend.append(lambda h=h, kr=kr, bs=bs, w_=w_: S.op("pe", lambda e: e.matmul(
                            psum[bs][:, 0:w_], lhsT=dgt[kd][:, h, :], rhs=rt[kr][:, 0:w_], start=(h == 0), stop=(h == 7)),
                            reads=[DGT[kd], RT[kr]], writes=[PB[bs]]))
                    while len(pend) > 4:
                        pend.pop(0)()
                    yield
                while pend:
                    pend.pop(0)()
                last = (kc == nkc - 1)
                wc = w_ - 128 if last else w_
                if wc > 0:
                    S.op("dve", lambda e, bs=bs, kc=kc, wc=wc: e.tensor_copy(out=score[:, kc * 512:kc * 512 + wc],
                                                                             in_=psum[bs][:, 0:wc]),
                         reads=[PB[bs]], writes=[SCORE])
                if last:
                    S.op("dve", lambda e, bs=bs, w_=w_: e.tensor_tensor(out=score[:, nk - 128:nk], in0=psum[bs][:, w_ - 128:w_],
                                                                       in1=negb, op=ALU.add),
                         reads=[PB[bs], CFb], writes=[SCORE])
                bput(bs)
            B = SMS["bis"]
            S.op("dve", lambda e: e.tensor_reduce(out=mx, in_=score[:, 0:nk], axis=AX.X, op=ALU.max), reads=[SCORE], writes=[B])
            S.op("dve", lambda e: e.tensor_reduce(out=mn, in_=score[:, 0:nk - 128], axis=AX.X, op=ALU.min),
                 reads=[SCORE], writes=[B])
            S.op("dve", lambda e: e.tensor_tensor(out=rng, in0=mx, in1=mn, op=ALU.subtract), reads=[B], writes=[B])
            S.op("dve", lambda e: e.tensor_tensor(out=thr, in0=mx, in1=mn, op=ALU.add), reads=[B], writes=[B])
            S.op("dve", lambda e: e.tensor_scalar(out=thr, in0=thr, scalar1=0.5, scalar2=None, op0=ALU.mult), reads=[B], writes=[B])
            S.op("dve", lambda e: e.tensor_scalar(out=steps[:, 0:16], in0=p2a, scalar1=rng, scalar2=None, op0=ALU.mult),
                 reads=[B, CFb], writes=[STEPS])
            S.op("dve", lambda e: e.tensor_scalar(out=steps[:, 16:32], in0=p2b, scalar1=rng, scalar2=None, op0=ALU.mult),
                 reads=[B, CFb], writes=[STEPS])
            for it in range(NBIS):
                S.op("dve", lambda e: e.tensor_scalar(out=junk[:, 0:nk], in0=score[:, 0:nk], scalar1=thr, scalar2=0.0,
                                                      op0=ALU.is_ge, op1=ALU.add, accum_out=cnt),
                     reads=[SCORE, B], writes=[JUNK, B])
                S.op("dve", lambda e, it=it: e.tensor_scalar(out=dd, in0=cnt, scalar1=float(TOPK), scalar2=steps[:, 16 + it:17 + it],
                                                             op0=ALU.is_ge, op1=ALU.mult), reads=[B, STEPS], writes=[B])
                S.op("dve", lambda e, it=it: e.scalar_tensor_tensor(out=thr, in0=dd, scalar=steps[:, it:it + 1], in1=thr,
                                                                    op0=ALU.subtract, op1=ALU.add),
                     reads=[B, STEPS], writes=[B])
                yield
                yield
            S.op("dve", lambda e: e.tensor_scalar(out=pen[:, i, 0:nk], in0=score[:, 0:nk], scalar1=thr, scalar2=NEG,
                                                  op0=ALU.is_lt, op1=ALU.mult), reads=[SCORE, B], writes=[PEN[i]])

        def sb_head(h):
            ch, hp = h // 2, (h % 2) * 64
            bcs = bget()
            pend = []
            for kb in range(nkb):
                c0 = max(0, kb - 4 * tq) * 128
                diag = kb >= 4 * tq
                bz = bget()
                S.op("pe", lambda e, kb=kb, c0=c0, bz=bz, diag=diag: e.matmul(
                    psum[bz][:, c0:512], lhsT=sbkT[:, ch, kb * 128:(kb + 1) * 128], rhs=sbq[:, h, c0:512],
                    start=True, stop=not diag), reads=KS(SBK) + [SBQ], writes=[PB[bz]])
                if diag:
                    S.op("pe", lambda e, c0=c0, bz=bz: e.matmul(psum[bz][:, c0:c0 + 128], lhsT=penS, rhs=ident,
                                                                 start=False, stop=True), reads=[CBb], writes=[PB[bz]])
                ke = rr("e", NT)
                S.op("act", lambda e, ke=ke, c0=c0, bz=bz: e.activation(out=etile[ke][:, c0:512], in_=psum[bz][:, c0:512],
                                                                        func=AF.Exp, scale=0.125),
                     reads=[PB[bz]], writes=[ET[ke]])
                bput(bz)
                sp = big16[:, kb * 512:(kb + 1) * 512]
                S.op("act", lambda e, ke=ke, c0=c0, sp=sp: e.activation(out=sp[:, c0:512], in_=etile[ke][:, c0:512],
                                                                        func=AF.Ln, bias=1.0),
                     reads=[ET[ke]], writes=[BIG[kb]])
                pend.append(lambda kb=kb, c0=c0, sp=sp: S.op("pe", lambda e: e.matmul(
                    psum[bcs][0:16, c0:512], lhsT=ekb(kb), rhs=sp[:, c0:512], start=(kb == 0), stop=(kb == nkb - 1)),
                    reads=[CBb, BIG[kb]], writes=[PB[bcs]]))
                if len(pend) > SKEW:
                    pend.pop(0)()
                yield
            while pend:
                pend.pop(0)()
            S.op("dve", lambda e: e.tensor_copy(out=cs48[0:16, :], in_=psum[bcs][0:16, :]), reads=[PB[bcs]], writes=[CS])
            S.op("dve", lambda e: e.tensor_tensor(out=cslo[:, :], in0=psum[bcs][0:16, :], in1=cs48[0:16, :], op=ALU.subtract),
                 reads=[PB[bcs], CS], writes=[CS])
            S.op("dve", lambda e: e.tensor_copy(out=cs48[32:48, :], in_=cslo[:, :]), reads=[CS], writes=[CS])
            bput(bcs)
            bo = bget()
            pend = []
            for kb in range(nkb):
                c0 = max(0, kb - 4 * tq) * 128
                diag = kb >= 4 * tq
                bi = bget()
                sp = big16[:, kb * 512:(kb + 1) * 512]
                S.op("pe", lambda e, kb=kb, c0=c0, bi=bi: e.matmul(
                    psum[bi][:, c0:512], lhsT=sbkT[:, ch, kb * 128:(kb + 1) * 128], rhs=sbq[:, h, c0:512],
                    start=True, stop=False), reads=KS(SBK) + [SBQ], writes=[PB[bi]])
                S.op("pe", lambda e, c0=c0, bi=bi, sp=sp: e.matmul(psum[bi][:, c0:512], lhsT=triM8, rhs=sp[:, c0:512],
                                                                   start=False, stop=False),
                     reads=[CBb, BIG[kb]], writes=[PB[bi]])
                S.op("pe", lambda e, kb=kb, c0=c0, bi=bi, diag=diag: e.matmul(psum[bi][:, c0:512], lhsT=selM8(kb),
                                                                              rhs=cs48[:, c0:512], start=False, stop=not diag),
                     reads=[CBb, CS], writes=[PB[bi]])
                if diag:
                    S.op("pe", lambda e, c0=c0, bi=bi: e.matmul(psum[bi][:, c0:c0 + 128], lhsT=penS, rhs=ident,
                                                                 start=False, stop=True), reads=[CBb], writes=[PB[bi]])
                ka = rr("a", NT)
                S.op("act", lambda e, ka=ka, c0=c0, bi=bi: e.activation(out=atile[ka][:, c0:512], in_=psum[bi][:, c0:512],
                                                                        func=AF.Exp, scale=0.125),
                     reads=[PB[bi]], writes=[AT[ka]])
                bput(bi)
                pend.append(lambda kb=kb, c0=c0, ka=ka: S.op("pe", lambda e: e.matmul(
                    psum[bo][:, c0:512], lhsT=sbv[:, kb, ch * 128:(ch + 1) * 128], rhs=atile[ka][:, c0:512],
                    start=(kb == 0), stop=(kb == nkb - 1)), reads=KS(SBV) + [AT[ka]], writes=[PB[bo]]))
                if len(pend) > SKEW:
                    pend.pop(0)()
                yield
            while pend:
                pend.pop(0)()
            S.op("dve", lambda e: e.tensor_tensor(out=mix[hp:hp + 64, ch, :], in0=psum[bo][hp:hp + 64, :],
                                                  in1=sbg[hp:hp + 64, ch, :], op=ALU.mult),
                 reads=[PB[bo], SBG], writes=[HNMIX])
            bput(bo)

        def mem_head(hm):
            ch, hp = hm // 2, (hm % 2) * 64
            bo = bget(); bd = bget()
            for mb in range(2):
                bl = bget()
                S.op("pe", lambda e, mb=mb, bl=bl: e.matmul(psum[bl][:, :], lhsT=memk[hp:hp + 64, ch, mb * 128:(mb + 1) * 128],
                                                            rhs=mq[hp:hp + 64, ch, :], start=True, stop=True),
                     reads=[MEMK, MQ], writes=[PB[bl]])
                kp = rr("p", NT)
                S.op("act", lambda e, kp=kp, bl=bl: e.activation(out=ptile[kp][:, :], in_=psum[bl][:, :], func=AF.Exp, scale=0.125),
                     reads=[PB[bl]], writes=[PT[kp]])
                bput(bl)
                S.op("pe", lambda e, mb=mb, kp=kp: e.matmul(psum[bo][:, :], lhsT=memv[:, mb, ch * 128:(ch + 1) * 128],
                                                            rhs=ptile[kp][:, :], start=(mb == 0), stop=(mb == 1)),
                     reads=[MEMV, PT[kp]], writes=[PB[bo]])
                S.op("pe", lambda e, mb=mb, kp=kp: e.matmul(psum[bd][:, :], lhsT=ones, rhs=ptile[kp][:, :],
                                                            start=(mb == 0), stop=(mb == 1)),
                     reads=[CBb, PT[kp]], writes=[PB[bd]])
            S.op("act", lambda e: e.activation(out=recf[hp:hp + 64, :], in_=psum[bd][hp:hp + 64, :], func=AF.Ln), reads=[PB[bd]], writes=[RECF])
            S.op("act", lambda e: e.activation(out=recf[hp:hp + 64, :], in_=recf[hp:hp + 64, :], func=AF.Exp, scale=-1.0), reads=[RECF], writes=[RECF])
            S.op("dve", lambda e: e.tensor_tensor(out=tmpf[hp:hp + 64, 0:512], in0=psum[bo][hp:hp + 64, :],
                                                  in1=recf[hp:hp + 64, :], op=ALU.mult), reads=[PB[bo], RECF], writes=[TMPF])
            S.op("dve", lambda e: e.tensor_tensor(out=mix[hp:hp + 64, 6 + ch, :], in0=tmpf[hp:hp + 64, 0:512],
                                                  in1=mg[hp:hp + 64, ch, :], op=ALU.mult), reads=[TMPF, MG], writes=[HNMIX])
            bput(bo); bput(bd)

        def dsa_head(h):
            ch, hp = h // 2, (h % 2) * 64
            bq = bget()
            S.op("pe", lambda e: e.matmul(psum[bq][:, :], lhsT=wuk[hp:hp + 64, ch * 128:(ch + 1) * 128], rhs=dq[hp:hp + 64, ch, :],
                                          start=True, stop=True), reads=[SMB, DQ], writes=[PB[bq]])
            kq = rr("q", 2)
            evac_copy(qlat[kq][:, :], psum[bq][:, :], [PB[bq]], [QL[kq]])
            bput(bq)
            bo = bget(); bd = bget()
            pend = []
            for kb in range(nkb):
                i0 = max(0, kb - 4 * tq)
                c0 = i0 * 128
                bl = bget()
                mm = []
                mm.append((lambda e, st, sp_, kb=kb, c0=c0, bl=bl: e.matmul(
                    psum[bl][:, c0:512], lhsT=ckvT[:, kb * 128:(kb + 1) * 128], rhs=qlat[kq][:, c0:512], start=st, stop=sp_),
                    KS(CKVT) + [QL[kq]]))
                for i in range(i0, 4):
                    qb = 4 * tq + i
                    cs_ = slice(i * 128, (i + 1) * 128)
                    if qb >= 2:
                        mm.append((lambda e, st, sp_, i=i, kb=kb, cs_=cs_, bl=bl: e.matmul(
                            psum[bl][:, cs_], lhsT=pen[:, i, kb * 128:(kb + 1) * 128], rhs=ident, start=st, stop=sp_),
                            [PEN[i], CBb]))
                    elif qb == kb:
                        mm.append((lambda e, st, sp_, cs_=cs_, bl=bl: e.matmul(psum[bl][:, cs_], lhsT=penD, rhs=ident,
                                                                              start=st, stop=sp_), [CBb]))
                    if qb - kb <= 1:
                        jj = qb - kb
                        mm.append((lambda e, st, sp_, cs_=cs_, jj=jj, bl=bl: e.matmul(
                            psum[bl][:, cs_], lhsT=ident, rhs=tb8[:, (h * 2 + jj) * 128:(h * 2 + jj + 1) * 128],
                            start=st, stop=sp_), [CBb, SMB]))
                for n_, (fn, rds) in enumerate(mm):
                    S.op("pe", lambda e, fn=fn, n_=n_, nm=len(mm): fn(e, n_ == 0, n_ == nm - 1), reads=rds, writes=[PB[bl]])
                kp = rr("p", NT)
                near_hi = min(512, max(c0, (kb + 2 - 4 * tq) * 128))
                if near_hi > c0:
                    S.op("act", lambda e, kp=kp, c0=c0, near_hi=near_hi, bl=bl: e.activation(
                        out=ptile[kp][:, c0:near_hi], in_=psum[bl][:, c0:near_hi], func=AF.Exp, scale=0.125),
                        reads=[PB[bl]], writes=[PT[kp]])
                if near_hi < 512:
                    S.op("act", lambda e, kp=kp, near_hi=near_hi, bl=bl: e.activation(
                        out=ptile[kp][:, near_hi:512], in_=psum[bl][:, near_hi:512], func=AF.Exp, scale=0.125,
                        bias=b31[:, h:h + 1]), reads=[PB[bl], SMF], writes=[PT[kp]])
                bput(bl)
                def tail(kb=kb, c0=c0, kp=kp):
                    S.op("pe", lambda e: e.matmul(psum[bo][:, c0:512], lhsT=ckv[:, kb, :], rhs=ptile[kp][:, c0:512],
                                                  start=(kb == 0), stop=(kb == nkb - 1)),
                         reads=KS(CKV) + [PT[kp]], writes=[PB[bo]])
                    S.op("pe", lambda e: e.matmul(psum[bd][:, c0:512], lhsT=ones, rhs=ptile[kp][:, c0:512],
                                                  start=(kb == 0), stop=(kb == nkb - 1)),
                         reads=[CBb, PT[kp]], writes=[PB[bd]])
                pend.append(tail)
                if len(pend) > SKEW:
                    pend.pop(0)()
                yield
            while pend:
                pend.pop(0)()
            S.op("act", lambda e: e.activation(out=recf[:, :], in_=psum[bd][:, :], func=AF.Ln), reads=[PB[bd]], writes=[RECF])
            S.op("act", lambda e: e.activation(out=recf[:, :], in_=recf[:, :], func=AF.Exp, scale=-1.0), reads=[RECF], writes=[RECF])
            S.op("dve", lambda e: e.tensor_tensor(out=onb[:, :], in0=psum[bo][:, :], in1=recf[:, :], op=ALU.mult),
                 reads=[PB[bo], RECF], writes=[ONB])
            bput(bo); bput(bd)
            bu = bget()
            S.op("pe", lambda e: e.matmul(psum[bu][:, :], lhsT=wuv[:, ch * 128:(ch + 1) * 128], rhs=onb[:, :], start=True, stop=True),
                 reads=[SMB, ONB], writes=[PB[bu]])
            S.op("dve", lambda e: e.tensor_tensor(out=mix[hp:hp + 64, 3 + ch, :], in0=psum[bu][hp:hp + 64, :],
                                                  in1=dg[hp:hp + 64, ch, :], op=ALU.mult), reads=[PB[bu], DG], writes=[HNMIX])
            bput(bu)

        def run_par(*gens):
            gens = list(gens)
            while gens:
                for g in list(gens):
                    try:
                        next(g)
                    except StopIteration:
                        gens.remove(g)

        def seq(*fns):
            for f in fns:
                r = f()
                if r is not None:
                    yield from r
                yield

        run_par(seq(*[lambda i=i: indexer(i) for i in range(4)]),
                seq(*([lambda h=h: sb_head(h) for h in range(5)] + [lambda hm=hm: mem_head(hm) for hm in range(4)])))
        run_par(sb_head(5), seq(lambda: dsa_head(0), lambda: dsa_head(1)))
        wO3 = big16[:].rearrange("p (c n) -> p c n", c=8)
        for c in range(8):
            S.dma(lambda e, c=c: e.dma_start(out=big16[:, c * 1024:(c + 1) * 1024], in_=wsc_d[l, 30 + c, :, :]), sl_big[c],
                  reads=[WSC[l][30 + c]], writes=[BIG[2 * c], BIG[2 * c + 1]])
        for h in range(2, 6):
            run_par(dsa_head(h))

        for i in range(4):
            b0 = bget(); b1 = bget()
            bb = (b0, b1)
            for half in range(2):
                for c in range(8):
                    S.op("pe", lambda e, c=c, half=half, i=i, bb=bb: e.matmul(
                        psum[bb[half]][:, :], lhsT=mix[:, c, i * 128:(i + 1) * 128], rhs=wO3[:, c, half * 512:(half + 1) * 512],
                        start=(c == 0), stop=(c == 7)), reads=[HNMIX, BIG[2 * c + half]], writes=[PB[bb[half]]])
            for half in range(2):
                evac_copy(tmpf[:, half * 512:(half + 1) * 512], psum[bb[half]][:, :], [PB[bb[half]]], [TMPF])
            S.op("act", lambda e: e.activation(out=junk[:, 0:1024], in_=tmpf[:, :], func=AF.Square, accum_out=ssy2),
                 reads=[TMPF], writes=[JUNK, SMS["ssy"]])
            rms_scale(ssy2, rsy, 1, 1.0 / D, SMS["ssy"], SMS["ssy"])
            S.op("dve", lambda e: e.scalar_tensor_tensor(out=tmpf[:, :], in0=tmpf[:, :], scalar=rsy, in1=postg[:, :],
                                                         op0=ALU.mult, op1=ALU.mult),
                 reads=[TMPF, SMS["ssy"], SMF], writes=[TMPF])
            bput(b0); bput(b1)
            S.op("pool", lambda e, i=i: e.tensor_tensor(out=xc3[:, i, :], in0=tmpf[:, :], in1=xc3[:, i, :], op=ALU.add),
                 reads=[TMPF, XC], writes=[XC])
        wr = [OUTB] if dst_x is out_d else [SCRB[b][tq]]
        S.dma(lambda e: e.dma_start(out=dst_x[b, t0:t0 + 512, :].rearrange("(i p) d -> p i d", p=128), in_=xc3),
              sl_o, reads=[XC], writes=wr)

    for u in range(len(layer_of_unit)):
        unit(u)
    S.final_wait("sp", [OUTB, XC])
    S.emit()
    S.close()
    for g in reversed(ctx):
        g.__exit__(None, None, None)
    return nc


def _t5_bucket(rel):
    n = np.maximum(rel, 0)
    max_exact = 16
    nf = np.maximum(n, 1).astype(np.float32)
    large = max_exact + (np.log(nf / max_exact) / math.log(128 / max_exact) * (32 - max_exact)).astype(np.int32)
    large = np.minimum(large, 31)
    return np.where(n < max_exact, n, large)


def _constants():
    bf = ml_dtypes.bfloat16
    cbv = np.zeros((128, NCB), np.float32)
    p = np.arange(128)
    cbv[:, CB_ID:CB_ID + 128] = np.eye(128)
    cbv[:, CB_TRI:CB_TRI + 128] = np.where(p[:, None] >= p[None, :], -8.0, 0.0)
    cbv[:, CB_ONES:CB_ONES + 128] = 1.0
    cbv[:, CB_PENS:CB_PENS + 128] = np.where(p[None, :] < p[:, None], 0.0, NEG)
    cbv[:, CB_PEND:CB_PEND + 128] = np.where(p[None, :] <= p[:, None], 0.0, NEG)
    for kb in range(16):
        cbv[:, CB_EKB + kb * 16 + kb] = 1.0
        for jb in range(16):
            if jb > kb:
                cbv[jb, CB_SEL + kb * 128:CB_SEL + (kb + 1) * 128] = -8.0
                cbv[32 + jb, CB_SEL + kb * 128:CB_SEL + (kb + 1) * 128] = -8.0
    cfv = np.zeros((128, NCF), np.float32)
    cfv[:, CF_NEGB:CF_NEGB + 128] = np.where(p[None, :] <= p[:, None], 0.0, -1e30)
    cfv[:, CF_P2A:CF_P2A + 16] = 2.0 ** -(np.arange(16) + 2.0)
    cfv[:, CF_P2B:CF_P2B + 16] = 2.0 ** -(np.arange(16) + 1.0)
    return cfv, cbv.astype(bf)


def _chunk_cols(w, cols):
    sel = w[:, cols]
    n = sel.shape[1] // 128
    a = sel.reshape(8, 128, n, 128)
    return np.ascontiguousarray(a.transpose(2, 1, 0, 3).reshape(n, 128, 1024))


def _prep_weights(pre_norm_g, post_norm_g, w_in, w_uk, w_uv, kv_norm_g, w_mem_kv, w_out, rel_bias, layers):
    o = np.cumsum([0, 384, 384, 384, 384, 384, 128, 384, 256, 32, 8, 256, 256])
    (o_sbq, o_sbk, o_sbv, o_sbg, o_dq, o_ckv, o_dg, o_iq, o_ik, o_iw, o_mq, o_mg) = o[:12]
    r = lambda a, n: list(range(a, a + n))
    fcols = (r(o_sbq, 384) + r(o_sbk, 384) + r(o_sbg, 384) + r(o_dq, 384) + r(o_dg, 384) + r(o_iq, 256)
             + r(o_ik, 32) * 4 + r(o_mq, 256) + r(o_mg, 256) + r(o_sbv, 384) + r(o_ckv, 128))
    assert len(fcols) == 26 * 128
    s_l = np.arange(128)
    wF, wO, wM, wsm = [], [], [], []
    for l in layers:
        wF.append(_chunk_cols(w_in[l], fcols))
        wO.append(np.ascontiguousarray(w_out[l].reshape(8, 128, 1024)))
        wM.append(_chunk_cols(w_mem_kv[l], list(range(512))))
        sm = np.zeros((128, NSM), np.float32)
        sm[:, SM_GCOL:SM_GCOL + 8] = pre_norm_g[l].reshape(8, 128).T
        sm[:, SM_IW:SM_IW + 64] = w_in[l][:, o_iw:o_iw + 8].reshape(8, 128, 8).transpose(1, 0, 2).reshape(128, 64)
        uk = w_uk[l]
        t = uk.reshape(128, 3, 2, 64).transpose(2, 3, 1, 0).reshape(128, 3 * 128)
        sm[:, SM_WUK:SM_WUK + 384] = t
        sm[:, SM_WUV:SM_WUV + 384] = w_uv[l].reshape(128, 384)
        sm[:, SM_KVG:SM_KVG + 128] = kv_norm_g[l][None, :]
        sm[:, SM_B31:SM_B31 + 6] = rel_bias[31][None, :]
        sm[:, SM_POSTG:SM_POSTG + 1024] = post_norm_g[l][None, :]
        for h in range(6):
            for j in range(2):
                rel = s_l[None, :] - s_l[:, None] + 128 * j
                sm[:, SM_TB + (h * 2 + j) * 128:SM_TB + (h * 2 + j + 1) * 128] = rel_bias[_t5_bucket(rel), h]
        wsm.append(sm)
    return (np.stack(wF), np.stack(wO), np.stack(wM), np.stack(wsm))


_PROG_CACHE = {}


def _get_prog(key, *args):
    if key not in _PROG_CACHE:
        _PROG_CACHE[key] = build_program(*args)
    return _PROG_CACHE[key]


FUSED = True


def kernel(x, mem, pre_norm_g, post_norm_g, w_in, w_uk, w_uv, kv_norm_g, w_mem_kv, w_out, rel_bias):
    x = np.asarray(x, np.float32)
    mem = np.asarray(mem, np.float32)
    args = [np.asarray(a, np.float32) for a in (pre_norm_g, post_norm_g, w_in, w_uk, w_uv, kv_norm_g, w_mem_kv, w_out, rel_bias)]
    B, S_TOK, _ = x.shape
    depth = w_in.shape[0]
    per = B // N_CORES
    cfv, cbv = _constants()
    if FUSED:
        units_l = []; units_b = []; chain = []
        for b in range(per):
            for l in range(depth):
                units_l.append(l); units_b.append(b); chain.append("in" if l == 0 else "scr")
        nc = _get_prog(("fused", S_TOK, per, depth), S_TOK, per, units_l, units_b, chain)
        wF, wO, wM, wsm = _prep_weights(*args, layers=list(range(depth)))
        in_maps = [{"x": np.ascontiguousarray(x[c * per:(c + 1) * per]), "mem": np.ascontiguousarray(mem[c * per:(c + 1) * per]),
                    "wF": wF, "wO": wO, "wM": wM, "wsm": wsm, "cf32": cfv, "cbf": cbv} for c in range(N_CORES)]
        res = run_bass_kernel_spmd(nc, in_maps, core_ids=list(range(N_CORES)))
        return np.concatenate([r["out"] for r in res.results], axis=0)
    cur = x
    nc = _get_prog(("unit", S_TOK), S_TOK, 1, [0], [0], ["in"])
    for l in range(depth):
        wF, wO, wM, wsm = _prep_weights(*args, layers=[l])
        nxt = np.empty_like(cur)
        for b in range(per):
            in_maps = [{"x": np.ascontiguousarray(cur[c * per + b:c * per + b + 1]),
                        "mem": np.ascontiguousarray(mem[c * per + b:c * per + b + 1]),
                        "wF": wF, "wO": wO, "wM": wM, "wsm": wsm, "cf32": cfv, "cbf": cbv} for c in range(N_CORES)]
            res = run_bass_kernel_spmd(nc, in_maps, core_ids=list(range(N_CORES)))
            for c in range(N_CORES):
                nxt[c * per + b] = res.results[c]["out"][0]
        cur = nxt
    return cur
```

```python
import math
import numpy as np
import ml_dtypes
import concourse.bass as bass
import concourse.mybir as mybir
from concourse.bass_utils import run_bass_kernel_spmd

F32 = mybir.dt.float32
BF16 = mybir.dt.bfloat16
AF = mybir.ActivationFunctionType
ALU = mybir.AluOpType
AX = mybir.AxisListType

D = 1024
NMEM = 256
TOPK = 256
NBIS = 13
EPS = 1e-6
NEG = -30000.0
N_CORES = 8
DBG_STAGE = 99
DBG_SUB = 99
SKEW = 2

SM_GCOL, SM_IW, SM_WUK, SM_WUV, SM_KVG, SM_B31, SM_POSTG, SM_TB = 0, 8, 72, 456, 840, 968, 974, 1998
NSM = 1998 + 6 * 2 * 128
CB_ID, CB_TRI, CB_ONES, CB_PENS, CB_PEND, CB_EKB, CB_SEL = 0, 128, 256, 384, 512, 640, 896
NCB = 896 + 16 * 128
CF_NEGB, CF_P2A, CF_P2B = 0, 128, 144
NCF = 160


class Buf:
    __slots__ = ("name", "w", "r", "excl")

    def __init__(self, name, excl=False):
        self.name = name
        self.w = None
        self.r = {}
        self.excl = excl


class Sched:
    ENGS = ("pe", "act", "dve", "pool", "sp")

    def __init__(self, nc):
        self.nc = nc
        self.ops = {e: [] for e in self.ENGS}
        self.sems = {}
        self.cnt = {}
        self.waited = {e: {} for e in self.ENGS}
        self._ctx = []
        for e in self.ENGS:
            self._newsem("E_" + e)

    def _newsem(self, key):
        g = self.nc.semaphore(key)
        h = g.__enter__()
        self._ctx.append(g)
        self.sems[key] = h
        self.cnt[key] = 0
        return key

    def dma_slot(self, name):
        return self._newsem("D_" + name)

    def _deps(self, e, reads, writes):
        deps = {}

        def add(ev):
            if ev is None:
                return
            k, v = ev
            if deps.get(k, 0) < v:
                deps[k] = v
        for b in reads:
            add(b.w)
        for b in writes:
            add(b.w)
            for k, v in b.r.items():
                add((k, v))
        waits = []
        mykey = "E_" + e
        for k, v in deps.items():
            if k == mykey and e in ("pe", "sp"):
                continue
            if self.waited[e].get(k, 0) >= v:
                continue
            self.waited[e][k] = v
            waits.append((k, v))
        return waits

    def _record(self, ev, reads, writes):
        k, v = ev
        for b in reads:
            if b.r.get(k, 0) < v:
                b.r[k] = v
        for b in writes:
            b.w = ev
            b.r = {}

    def op(self, e, fn, reads=(), writes=()):
        ex = [b for b in reads if b.excl]
        if ex:
            writes = list(writes) + ex
        waits = self._deps(e, reads, writes)
        k = "E_" + e
        self.cnt[k] += 1
        ev = (k, self.cnt[k])
        self.ops[e].append((waits, fn, (k, 1)))
        self._record(ev, reads, writes)
        return ev

    def dma(self, fn, slot, reads=(), writes=(), e="sp"):
        waits = self._deps(e, reads, writes)
        self.cnt[slot] += 16
        ev = (slot, self.cnt[slot])
        self.ops[e].append((waits, fn, (slot, 16)))
        self._record(ev, reads, writes)
        return ev

    def final_wait(self, e, bufs):
        waits = self._deps(e, bufs, bufs)
        self.ops[e].append((waits, None, None))

    def emit(self):
        nc = self.nc
        needed = {}
        for e in self.ENGS:
            for waits, fn, inc in self.ops[e]:
                for k, v in waits:
                    if k.startswith("E_"):
                        needed.setdefault(k, set()).add(v)
        rank = {k: {v: i + 1 for i, v in enumerate(sorted(vs))} for k, vs in needed.items()}
        with nc.Block() as block:
            def run(ename):
                def body(eng):
                    seq = 0
                    mykey = "E_" + ename
                    myrank = rank.get(mykey, {})
                    for waits, fn, inc in self.ops[ename]:
                        for k, v in waits:
                            eng.wait_ge(self.sems[k], rank[k][v] if k.startswith("E_") else v)
                        if fn is None:
                            continue
                        inst = fn(eng)
                        if inc[0] == mykey:
                            seq += 1
                            if seq in myrank:
                                inst.then_inc(self.sems[mykey], 1)
                        else:
                            inst.then_inc(self.sems[inc[0]], inc[1])
                return body
            block.tensor(run("pe"))
            block.scalar(run("act"))
            block.vector(run("dve"))
            block.gpsimd(run("pool"))
            block.sync(run("sp"))

    def close(self):
        for g in reversed(self._ctx):
            g.__exit__(None, None, None)


def build_program(S_TOK, NB, layer_of_unit, batch_of_unit, chain):
    NQ = S_TOK // 512
    NBLK = S_TOK // 128
    L = max(layer_of_unit) + 1
    nc = bass.Bass("TRN2", target_bir_lowering=False)
    x_d = nc.dram_tensor("x", [NB, S_TOK, D], F32, kind="ExternalInput").ap()
    mem_d = nc.dram_tensor("mem", [NB, NMEM, D], F32, kind="ExternalInput").ap()
    wF_d = nc.dram_tensor("wF", [L, 26, 128, 1024], F32, kind="ExternalInput").ap()
    wO_d = nc.dram_tensor("wO", [L, 8, 128, 1024], F32, kind="ExternalInput").ap()
    wM_d = nc.dram_tensor("wM", [L, 4, 128, 1024], F32, kind="ExternalInput").ap()
    wsm_d = nc.dram_tensor("wsm", [L, 128, NSM], F32, kind="ExternalInput").ap()
    cf_d = nc.dram_tensor("cf32", [128, NCF], F32, kind="ExternalInput").ap()
    cb_d = nc.dram_tensor("cbf", [128, NCB], BF16, kind="ExternalInput").ap()
    out_d = nc.dram_tensor("out", [NB, S_TOK, D], F32, kind="ExternalOutput").ap()
    need_scr = any(c == "scr" for c in chain)
    scr_d = nc.dram_tensor("xscr", [NB, S_TOK, D], F32, kind="Internal").ap() if need_scr else None

    wsc_d = nc.dram_tensor("wscr", [L, 38, 128, 1024], BF16, kind="Internal").ap()
    S = Sched(nc)
    ctx = []

    def sb(name, shape, dt):
        g = nc.sbuf_tensor(name, shape, dt)
        h = g.__enter__()
        ctx.append(g)
        return h

    psum = []
    PB = []
    for i in range(8):
        g = nc.psum_tensor(f"ps{i}", [128, 512], F32)
        psum.append(g.__enter__())
        ctx.append(g)
        PB.append(Buf(f"ps{i}", excl=True))
    free_banks = list(range(8))

    def bget():
        return free_banks.pop(0)

    def bput(i):
        free_banks.append(i)

    cb = sb("cb", [128, NCB], BF16); CBb = Buf("cb")
    cf = sb("cf", [128, NCF], F32); CFb = Buf("cf")
    ident = cb[:, CB_ID:CB_ID + 128]
    triM8 = cb[:, CB_TRI:CB_TRI + 128]
    ones = cb[:, CB_ONES:CB_ONES + 128]
    penS = cb[:, CB_PENS:CB_PENS + 128]
    penD = cb[:, CB_PEND:CB_PEND + 128]

    def ekb(kb):
        return cb[:, CB_EKB + 128 - 2 * kb:CB_EKB + 256 - 2 * kb]

    def selM8(kb):
        return cb[:, CB_SEL + kb * 128:CB_SEL + (kb + 1) * 128]
    negb = cf[:, CF_NEGB:CF_NEGB + 128]
    p2a = cf[:, CF_P2A:CF_P2A + 16]
    p2b = cf[:, CF_P2B:CF_P2B + 16]

    sbkT = sb("sbkT", [128, 3, S_TOK], BF16); SBK = [Buf(f"sbk{q}") for q in range(NQ)]
    sbv = sb("sbv", [128, NBLK, 384], BF16); SBV = [Buf(f"sbv{q}") for q in range(NQ)]
    ckv = sb("ckv", [128, NBLK, 128], BF16); CKV = [Buf(f"ckv{q}") for q in range(NQ)]
    ckvT = sb("ckvT", [128, S_TOK], BF16); CKVT = [Buf(f"ckvT{q}") for q in range(NQ)]
    ikT4 = sb("ikT4", [128, S_TOK], BF16); IKT = [Buf(f"ikT{q}") for q in range(NQ)]
    sbq = sb("sbq", [128, 6, 512], BF16); SBQ = Buf("sbq")
    sbg = sb("sbg", [128, 3, 512], BF16); SBG = Buf("sbg")
    dq = sb("dq", [128, 3, 512], BF16); DQ = Buf("dq")
    dg = sb("dg", [128, 3, 512], BF16); DG = Buf("dg")
    iq = sb("iq", [128, 2, 512], BF16); IQ = Buf("iq")
    mq = sb("mq", [128, 2, 512], BF16); MQ = Buf("mq")
    mg = sb("mg", [128, 2, 512], BF16); MG = Buf("mg")
    idxw = sb("idxw", [128, 32], F32); IDXW = Buf("idxw")
    hT = sb("hT", [128, 8, 512], BF16); HT = Buf("hT")
    hnmix = sb("hnmix", [128, 4096], BF16); HNMIX = Buf("hnmix")
    xc = sb("xc", [128, 4096], F32); XC = Buf("xc")
    NWB = 2
    wst = [sb(f"wst{i}", [128, 1024], F32) for i in range(NWB)]; WST = [Buf(f"wst{i}") for i in range(NWB)]
    NWF = 4
    wbf = [sb(f"wbf{i}", [128, 1024], BF16) for i in range(NWF)]; WBF = [Buf(f"wbf{i}") for i in range(NWF)]
    big16 = sb("big16", [128, 8192], BF16); BIG = [Buf(f"big{r}") for r in range(16)]
    gcol = sb("gcol", [128, 8], F32); kvg = sb("kvg", [128, 128], F32); b31 = sb("b31", [128, 6], F32)
    postg = sb("postg", [128, 1024], F32)
    SMF = Buf("smallf32")
    wiw = sb("wiw", [128, 64], BF16); wuk = sb("wuk", [128, 384], BF16); wuv = sb("wuv", [128, 384], BF16)
    tb8 = sb("tb8", [128, 1536], BF16)
    SMB = Buf("smallbf")
    memT = sb("memT", [128, 8, 256], BF16); MEMT = Buf("memT")
    memk = sb("memk", [128, 2, 256], BF16); MEMK = Buf("memk")
    memv = sb("memv", [128, 2, 256], BF16); MEMV = Buf("memv")
    membf = sb("membf", [128, 2, 1024], BF16); MEMBF = Buf("membf")
    NT = 3
    etile = [sb(f"et{i}", [128, 512], BF16) for i in range(NT)]; ET = [Buf(f"et{i}") for i in range(NT)]
    atile = [sb(f"at{i}", [128, 512], BF16) for i in range(NT)]; AT = [Buf(f"at{i}") for i in range(NT)]
    cs48 = sb("cs48", [128, 512], BF16); cslo = sb("cslo", [32, 512], BF16); CS = Buf("cs")
    cshi = cs48[0:16, :]
    score = sb("score", [128, S_TOK], F32); SCORE = Buf("score")
    junk = sb("junk", [128, S_TOK], BF16); JUNK = Buf("junk")
    pen = sb("pen", [128, 4, S_TOK], BF16); PEN = [Buf(f"pen{i}") for i in range(4)]
    NR = 8
    rt = [sb(f"rt{i}", [128, 512], BF16) for i in range(NR)]; RT = [Buf(f"rt{i}") for i in range(NR)]
    dgt = [sb(f"dgt{i}", [128, 8, 128], BF16) for i in range(2)]; DGT = [Buf(f"dgt{i}") for i in range(2)]
    ptile = [sb(f"pt{i}", [128, 512], BF16) for i in range(NT)]; PT = [Buf(f"pt{i}") for i in range(NT)]
    qlat = [sb(f"ql{i}", [128, 512], BF16) for i in range(2)]; QL = [Buf(f"ql{i}") for i in range(2)]
    recf = sb("recf", [128, 512], F32); RECF = Buf("recf")
    onb = sb("onb", [128, 512], BF16); ONB = Buf("onb")
    tmpf = sb("tmpf", [128, 1024], F32); TMPF = Buf("tmpf")
    sm = sb("smalls", [128, 64], F32)
    SMS = {n: Buf("sm_" + n) for n in ("ss", "rs", "ssk", "rsk", "bis", "ssy")}
    steps = sb("steps", [128, 32], F32); STEPS = Buf("steps")
    ss = sm[:, 0:4]; rs = sm[:, 4:8]; ssk = sm[:, 8:12]; rsk = sm[:, 12:16]
    mx = sm[:, 16:17]; mn = sm[:, 17:18]; thr = sm[:, 18:19]; rng = sm[:, 19:20]; cnt = sm[:, 20:21]; dd = sm[:, 21:22]
    ssy = sm[:, 24:26]; ssy2 = sm[:, 26:27]; rsy = sm[:, 27:28]

    sl_c = S.dma_slot("const"); sl_c2 = S.dma_slot("const2"); sl_x = S.dma_slot("x"); sl_o = S.dma_slot("o"); sl_m = S.dma_slot("mem")
    sl_s = S.dma_slot("small"); sl_w = [S.dma_slot(f"w{i}") for i in range(NWB)]
    sl_wb = [S.dma_slot(f"wb{i}") for i in range(NWF)]; sl_wo = [S.dma_slot(f"wo{i}") for i in range(NWB)]
    sl_big = [S.dma_slot(f"big{i}") for i in range(8)]
    WSC = [[Buf(f"wsc{l_}_{i}") for i in range(38)] for l_ in range(L)]
    OUTB = Buf("outdram")
    SCRB = {}

    cnt_rr = {"wf": 0, "w": 0, "e": 0, "a": 0, "r": 0, "p": 0, "q": 0, "d": 0, "ev": 0}

    def rr(key, n):
        v = cnt_rr[key] % n
        cnt_rr[key] += 1
        return v

    S.op("dve", lambda e: e.memset(cs48[:, :], 0.0), writes=[CS])
    S.op("dve", lambda e: e.memset(sbq[:].rearrange("p h t -> p (h t)"), 0.0), writes=[SBQ])
    S.dma(lambda e: e.dma_start(out=cb[:], in_=cb_d[:, :]), sl_c, writes=[CBb])
    S.dma(lambda e: e.dma_start(out=cf[:], in_=cf_d[:, :]), sl_c2, writes=[CFb])

    for l_ in range(L):
        for idx in range(38):
            src = wF_d[l_, idx, :, :] if idx < 26 else (wM_d[l_, idx - 26, :, :] if idx < 30 else wO_d[l_, idx - 30, :, :])
            k = rr("w", NWB)
            S.dma(lambda e, k=k, src=src: e.dma_start(out=wst[k][:], in_=src), sl_w[k], writes=[WST[k]])
            kf = rr("wf", NWF)
            S.op("act", lambda e, k=k, kf=kf: e.activation(out=wbf[kf][:], in_=wst[k][:], func=AF.Copy),
                 reads=[WST[k]], writes=[WBF[kf]])
            S.dma(lambda e, kf=kf, l_=l_, idx=idx: e.dma_start(out=wsc_d[l_, idx, :, :], in_=wbf[kf][:]), sl_wb[kf],
                  reads=[WBF[kf]], writes=[WSC[l_][idx]])

    def wchunk(l_, idx):
        kf = rr("wf", NWF)
        S.dma(lambda e: e.dma_start(out=wbf[kf][:], in_=wsc_d[l_, idx, :, :]), sl_wb[kf], reads=[WSC[l_][idx]], writes=[WBF[kf]])
        return wbf[kf], WBF[kf]

    LA = 3

    def wstream(l_, idxs):
        pend_ = []
        it = iter(idxs)
        for _ in range(LA):
            nx = next(it, None)
            if nx is not None:
                pend_.append(wchunk(l_, nx))
        while pend_:
            cur = pend_.pop(0)
            nx = next(it, None)
            if nx is not None:
                pend_.append(wchunk(l_, nx))
            yield cur

    def evac_copy(out_ap, in_ap, reads, writes):
        if rr("ev", 2) == 0:
            S.op("act", lambda e: e.activation(out=out_ap, in_=in_ap, func=AF.Copy), reads=reads, writes=writes)
        else:
            S.op("dve", lambda e: e.tensor_copy(out=out_ap, in_=in_ap), reads=reads, writes=writes)

    def rms_scale(src_ss, dst_rs, n, inv_n, B_ss, B_rs):
        S.op("act", lambda e: e.activation(out=dst_rs, in_=src_ss, func=AF.Sqrt, scale=inv_n, bias=EPS),
             reads=[B_ss], writes=[B_rs])
        S.op("dve", lambda e: e.reciprocal(out=dst_rs, in_=dst_rs), reads=[B_rs], writes=[B_rs])

    def unit(u):
        l = layer_of_unit[u]
        b = batch_of_unit[u]
        src_x = x_d if chain[u] == "in" else scr_d
        later = any(batch_of_unit[v] == b for v in range(u + 1, len(layer_of_unit)))
        dst_x = scr_d if later else out_d
        if later and b not in SCRB:
            SCRB[b] = [Buf(f"scr{b}_{q}") for q in range(NQ)]

        S.dma(lambda e: e.dma_start(out=xc[:, 0:NSM], in_=wsm_d[l, :, :]), sl_s, writes=[XC])
        for (dst, off, n) in ((gcol, SM_GCOL, 8), (kvg, SM_KVG, 128), (b31, SM_B31, 6), (postg, SM_POSTG, 1024)):
            S.op("dve", lambda e, dst=dst, off=off, n=n: e.tensor_copy(out=dst[:, 0:n], in_=xc[:, off:off + n]),
                 reads=[XC], writes=[SMF])
        for (dst, off, n) in ((wiw, SM_IW, 64), (wuk, SM_WUK, 384), (wuv, SM_WUV, 384)):
            S.op("dve", lambda e, dst=dst, off=off, n=n: e.tensor_copy(out=dst[:, 0:n], in_=xc[:, off:off + n]),
                 reads=[XC], writes=[SMB])
        S.op("dve", lambda e: e.tensor_scalar(out=tb8[:, :], in0=xc[:, SM_TB:SM_TB + 1536], scalar1=8.0, scalar2=None,
                                              op0=ALU.mult), reads=[XC], writes=[SMB])
        S.dma(lambda e: e.dma_start(out=xc[:, 0:2048].rearrange("p (j d) -> p j d", j=2),
                                    in_=mem_d[b, :, :].rearrange("(j p) d -> p j d", p=128)), sl_m, writes=[XC])
        S.op("dve", lambda e: e.tensor_copy(out=membf[:].rearrange("p j d -> p (j d)"), in_=xc[:, 0:2048]),
             reads=[XC], writes=[MEMBF])
        for c2 in range(4):
            bk = bget()
            pb = psum[bk][:].bitcast(BF16)
            for cc in range(2):
                c = 2 * c2 + cc
                for j in range(2):
                    S.op("pe", lambda e, c=c, j=j, cc=cc, pb=pb: e.transpose(
                        pb[:, cc * 256 + j * 128:cc * 256 + (j + 1) * 128], membf[:, j, c * 128:(c + 1) * 128], ident),
                        reads=[MEMBF, CBb], writes=[PB[bk]])
            evac_copy(memT[:, 2 * c2:2 * c2 + 2, :], pb[:, 0:512].rearrange("p (c m) -> p c m", c=2), [PB[bk]], [MEMT])
            bput(bk)
        for g4, (w, W) in enumerate(wstream(l, [26, 27, 28, 29])):
            w3 = w[:].rearrange("p (c g) -> p c g", c=8)
            bk = bget()
            if g4 < 2:
                for c in range(8):
                    S.op("pe", lambda e, c=c, w3=w3, bk=bk: e.matmul(psum[bk][:, 0:256], lhsT=w3[:, c, :], rhs=memT[:, c, :],
                                                                    start=(c == 0), stop=(c == 7)),
                         reads=[W, MEMT], writes=[PB[bk]])
                evac_copy(memk[:, g4, :], psum[bk][:, 0:256], [PB[bk]], [MEMK])
            else:
                for j in range(2):
                    for c in range(8):
                        S.op("pe", lambda e, c=c, j=j, w3=w3, bk=bk: e.matmul(
                            psum[bk][:, j * 128:(j + 1) * 128], lhsT=memT[:, c, j * 128:(j + 1) * 128], rhs=w3[:, c, :],
                            start=(c == 0), stop=(c == 7)), reads=[W, MEMT], writes=[PB[bk]])
                evac_copy(memv[:, :, (g4 - 2) * 128:(g4 - 1) * 128], psum[bk][:, 0:256].rearrange("p (j g) -> p j g", j=2),
                          [PB[bk]], [MEMV])
            bput(bk)

        for tq in range(NQ):
            chunk_phase(u, l, b, tq, src_x, dst_x)

    def chunk_phase(u, l, b, tq, src_x, dst_x):
        t0 = tq * 512
        hn = hnmix[:].rearrange("p (i d) -> p i d", i=4)
        mix = hnmix[:].rearrange("p (c t) -> p c t", c=8)
        xc3 = xc[:].rearrange("p (i d) -> p i d", i=4)
        rd = [XC]
        if src_x is scr_d:
            rd = [XC] + [SCRB[b][tq]]
        S.dma(lambda e: e.dma_start(out=xc3, in_=src_x[b, t0:t0 + 512, :].rearrange("(i p) d -> p i d", p=128)),
              sl_x, reads=rd[1:], writes=[XC])
        for i in range(4):
            S.op("act", lambda e, i=i: e.activation(out=junk[:, 0:1024], in_=xc3[:, i, :], func=AF.Square,
                                                    accum_out=ss[:, i:i + 1]), reads=[XC], writes=[JUNK, SMS["ss"]])
        rms_scale(ss, rs, 4, 1.0 / D, SMS["ss"], SMS["rs"])
        for i in range(4):
            S.op("dve", lambda e, i=i: e.tensor_scalar(out=hn[:, i, :], in0=xc3[:, i, :], scalar1=rs[:, i:i + 1],
                                                       scalar2=None, op0=ALU.mult),
                 reads=[XC, SMS["rs"]], writes=[HNMIX])
        for c2 in range(4):
            bk = bget()
            pb = psum[bk][:].bitcast(BF16)
            for cc in range(2):
                c = 2 * c2 + cc
                for i in range(4):
                    S.op("pe", lambda e, c=c, i=i, cc=cc, pb=pb: e.transpose(
                        pb[:, cc * 512 + i * 128:cc * 512 + (i + 1) * 128], hn[:, i, c * 128:(c + 1) * 128], ident),
                        reads=[HNMIX, CBb], writes=[PB[bk]])
            for cc in range(2):
                c = 2 * c2 + cc
                S.op("dve", lambda e, c=c, cc=cc, pb=pb: e.tensor_scalar(
                    out=hT[:, c, :], in0=pb[:, cc * 512:(cc + 1) * 512], scalar1=gcol[:, c:c + 1], scalar2=None,
                    op0=ALU.mult), reads=[PB[bk], SMF], writes=[HT])
            bput(bk)

        if DBG_STAGE < 2:
            return
        fdest = ([("sbq", i) for i in range(3)] + [("sbk", i) for i in range(3)] + [("sbg", i) for i in range(3)]
                 + [("dq", i) for i in range(3)] + [("dg", i) for i in range(3)] + [("iq", 0), ("iq", 1), ("ik", 0)]
                 + [("mq", 0), ("mq", 1), ("mg", 0), ("mg", 1)])
        dst_tab = {"sbq": (sbq, SBQ), "sbg": (sbg, SBG), "dq": (dq, DQ), "dg": (dg, DG), "iq": (iq, IQ),
                   "mq": (mq, MQ), "mg": (mg, MG)}
        ptb = None
        for cc, (w, W) in enumerate(wstream(l, list(range(26)))):
            w3 = w[:].rearrange("p (c g) -> p c g", c=8)
            if cc < 22:
                name, ci = fdest[cc]
                bk = bget()
                for c in range(8):
                    S.op("pe", lambda e, c=c, w3=w3, bk=bk: e.matmul(psum[bk][:, :], lhsT=w3[:, c, :], rhs=hT[:, c, :],
                                                                    start=(c == 0), stop=(c == 7)),
                         reads=[W, HT], writes=[PB[bk]])
                if name == "sbk":
                    evac_copy(sbkT[:, ci, t0:t0 + 512], psum[bk][:, :], [PB[bk]], [SBK[tq]])
                elif name == "ik":
                    evac_copy(ikT4[:, t0:t0 + 512], psum[bk][:, :], [PB[bk]], [IKT[tq]])
                elif name in ("sbg", "dg", "mg") and DBG_SUB >= 2:
                    dt_, DB = dst_tab[name]
                    hs = rr("ev", 2) * 512
                    S.op("act", lambda e, bk=bk, hs=hs: e.activation(out=tmpf[:, hs:hs + 512], in_=psum[bk][:, :], func=AF.Exp,
                                                                     scale=-1.0), reads=[PB[bk]], writes=[TMPF])
                    S.op("act", lambda e, hs=hs: e.activation(out=tmpf[:, hs:hs + 512], in_=tmpf[:, hs:hs + 512], func=AF.Ln, bias=1.0),
                         reads=[TMPF], writes=[TMPF])
                    S.op("act", lambda e, hs=hs: e.activation(out=tmpf[:, hs:hs + 512], in_=tmpf[:, hs:hs + 512], func=AF.Exp, scale=-1.0),
                         reads=[TMPF], writes=[TMPF])
                    S.op("dve", lambda e, dt_=dt_, ci=ci, bk=bk, hs=hs: e.tensor_tensor(
                        out=dt_[:, ci, :], in0=psum[bk][:, :], in1=tmpf[:, hs:hs + 512], op=ALU.mult),
                        reads=[PB[bk], TMPF], writes=[DB])
                elif name == "sbq":
                    evac_copy(sbq[0:64, 2 * ci, :], psum[bk][0:64, :], [PB[bk]], [SBQ])
                    evac_copy(sbq[64:128, 2 * ci + 1, :], psum[bk][64:128, :], [PB[bk]], [SBQ])
                else:
                    dt_, DB = dst_tab[name]
                    evac_copy(dt_[:, ci, :], psum[bk][:, :], [PB[bk]], [DB])
                bput(bk)
            elif DBG_SUB >= 30:
                g4 = cc - 22
                if g4 == 0:
                    ptb = [bget() for _ in range(4)]
                for i in range(4):
                    for c in range(8):
                        S.op("pe", lambda e, c=c, i=i, w3=w3, g4=g4: e.matmul(
                            psum[ptb[i]][:, g4 * 128:(g4 + 1) * 128], lhsT=hT[:, c, i * 128:(i + 1) * 128], rhs=w3[:, c, :],
                            start=(c == 0), stop=(c == 7)), reads=[W, HT], writes=[PB[ptb[i]]])
        if DBG_SUB < 30:
            return
        for i in range(4):
            j = 4 * tq + i
            evac_copy(sbv[:, j, :], psum[ptb[i]][:, 0:384], [PB[ptb[i]]], [SBV[tq]])
            evac_copy(tmpf[:, i * 128:(i + 1) * 128], psum[ptb[i]][:, 384:512], [PB[ptb[i]]], [TMPF])
        for i in range(4):
            S.op("act", lambda e, i=i: e.activation(out=junk[:, 0:128], in_=tmpf[:, i * 128:(i + 1) * 128], func=AF.Square,
                                                    accum_out=ssk[:, i:i + 1]),
                 reads=[TMPF], writes=[JUNK, SMS["ssk"]])
        rms_scale(ssk, rsk, 4, 1.0 / 128, SMS["ssk"], SMS["rsk"])
        for i in range(4):
            j = 4 * tq + i
            S.op("dve", lambda e, i=i, j=j: e.scalar_tensor_tensor(out=ckv[:, j, :], in0=tmpf[:, i * 128:(i + 1) * 128],
                                                                   scalar=rsk[:, i:i + 1], in1=kvg[:, :],
                                                                   op0=ALU.mult, op1=ALU.mult),
                 reads=[TMPF, SMS["rsk"], SMF], writes=[CKV[tq]])
        for i in range(4):
            bput(ptb[i])
        if DBG_SUB < 40:
            return
        bk = bget()
        pb = psum[bk][:].bitcast(BF16)
        for i in range(4):
            j = 4 * tq + i
            S.op("pe", lambda e, i=i, j=j, pb=pb: e.transpose(pb[:, i * 128:(i + 1) * 128], ckv[:, j, :], ident),
                 reads=[CKV[tq], CBb], writes=[PB[bk]])
        evac_copy(ckvT[:, t0:t0 + 512], pb[:, 0:512], [PB[bk]], [CKVT[tq]])
        bput(bk)
        if DBG_SUB < 50:
            return
        bk = bget()
        wiw3 = wiw[:].rearrange("p (c g) -> p c g", c=8)
        for i in range(4):
            for c in range(8):
                S.op("pe", lambda e, c=c, i=i, bk=bk: e.matmul(psum[bk][:, i * 8:(i + 1) * 8],
                                                                lhsT=hT[:, c, i * 128:(i + 1) * 128], rhs=wiw3[:, c, :],
                                                                start=(c == 0), stop=(c == 7)),
                     reads=[SMB, HT], writes=[PB[bk]])
        S.op("dve", lambda e, bk=bk: e.tensor_scalar(out=idxw[:, :], in0=psum[bk][:, 0:32], scalar1=1.0 / 16, scalar2=None,
                                                     op0=ALU.mult), reads=[PB[bk]], writes=[IDXW])
        bput(bk)

        if DBG_STAGE < 3:
            return
        nkb = 4 * tq + 4
        KS = lambda lst: [lst[q] for q in range(tq + 1)]

        def indexer(i):
            qb = 4 * tq + i
            if qb < 2:
                return
            yield
            nk = (qb + 1) * 128
            nkc = (nk + 511) // 512
            kd = rr("d", 2)
            for h in range(8):
                S.op("act", lambda e, h=h, kd=kd: e.activation(out=dgt[kd][:, h, :], in_=ident, func=AF.Copy,
                                                               scale=idxw[:, i * 8 + h:i * 8 + h + 1]),
                     reads=[CBb, IDXW], writes=[DGT[kd]])
            for kc in range(nkc):
                w_ = min(512, nk - kc * 512)
                bs = bget()
                pend = []
                for g in range(2):
                    bds = [bget() for _ in range(4)]
                    for j in range(4):
                        hp = j * 32
                        tp = (96, 0) if hp == 96 else None
                        S.op("pe", lambda e, g=g, hp=hp, tp=tp, bd=bds[j], kc=kc, w_=w_: e.matmul(
                            psum[bd][:, 0:w_], lhsT=iq[hp:hp + 32, g, i * 128:(i + 1) * 128],
                            rhs=ikT4[hp:hp + 32, kc * 512:kc * 512 + w_], start=True, stop=True, tile_position=tp),
                            reads=[IQ] + KS(IKT), writes=[PB[bds[j]]])
                    for j in range(4):
                        h = 4 * g + j
                        kr = rr("r", NR)
                        S.op("dve", lambda e, kr=kr, bd=bds[j], w_=w_: e.tensor_scalar(out=rt[kr][:, 0:w_], in0=psum[bd][:, 0:w_],
                                                                                      scalar1=0.0, scalar2=None, op0=ALU.max),
                             reads=[PB[bds[j]]], writes=[RT[kr]])
                        bput(bds[j])
                        pend.append(lambda h=h, kr=kr, bs=bs, w_=w_: S.op("pe", lambda e: e.matmul(
                            psum[bs][:, 0:w_], lhsT=dgt[kd][:, h, :], rhs=rt[kr][:, 0:w_], start=(h == 0), stop=(h == 7)),
                            reads=[DGT[kd], RT[kr]], writes=[PB[bs]]))
                    while len(pend) > 4:
                        pend.pop(0)()
                    yield
                while pend:
                    pend.pop(0)()
                last = (kc == nkc - 1)
                wc = w_ - 128 if last else w_
                if wc > 0:
                    S.op("dve", lambda e, bs=bs, kc=kc, wc=wc: e.tensor_copy(out=score[:, kc * 512:kc * 512 + wc],
                                                                             in_=psum[bs][:, 0:wc]),
                         reads=[PB[bs]], writes=[SCORE])
                if last:
                    S.op("dve", lambda e, bs=bs, w_=w_: e.tensor_tensor(out=score[:, nk - 128:nk], in0=psum[bs][:, w_ - 128:w_],
                                                                       in1=negb, op=ALU.add),
                         reads=[PB[bs], CFb], writes=[SCORE])
                bput(bs)
            B = SMS["bis"]
            S.op("dve", lambda e: e.tensor_reduce(out=mx, in_=score[:, 0:nk], axis=AX.X, op=ALU.max), reads=[SCORE], writes=[B])
            S.op("dve", lambda e: e.tensor_reduce(out=mn, in_=score[:, 0:nk - 128], axis=AX.X, op=ALU.min),
                 reads=[SCORE], writes=[B])
            S.op("dve", lambda e: e.tensor_tensor(out=rng, in0=mx, in1=mn, op=ALU.subtract), reads=[B], writes=[B])
            S.op("dve", lambda e: e.tensor_tensor(out=thr, in0=mx, in1=mn, op=ALU.add), reads=[B], writes=[B])
            S.op("dve", lambda e: e.tensor_scalar(out=thr, in0=thr, scalar1=0.5, scalar2=None, op0=ALU.mult), reads=[B], writes=[B])
            S.op("dve", lambda e: e.tensor_scalar(out=steps[:, 0:16], in0=p2a, scalar1=rng, scalar2=None, op0=ALU.mult),
                 reads=[B, CFb], writes=[STEPS])
            S.op("dve", lambda e: e.tensor_scalar(out=steps[:, 16:32], in0=p2b, scalar1=rng, scalar2=None, op0=ALU.mult),
                 reads=[B, CFb], writes=[STEPS])
            for it in range(NBIS):
                S.op("dve", lambda e: e.tensor_scalar(out=junk[:, 0:nk], in0=score[:, 0:nk], scalar1=thr, scalar2=0.0,
                                                      op0=ALU.is_ge, op1=ALU.add, accum_out=cnt),
                     reads=[SCORE, B], writes=[JUNK, B])
                S.op("dve", lambda e, it=it: e.tensor_scalar(out=dd, in0=cnt, scalar1=float(TOPK), scalar2=steps[:, 16 + it:17 + it],
                                                             op0=ALU.is_ge, op1=ALU.mult), reads=[B, STEPS], writes=[B])
                S.op("dve", lambda e, it=it: e.scalar_tensor_tensor(out=thr, in0=dd, scalar=steps[:, it:it + 1], in1=thr,
                                                                    op0=ALU.subtract, op1=ALU.add),
                     reads=[B, STEPS], writes=[B])
                yield
                yield
            S.op("dve", lambda e: e.tensor_scalar(out=pen[:, i, 0:nk], in0=score[:, 0:nk], scalar1=thr, scalar2=NEG,
                                                  op0=ALU.is_lt, op1=ALU.mult), reads=[SCORE, B], writes=[PEN[i]])

        def sb_head(h):
            ch, hp = h // 2, (h % 2) * 64
            bcs = bget()
            pend = []
            pendL = []
            for kb in range(nkb):
                c0 = max(0, kb - 4 * tq) * 128
                diag = kb >= 4 * tq
                bz = bget()
                S.op("pe", lambda e, kb=kb, c0=c0, bz=bz, diag=diag: e.matmul(
                    psum[bz][:, c0:512], lhsT=sbkT[:, ch, kb * 128:(kb + 1) * 128], rhs=sbq[:, h, c0:512],
                    start=True, stop=not diag), reads=KS(SBK) + [SBQ], writes=[PB[bz]])
                if diag:
                    S.op("pe", lambda e, c0=c0, bz=bz: e.matmul(psum[bz][:, c0:c0 + 128], lhsT=penS, rhs=ident,
                                                                 start=False, stop=True), reads=[CBb], writes=[PB[bz]])
                ke = rr("e", NT)
                S.op("act", lambda e, ke=ke, c0=c0, bz=bz: e.activation(out=etile[ke][:, c0:512], in_=psum[bz][:, c0:512],
                                                                        func=AF.Exp, scale=0.125),
                     reads=[PB[bz]], writes=[ET[ke]])
                bput(bz)
                sp = big16[:, kb * 512:(kb + 1) * 512]

                def ln_stage(kb=kb, c0=c0, ke=ke, sp=sp):
                    S.op("act", lambda e: e.activation(out=sp[:, c0:512], in_=etile[ke][:, c0:512], func=AF.Ln, bias=1.0),
                         reads=[ET[ke]], writes=[BIG[kb]])
                    pend.append(lambda: S.op("pe", lambda e: e.matmul(
                        psum[bcs][:, c0:512], lhsT=ekb(kb), rhs=sp[:, c0:512], start=(kb == 0), stop=(kb == nkb - 1)),
                        reads=[CBb, BIG[kb]], writes=[PB[bcs]]))
                pendL.append(ln_stage)
                if len(pendL) > 1:
                    pendL.pop(0)()
                if len(pend) > SKEW:
                    pend.pop(0)()
                yield
            while pendL:
                pendL.pop(0)()
            while pend:
                pend.pop(0)()
            S.op("dve", lambda e: e.tensor_copy(out=cs48[0:32, :], in_=psum[bcs][0:32, :]), reads=[PB[bcs]], writes=[CS])
            S.op("dve", lambda e: e.tensor_tensor(out=cslo[:, :], in0=psum[bcs][0:32, :], in1=cs48[0:32, :], op=ALU.subtract),
                 reads=[PB[bcs], CS], writes=[CS])
            S.op("dve", lambda e: e.tensor_copy(out=cs48[32:64, :], in_=cslo[:, :]), reads=[CS], writes=[CS])
            bput(bcs)
            bo = bget()
            pend = []
            for kb in range(nkb):
                c0 = max(0, kb - 4 * tq) * 128
                diag = kb >= 4 * tq
                bi = bget()
                sp = big16[:, kb * 512:(kb + 1) * 512]
                S.op("pe", lambda e, kb=kb, c0=c0, bi=bi: e.matmul(
                    psum[bi][:, c0:512], lhsT=sbkT[:, ch, kb * 128:(kb + 1) * 128], rhs=sbq[:, h, c0:512],
                    start=True, stop=False), reads=KS(SBK) + [SBQ], writes=[PB[bi]])
                S.op("pe", lambda e, c0=c0, bi=bi, sp=sp: e.matmul(psum[bi][:, c0:512], lhsT=triM8, rhs=sp[:, c0:512],
                                                                   start=False, stop=False),
                     reads=[CBb, BIG[kb]], writes=[PB[bi]])
                S.op("pe", lambda e, kb=kb, c0=c0, bi=bi, diag=diag: e.matmul(psum[bi][:, c0:512], lhsT=selM8(kb),
                                                                              rhs=cs48[:, c0:512], start=False, stop=not diag),
                     reads=[CBb, CS], writes=[PB[bi]])
                if diag:
                    S.op("pe", lambda e, c0=c0, bi=bi: e.matmul(psum[bi][:, c0:c0 + 128], lhsT=penS, rhs=ident,
                                                                 start=False, stop=True), reads=[CBb], writes=[PB[bi]])
                ka = rr("a", NT)
                S.op("act", lambda e, ka=ka, c0=c0, bi=bi: e.activation(out=atile[ka][:, c0:512], in_=psum[bi][:, c0:512],
                                                                        func=AF.Exp, scale=0.125),
                     reads=[PB[bi]], writes=[AT[ka]])
                bput(bi)
                pend.append(lambda kb=kb, c0=c0, ka=ka: S.op("pe", lambda e: e.matmul(
                    psum[bo][:, c0:512], lhsT=sbv[:, kb, ch * 128:(ch + 1) * 128], rhs=atile[ka][:, c0:512],
                    start=(kb == 0), stop=(kb == nkb - 1)), reads=KS(SBV) + [AT[ka]], writes=[PB[bo]]))
                if len(pend) > SKEW:
                    pend.pop(0)()
                yield
            while pend:
                pend.pop(0)()
            S.op("dve", lambda e: e.tensor_tensor(out=mix[hp:hp + 64, ch, :], in0=psum[bo][hp:hp + 64, :],
                                                  in1=sbg[hp:hp + 64, ch, :], op=ALU.mult),
                 reads=[PB[bo], SBG], writes=[HNMIX])
            bput(bo)

        def mem_head(hm):
            ch, hp = hm // 2, (hm % 2) * 64
            bo = bget(); bd = bget()
            for mb in range(2):
                bl = bget()
                S.op("pe", lambda e, mb=mb, bl=bl: e.matmul(psum[bl][:, :], lhsT=memk[hp:hp + 64, ch, mb * 128:(mb + 1) * 128],
                                                            rhs=mq[hp:hp + 64, ch, :], start=True, stop=True),
                     reads=[MEMK, MQ], writes=[PB[bl]])
                kp = rr("p", NT)
                S.op("act", lambda e, kp=kp, bl=bl: e.activation(out=ptile[kp][:, :], in_=psum[bl][:, :], func=AF.Exp, scale=0.125),
                     reads=[PB[bl]], writes=[PT[kp]])
                bput(bl)
                S.op("pe", lambda e, mb=mb, kp=kp: e.matmul(psum[bo][:, :], lhsT=memv[:, mb, ch * 128:(ch + 1) * 128],
                                                            rhs=ptile[kp][:, :], start=(mb == 0), stop=(mb == 1)),
                     reads=[MEMV, PT[kp]], writes=[PB[bo]])
                S.op("pe", lambda e, mb=mb, kp=kp: e.matmul(psum[bd][:, :], lhsT=ones, rhs=ptile[kp][:, :],
                                                            start=(mb == 0), stop=(mb == 1)),
                     reads=[CBb, PT[kp]], writes=[PB[bd]])
            S.op("act", lambda e: e.activation(out=recf[hp:hp + 64, :], in_=psum[bd][hp:hp + 64, :], func=AF.Ln), reads=[PB[bd]], writes=[RECF])
            S.op("act", lambda e: e.activation(out=recf[hp:hp + 64, :], in_=recf[hp:hp + 64, :], func=AF.Exp, scale=-1.0), reads=[RECF], writes=[RECF])
            S.op("dve", lambda e: e.tensor_tensor(out=tmpf[hp:hp + 64, 0:512], in0=psum[bo][hp:hp + 64, :],
                                                  in1=recf[hp:hp + 64, :], op=ALU.mult), reads=[PB[bo], RECF], writes=[TMPF])
            S.op("dve", lambda e: e.tensor_tensor(out=mix[hp:hp + 64, 6 + ch, :], in0=tmpf[hp:hp + 64, 0:512],
                                                  in1=mg[hp:hp + 64, ch, :], op=ALU.mult), reads=[TMPF, MG], writes=[HNMIX])
            bput(bo); bput(bd)

        def dsa_head(h):
            ch, hp = h // 2, (h % 2) * 64
            bq = bget()
            S.op("pe", lambda e: e.matmul(psum[bq][:, :], lhsT=wuk[hp:hp + 64, ch * 128:(ch + 1) * 128], rhs=dq[hp:hp + 64, ch, :],
                                          start=True, stop=True), reads=[SMB, DQ], writes=[PB[bq]])
            kq = rr("q", 2)
            evac_copy(qlat[kq][:, :], psum[bq][:, :], [PB[bq]], [QL[kq]])
            bput(bq)
            bo = bget(); bd = bget()
            pend = []
            for kb in range(nkb):
                i0 = max(0, kb - 4 * tq)
                c0 = i0 * 128
                bl = bget()
                mm = []
                mm.append((lambda e, st, sp_, kb=kb, c0=c0, bl=bl: e.matmul(
                    psum[bl][:, c0:512], lhsT=ckvT[:, kb * 128:(kb + 1) * 128], rhs=qlat[kq][:, c0:512], start=st, stop=sp_),
                    KS(CKVT) + [QL[kq]]))
                for i in range(i0, 4):
                    qb = 4 * tq + i
                    cs_ = slice(i * 128, (i + 1) * 128)
                    if qb >= 2:
                        mm.append((lambda e, st, sp_, i=i, kb=kb, cs_=cs_, bl=bl: e.matmul(
                            psum[bl][:, cs_], lhsT=pen[:, i, kb * 128:(kb + 1) * 128], rhs=ident, start=st, stop=sp_),
                            [PEN[i], CBb]))
                    elif qb == kb:
                        mm.append((lambda e, st, sp_, cs_=cs_, bl=bl: e.matmul(psum[bl][:, cs_], lhsT=penD, rhs=ident,
                                                                              start=st, stop=sp_), [CBb]))
                    if qb - kb <= 1:
                        jj = qb - kb
                        mm.append((lambda e, st, sp_, cs_=cs_, jj=jj, bl=bl: e.matmul(
                            psum[bl][:, cs_], lhsT=ident, rhs=tb8[:, (h * 2 + jj) * 128:(h * 2 + jj + 1) * 128],
                            start=st, stop=sp_), [CBb, SMB]))
                for n_, (fn, rds) in enumerate(mm):
                    S.op("pe", lambda e, fn=fn, n_=n_, nm=len(mm): fn(e, n_ == 0, n_ == nm - 1), reads=rds, writes=[PB[bl]])
                kp = rr("p", NT)
                near_hi = min(512, max(c0, (kb + 2 - 4 * tq) * 128))
                if near_hi > c0:
                    S.op("act", lambda e, kp=kp, c0=c0, near_hi=near_hi, bl=bl: e.activation(
                        out=ptile[kp][:, c0:near_hi], in_=psum[bl][:, c0:near_hi], func=AF.Exp, scale=0.125),
                        reads=[PB[bl]], writes=[PT[kp]])
                if near_hi < 512:
                    S.op("act", lambda e, kp=kp, near_hi=near_hi, bl=bl: e.activation(
                        out=ptile[kp][:, near_hi:512], in_=psum[bl][:, near_hi:512], func=AF.Exp, scale=0.125,
                        bias=b31[:, h:h + 1]), reads=[PB[bl], SMF], writes=[PT[kp]])
                bput(bl)
                def tail(kb=kb, c0=c0, kp=kp):
                    S.op("pe", lambda e: e.matmul(psum[bo][:, c0:512], lhsT=ckv[:, kb, :], rhs=ptile[kp][:, c0:512],
                                                  start=(kb == 0), stop=(kb == nkb - 1)),
                         reads=KS(CKV) + [PT[kp]], writes=[PB[bo]])
                    S.op("pe", lambda e: e.matmul(psum[bd][:, c0:512], lhsT=ones, rhs=ptile[kp][:, c0:512],
                                                  start=(kb == 0), stop=(kb == nkb - 1)),
                         reads=[CBb, PT[kp]], writes=[PB[bd]])
                pend.append(tail)
                if len(pend) > SKEW:
                    pend.pop(0)()
                yield
            while pend:
                pend.pop(0)()
            S.op("act", lambda e: e.activation(out=recf[:, :], in_=psum[bd][:, :], func=AF.Ln), reads=[PB[bd]], writes=[RECF])
            S.op("act", lambda e: e.activation(out=recf[:, :], in_=recf[:, :], func=AF.Exp, scale=-1.0), reads=[RECF], writes=[RECF])
            S.op("dve", lambda e: e.tensor_tensor(out=onb[:, :], in0=psum[bo][:, :], in1=recf[:, :], op=ALU.mult),
                 reads=[PB[bo], RECF], writes=[ONB])
            bput(bo); bput(bd)
            bu = bget()
            S.op("pe", lambda e: e.matmul(psum[bu][:, :], lhsT=wuv[:, ch * 128:(ch + 1) * 128], rhs=onb[:, :], start=True, stop=True),
                 reads=[SMB, ONB], writes=[PB[bu]])
            S.op("dve", lambda e: e.tensor_tensor(out=mix[hp:hp + 64, 3 + ch, :], in0=psum[bu][hp:hp + 64, :],
                                                  in1=dg[hp:hp + 64, ch, :], op=ALU.mult), reads=[PB[bu], DG], writes=[HNMIX])
            bput(bu)

        def run_par(*gens):
            gens = list(gens)
            while gens:
                for g in list(gens):
                    try:
                        next(g)
                    except StopIteration:
                        gens.remove(g)

        def seq(*fns):
            for f in fns:
                r = f()
                if r is not None:
                    yield from r
                yield

        run_par(seq(*[lambda i=i: indexer(i) for i in range(4)]),
                seq(*([lambda h=h: sb_head(h) for h in range(5)] + [lambda hm=hm: mem_head(hm) for hm in range(4)])))
        run_par(sb_head(5), seq(lambda: dsa_head(0), lambda: dsa_head(1)))
        wO3 = big16[:].rearrange("p (c n) -> p c n", c=8)
        for c in range(8):
            S.dma(lambda e, c=c: e.dma_start(out=big16[:, c * 1024:(c + 1) * 1024], in_=wsc_d[l, 30 + c, :, :]), sl_big[c],
                  reads=[WSC[l][30 + c]], writes=[BIG[2 * c], BIG[2 * c + 1]])
        for h in range(2, 6):
            run_par(dsa_head(h))

        for i in range(4):
            b0 = bget(); b1 = bget()
            bb = (b0, b1)
            for half in range(2):
                for c in range(8):
                    S.op("pe", lambda e, c=c, half=half, i=i, bb=bb: e.matmul(
                        psum[bb[half]][:, :], lhsT=mix[:, c, i * 128:(i + 1) * 128], rhs=wO3[:, c, half * 512:(half + 1) * 512],
                        start=(c == 0), stop=(c == 7)), reads=[HNMIX, BIG[2 * c + half]], writes=[PB[bb[half]]])
            for half in range(2):
                evac_copy(tmpf[:, half * 512:(half + 1) * 512], psum[bb[half]][:, :], [PB[bb[half]]], [TMPF])
            S.op("act", lambda e: e.activation(out=junk[:, 0:1024], in_=tmpf[:, :], func=AF.Square, accum_out=ssy2),
                 reads=[TMPF], writes=[JUNK, SMS["ssy"]])
            rms_scale(ssy2, rsy, 1, 1.0 / D, SMS["ssy"], SMS["ssy"])
            S.op("dve", lambda e: e.scalar_tensor_tensor(out=tmpf[:, :], in0=tmpf[:, :], scalar=rsy, in1=postg[:, :],
                                                         op0=ALU.mult, op1=ALU.mult),
                 reads=[TMPF, SMS["ssy"], SMF], writes=[TMPF])
            bput(b0); bput(b1)
            S.op("pool", lambda e, i=i: e.tensor_tensor(out=xc3[:, i, :], in0=tmpf[:, :], in1=xc3[:, i, :], op=ALU.add),
                 reads=[TMPF, XC], writes=[XC])
        wr = [OUTB] if dst_x is out_d else [SCRB[b][tq]]
        S.dma(lambda e: e.dma_start(out=dst_x[b, t0:t0 + 512, :].rearrange("(i p) d -> p i d", p=128), in_=xc3),
              sl_o, reads=[XC], writes=wr)

    for u in range(len(layer_of_unit)):
        unit(u)
    S.final_wait("sp", [OUTB, XC])
    S.emit()
    S.close()
    for g in reversed(ctx):
        g.__exit__(None, None, None)
    return nc


def _t5_bucket(rel):
    n = np.maximum(rel, 0)
    max_exact = 16
    nf = np.maximum(n, 1).astype(np.float32)
    large = max_exact + (np.log(nf / max_exact) / math.log(128 / max_exact) * (32 - max_exact)).astype(np.int32)
    large = np.minimum(large, 31)
    return np.where(n < max_exact, n, large)


def _constants():
    bf = ml_dtypes.bfloat16
    cbv = np.zeros((128, NCB), np.float32)
    p = np.arange(128)
    cbv[:, CB_ID:CB_ID + 128] = np.eye(128)
    cbv[:, CB_TRI:CB_TRI + 128] = np.where(p[:, None] >= p[None, :], -8.0, 0.0)
    cbv[:, CB_ONES:CB_ONES + 128] = 1.0
    cbv[:, CB_PENS:CB_PENS + 128] = np.where(p[None, :] < p[:, None], 0.0, NEG)
    cbv[:, CB_PEND:CB_PEND + 128] = np.where(p[None, :] <= p[:, None], 0.0, NEG)
    for kb in range(16):
        cbv[:, CB_EKB + 128] = 1.0
        for jb in range(16):
            if jb > kb:
                cbv[2 * jb, CB_SEL + kb * 128:CB_SEL + (kb + 1) * 128] = -8.0
                cbv[32 + 2 * jb, CB_SEL + kb * 128:CB_SEL + (kb + 1) * 128] = -8.0
    cfv = np.zeros((128, NCF), np.float32)
    cfv[:, CF_NEGB:CF_NEGB + 128] = np.where(p[None, :] <= p[:, None], 0.0, -1e30)
    cfv[:, CF_P2A:CF_P2A + 16] = 2.0 ** -(np.arange(16) + 2.0)
    cfv[:, CF_P2B:CF_P2B + 16] = 2.0 ** -(np.arange(16) + 1.0)
    return cfv, cbv.astype(bf)


def _chunk_cols(w, cols):
    sel = w[:, cols]
    n = sel.shape[1] // 128
    a = sel.reshape(8, 128, n, 128)
    return np.ascontiguousarray(a.transpose(2, 1, 0, 3).reshape(n, 128, 1024))


def _prep_weights(pre_norm_g, post_norm_g, w_in, w_uk, w_uv, kv_norm_g, w_mem_kv, w_out, rel_bias, layers):
    o = np.cumsum([0, 384, 384, 384, 384, 384, 128, 384, 256, 32, 8, 256, 256])
    (o_sbq, o_sbk, o_sbv, o_sbg, o_dq, o_ckv, o_dg, o_iq, o_ik, o_iw, o_mq, o_mg) = o[:12]
    r = lambda a, n: list(range(a, a + n))
    fcols = (r(o_sbq, 384) + r(o_sbk, 384) + r(o_sbg, 384) + r(o_dq, 384) + r(o_dg, 384) + r(o_iq, 256)
             + r(o_ik, 32) * 4 + r(o_mq, 256) + r(o_mg, 256) + r(o_sbv, 384) + r(o_ckv, 128))
    assert len(fcols) == 26 * 128
    s_l = np.arange(128)
    wF, wO, wM, wsm = [], [], [], []
    for l in layers:
        wF.append(_chunk_cols(w_in[l], fcols))
        wO.append(np.ascontiguousarray(w_out[l].reshape(8, 128, 1024)))
        wM.append(_chunk_cols(w_mem_kv[l], list(range(512))))
        sm = np.zeros((128, NSM), np.float32)
        sm[:, SM_GCOL:SM_GCOL + 8] = pre_norm_g[l].reshape(8, 128).T
        sm[:, SM_IW:SM_IW + 64] = w_in[l][:, o_iw:o_iw + 8].reshape(8, 128, 8).transpose(1, 0, 2).reshape(128, 64)
        uk = w_uk[l]
        t = uk.reshape(128, 3, 2, 64).transpose(2, 3, 1, 0).reshape(128, 3 * 128)
        sm[:, SM_WUK:SM_WUK + 384] = t
        sm[:, SM_WUV:SM_WUV + 384] = w_uv[l].reshape(128, 384)
        sm[:, SM_KVG:SM_KVG + 128] = kv_norm_g[l][None, :]
        sm[:, SM_B31:SM_B31 + 6] = rel_bias[31][None, :]
        sm[:, SM_POSTG:SM_POSTG + 1024] = post_norm_g[l][None, :]
        for h in range(6):
            for j in range(2):
                rel = s_l[None, :] - s_l[:, None] + 128 * j
                sm[:, SM_TB + (h * 2 + j) * 128:SM_TB + (h * 2 + j + 1) * 128] = rel_bias[_t5_bucket(rel), h]
        wsm.append(sm)
    return (np.stack(wF), np.stack(wO), np.stack(wM), np.stack(wsm))


_PROG_CACHE = {}


def _get_prog(key, *args):
    if key not in _PROG_CACHE:
        _PROG_CACHE[key] = build_program(*args)
    return _PROG_CACHE[key]


FUSED = True


def kernel(x, mem, pre_norm_g, post_norm_g, w_in, w_uk, w_uv, kv_norm_g, w_mem_kv, w_out, rel_bias):
    x = np.asarray(x, np.float32)
    mem = np.asarray(mem, np.float32)
    args = [np.asarray(a, np.float32) for a in (pre_norm_g, post_norm_g, w_in, w_uk, w_uv, kv_norm_g, w_mem_kv, w_out, rel_bias)]
    B, S_TOK, _ = x.shape
    depth = w_in.shape[0]
    per = B // N_CORES
    cfv, cbv = _constants()
    if FUSED:
        units_l = []; units_b = []; chain = []
        for b in range(per):
            for l in range(depth):
                units_l.append(l); units_b.append(b); chain.append("in" if l == 0 else "scr")
        nc = _get_prog(("fused", S_TOK, per, depth), S_TOK, per, units_l, units_b, chain)
        wF, wO, wM, wsm = _prep_weights(*args, layers=list(range(depth)))
        in_maps = [{"x": np.ascontiguousarray(x[c * per:(c + 1) * per]), "mem": np.ascontiguousarray(mem[c * per:(c + 1) * per]),
                    "wF": wF, "wO": wO, "wM": wM, "wsm": wsm, "cf32": cfv, "cbf": cbv} for c in range(N_CORES)]
        res = run_bass_kernel_spmd(nc, in_maps, core_ids=list(range(N_CORES)))
        return np.concatenate([r["out"] for r in res.results], axis=0)
    cur = x
    nc = _get_prog(("unit", S_TOK), S_TOK, 1, [0], [0], ["in"])
    for l in range(depth):
        wF, wO, wM, wsm = _prep_weights(*args, layers=[l])
        nxt = np.empty_like(cur)
        for b in range(per):
            in_maps = [{"x": np.ascontiguousarray(cur[c * per + b:c * per + b + 1]),
                        "mem": np.ascontiguousarray(mem[c * per + b:c * per + b + 1]),
                        "wF": wF, "wO": wO, "wM": wM, "wsm": wsm, "cf32": cfv, "cbf": cbv} for c in range(N_CORES)]
            res = run_bass_kernel_spmd(nc, in_maps, core_ids=list(range(N_CORES)))
            for c in range(N_CORES):
                nxt[c * per + b] = res.results[c]["out"][0]
        cur = nxt
    return cur
```

```python
import math
import numpy as np
import ml_dtypes
import concourse.bass as bass
import concourse.mybir as mybir
from concourse.bass_utils import run_bass_kernel_spmd

F32 = mybir.dt.float32
BF16 = mybir.dt.bfloat16
AF = mybir.ActivationFunctionType
ALU = mybir.AluOpType
AX = mybir.AxisListType

D = 1024
NMEM = 256
TOPK = 256
NBIS = 13
EPS = 1e-6
NEG = -30000.0
N_CORES = 8
DBG_STAGE = 99
DBG_SUB = 99
SKEW = 2

SM_GCOL, SM_IW, SM_WUK, SM_WUV, SM_KVG, SM_B31, SM_POSTG, SM_TB = 0, 8, 72, 456, 840, 968, 974, 1998
NSM = 1998 + 6 * 2 * 128
CB_ID, CB_TRI, CB_ONES, CB_PENS, CB_PEND, CB_EKB, CB_SEL = 0, 128, 256, 384, 512, 640, 896
NCB = 896 + 16 * 128
CF_NEGB, CF_P2A, CF_P2B = 0, 128, 144
NCF = 160


class Buf:
    __slots__ = ("name", "w", "r", "excl")

    def __init__(self, name, excl=False):
        self.name = name
        self.w = None
        self.r = {}
        self.excl = excl


class Sched:
    ENGS = ("pe", "act", "dve", "pool", "sp")

    def __init__(self, nc):
        self.nc = nc
        self.ops = {e: [] for e in self.ENGS}
        self.sems = {}
        self.cnt = {}
        self.waited = {e: {} for e in self.ENGS}
        self._ctx = []
        for e in self.ENGS:
            self._newsem("E_" + e)

    def _newsem(self, key):
        g = self.nc.semaphore(key)
        h = g.__enter__()
        self._ctx.append(g)
        self.sems[key] = h
        self.cnt[key] = 0
        return key

    def dma_slot(self, name):
        return self._newsem("D_" + name)

    def _deps(self, e, reads, writes):
        deps = {}

        def add(ev):
            if ev is None:
                return
            k, v = ev
            if deps.get(k, 0) < v:
                deps[k] = v
        for b in reads:
            add(b.w)
        for b in writes:
            add(b.w)
            for k, v in b.r.items():
                add((k, v))
        waits = []
        mykey = "E_" + e
        for k, v in deps.items():
            if k == mykey and e in ("pe", "sp"):
                continue
            if self.waited[e].get(k, 0) >= v:
                continue
            self.waited[e][k] = v
            waits.append((k, v))
        return waits

    def _record(self, ev, reads, writes):
        k, v = ev
        for b in reads:
            if b.r.get(k, 0) < v:
                b.r[k] = v
        for b in writes:
            b.w = ev
            b.r = {}

    def op(self, e, fn, reads=(), writes=()):
        ex = [b for b in reads if b.excl]
        if ex:
            writes = list(writes) + ex
        waits = self._deps(e, reads, writes)
        k = "E_" + e
        self.cnt[k] += 1
        ev = (k, self.cnt[k])
        self.ops[e].append((waits, fn, (k, 1)))
        self._record(ev, reads, writes)
        return ev

    def dma(self, fn, slot, reads=(), writes=(), e="sp"):
        waits = self._deps(e, reads, writes)
        self.cnt[slot] += 16
        ev = (slot, self.cnt[slot])
        self.ops[e].append((waits, fn, (slot, 16)))
        self._record(ev, reads, writes)
        return ev

    def final_wait(self, e, bufs):
        waits = self._deps(e, bufs, bufs)
        self.ops[e].append((waits, None, None))

    def emit(self):
        nc = self.nc
        needed = {}
        for e in self.ENGS:
            for waits, fn, inc in self.ops[e]:
                for k, v in waits:
                    if k.startswith("E_"):
                        needed.setdefault(k, set()).add(v)
        rank = {k: {v: i + 1 for i, v in enumerate(sorted(vs))} for k, vs in needed.items()}
        with nc.Block() as block:
            def run(ename):
                def body(eng):
                    seq = 0
                    mykey = "E_" + ename
                    myrank = rank.get(mykey, {})
                    for waits, fn, inc in self.ops[ename]:
                        for k, v in waits:
                            eng.wait_ge(self.sems[k], rank[k][v] if k.startswith("E_") else v)
                        if fn is None:
                            continue
                        inst = fn(eng)
                        if inc[0] == mykey:
                            seq += 1
                            if seq in myrank:
                                inst.then_inc(self.sems[mykey], 1)
                        else:
                            inst.then_inc(self.sems[inc[0]], inc[1])
                return body
            block.tensor(run("pe"))
            block.scalar(run("act"))
            block.vector(run("dve"))
            block.gpsimd(run("pool"))
            block.sync(run("sp"))

    def close(self):
        for g in reversed(self._ctx):
            g.__exit__(None, None, None)


def build_program(S_TOK, NB, layer_of_unit, batch_of_unit, chain):
    NQ = S_TOK // 512
    NBLK = S_TOK // 128
    L = max(layer_of_unit) + 1
    nc = bass.Bass("TRN2", target_bir_lowering=False)
    x_d = nc.dram_tensor("x", [NB, S_TOK, D], F32, kind="ExternalInput").ap()
    mem_d = nc.dram_tensor("mem", [NB, NMEM, D], F32, kind="ExternalInput").ap()
    wF_d = nc.dram_tensor("wF", [L, 26, 128, 1024], F32, kind="ExternalInput").ap()
    wO_d = nc.dram_tensor("wO", [L, 8, 128, 1024], F32, kind="ExternalInput").ap()
    wM_d = nc.dram_tensor("wM", [L, 4, 128, 1024], F32, kind="ExternalInput").ap()
    wsm_d = nc.dram_tensor("wsm", [L, 128, NSM], F32, kind="ExternalInput").ap()
    cf_d = nc.dram_tensor("cf32", [128, NCF], F32, kind="ExternalInput").ap()
    cb_d = nc.dram_tensor("cbf", [128, NCB], BF16, kind="ExternalInput").ap()
    out_d = nc.dram_tensor("out", [NB, S_TOK, D], F32, kind="ExternalOutput").ap()
    need_scr = any(c == "scr" for c in chain)
    scr_d = nc.dram_tensor("xscr", [NB, S_TOK, D], F32, kind="Internal").ap() if need_scr else None

    wsc_d = nc.dram_tensor("wscr", [L, 38, 128, 1024], BF16, kind="Internal").ap()
    S = Sched(nc)
    ctx = []

    def sb(name, shape, dt):
        g = nc.sbuf_tensor(name, shape, dt)
        h = g.__enter__()
        ctx.append(g)
        return h

    psum = []
    PB = []
    for i in range(8):
        g = nc.psum_tensor(f"ps{i}", [128, 512], F32)
        psum.append(g.__enter__())
        ctx.append(g)
        PB.append(Buf(f"ps{i}", excl=True))
    free_banks = list(range(8))

    def bget():
        return free_banks.pop(0)

    def bput(i):
        free_banks.append(i)

    cb = sb("cb", [128, NCB], BF16); CBb = Buf("cb")
    cf = sb("cf", [128, NCF], F32); CFb = Buf("cf")
    ident = cb[:, CB_ID:CB_ID + 128]
    triM8 = cb[:, CB_TRI:CB_TRI + 128]
    ones = cb[:, CB_ONES:CB_ONES + 128]
    penS = cb[:, CB_PENS:CB_PENS + 128]
    penD = cb[:, CB_PEND:CB_PEND + 128]

    def ekb(kb):
        return cb[:, CB_EKB + 128 - 2 * kb:CB_EKB + 256 - 2 * kb]

    def selM8(kb):
        return cb[:, CB_SEL + kb * 128:CB_SEL + (kb + 1) * 128]
    negb = cf[:, CF_NEGB:CF_NEGB + 128]
    p2a = cf[:, CF_P2A:CF_P2A + 16]
    p2b = cf[:, CF_P2B:CF_P2B + 16]

    sbkT = sb("sbkT", [128, 3, S_TOK], BF16); SBK = [Buf(f"sbk{q}") for q in range(NQ)]
    sbv = sb("sbv", [128, NBLK, 384], BF16); SBV = [Buf(f"sbv{q}") for q in range(NQ)]
    ckv = sb("ckv", [128, NBLK, 128], BF16); CKV = [Buf(f"ckv{q}") for q in range(NQ)]
    ckvT = sb("ckvT", [128, S_TOK], BF16); CKVT = [Buf(f"ckvT{q}") for q in range(NQ)]
    ikT4 = sb("ikT4", [128, S_TOK], BF16); IKT = [Buf(f"ikT{q}") for q in range(NQ)]
    sbq = sb("sbq", [128, 6, 512], BF16); SBQ = Buf("sbq")
    sbg = sb("sbg", [128, 3, 512], BF16); SBG = Buf("sbg")
    dq = sb("dq", [128, 3, 512], BF16); DQ = Buf("dq")
    dg = sb("dg", [128, 3, 512], BF16); DG = Buf("dg")
    iq = sb("iq", [128, 2, 512], BF16); IQ = Buf("iq")
    mq = sb("mq", [128, 2, 512], BF16); MQ = Buf("mq")
    mg = sb("mg", [128, 2, 512], BF16); MG = Buf("mg")
    idxw = sb("idxw", [128, 32], F32); IDXW = Buf("idxw")
    hT = sb("hT", [128, 8, 512], BF16); HT = Buf("hT")
    hnmix = sb("hnmix", [128, 4096], BF16); HNMIX = Buf("hnmix")
    xc = sb("xc", [128, 4096], F32); XC = Buf("xc")
    NWB = 2
    wst = [sb(f"wst{i}", [128, 1024], F32) for i in range(NWB)]; WST = [Buf(f"wst{i}") for i in range(NWB)]
    NWF = 4
    wbf = [sb(f"wbf{i}", [128, 1024], BF16) for i in range(NWF)]; WBF = [Buf(f"wbf{i}") for i in range(NWF)]
    big16 = sb("big16", [128, 8192], BF16); BIG = [Buf(f"big{r}") for r in range(16)]
    gcol = sb("gcol", [128, 8], F32); kvg = sb("kvg", [128, 128], F32); b31 = sb("b31", [128, 6], F32)
    postg = sb("postg", [128, 1024], F32)
    SMF = Buf("smallf32")
    wiw = sb("wiw", [128, 64], BF16); wuk = sb("wuk", [128, 384], BF16); wuv = sb("wuv", [128, 384], BF16)
    tb8 = sb("tb8", [128, 1536], BF16)
    SMB = Buf("smallbf")
    memT = sb("memT", [128, 8, 256], BF16); MEMT = Buf("memT")
    memk = sb("memk", [128, 2, 256], BF16); MEMK = Buf("memk")
    memv = sb("memv", [128, 2, 256], BF16); MEMV = Buf("memv")
    membf = sb("membf", [128, 2, 1024], BF16); MEMBF = Buf("membf")
    NT = 3
    etile = [sb(f"et{i}", [128, 512], BF16) for i in range(NT)]; ET = [Buf(f"et{i}") for i in range(NT)]
    atile = [sb(f"at{i}", [128, 512], BF16) for i in range(NT)]; AT = [Buf(f"at{i}") for i in range(NT)]
    cs48 = sb("cs48", [128, 512], BF16); cslo = sb("cslo", [32, 512], BF16); CS = Buf("cs")
    cshi = cs48[0:16, :]
    score = sb("score", [128, S_TOK], F32); SCORE = Buf("score")
    junk = sb("junk", [128, S_TOK], BF16); JUNK = Buf("junk")
    pen = sb("pen", [128, 4, S_TOK], BF16); PEN = [Buf(f"pen{i}") for i in range(4)]
    NR = 8
    rt = [sb(f"rt{i}", [128, 512], BF16) for i in range(NR)]; RT = [Buf(f"rt{i}") for i in range(NR)]
    dgt = [sb(f"dgt{i}", [128, 8, 128], BF16) for i in range(2)]; DGT = [Buf(f"dgt{i}") for i in range(2)]
    ptile = [sb(f"pt{i}", [128, 512], BF16) for i in range(NT)]; PT = [Buf(f"pt{i}") for i in range(NT)]
    qlat = [sb(f"ql{i}", [128, 512], BF16) for i in range(2)]; QL = [Buf(f"ql{i}") for i in range(2)]
    recf = sb("recf", [128, 512], F32); RECF = Buf("recf")
    onb = sb("onb", [128, 512], BF16); ONB = Buf("onb")
    tmpf = sb("tmpf", [128, 1024], F32); TMPF = Buf("tmpf")
    sm = sb("smalls", [128, 64], F32)
    SMS = {n: Buf("sm_" + n) for n in ("ss", "rs", "ssk", "rsk", "bis", "ssy")}
    steps = sb("steps", [128, 32], F32); STEPS = Buf("steps")
    ss = sm[:, 0:4]; rs = sm[:, 4:8]; ssk = sm[:, 8:12]; rsk = sm[:, 12:16]
    mx = sm[:, 16:17]; mn = sm[:, 17:18]; thr = sm[:, 18:19]; rng = sm[:, 19:20]; cnt = sm[:, 20:21]; dd = sm[:, 21:22]
    ssy = sm[:, 24:26]; ssy2 = sm[:, 26:27]; rsy = sm[:, 27:28]

    sl_c = S.dma_slot("const"); sl_c2 = S.dma_slot("const2"); sl_x = S.dma_slot("x"); sl_o = S.dma_slot("o"); sl_m = S.dma_slot("mem")
    sl_s = S.dma_slot("small"); sl_w = [S.dma_slot(f"w{i}") for i in range(NWB)]
    sl_wb = [S.dma_slot(f"wb{i}") for i in range(NWF)]; sl_wo = [S.dma_slot(f"wo{i}") for i in range(NWB)]
    sl_big = [S.dma_slot(f"big{i}") for i in range(8)]
    sl_co = [S.dma_slot(f"co{i}") for i in range(NWF)]
    WSC = [[Buf(f"wsc{l_}_{i}") for i in range(38)] for l_ in range(L)]
    OUTB = Buf("outdram")
    SCRB = {}

    cnt_rr = {"wf": 0, "w": 0, "e": 0, "a": 0, "r": 0, "p": 0, "q": 0, "d": 0, "ev": 0}

    def rr(key, n):
        v = cnt_rr[key] % n
        cnt_rr[key] += 1
        return v

    S.op("dve", lambda e: e.memset(cs48[:, :], 0.0), writes=[CS])
    S.op("dve", lambda e: e.memset(sbq[:].rearrange("p h t -> p (h t)"), 0.0), writes=[SBQ])
    S.dma(lambda e: e.dma_start(out=cb[:], in_=cb_d[:, :]), sl_c, writes=[CBb])
    S.dma(lambda e: e.dma_start(out=cf[:], in_=cf_d[:, :]), sl_c2, writes=[CFb])

    conv_order = [26, 27, 28, 29] + list(range(26)) + list(range(30, 38))
    for l_ in range(L):
        pendc = []
        for idx in conv_order:
            src = wF_d[l_, idx, :, :] if idx < 26 else (wM_d[l_, idx - 26, :, :] if idx < 30 else wO_d[l_, idx - 30, :, :])
            k = rr("w", NWB)
            S.dma(lambda e, k=k, src=src: e.dma_start(out=wst[k][:], in_=src), sl_w[k], writes=[WST[k]], e="pool")

            def tailc(k=k, l_=l_, idx=idx):
                kf = rr("wf", NWF)
                S.op("pool", lambda e: e.tensor_copy(out=wbf[kf][:], in_=wst[k][:]), reads=[WST[k]], writes=[WBF[kf]])
                S.dma(lambda e: e.dma_start(out=wsc_d[l_, idx, :, :], in_=wbf[kf][:]), sl_co[kf],
                      reads=[WBF[kf]], writes=[WSC[l_][idx]], e="pool")
            pendc.append(tailc)
            if len(pendc) > 1:
                pendc.pop(0)()
        while pendc:
            pendc.pop(0)()

    def wchunk(l_, idx):
        kf = rr("wf", NWF)
        S.dma(lambda e: e.dma_start(out=wbf[kf][:], in_=wsc_d[l_, idx, :, :]), sl_wb[kf], reads=[WSC[l_][idx]], writes=[WBF[kf]])
        return wbf[kf], WBF[kf]

    LA = 3

    def wstream(l_, idxs):
        pend_ = []
        it = iter(idxs)
        for _ in range(LA):
            nx = next(it, None)
            if nx is not None:
                pend_.append(wchunk(l_, nx))
        while pend_:
            cur = pend_.pop(0)
            nx = next(it, None)
            if nx is not None:
                pend_.append(wchunk(l_, nx))
            yield cur

    def evac_copy(out_ap, in_ap, reads, writes):
        if rr("ev", 2) == 0:
            S.op("act", lambda e: e.activation(out=out_ap, in_=in_ap, func=AF.Copy), reads=reads, writes=writes)
        else:
            S.op("dve", lambda e: e.tensor_copy(out=out_ap, in_=in_ap), reads=reads, writes=writes)

    def rms_scale(src_ss, dst_rs, n, inv_n, B_ss, B_rs):
        S.op("act", lambda e: e.activation(out=dst_rs, in_=src_ss, func=AF.Sqrt, scale=inv_n, bias=EPS),
             reads=[B_ss], writes=[B_rs])
        S.op("dve", lambda e: e.reciprocal(out=dst_rs, in_=dst_rs), reads=[B_rs], writes=[B_rs])

    def unit(u):
        l = layer_of_unit[u]
        b = batch_of_unit[u]
        src_x = x_d if chain[u] == "in" else scr_d
        later = any(batch_of_unit[v] == b for v in range(u + 1, len(layer_of_unit)))
        dst_x = scr_d if later else out_d
        if later and b not in SCRB:
            SCRB[b] = [Buf(f"scr{b}_{q}") for q in range(NQ)]

        S.dma(lambda e: e.dma_start(out=xc[:, 0:NSM], in_=wsm_d[l, :, :]), sl_s, writes=[XC])
        for (dst, off, n) in ((gcol, SM_GCOL, 8), (kvg, SM_KVG, 128), (b31, SM_B31, 6), (postg, SM_POSTG, 1024)):
            S.op("dve", lambda e, dst=dst, off=off, n=n: e.tensor_copy(out=dst[:, 0:n], in_=xc[:, off:off + n]),
                 reads=[XC], writes=[SMF])
        for (dst, off, n) in ((wiw, SM_IW, 64), (wuk, SM_WUK, 384), (wuv, SM_WUV, 384)):
            S.op("dve", lambda e, dst=dst, off=off, n=n: e.tensor_copy(out=dst[:, 0:n], in_=xc[:, off:off + n]),
                 reads=[XC], writes=[SMB])
        S.op("dve", lambda e: e.tensor_scalar(out=tb8[:, :], in0=xc[:, SM_TB:SM_TB + 1536], scalar1=8.0, scalar2=None,
                                              op0=ALU.mult), reads=[XC], writes=[SMB])
        S.dma(lambda e: e.dma_start(out=xc[:, 0:2048].rearrange("p (j d) -> p j d", j=2),
                                    in_=mem_d[b, :, :].rearrange("(j p) d -> p j d", p=128)), sl_m, writes=[XC])
        S.op("dve", lambda e: e.tensor_copy(out=membf[:].rearrange("p j d -> p (j d)"), in_=xc[:, 0:2048]),
             reads=[XC], writes=[MEMBF])
        for c2 in range(4):
            bk = bget()
            pb = psum[bk][:].bitcast(BF16)
            for cc in range(2):
                c = 2 * c2 + cc
                for j in range(2):
                    S.op("pe", lambda e, c=c, j=j, cc=cc, pb=pb: e.transpose(
                        pb[:, cc * 256 + j * 128:cc * 256 + (j + 1) * 128], membf[:, j, c * 128:(c + 1) * 128], ident),
                        reads=[MEMBF, CBb], writes=[PB[bk]])
            evac_copy(memT[:, 2 * c2:2 * c2 + 2, :], pb[:, 0:512].rearrange("p (c m) -> p c m", c=2), [PB[bk]], [MEMT])
            bput(bk)
        for g4, (w, W) in enumerate(wstream(l, [26, 27, 28, 29])):
            w3 = w[:].rearrange("p (c g) -> p c g", c=8)
            bk = bget()
            if g4 < 2:
                for c in range(8):
                    S.op("pe", lambda e, c=c, w3=w3, bk=bk: e.matmul(psum[bk][:, 0:256], lhsT=w3[:, c, :], rhs=memT[:, c, :],
                                                                    start=(c == 0), stop=(c == 7)),
                         reads=[W, MEMT], writes=[PB[bk]])
                evac_copy(memk[:, g4, :], psum[bk][:, 0:256], [PB[bk]], [MEMK])
            else:
                for j in range(2):
                    for c in range(8):
                        S.op("pe", lambda e, c=c, j=j, w3=w3, bk=bk: e.matmul(
                            psum[bk][:, j * 128:(j + 1) * 128], lhsT=memT[:, c, j * 128:(j + 1) * 128], rhs=w3[:, c, :],
                            start=(c == 0), stop=(c == 7)), reads=[W, MEMT], writes=[PB[bk]])
                evac_copy(memv[:, :, (g4 - 2) * 128:(g4 - 1) * 128], psum[bk][:, 0:256].rearrange("p (j g) -> p j g", j=2),
                          [PB[bk]], [MEMV])
            bput(bk)

        for tq in range(NQ):
            chunk_phase(u, l, b, tq, src_x, dst_x)

    def chunk_phase(u, l, b, tq, src_x, dst_x):
        t0 = tq * 512
        hn = hnmix[:].rearrange("p (i d) -> p i d", i=4)
        mix = hnmix[:].rearrange("p (c t) -> p c t", c=8)
        xc3 = xc[:].rearrange("p (i d) -> p i d", i=4)
        rd = [XC]
        if src_x is scr_d:
            rd = [XC] + [SCRB[b][tq]]
        S.dma(lambda e: e.dma_start(out=xc3, in_=src_x[b, t0:t0 + 512, :].rearrange("(i p) d -> p i d", p=128)),
              sl_x, reads=rd[1:], writes=[XC])
        for i in range(4):
            S.op("act", lambda e, i=i: e.activation(out=junk[:, 0:1024], in_=xc3[:, i, :], func=AF.Square,
                                                    accum_out=ss[:, i:i + 1]), reads=[XC], writes=[JUNK, SMS["ss"]])
        rms_scale(ss, rs, 4, 1.0 / D, SMS["ss"], SMS["rs"])
        for i in range(4):
            S.op("dve", lambda e, i=i: e.tensor_scalar(out=hn[:, i, :], in0=xc3[:, i, :], scalar1=rs[:, i:i + 1],
                                                       scalar2=None, op0=ALU.mult),
                 reads=[XC, SMS["rs"]], writes=[HNMIX])
        for c2 in range(4):
            bk = bget()
            pb = psum[bk][:].bitcast(BF16)
            for cc in range(2):
                c = 2 * c2 + cc
                for i in range(4):
                    S.op("pe", lambda e, c=c, i=i, cc=cc, pb=pb: e.transpose(
                        pb[:, cc * 512 + i * 128:cc * 512 + (i + 1) * 128], hn[:, i, c * 128:(c + 1) * 128], ident),
                        reads=[HNMIX, CBb], writes=[PB[bk]])
            for cc in range(2):
                c = 2 * c2 + cc
                S.op("dve", lambda e, c=c, cc=cc, pb=pb: e.tensor_scalar(
                    out=hT[:, c, :], in0=pb[:, cc * 512:(cc + 1) * 512], scalar1=gcol[:, c:c + 1], scalar2=None,
                    op0=ALU.mult), reads=[PB[bk], SMF], writes=[HT])
            bput(bk)

        if DBG_STAGE < 2:
            return
        fdest = ([("sbq", i) for i in range(3)] + [("sbk", i) for i in range(3)] + [("sbg", i) for i in range(3)]
                 + [("dq", i) for i in range(3)] + [("dg", i) for i in range(3)] + [("iq", 0), ("iq", 1), ("ik", 0)]
                 + [("mq", 0), ("mq", 1), ("mg", 0), ("mg", 1)])
        dst_tab = {"sbq": (sbq, SBQ), "sbg": (sbg, SBG), "dq": (dq, DQ), "dg": (dg, DG), "iq": (iq, IQ),
                   "mq": (mq, MQ), "mg": (mg, MG)}
        ptb = None
        for cc, (w, W) in enumerate(wstream(l, list(range(26)))):
            w3 = w[:].rearrange("p (c g) -> p c g", c=8)
            if cc < 22:
                name, ci = fdest[cc]
                bk = bget()
                for c in range(8):
                    S.op("pe", lambda e, c=c, w3=w3, bk=bk: e.matmul(psum[bk][:, :], lhsT=w3[:, c, :], rhs=hT[:, c, :],
                                                                    start=(c == 0), stop=(c == 7)),
                         reads=[W, HT], writes=[PB[bk]])
                if name == "sbk":
                    evac_copy(sbkT[:, ci, t0:t0 + 512], psum[bk][:, :], [PB[bk]], [SBK[tq]])
                elif name == "ik":
                    evac_copy(ikT4[:, t0:t0 + 512], psum[bk][:, :], [PB[bk]], [IKT[tq]])
                elif name in ("sbg", "dg", "mg") and DBG_SUB >= 2:
                    dt_, DB = dst_tab[name]
                    hs = rr("ev", 2) * 512
                    S.op("act", lambda e, bk=bk, hs=hs: e.activation(out=tmpf[:, hs:hs + 512], in_=psum[bk][:, :], func=AF.Exp,
                                                                     scale=-1.0), reads=[PB[bk]], writes=[TMPF])
                    S.op("act", lambda e, hs=hs: e.activation(out=tmpf[:, hs:hs + 512], in_=tmpf[:, hs:hs + 512], func=AF.Ln, bias=1.0),
                         reads=[TMPF], writes=[TMPF])
                    S.op("act", lambda e, hs=hs: e.activation(out=tmpf[:, hs:hs + 512], in_=tmpf[:, hs:hs + 512], func=AF.Exp, scale=-1.0),
                         reads=[TMPF], writes=[TMPF])
                    S.op("dve", lambda e, dt_=dt_, ci=ci, bk=bk, hs=hs: e.tensor_tensor(
                        out=dt_[:, ci, :], in0=psum[bk][:, :], in1=tmpf[:, hs:hs + 512], op=ALU.mult),
                        reads=[PB[bk], TMPF], writes=[DB])
                elif name == "sbq":
                    evac_copy(sbq[0:64, 2 * ci, :], psum[bk][0:64, :], [PB[bk]], [SBQ])
                    evac_copy(sbq[64:128, 2 * ci + 1, :], psum[bk][64:128, :], [PB[bk]], [SBQ])
                else:
                    dt_, DB = dst_tab[name]
                    evac_copy(dt_[:, ci, :], psum[bk][:, :], [PB[bk]], [DB])
                bput(bk)
            elif DBG_SUB >= 30:
                g4 = cc - 22
                if g4 == 0:
                    ptb = [bget() for _ in range(4)]
                for i in range(4):
                    for c in range(8):
                        S.op("pe", lambda e, c=c, i=i, w3=w3, g4=g4: e.matmul(
                            psum[ptb[i]][:, g4 * 128:(g4 + 1) * 128], lhsT=hT[:, c, i * 128:(i + 1) * 128], rhs=w3[:, c, :],
                            start=(c == 0), stop=(c == 7)), reads=[W, HT], writes=[PB[ptb[i]]])
        if DBG_SUB < 30:
            return
        for i in range(4):
            j = 4 * tq + i
            evac_copy(sbv[:, j, :], psum[ptb[i]][:, 0:384], [PB[ptb[i]]], [SBV[tq]])
            evac_copy(tmpf[:, i * 128:(i + 1) * 128], psum[ptb[i]][:, 384:512], [PB[ptb[i]]], [TMPF])
        for i in range(4):
            S.op("act", lambda e, i=i: e.activation(out=junk[:, 0:128], in_=tmpf[:, i * 128:(i + 1) * 128], func=AF.Square,
                                                    accum_out=ssk[:, i:i + 1]),
                 reads=[TMPF], writes=[JUNK, SMS["ssk"]])
        rms_scale(ssk, rsk, 4, 1.0 / 128, SMS["ssk"], SMS["rsk"])
        for i in range(4):
            j = 4 * tq + i
            S.op("dve", lambda e, i=i, j=j: e.scalar_tensor_tensor(out=ckv[:, j, :], in0=tmpf[:, i * 128:(i + 1) * 128],
                                                                   scalar=rsk[:, i:i + 1], in1=kvg[:, :],
                                                                   op0=ALU.mult, op1=ALU.mult),
                 reads=[TMPF, SMS["rsk"], SMF], writes=[CKV[tq]])
        for i in range(4):
            bput(ptb[i])
        if DBG_SUB < 40:
            return
        bk = bget()
        pb = psum[bk][:].bitcast(BF16)
        for i in range(4):
            j = 4 * tq + i
            S.op("pe", lambda e, i=i, j=j, pb=pb: e.transpose(pb[:, i * 128:(i + 1) * 128], ckv[:, j, :], ident),
                 reads=[CKV[tq], CBb], writes=[PB[bk]])
        evac_copy(ckvT[:, t0:t0 + 512], pb[:, 0:512], [PB[bk]], [CKVT[tq]])
        bput(bk)
        if DBG_SUB < 50:
            return
        bk = bget()
        wiw3 = wiw[:].rearrange("p (c g) -> p c g", c=8)
        for i in range(4):
            for c in range(8):
                S.op("pe", lambda e, c=c, i=i, bk=bk: e.matmul(psum[bk][:, i * 8:(i + 1) * 8],
                                                                lhsT=hT[:, c, i * 128:(i + 1) * 128], rhs=wiw3[:, c, :],
                                                                start=(c == 0), stop=(c == 7)),
                     reads=[SMB, HT], writes=[PB[bk]])
        S.op("dve", lambda e, bk=bk: e.tensor_scalar(out=idxw[:, :], in0=psum[bk][:, 0:32], scalar1=1.0 / 16, scalar2=None,
                                                     op0=ALU.mult), reads=[PB[bk]], writes=[IDXW])
        bput(bk)

        if DBG_STAGE < 3:
            return
        nkb = 4 * tq + 4
        KS = lambda lst: [lst[q] for q in range(tq + 1)]

        def indexer(i):
            qb = 4 * tq + i
            if qb < 2:
                return
            yield
            nk = (qb + 1) * 128
            nkc = (nk + 511) // 512
            kd = rr("d", 2)
            for h in range(8):
                S.op("act", lambda e, h=h, kd=kd: e.activation(out=dgt[kd][:, h, :], in_=ident, func=AF.Copy,
                                                               scale=idxw[:, i * 8 + h:i * 8 + h + 1]),
                     reads=[CBb, IDXW], writes=[DGT[kd]])
            for kc in range(nkc):
                w_ = min(512, nk - kc * 512)
                bs = bget()
                pend = []
                for g in range(2):
                    bds = [bget() for _ in range(4)]
                    for j in range(4):
                        hp = j * 32
                        tp = (96, 0) if hp == 96 else None
                        S.op("pe", lambda e, g=g, hp=hp, tp=tp, bd=bds[j], kc=kc, w_=w_: e.matmul(
                            psum[bd][:, 0:w_], lhsT=iq[hp:hp + 32, g, i * 128:(i + 1) * 128],
                            rhs=ikT4[hp:hp + 32, kc * 512:kc * 512 + w_], start=True, stop=True, tile_position=tp),
                            reads=[IQ] + KS(IKT), writes=[PB[bds[j]]])
                    for j in range(4):
                        h = 4 * g + j
                        kr = rr("r", NR)
                        S.op("dve", lambda e, kr=kr, bd=bds[j], w_=w_: e.tensor_scalar(out=rt[kr][:, 0:w_], in0=psum[bd][:, 0:w_],
                                                                                      scalar1=0.0, scalar2=None, op0=ALU.max),
                             reads=[PB[bds[j]]], writes=[RT[kr]])
                        bput(bds[j])
                        pend.append(lambda h=h, kr=kr, bs=bs, w_=w_: S.op("pe", lambda e: e.matmul(
                            psum[bs][:, 0:w_], lhsT=dgt[kd][:, h, :], rhs=rt[kr][:, 0:w_], start=(h == 0), stop=(h == 7)),
                            reads=[DGT[kd], RT[kr]], writes=[PB[bs]]))
                    while len(pend) > 4:
                        pend.pop(0)()
                    yield
                while pend:
                    pend.pop(0)()
                last = (kc == nkc - 1)
                wc = w_ - 128 if last else w_
                if wc > 0:
                    S.op("dve", lambda e, bs=bs, kc=kc, wc=wc: e.tensor_copy(out=score[:, kc * 512:kc * 512 + wc],
                                                                             in_=psum[bs][:, 0:wc]),
                         reads=[PB[bs]], writes=[SCORE])
                if last:
                    S.op("dve", lambda e, bs=bs, w_=w_: e.tensor_tensor(out=score[:, nk - 128:nk], in0=psum[bs][:, w_ - 128:w_],
                                                                       in1=negb, op=ALU.add),
                         reads=[PB[bs], CFb], writes=[SCORE])
                bput(bs)
            B = SMS["bis"]
            S.op("dve", lambda e: e.tensor_reduce(out=mx, in_=score[:, 0:nk], axis=AX.X, op=ALU.max), reads=[SCORE], writes=[B])
            S.op("dve", lambda e: e.tensor_reduce(out=mn, in_=score[:, 0:nk - 128], axis=AX.X, op=ALU.min),
                 reads=[SCORE], writes=[B])
            S.op("dve", lambda e: e.tensor_tensor(out=rng, in0=mx, in1=mn, op=ALU.subtract), reads=[B], writes=[B])
            S.op("dve", lambda e: e.tensor_tensor(out=thr, in0=mx, in1=mn, op=ALU.add), reads=[B], writes=[B])
            S.op("dve", lambda e: e.tensor_scalar(out=thr, in0=thr, scalar1=0.5, scalar2=None, op0=ALU.mult), reads=[B], writes=[B])
            S.op("dve", lambda e: e.tensor_scalar(out=steps[:, 0:16], in0=p2a, scalar1=rng, scalar2=None, op0=ALU.mult),
                 reads=[B, CFb], writes=[STEPS])
            S.op("dve", lambda e: e.tensor_scalar(out=steps[:, 16:32], in0=p2b, scalar1=rng, scalar2=None, op0=ALU.mult),
                 reads=[B, CFb], writes=[STEPS])
            for it in range(NBIS):
                S.op("dve", lambda e: e.tensor_scalar(out=junk[:, 0:nk], in0=score[:, 0:nk], scalar1=thr, scalar2=0.0,
                                                      op0=ALU.is_ge, op1=ALU.add, accum_out=cnt),
                     reads=[SCORE, B], writes=[JUNK, B])
                S.op("dve", lambda e, it=it: e.tensor_scalar(out=dd, in0=cnt, scalar1=float(TOPK), scalar2=steps[:, 16 + it:17 + it],
                                                             op0=ALU.is_ge, op1=ALU.mult), reads=[B, STEPS], writes=[B])
                S.op("dve", lambda e, it=it: e.scalar_tensor_tensor(out=thr, in0=dd, scalar=steps[:, it:it + 1], in1=thr,
                                                                    op0=ALU.subtract, op1=ALU.add),
                     reads=[B, STEPS], writes=[B])
                yield
                yield
            S.op("dve", lambda e: e.tensor_scalar(out=pen[:, i, 0:nk], in0=score[:, 0:nk], scalar1=thr, scalar2=NEG,
                                                  op0=ALU.is_lt, op1=ALU.mult), reads=[SCORE, B], writes=[PEN[i]])

        def sb_head(h):
            ch, hp = h // 2, (h % 2) * 64
            bcs = bget()
            pend = []
            pendL = []
            for kb in range(nkb):
                c0 = max(0, kb - 4 * tq) * 128
                diag = kb >= 4 * tq
                bz = bget()
                S.op("pe", lambda e, kb=kb, c0=c0, bz=bz, diag=diag: e.matmul(
                    psum[bz][:, c0:512], lhsT=sbkT[:, ch, kb * 128:(kb + 1) * 128], rhs=sbq[:, h, c0:512],
                    start=True, stop=not diag), reads=KS(SBK) + [SBQ], writes=[PB[bz]])
                if diag:
                    S.op("pe", lambda e, c0=c0, bz=bz: e.matmul(psum[bz][:, c0:c0 + 128], lhsT=penS, rhs=ident,
                                                                 start=False, stop=True), reads=[CBb], writes=[PB[bz]])
                ke = rr("e", NT)
                S.op("act", lambda e, ke=ke, c0=c0, bz=bz: e.activation(out=etile[ke][:, c0:512], in_=psum[bz][:, c0:512],
                                                                        func=AF.Exp, scale=0.125),
                     reads=[PB[bz]], writes=[ET[ke]])
                bput(bz)
                sp = big16[:, kb * 512:(kb + 1) * 512]

                def ln_stage(kb=kb, c0=c0, ke=ke, sp=sp):
                    S.op("act", lambda e: e.activation(out=sp[:, c0:512], in_=etile[ke][:, c0:512], func=AF.Ln, bias=1.0),
                         reads=[ET[ke]], writes=[BIG[kb]])
                    pend.append(lambda: S.op("pe", lambda e: e.matmul(
                        psum[bcs][:, c0:512], lhsT=ekb(kb), rhs=sp[:, c0:512], start=(kb == 0), stop=(kb == nkb - 1)),
                        reads=[CBb, BIG[kb]], writes=[PB[bcs]]))
                pendL.append(ln_stage)
                if len(pendL) > 1:
                    pendL.pop(0)()
                if len(pend) > SKEW:
                    pend.pop(0)()
                yield
            while pendL:
                pendL.pop(0)()
            while pend:
                pend.pop(0)()
            S.op("dve", lambda e: e.tensor_copy(out=cs48[0:32, :], in_=psum[bcs][0:32, :]), reads=[PB[bcs]], writes=[CS])
            S.op("dve", lambda e: e.tensor_tensor(out=cslo[:, :], in0=psum[bcs][0:32, :], in1=cs48[0:32, :], op=ALU.subtract),
                 reads=[PB[bcs], CS], writes=[CS])
            S.op("dve", lambda e: e.tensor_copy(out=cs48[32:64, :], in_=cslo[:, :]), reads=[CS], writes=[CS])
            bput(bcs)
            bo = bget()
            pend = []
            for kb in range(nkb):
                c0 = max(0, kb - 4 * tq) * 128
                diag = kb >= 4 * tq
                bi = bget()
                sp = big16[:, kb * 512:(kb + 1) * 512]
                S.op("pe", lambda e, kb=kb, c0=c0, bi=bi: e.matmul(
                    psum[bi][:, c0:512], lhsT=sbkT[:, ch, kb * 128:(kb + 1) * 128], rhs=sbq[:, h, c0:512],
                    start=True, stop=False), reads=KS(SBK) + [SBQ], writes=[PB[bi]])
                S.op("pe", lambda e, c0=c0, bi=bi, sp=sp: e.matmul(psum[bi][:, c0:512], lhsT=triM8, rhs=sp[:, c0:512],
                                                                   start=False, stop=False),
                     reads=[CBb, BIG[kb]], writes=[PB[bi]])
                S.op("pe", lambda e, kb=kb, c0=c0, bi=bi, diag=diag: e.matmul(psum[bi][:, c0:512], lhsT=selM8(kb),
                                                                              rhs=cs48[:, c0:512], start=False, stop=not diag),
                     reads=[CBb, CS], writes=[PB[bi]])
                if diag:
                    S.op("pe", lambda e, c0=c0, bi=bi: e.matmul(psum[bi][:, c0:c0 + 128], lhsT=penS, rhs=ident,
                                                                 start=False, stop=True), reads=[CBb], writes=[PB[bi]])
                ka = rr("a", NT)
                S.op("act", lambda e, ka=ka, c0=c0, bi=bi: e.activation(out=atile[ka][:, c0:512], in_=psum[bi][:, c0:512],
                                                                        func=AF.Exp, scale=0.125),
                     reads=[PB[bi]], writes=[AT[ka]])
                bput(bi)
                pend.append(lambda kb=kb, c0=c0, ka=ka: S.op("pe", lambda e: e.matmul(
                    psum[bo][:, c0:512], lhsT=sbv[:, kb, ch * 128:(ch + 1) * 128], rhs=atile[ka][:, c0:512],
                    start=(kb == 0), stop=(kb == nkb - 1)), reads=KS(SBV) + [AT[ka]], writes=[PB[bo]]))
                if len(pend) > SKEW:
                    pend.pop(0)()
                yield
            while pend:
                pend.pop(0)()
            S.op("dve", lambda e: e.tensor_tensor(out=mix[hp:hp + 64, ch, :], in0=psum[bo][hp:hp + 64, :],
                                                  in1=sbg[hp:hp + 64, ch, :], op=ALU.mult),
                 reads=[PB[bo], SBG], writes=[HNMIX])
            bput(bo)

        def mem_head(hm):
            ch, hp = hm // 2, (hm % 2) * 64
            bo = bget(); bd = bget()
            for mb in range(2):
                bl = bget()
                S.op("pe", lambda e, mb=mb, bl=bl: e.matmul(psum[bl][:, :], lhsT=memk[hp:hp + 64, ch, mb * 128:(mb + 1) * 128],
                                                            rhs=mq[hp:hp + 64, ch, :], start=True, stop=True),
                     reads=[MEMK, MQ], writes=[PB[bl]])
                kp = rr("p", NT)
                S.op("act", lambda e, kp=kp, bl=bl: e.activation(out=ptile[kp][:, :], in_=psum[bl][:, :], func=AF.Exp, scale=0.125),
                     reads=[PB[bl]], writes=[PT[kp]])
                bput(bl)
                S.op("pe", lambda e, mb=mb, kp=kp: e.matmul(psum[bo][:, :], lhsT=memv[:, mb, ch * 128:(ch + 1) * 128],
                                                            rhs=ptile[kp][:, :], start=(mb == 0), stop=(mb == 1)),
                     reads=[MEMV, PT[kp]], writes=[PB[bo]])
                S.op("pe", lambda e, mb=mb, kp=kp: e.matmul(psum[bd][:, :], lhsT=ones, rhs=ptile[kp][:, :],
                                                            start=(mb == 0), stop=(mb == 1)),
                     reads=[CBb, PT[kp]], writes=[PB[bd]])
            S.op("act", lambda e: e.activation(out=recf[hp:hp + 64, :], in_=psum[bd][hp:hp + 64, :], func=AF.Ln), reads=[PB[bd]], writes=[RECF])
            S.op("act", lambda e: e.activation(out=recf[hp:hp + 64, :], in_=recf[hp:hp + 64, :], func=AF.Exp, scale=-1.0), reads=[RECF], writes=[RECF])
            S.op("dve", lambda e: e.tensor_tensor(out=tmpf[hp:hp + 64, 0:512], in0=psum[bo][hp:hp + 64, :],
                                                  in1=recf[hp:hp + 64, :], op=ALU.mult), reads=[PB[bo], RECF], writes=[TMPF])
            S.op("dve", lambda e: e.tensor_tensor(out=mix[hp:hp + 64, 6 + ch, :], in0=tmpf[hp:hp + 64, 0:512],
                                                  in1=mg[hp:hp + 64, ch, :], op=ALU.mult), reads=[TMPF, MG], writes=[HNMIX])
            bput(bo); bput(bd)

        def dsa_head(h):
            ch, hp = h // 2, (h % 2) * 64
            bq = bget()
            S.op("pe", lambda e: e.matmul(psum[bq][:, :], lhsT=wuk[hp:hp + 64, ch * 128:(ch + 1) * 128], rhs=dq[hp:hp + 64, ch, :],
                                          start=True, stop=True), reads=[SMB, DQ], writes=[PB[bq]])
            kq = rr("q", 2)
            evac_copy(qlat[kq][:, :], psum[bq][:, :], [PB[bq]], [QL[kq]])
            bput(bq)
            bo = bget(); bd = bget()
            pend = []
            for kb in range(nkb):
                i0 = max(0, kb - 4 * tq)
                c0 = i0 * 128
                bl = bget()
                mm = []
                mm.append((lambda e, st, sp_, kb=kb, c0=c0, bl=bl: e.matmul(
                    psum[bl][:, c0:512], lhsT=ckvT[:, kb * 128:(kb + 1) * 128], rhs=qlat[kq][:, c0:512], start=st, stop=sp_),
                    KS(CKVT) + [QL[kq]]))
                for i in range(i0, 4):
                    qb = 4 * tq + i
                    cs_ = slice(i * 128, (i + 1) * 128)
                    if qb >= 2:
                        mm.append((lambda e, st, sp_, i=i, kb=kb, cs_=cs_, bl=bl: e.matmul(
                            psum[bl][:, cs_], lhsT=pen[:, i, kb * 128:(kb + 1) * 128], rhs=ident, start=st, stop=sp_),
                            [PEN[i], CBb]))
                    elif qb == kb:
                        mm.append((lambda e, st, sp_, cs_=cs_, bl=bl: e.matmul(psum[bl][:, cs_], lhsT=penD, rhs=ident,
                                                                              start=st, stop=sp_), [CBb]))
                    if qb - kb <= 1:
                        jj = qb - kb
                        mm.append((lambda e, st, sp_, cs_=cs_, jj=jj, bl=bl: e.matmul(
                            psum[bl][:, cs_], lhsT=ident, rhs=tb8[:, (h * 2 + jj) * 128:(h * 2 + jj + 1) * 128],
                            start=st, stop=sp_), [CBb, SMB]))
                for n_, (fn, rds) in enumerate(mm):
                    S.op("pe", lambda e, fn=fn, n_=n_, nm=len(mm): fn(e, n_ == 0, n_ == nm - 1), reads=rds, writes=[PB[bl]])
                kp = rr("p", NT)
                near_hi = min(512, max(c0, (kb + 2 - 4 * tq) * 128))
                if near_hi > c0:
                    S.op("act", lambda e, kp=kp, c0=c0, near_hi=near_hi, bl=bl: e.activation(
                        out=ptile[kp][:, c0:near_hi], in_=psum[bl][:, c0:near_hi], func=AF.Exp, scale=0.125),
                        reads=[PB[bl]], writes=[PT[kp]])
                if near_hi < 512:
                    S.op("act", lambda e, kp=kp, near_hi=near_hi, bl=bl: e.activation(
                        out=ptile[kp][:, near_hi:512], in_=psum[bl][:, near_hi:512], func=AF.Exp, scale=0.125,
                        bias=b31[:, h:h + 1]), reads=[PB[bl], SMF], writes=[PT[kp]])
                bput(bl)
                def tail(kb=kb, c0=c0, kp=kp):
                    S.op("pe", lambda e: e.matmul(psum[bo][:, c0:512], lhsT=ckv[:, kb, :], rhs=ptile[kp][:, c0:512],
                                                  start=(kb == 0), stop=(kb == nkb - 1)),
                         reads=KS(CKV) + [PT[kp]], writes=[PB[bo]])
                    S.op("pe", lambda e: e.matmul(psum[bd][:, c0:512], lhsT=ones, rhs=ptile[kp][:, c0:512],
                                                  start=(kb == 0), stop=(kb == nkb - 1)),
                         reads=[CBb, PT[kp]], writes=[PB[bd]])
                pend.append(tail)
                if len(pend) > SKEW:
                    pend.pop(0)()
                yield
            while pend:
                pend.pop(0)()
            S.op("act", lambda e: e.activation(out=recf[:, :], in_=psum[bd][:, :], func=AF.Ln), reads=[PB[bd]], writes=[RECF])
            S.op("act", lambda e: e.activation(out=recf[:, :], in_=recf[:, :], func=AF.Exp, scale=-1.0), reads=[RECF], writes=[RECF])
            S.op("dve", lambda e: e.tensor_tensor(out=onb[:, :], in0=psum[bo][:, :], in1=recf[:, :], op=ALU.mult),
                 reads=[PB[bo], RECF], writes=[ONB])
            bput(bo); bput(bd)
            bu = bget()
            S.op("pe", lambda e: e.matmul(psum[bu][:, :], lhsT=wuv[:, ch * 128:(ch + 1) * 128], rhs=onb[:, :], start=True, stop=True),
                 reads=[SMB, ONB], writes=[PB[bu]])
            S.op("dve", lambda e: e.tensor_tensor(out=mix[hp:hp + 64, 3 + ch, :], in0=psum[bu][hp:hp + 64, :],
                                                  in1=dg[hp:hp + 64, ch, :], op=ALU.mult), reads=[PB[bu], DG], writes=[HNMIX])
            bput(bu)

        def run_par(*gens):
            gens = list(gens)
            while gens:
                for g in list(gens):
                    try:
                        next(g)
                    except StopIteration:
                        gens.remove(g)

        def seq(*fns):
            for f in fns:
                r = f()
                if r is not None:
                    yield from r
                yield

        run_par(seq(*[lambda i=i: indexer(i) for i in range(4)]),
                seq(*([lambda h=h: sb_head(h) for h in range(5)] + [lambda hm=hm: mem_head(hm) for hm in range(4)])))
        run_par(sb_head(5), seq(lambda: dsa_head(0), lambda: dsa_head(1)))
        wO3 = big16[:].rearrange("p (c n) -> p c n", c=8)
        for c in range(8):
            S.dma(lambda e, c=c: e.dma_start(out=big16[:, c * 1024:(c + 1) * 1024], in_=wsc_d[l, 30 + c, :, :]), sl_big[c],
                  reads=[WSC[l][30 + c]], writes=[BIG[2 * c], BIG[2 * c + 1]])
        for h in range(2, 6):
            run_par(dsa_head(h))

        for i in range(4):
            b0 = bget(); b1 = bget()
            bb = (b0, b1)
            for half in range(2):
                for c in range(8):
                    S.op("pe", lambda e, c=c, half=half, i=i, bb=bb: e.matmul(
                        psum[bb[half]][:, :], lhsT=mix[:, c, i * 128:(i + 1) * 128], rhs=wO3[:, c, half * 512:(half + 1) * 512],
                        start=(c == 0), stop=(c == 7)), reads=[HNMIX, BIG[2 * c + half]], writes=[PB[bb[half]]])
            for half in range(2):
                evac_copy(tmpf[:, half * 512:(half + 1) * 512], psum[bb[half]][:, :], [PB[bb[half]]], [TMPF])
            S.op("act", lambda e: e.activation(out=junk[:, 0:1024], in_=tmpf[:, :], func=AF.Square, accum_out=ssy2),
                 reads=[TMPF], writes=[JUNK, SMS["ssy"]])
            rms_scale(ssy2, rsy, 1, 1.0 / D, SMS["ssy"], SMS["ssy"])
            S.op("dve", lambda e: e.scalar_tensor_tensor(out=tmpf[:, :], in0=tmpf[:, :], scalar=rsy, in1=postg[:, :],
                                                         op0=ALU.mult, op1=ALU.mult),
                 reads=[TMPF, SMS["ssy"], SMF], writes=[TMPF])
            bput(b0); bput(b1)
            S.op("pool", lambda e, i=i: e.tensor_tensor(out=xc3[:, i, :], in0=tmpf[:, :], in1=xc3[:, i, :], op=ALU.add),
                 reads=[TMPF, XC], writes=[XC])
        wr = [OUTB] if dst_x is out_d else [SCRB[b][tq]]
        S.dma(lambda e: e.dma_start(out=dst_x[b, t0:t0 + 512, :].rearrange("(i p) d -> p i d", p=128), in_=xc3),
              sl_o, reads=[XC], writes=wr)

    for u in range(len(layer_of_unit)):
        unit(u)
    S.final_wait("sp", [OUTB, XC])
    S.emit()
    S.close()
    for g in reversed(ctx):
        g.__exit__(None, None, None)
    return nc


def _t5_bucket(rel):
    n = np.maximum(rel, 0)
    max_exact = 16
    nf = np.maximum(n, 1).astype(np.float32)
    large = max_exact + (np.log(nf / max_exact) / math.log(128 / max_exact) * (32 - max_exact)).astype(np.int32)
    large = np.minimum(large, 31)
    return np.where(n < max_exact, n, large)


def _constants():
    bf = ml_dtypes.bfloat16
    cbv = np.zeros((128, NCB), np.float32)
    p = np.arange(128)
    cbv[:, CB_ID:CB_ID + 128] = np.eye(128)
    cbv[:, CB_TRI:CB_TRI + 128] = np.where(p[:, None] >= p[None, :], -8.0, 0.0)
    cbv[:, CB_ONES:CB_ONES + 128] = 1.0
    cbv[:, CB_PENS:CB_PENS + 128] = np.where(p[None, :] < p[:, None], 0.0, NEG)
    cbv[:, CB_PEND:CB_PEND + 128] = np.where(p[None, :] <= p[:, None], 0.0, NEG)
    for kb in range(16):
        cbv[:, CB_EKB + 128] = 1.0
        for jb in range(16):
            if jb > kb:
                cbv[2 * jb, CB_SEL + kb * 128:CB_SEL + (kb + 1) * 128] = -8.0
                cbv[32 + 2 * jb, CB_SEL + kb * 128:CB_SEL + (kb + 1) * 128] = -8.0
    cfv = np.zeros((128, NCF), np.float32)
    cfv[:, CF_NEGB:CF_NEGB + 128] = np.where(p[None, :] <= p[:, None], 0.0, -1e30)
    cfv[:, CF_P2A:CF_P2A + 16] = 2.0 ** -(np.arange(16) + 2.0)
    cfv[:, CF_P2B:CF_P2B + 16] = 2.0 ** -(np.arange(16) + 1.0)
    return cfv, cbv.astype(bf)


def _chunk_cols(w, cols):
    sel = w[:, cols]
    n = sel.shape[1] // 128
    a = sel.reshape(8, 128, n, 128)
    return np.ascontiguousarray(a.transpose(2, 1, 0, 3).reshape(n, 128, 1024))


def _prep_weights(pre_norm_g, post_norm_g, w_in, w_uk, w_uv, kv_norm_g, w_mem_kv, w_out, rel_bias, layers):
    o = np.cumsum([0, 384, 384, 384, 384, 384, 128, 384, 256, 32, 8, 256, 256])
    (o_sbq, o_sbk, o_sbv, o_sbg, o_dq, o_ckv, o_dg, o_iq, o_ik, o_iw, o_mq, o_mg) = o[:12]
    r = lambda a, n: list(range(a, a + n))
    fcols = (r(o_sbq, 384) + r(o_sbk, 384) + r(o_sbg, 384) + r(o_dq, 384) + r(o_dg, 384) + r(o_iq, 256)
             + r(o_ik, 32) * 4 + r(o_mq, 256) + r(o_mg, 256) + r(o_sbv, 384) + r(o_ckv, 128))
    assert len(fcols) == 26 * 128
    s_l = np.arange(128)
    wF, wO, wM, wsm = [], [], [], []
    for l in layers:
        wF.append(_chunk_cols(w_in[l], fcols))
        wO.append(np.ascontiguousarray(w_out[l].reshape(8, 128, 1024)))
        wM.append(_chunk_cols(w_mem_kv[l], list(range(512))))
        sm = np.zeros((128, NSM), np.float32)
        sm[:, SM_GCOL:SM_GCOL + 8] = pre_norm_g[l].reshape(8, 128).T
        sm[:, SM_IW:SM_IW + 64] = w_in[l][:, o_iw:o_iw + 8].reshape(8, 128, 8).transpose(1, 0, 2).reshape(128, 64)
        uk = w_uk[l]
        t = uk.reshape(128, 3, 2, 64).transpose(2, 3, 1, 0).reshape(128, 3 * 128)
        sm[:, SM_WUK:SM_WUK + 384] = t
        sm[:, SM_WUV:SM_WUV + 384] = w_uv[l].reshape(128, 384)
        sm[:, SM_KVG:SM_KVG + 128] = kv_norm_g[l][None, :]
        sm[:, SM_B31:SM_B31 + 6] = rel_bias[31][None, :]
        sm[:, SM_POSTG:SM_POSTG + 1024] = post_norm_g[l][None, :]
        for h in range(6):
            for j in range(2):
                rel = s_l[None, :] - s_l[:, None] + 128 * j
                sm[:, SM_TB + (h * 2 + j) * 128:SM_TB + (h * 2 + j + 1) * 128] = rel_bias[_t5_bucket(rel), h]
        wsm.append(sm)
    return (np.stack(wF), np.stack(wO), np.stack(wM), np.stack(wsm))


_PROG_CACHE = {}


def _get_prog(key, *args):
    if key not in _PROG_CACHE:
        _PROG_CACHE[key] = build_program(*args)
    return _PROG_CACHE[key]


FUSED = True


def kernel(x, mem, pre_norm_g, post_norm_g, w_in, w_uk, w_uv, kv_norm_g, w_mem_kv, w_out, rel_bias):
    x = np.asarray(x, np.float32)
    mem = np.asarray(mem, np.float32)
    args = [np.asarray(a, np.float32) for a in (pre_norm_g, post_norm_g, w_in, w_uk, w_uv, kv_norm_g, w_mem_kv, w_out, rel_bias)]
    B, S_TOK, _ = x.shape
    depth = w_in.shape[0]
    per = B // N_CORES
    cfv, cbv = _constants()
    if FUSED:
        units_l = []; units_b = []; chain = []
        for b in range(per):
            for l in range(depth):
                units_l.append(l); units_b.append(b); chain.append("in" if l == 0 else "scr")
        nc = _get_prog(("fused", S_TOK, per, depth), S_TOK, per, units_l, units_b, chain)
        wF, wO, wM, wsm = _prep_weights(*args, layers=list(range(depth)))
        in_maps = [{"x": np.ascontiguousarray(x[c * per:(c + 1) * per]), "mem": np.ascontiguousarray(mem[c * per:(c + 1) * per]),
                    "wF": wF, "wO": wO, "wM": wM, "wsm": wsm, "cf32": cfv, "cbf": cbv} for c in range(N_CORES)]
        res = run_bass_kernel_spmd(nc, in_maps, core_ids=list(range(N_CORES)))
        return np.concatenate([r["out"] for r in res.results], axis=0)
    cur = x
    nc = _get_prog(("unit", S_TOK), S_TOK, 1, [0], [0], ["in"])
    for l in range(depth):
        wF, wO, wM, wsm = _prep_weights(*args, layers=[l])
        nxt = np.empty_like(cur)
        for b in range(per):
            in_maps = [{"x": np.ascontiguousarray(cur[c * per + b:c * per + b + 1]),
                        "mem": np.ascontiguousarray(mem[c * per + b:c * per + b + 1]),
                        "wF": wF, "wO": wO, "wM": wM, "wsm": wsm, "cf32": cfv, "cbf": cbv} for c in range(N_CORES)]
            res = run_bass_kernel_spmd(nc, in_maps, core_ids=list(range(N_CORES)))
            for c in range(N_CORES):
                nxt[c * per + b] = res.results[c]["out"][0]
        cur = nxt
    return cur
```

```python
import math
import numpy as np
import ml_dtypes
import concourse.bass as bass
import concourse.mybir as mybir
from concourse.bass_utils import run_bass_kernel_spmd

F32 = mybir.dt.float32
BF16 = mybir.dt.bfloat16
AF = mybir.ActivationFunctionType
ALU = mybir.AluOpType
AX = mybir.AxisListType

D = 1024
NMEM = 256
TOPK = 256
NBIS = 13
EPS = 1e-6
NEG = -30000.0
N_CORES = 8
DBG_STAGE = 99
DBG_SUB = 99
SKEW = 2

SM_GCOL, SM_IW, SM_WUK, SM_WUV, SM_KVG, SM_B31, SM_POSTG, SM_TB = 0, 8, 72, 456, 840, 968, 974, 1998
NSM = 1998 + 6 * 2 * 128
CB_ID, CB_TRI, CB_ONES, CB_PENS, CB_PEND, CB_EKB, CB_SEL = 0, 128, 256, 384, 512, 640, 896
NCB = 896 + 16 * 128
CF_NEGB, CF_P2A, CF_P2B = 0, 128, 144
NCF = 160


class Buf:
    __slots__ = ("name", "w", "r", "excl")

    def __init__(self, name, excl=False):
        self.name = name
        self.w = None
        self.r = {}
        self.excl = excl


class Sched:
    ENGS = ("pe", "act", "dve", "pool", "sp")

    def __init__(self, nc):
        self.nc = nc
        self.ops = {e: [] for e in self.ENGS}
        self.sems = {}
        self.cnt = {}
        self.waited = {e: {} for e in self.ENGS}
        self._ctx = []
        for e in self.ENGS:
            self._newsem("E_" + e)

    def _newsem(self, key):
        g = self.nc.semaphore(key)
        h = g.__enter__()
        self._ctx.append(g)
        self.sems[key] = h
        self.cnt[key] = 0
        return key

    def dma_slot(self, name):
        return self._newsem("D_" + name)

    def _deps(self, e, reads, writes):
        deps = {}

        def add(ev):
            if ev is None:
                return
            k, v = ev
            if deps.get(k, 0) < v:
                deps[k] = v
        for b in reads:
            add(b.w)
        for b in writes:
            add(b.w)
            for k, v in b.r.items():
                add((k, v))
        waits = []
        mykey = "E_" + e
        for k, v in deps.items():
            if k == mykey and e in ("pe", "sp"):
                continue
            if self.waited[e].get(k, 0) >= v:
                continue
            self.waited[e][k] = v
            waits.append((k, v))
        return waits

    def _record(self, ev, reads, writes):
        k, v = ev
        for b in reads:
            if b.r.get(k, 0) < v:
                b.r[k] = v
        for b in writes:
            b.w = ev
            b.r = {}

    def op(self, e, fn, reads=(), writes=()):
        ex = [b for b in reads if b.excl]
        if ex:
            writes = list(writes) + ex
        waits = self._deps(e, reads, writes)
        k = "E_" + e
        self.cnt[k] += 1
        ev = (k, self.cnt[k])
        self.ops[e].append((waits, fn, (k, 1)))
        self._record(ev, reads, writes)
        return ev

    def dma(self, fn, slot, reads=(), writes=(), e="sp"):
        waits = self._deps(e, reads, writes)
        self.cnt[slot] += 16
        ev = (slot, self.cnt[slot])
        self.ops[e].append((waits, fn, (slot, 16)))
        self._record(ev, reads, writes)
        return ev

    def final_wait(self, e, bufs):
        waits = self._deps(e, bufs, bufs)
        self.ops[e].append((waits, None, None))

    def emit(self):
        nc = self.nc
        needed = {}
        for e in self.ENGS:
            for waits, fn, inc in self.ops[e]:
                for k, v in waits:
                    if k.startswith("E_"):
                        needed.setdefault(k, set()).add(v)
        rank = {k: {v: i + 1 for i, v in enumerate(sorted(vs))} for k, vs in needed.items()}
        with nc.Block() as block:
            def run(ename):
                def body(eng):
                    seq = 0
                    mykey = "E_" + ename
                    myrank = rank.get(mykey, {})
                    for waits, fn, inc in self.ops[ename]:
                        for k, v in waits:
                            eng.wait_ge(self.sems[k], rank[k][v] if k.startswith("E_") else v)
                        if fn is None:
                            continue
                        inst = fn(eng)
                        if inc[0] == mykey:
                            seq += 1
                            if seq in myrank:
                                inst.then_inc(self.sems[mykey], 1)
                        else:
                            inst.then_inc(self.sems[inc[0]], inc[1])
                return body
            block.tensor(run("pe"))
            block.scalar(run("act"))
            block.vector(run("dve"))
            block.gpsimd(run("pool"))
            block.sync(run("sp"))

    def close(self):
        for g in reversed(self._ctx):
            g.__exit__(None, None, None)


def build_program(S_TOK, NB, layer_of_unit, batch_of_unit, chain):
    NQ = S_TOK // 512
    NBLK = S_TOK // 128
    L = max(layer_of_unit) + 1
    nc = bass.Bass("TRN2", target_bir_lowering=False)
    x_d = nc.dram_tensor("x", [NB, S_TOK, D], F32, kind="ExternalInput").ap()
    mem_d = nc.dram_tensor("mem", [NB, NMEM, D], F32, kind="ExternalInput").ap()
    wF_d = nc.dram_tensor("wF", [L, 26, 128, 1024], F32, kind="ExternalInput").ap()
    wO_d = nc.dram_tensor("wO", [L, 8, 128, 1024], F32, kind="ExternalInput").ap()
    wM_d = nc.dram_tensor("wM", [L, 4, 128, 1024], F32, kind="ExternalInput").ap()
    wsm_d = nc.dram_tensor("wsm", [L, 128, NSM], F32, kind="ExternalInput").ap()
    cf_d = nc.dram_tensor("cf32", [128, NCF], F32, kind="ExternalInput").ap()
    cb_d = nc.dram_tensor("cbf", [128, NCB], BF16, kind="ExternalInput").ap()
    out_d = nc.dram_tensor("out", [NB, S_TOK, D], F32, kind="ExternalOutput").ap()
    need_scr = any(c == "scr" for c in chain)
    scr_d = nc.dram_tensor("xscr", [NB, S_TOK, D], F32, kind="Internal").ap() if need_scr else None

    wsc_d = nc.dram_tensor("wscr", [L, 38, 128, 1024], BF16, kind="Internal").ap()
    S = Sched(nc)
    ctx = []

    def sb(name, shape, dt):
        g = nc.sbuf_tensor(name, shape, dt)
        h = g.__enter__()
        ctx.append(g)
        return h

    psum = []
    PB = []
    for i in range(8):
        g = nc.psum_tensor(f"ps{i}", [128, 512], F32)
        psum.append(g.__enter__())
        ctx.append(g)
        PB.append(Buf(f"ps{i}", excl=True))
    free_banks = list(range(8))

    def bget():
        return free_banks.pop(0)

    def bput(i):
        free_banks.append(i)

    cb = sb("cb", [128, NCB], BF16); CBb = Buf("cb")
    cf = sb("cf", [128, NCF], F32); CFb = Buf("cf")
    ident = cb[:, CB_ID:CB_ID + 128]
    triM8 = cb[:, CB_TRI:CB_TRI + 128]
    ones = cb[:, CB_ONES:CB_ONES + 128]
    penS = cb[:, CB_PENS:CB_PENS + 128]
    penD = cb[:, CB_PEND:CB_PEND + 128]

    def ekb(kb):
        return cb[:, CB_EKB + 128 - 2 * kb:CB_EKB + 256 - 2 * kb]

    def selM8(kb):
        return cb[:, CB_SEL + kb * 128:CB_SEL + (kb + 1) * 128]
    negb = cf[:, CF_NEGB:CF_NEGB + 128]
    p2a = cf[:, CF_P2A:CF_P2A + 16]
    p2b = cf[:, CF_P2B:CF_P2B + 16]

    sbkT = sb("sbkT", [128, 3, S_TOK], BF16); SBK = [Buf(f"sbk{q}") for q in range(NQ)]
    sbv = sb("sbv", [128, NBLK, 384], BF16); SBV = [Buf(f"sbv{q}") for q in range(NQ)]
    ckv = sb("ckv", [128, NBLK, 128], BF16); CKV = [Buf(f"ckv{q}") for q in range(NQ)]
    ckvT = sb("ckvT", [128, S_TOK], BF16); CKVT = [Buf(f"ckvT{q}") for q in range(NQ)]
    ikT4 = sb("ikT4", [128, S_TOK], BF16); IKT = [Buf(f"ikT{q}") for q in range(NQ)]
    sbq = sb("sbq", [128, 6, 512], BF16); SBQ = Buf("sbq")
    sbg = sb("sbg", [128, 3, 512], BF16); SBG = Buf("sbg")
    dq = sb("dq", [128, 3, 512], BF16); DQ = Buf("dq")
    dg = sb("dg", [128, 3, 512], BF16); DG = Buf("dg")
    iq = sb("iq", [128, 2, 512], BF16); IQ = Buf("iq")
    mq = sb("mq", [128, 2, 512], BF16); MQ = Buf("mq")
    mg = sb("mg", [128, 2, 512], BF16); MG = Buf("mg")
    idxw = sb("idxw", [128, 32], F32); IDXW = Buf("idxw")
    hT = sb("hT", [128, 8, 512], BF16); HT = Buf("hT")
    hnmix = sb("hnmix", [128, 4096], BF16); HNMIX = Buf("hnmix")
    xc = sb("xc", [128, 4096], F32); XC = Buf("xc")
    NWB = 2
    wst = [sb(f"wst{i}", [128, 1024], F32) for i in range(NWB)]; WST = [Buf(f"wst{i}") for i in range(NWB)]
    NWF = 4
    wbf = [sb(f"wbf{i}", [128, 1024], BF16) for i in range(NWF)]; WBF = [Buf(f"wbf{i}") for i in range(NWF)]
    big16 = sb("big16", [128, 8192], BF16); BIG = [Buf(f"big{r}") for r in range(16)]
    gcol = sb("gcol", [128, 8], F32); kvg = sb("kvg", [128, 128], F32); b31 = sb("b31", [128, 6], F32)
    postg = sb("postg", [128, 1024], F32)
    SMF = Buf("smallf32")
    wiw = sb("wiw", [128, 64], BF16); wuk = sb("wuk", [128, 384], BF16); wuv = sb("wuv", [128, 384], BF16)
    tb8 = sb("tb8", [128, 1536], BF16)
    SMB = Buf("smallbf")
    memT = sb("memT", [128, 8, 256], BF16); MEMT = Buf("memT")
    memk = sb("memk", [128, 2, 256], BF16); MEMK = Buf("memk")
    memv = sb("memv", [128, 2, 256], BF16); MEMV = Buf("memv")
    membf = sb("membf", [128, 2, 1024], BF16); MEMBF = Buf("membf")
    NT = 3
    etile = [sb(f"et{i}", [128, 512], BF16) for i in range(NT)]; ET = [Buf(f"et{i}") for i in range(NT)]
    atile = [sb(f"at{i}", [128, 512], BF16) for i in range(NT)]; AT = [Buf(f"at{i}") for i in range(NT)]
    cs48 = sb("cs48", [128, 512], BF16); cslo = sb("cslo", [32, 512], BF16); CS = Buf("cs")
    cshi = cs48[0:16, :]
    score = sb("score", [128, S_TOK], F32); SCORE = Buf("score")
    junk = sb("junk", [128, S_TOK], BF16); JUNK = Buf("junk")
    pen = sb("pen", [128, 4, S_TOK], BF16); PEN = [Buf(f"pen{i}") for i in range(4)]
    NR = 8
    rt = [sb(f"rt{i}", [128, 512], BF16) for i in range(NR)]; RT = [Buf(f"rt{i}") for i in range(NR)]
    dgt = [sb(f"dgt{i}", [128, 8, 128], BF16) for i in range(2)]; DGT = [Buf(f"dgt{i}") for i in range(2)]
    ptile = [sb(f"pt{i}", [128, 512], BF16) for i in range(NT)]; PT = [Buf(f"pt{i}") for i in range(NT)]
    qlat = [sb(f"ql{i}", [128, 512], BF16) for i in range(2)]; QL = [Buf(f"ql{i}") for i in range(2)]
    recf = sb("recf", [128, 512], F32); RECF = Buf("recf")
    onb = sb("onb", [128, 512], BF16); ONB = Buf("onb")
    tmpf = sb("tmpf", [128, 1024], F32); TMPF = Buf("tmpf")
    sm = sb("smalls", [128, 64], F32)
    SMS = {n: Buf("sm_" + n) for n in ("ss", "rs", "ssk", "rsk", "bis", "ssy")}
    steps = sb("steps", [128, 32], F32); STEPS = Buf("steps")
    ss = sm[:, 0:4]; rs = sm[:, 4:8]; ssk = sm[:, 8:12]; rsk = sm[:, 12:16]
    mx = sm[:, 16:17]; mn = sm[:, 17:18]; thr = sm[:, 18:19]; rng = sm[:, 19:20]; cnt = sm[:, 20:21]; dd = sm[:, 21:22]
    ssy = sm[:, 24:26]; ssy2 = sm[:, 26:27]; rsy = sm[:, 27:28]

    sl_c = S.dma_slot("const"); sl_c2 = S.dma_slot("const2"); sl_x = S.dma_slot("x"); sl_o = S.dma_slot("o"); sl_m = S.dma_slot("mem")
    sl_s = S.dma_slot("small"); sl_w = [S.dma_slot(f"w{i}") for i in range(NWB)]
    sl_wb = [S.dma_slot(f"wb{i}") for i in range(NWF)]; sl_wo = [S.dma_slot(f"wo{i}") for i in range(NWB)]
    sl_big = [S.dma_slot(f"big{i}") for i in range(8)]
    sl_co = [S.dma_slot(f"co{i}") for i in range(NWF)]
    WSC = [[Buf(f"wsc{l_}_{i}") for i in range(38)] for l_ in range(L)]
    OUTB = Buf("outdram")
    SCRB = {}

    cnt_rr = {"wf": 0, "w": 0, "e": 0, "a": 0, "r": 0, "p": 0, "q": 0, "d": 0, "ev": 0}

    def rr(key, n):
        v = cnt_rr[key] % n
        cnt_rr[key] += 1
        return v

    S.op("dve", lambda e: e.memset(cs48[:, :], 0.0), writes=[CS])
    S.op("dve", lambda e: e.memset(sbq[:].rearrange("p h t -> p (h t)"), 0.0), writes=[SBQ])
    S.dma(lambda e: e.dma_start(out=cb[:], in_=cb_d[:, :]), sl_c, writes=[CBb])
    S.dma(lambda e: e.dma_start(out=cf[:], in_=cf_d[:, :]), sl_c2, writes=[CFb])

    conv_order = [26, 27, 28, 29] + list(range(26)) + list(range(30, 38))
    for l_ in range(L):
        pendc = []
        for idx in conv_order:
            src = wF_d[l_, idx, :, :] if idx < 26 else (wM_d[l_, idx - 26, :, :] if idx < 30 else wO_d[l_, idx - 30, :, :])
            k = rr("w", NWB)
            S.dma(lambda e, k=k, src=src: e.dma_start(out=wst[k][:], in_=src), sl_w[k], writes=[WST[k]], e="pool")

            def tailc(k=k, l_=l_, idx=idx):
                kf = rr("wf", NWF)
                S.op("pool", lambda e: e.tensor_copy(out=wbf[kf][:], in_=wst[k][:]), reads=[WST[k]], writes=[WBF[kf]])
                S.dma(lambda e: e.dma_start(out=wsc_d[l_, idx, :, :], in_=wbf[kf][:]), sl_co[kf],
                      reads=[WBF[kf]], writes=[WSC[l_][idx]], e="pool")
            pendc.append(tailc)
            if len(pendc) > 1:
                pendc.pop(0)()
        while pendc:
            pendc.pop(0)()

    def wchunk(l_, idx):
        kf = rr("wf", NWF)
        S.dma(lambda e: e.dma_start(out=wbf[kf][:], in_=wsc_d[l_, idx, :, :]), sl_wb[kf], reads=[WSC[l_][idx]], writes=[WBF[kf]])
        return wbf[kf], WBF[kf]

    LA = 3

    def wstream(l_, idxs):
        pend_ = []
        it = iter(idxs)
        for _ in range(LA):
            nx = next(it, None)
            if nx is not None:
                pend_.append(wchunk(l_, nx))
        while pend_:
            cur = pend_.pop(0)
            nx = next(it, None)
            if nx is not None:
                pend_.append(wchunk(l_, nx))
            yield cur

    def evac_copy(out_ap, in_ap, reads, writes):
        if rr("ev", 2) == 0:
            S.op("act", lambda e: e.activation(out=out_ap, in_=in_ap, func=AF.Copy), reads=reads, writes=writes)
        else:
            S.op("dve", lambda e: e.tensor_copy(out=out_ap, in_=in_ap), reads=reads, writes=writes)

    def rms_scale(src_ss, dst_rs, n, inv_n, B_ss, B_rs):
        S.op("act", lambda e: e.activation(out=dst_rs, in_=src_ss, func=AF.Sqrt, scale=inv_n, bias=EPS),
             reads=[B_ss], writes=[B_rs])
        S.op("dve", lambda e: e.reciprocal(out=dst_rs, in_=dst_rs), reads=[B_rs], writes=[B_rs])

    def unit(u):
        l = layer_of_unit[u]
        b = batch_of_unit[u]
        src_x = x_d if chain[u] == "in" else scr_d
        later = any(batch_of_unit[v] == b for v in range(u + 1, len(layer_of_unit)))
        dst_x = scr_d if later else out_d
        if later and b not in SCRB:
            SCRB[b] = [Buf(f"scr{b}_{q}") for q in range(NQ)]

        S.dma(lambda e: e.dma_start(out=xc[:, 0:NSM], in_=wsm_d[l, :, :]), sl_s, writes=[XC])
        for (dst, off, n) in ((gcol, SM_GCOL, 8), (kvg, SM_KVG, 128), (b31, SM_B31, 6), (postg, SM_POSTG, 1024)):
            S.op("dve", lambda e, dst=dst, off=off, n=n: e.tensor_copy(out=dst[:, 0:n], in_=xc[:, off:off + n]),
                 reads=[XC], writes=[SMF])
        for (dst, off, n) in ((wiw, SM_IW, 64), (wuk, SM_WUK, 384), (wuv, SM_WUV, 384)):
            S.op("dve", lambda e, dst=dst, off=off, n=n: e.tensor_copy(out=dst[:, 0:n], in_=xc[:, off:off + n]),
                 reads=[XC], writes=[SMB])
        S.op("dve", lambda e: e.tensor_scalar(out=tb8[:, :], in0=xc[:, SM_TB:SM_TB + 1536], scalar1=8.0, scalar2=None,
                                              op0=ALU.mult), reads=[XC], writes=[SMB])
        S.dma(lambda e: e.dma_start(out=xc[:, 0:2048].rearrange("p (j d) -> p j d", j=2),
                                    in_=mem_d[b, :, :].rearrange("(j p) d -> p j d", p=128)), sl_m, writes=[XC])
        S.op("dve", lambda e: e.tensor_copy(out=membf[:].rearrange("p j d -> p (j d)"), in_=xc[:, 0:2048]),
             reads=[XC], writes=[MEMBF])
        for c2 in range(4):
            bk = bget()
            pb = psum[bk][:].bitcast(BF16)
            for cc in range(2):
                c = 2 * c2 + cc
                for j in range(2):
                    S.op("pe", lambda e, c=c, j=j, cc=cc, pb=pb: e.transpose(
                        pb[:, cc * 256 + j * 128:cc * 256 + (j + 1) * 128], membf[:, j, c * 128:(c + 1) * 128], ident),
                        reads=[MEMBF, CBb], writes=[PB[bk]])
            evac_copy(memT[:, 2 * c2:2 * c2 + 2, :], pb[:, 0:512].rearrange("p (c m) -> p c m", c=2), [PB[bk]], [MEMT])
            bput(bk)
        for g4, (w, W) in enumerate(wstream(l, [26, 27, 28, 29])):
            w3 = w[:].rearrange("p (c g) -> p c g", c=8)
            bk = bget()
            if g4 < 2:
                for c in range(8):
                    S.op("pe", lambda e, c=c, w3=w3, bk=bk: e.matmul(psum[bk][:, 0:256], lhsT=w3[:, c, :], rhs=memT[:, c, :],
                                                                    start=(c == 0), stop=(c == 7)),
                         reads=[W, MEMT], writes=[PB[bk]])
                evac_copy(memk[:, g4, :], psum[bk][:, 0:256], [PB[bk]], [MEMK])
            else:
                for j in range(2):
                    for c in range(8):
                        S.op("pe", lambda e, c=c, j=j, w3=w3, bk=bk: e.matmul(
                            psum[bk][:, j * 128:(j + 1) * 128], lhsT=memT[:, c, j * 128:(j + 1) * 128], rhs=w3[:, c, :],
                            start=(c == 0), stop=(c == 7)), reads=[W, MEMT], writes=[PB[bk]])
                evac_copy(memv[:, :, (g4 - 2) * 128:(g4 - 1) * 128], psum[bk][:, 0:256].rearrange("p (j g) -> p j g", j=2),
                          [PB[bk]], [MEMV])
            bput(bk)

        for tq in range(NQ):
            chunk_phase(u, l, b, tq, src_x, dst_x)

    def chunk_phase(u, l, b, tq, src_x, dst_x):
        t0 = tq * 512
        hn = hnmix[:].rearrange("p (i d) -> p i d", i=4)
        mix = hnmix[:].rearrange("p (c t) -> p c t", c=8)
        xc3 = xc[:].rearrange("p (i d) -> p i d", i=4)
        rd = [XC]
        if src_x is scr_d:
            rd = [XC] + [SCRB[b][tq]]
        S.dma(lambda e: e.dma_start(out=xc3, in_=src_x[b, t0:t0 + 512, :].rearrange("(i p) d -> p i d", p=128)),
              sl_x, reads=rd[1:], writes=[XC])
        for i in range(4):
            S.op("act", lambda e, i=i: e.activation(out=junk[:, 0:1024], in_=xc3[:, i, :], func=AF.Square,
                                                    accum_out=ss[:, i:i + 1]), reads=[XC], writes=[JUNK, SMS["ss"]])
        rms_scale(ss, rs, 4, 1.0 / D, SMS["ss"], SMS["rs"])
        for i in range(4):
            S.op("dve", lambda e, i=i: e.tensor_scalar(out=hn[:, i, :], in0=xc3[:, i, :], scalar1=rs[:, i:i + 1],
                                                       scalar2=None, op0=ALU.mult),
                 reads=[XC, SMS["rs"]], writes=[HNMIX])
        for c2 in range(4):
            bk = bget()
            pb = psum[bk][:].bitcast(BF16)
            for cc in range(2):
                c = 2 * c2 + cc
                for i in range(4):
                    S.op("pe", lambda e, c=c, i=i, cc=cc, pb=pb: e.transpose(
                        pb[:, cc * 512 + i * 128:cc * 512 + (i + 1) * 128], hn[:, i, c * 128:(c + 1) * 128], ident),
                        reads=[HNMIX, CBb], writes=[PB[bk]])
            for cc in range(2):
                c = 2 * c2 + cc
                S.op("dve", lambda e, c=c, cc=cc, pb=pb: e.tensor_scalar(
                    out=hT[:, c, :], in0=pb[:, cc * 512:(cc + 1) * 512], scalar1=gcol[:, c:c + 1], scalar2=None,
                    op0=ALU.mult), reads=[PB[bk], SMF], writes=[HT])
            bput(bk)

        if DBG_STAGE < 2:
            return
        fdest = ([("sbq", i) for i in range(3)] + [("sbk", i) for i in range(3)] + [("sbg", i) for i in range(3)]
                 + [("dq", i) for i in range(3)] + [("dg", i) for i in range(3)] + [("iq", 0), ("iq", 1), ("ik", 0)]
                 + [("mq", 0), ("mq", 1), ("mg", 0), ("mg", 1)])
        dst_tab = {"sbq": (sbq, SBQ), "sbg": (sbg, SBG), "dq": (dq, DQ), "dg": (dg, DG), "iq": (iq, IQ),
                   "mq": (mq, MQ), "mg": (mg, MG)}
        ptb = None
        for cc, (w, W) in enumerate(wstream(l, list(range(26)))):
            w3 = w[:].rearrange("p (c g) -> p c g", c=8)
            if cc < 22:
                name, ci = fdest[cc]
                bk = bget()
                for c in range(8):
                    S.op("pe", lambda e, c=c, w3=w3, bk=bk: e.matmul(psum[bk][:, :], lhsT=w3[:, c, :], rhs=hT[:, c, :],
                                                                    start=(c == 0), stop=(c == 7)),
                         reads=[W, HT], writes=[PB[bk]])
                if name == "sbk":
                    evac_copy(sbkT[:, ci, t0:t0 + 512], psum[bk][:, :], [PB[bk]], [SBK[tq]])
                elif name == "ik":
                    evac_copy(ikT4[:, t0:t0 + 512], psum[bk][:, :], [PB[bk]], [IKT[tq]])
                elif name in ("sbg", "dg", "mg") and DBG_SUB >= 2:
                    dt_, DB = dst_tab[name]
                    hs = rr("ev", 2) * 512
                    S.op("act", lambda e, bk=bk, hs=hs: e.activation(out=tmpf[:, hs:hs + 512], in_=psum[bk][:, :], func=AF.Exp,
                                                                     scale=-1.0), reads=[PB[bk]], writes=[TMPF])
                    S.op("act", lambda e, hs=hs: e.activation(out=tmpf[:, hs:hs + 512], in_=tmpf[:, hs:hs + 512], func=AF.Ln, bias=1.0),
                         reads=[TMPF], writes=[TMPF])
                    S.op("act", lambda e, hs=hs: e.activation(out=tmpf[:, hs:hs + 512], in_=tmpf[:, hs:hs + 512], func=AF.Exp, scale=-1.0),
                         reads=[TMPF], writes=[TMPF])
                    S.op("dve", lambda e, dt_=dt_, ci=ci, bk=bk, hs=hs: e.tensor_tensor(
                        out=dt_[:, ci, :], in0=psum[bk][:, :], in1=tmpf[:, hs:hs + 512], op=ALU.mult),
                        reads=[PB[bk], TMPF], writes=[DB])
                elif name == "sbq":
                    evac_copy(sbq[0:64, 2 * ci, :], psum[bk][0:64, :], [PB[bk]], [SBQ])
                    evac_copy(sbq[64:128, 2 * ci + 1, :], psum[bk][64:128, :], [PB[bk]], [SBQ])
                else:
                    dt_, DB = dst_tab[name]
                    evac_copy(dt_[:, ci, :], psum[bk][:, :], [PB[bk]], [DB])
                bput(bk)
            elif DBG_SUB >= 30:
                g4 = cc - 22
                if g4 == 0:
                    ptb = [bget() for _ in range(4)]
                for i in range(4):
                    for c in range(8):
                        S.op("pe", lambda e, c=c, i=i, w3=w3, g4=g4: e.matmul(
                            psum[ptb[i]][:, g4 * 128:(g4 + 1) * 128], lhsT=hT[:, c, i * 128:(i + 1) * 128], rhs=w3[:, c, :],
                            start=(c == 0), stop=(c == 7)), reads=[W, HT], writes=[PB[ptb[i]]])
        if DBG_SUB < 30:
            return
        for i in range(4):
            j = 4 * tq + i
            evac_copy(sbv[:, j, :], psum[ptb[i]][:, 0:384], [PB[ptb[i]]], [SBV[tq]])
            evac_copy(tmpf[:, i * 128:(i + 1) * 128], psum[ptb[i]][:, 384:512], [PB[ptb[i]]], [TMPF])
        for i in range(4):
            S.op("act", lambda e, i=i: e.activation(out=junk[:, 0:128], in_=tmpf[:, i * 128:(i + 1) * 128], func=AF.Square,
                                                    accum_out=ssk[:, i:i + 1]),
                 reads=[TMPF], writes=[JUNK, SMS["ssk"]])
        rms_scale(ssk, rsk, 4, 1.0 / 128, SMS["ssk"], SMS["rsk"])
        for i in range(4):
            j = 4 * tq + i
            S.op("dve", lambda e, i=i, j=j: e.scalar_tensor_tensor(out=ckv[:, j, :], in0=tmpf[:, i * 128:(i + 1) * 128],
                                                                   scalar=rsk[:, i:i + 1], in1=kvg[:, :],
                                                                   op0=ALU.mult, op1=ALU.mult),
                 reads=[TMPF, SMS["rsk"], SMF], writes=[CKV[tq]])
        for i in range(4):
            bput(ptb[i])
        if DBG_SUB < 40:
            return
        bk = bget()
        pb = psum[bk][:].bitcast(BF16)
        for i in range(4):
            j = 4 * tq + i
            S.op("pe", lambda e, i=i, j=j, pb=pb: e.transpose(pb[:, i * 128:(i + 1) * 128], ckv[:, j, :], ident),
                 reads=[CKV[tq], CBb], writes=[PB[bk]])
        evac_copy(ckvT[:, t0:t0 + 512], pb[:, 0:512], [PB[bk]], [CKVT[tq]])
        bput(bk)
        if DBG_SUB < 50:
            return
        bk = bget()
        wiw3 = wiw[:].rearrange("p (c g) -> p c g", c=8)
        for i in range(4):
            for c in range(8):
                S.op("pe", lambda e, c=c, i=i, bk=bk: e.matmul(psum[bk][:, i * 8:(i + 1) * 8],
                                                                lhsT=hT[:, c, i * 128:(i + 1) * 128], rhs=wiw3[:, c, :],
                                                                start=(c == 0), stop=(c == 7)),
                     reads=[SMB, HT], writes=[PB[bk]])
        S.op("dve", lambda e, bk=bk: e.tensor_scalar(out=idxw[:, :], in0=psum[bk][:, 0:32], scalar1=1.0 / 16, scalar2=None,
                                                     op0=ALU.mult), reads=[PB[bk]], writes=[IDXW])
        bput(bk)

        if DBG_STAGE < 3:
            return
        nkb = 4 * tq + 4
        KS = lambda lst: [lst[q] for q in range(tq + 1)]

        def indexer(i):
            qb = 4 * tq + i
            if qb < 2:
                return
            yield
            nk = (qb + 1) * 128
            nkc = (nk + 511) // 512
            kd = rr("d", 2)
            for h in range(8):
                S.op("act", lambda e, h=h, kd=kd: e.activation(out=dgt[kd][:, h, :], in_=ident, func=AF.Copy,
                                                               scale=idxw[:, i * 8 + h:i * 8 + h + 1]),
                     reads=[CBb, IDXW], writes=[DGT[kd]])
            for kc in range(nkc):
                w_ = min(512, nk - kc * 512)
                bs = bget()
                pend = []
                for g in range(2):
                    bds = [bget() for _ in range(4)]
                    for j in range(4):
                        hp = j * 32
                        tp = (96, 0) if hp == 96 else None
                        S.op("pe", lambda e, g=g, hp=hp, tp=tp, bd=bds[j], kc=kc, w_=w_: e.matmul(
                            psum[bd][:, 0:w_], lhsT=iq[hp:hp + 32, g, i * 128:(i + 1) * 128],
                            rhs=ikT4[hp:hp + 32, kc * 512:kc * 512 + w_], start=True, stop=True, tile_position=tp),
                            reads=[IQ] + KS(IKT), writes=[PB[bds[j]]])
                    for j in range(4):
                        h = 4 * g + j
                        kr = rr("r", NR)
                        S.op("dve", lambda e, kr=kr, bd=bds[j], w_=w_: e.tensor_scalar(out=rt[kr][:, 0:w_], in0=psum[bd][:, 0:w_],
                                                                                      scalar1=0.0, scalar2=None, op0=ALU.max),
                             reads=[PB[bds[j]]], writes=[RT[kr]])
                        bput(bds[j])
                        pend.append(lambda h=h, kr=kr, bs=bs, w_=w_: S.op("pe", lambda e: e.matmul(
                            psum[bs][:, 0:w_], lhsT=dgt[kd][:, h, :], rhs=rt[kr][:, 0:w_], start=(h == 0), stop=(h == 7)),
                            reads=[DGT[kd], RT[kr]], writes=[PB[bs]]))
                    while len(pend) > 4:
                        pend.pop(0)()
                    yield
                while pend:
                    pend.pop(0)()
                last = (kc == nkc - 1)
                wc = w_ - 128 if last else w_
                if wc > 0:
                    S.op("act", lambda e, bs=bs, kc=kc, wc=wc: e.activation(out=score[:, kc * 512:kc * 512 + wc],
                                                                            in_=psum[bs][:, 0:wc], func=AF.Copy),
                         reads=[PB[bs]], writes=[SCORE])
                if last:
                    S.op("dve", lambda e, bs=bs, w_=w_: e.tensor_tensor(out=score[:, nk - 128:nk], in0=psum[bs][:, w_ - 128:w_],
                                                                       in1=negb, op=ALU.add),
                         reads=[PB[bs], CFb], writes=[SCORE])
                bput(bs)
            B = SMS["bis"]
            S.op("dve", lambda e: e.tensor_reduce(out=mx, in_=score[:, 0:nk], axis=AX.X, op=ALU.max), reads=[SCORE], writes=[B])
            S.op("dve", lambda e: e.tensor_reduce(out=mn, in_=score[:, 0:nk - 128], axis=AX.X, op=ALU.min),
                 reads=[SCORE], writes=[B])
            S.op("dve", lambda e: e.tensor_tensor(out=rng, in0=mx, in1=mn, op=ALU.subtract), reads=[B], writes=[B])
            S.op("dve", lambda e: e.tensor_tensor(out=thr, in0=mx, in1=mn, op=ALU.add), reads=[B], writes=[B])
            S.op("dve", lambda e: e.tensor_scalar(out=thr, in0=thr, scalar1=0.5, scalar2=None, op0=ALU.mult), reads=[B], writes=[B])
            S.op("dve", lambda e: e.tensor_scalar(out=steps[:, 0:16], in0=p2a, scalar1=rng, scalar2=None, op0=ALU.mult),
                 reads=[B, CFb], writes=[STEPS])
            S.op("dve", lambda e: e.tensor_scalar(out=steps[:, 16:32], in0=p2b, scalar1=rng, scalar2=None, op0=ALU.mult),
                 reads=[B, CFb], writes=[STEPS])
            for it in range(NBIS):
                S.op("dve", lambda e: e.tensor_scalar(out=junk[:, 0:nk], in0=score[:, 0:nk], scalar1=thr, scalar2=0.0,
                                                      op0=ALU.is_ge, op1=ALU.add, accum_out=cnt),
                     reads=[SCORE, B], writes=[JUNK, B])
                S.op("dve", lambda e, it=it: e.tensor_scalar(out=dd, in0=cnt, scalar1=float(TOPK), scalar2=steps[:, 16 + it:17 + it],
                                                             op0=ALU.is_ge, op1=ALU.mult), reads=[B, STEPS], writes=[B])
                S.op("dve", lambda e, it=it: e.scalar_tensor_tensor(out=thr, in0=dd, scalar=steps[:, it:it + 1], in1=thr,
                                                                    op0=ALU.subtract, op1=ALU.add),
                     reads=[B, STEPS], writes=[B])
                yield
                yield
            S.op("dve", lambda e: e.tensor_scalar(out=pen[:, i, 0:nk], in0=score[:, 0:nk], scalar1=thr, scalar2=NEG,
                                                  op0=ALU.is_lt, op1=ALU.mult), reads=[SCORE, B], writes=[PEN[i]])

        def sb_head(h):
            ch, hp = h // 2, (h % 2) * 64
            bcs = bget()
            pend = []
            pendL = []
            for kb in range(nkb):
                c0 = max(0, kb - 4 * tq) * 128
                diag = kb >= 4 * tq
                bz = bget()
                S.op("pe", lambda e, kb=kb, c0=c0, bz=bz, diag=diag: e.matmul(
                    psum[bz][:, c0:512], lhsT=sbkT[:, ch, kb * 128:(kb + 1) * 128], rhs=sbq[:, h, c0:512],
                    start=True, stop=not diag), reads=KS(SBK) + [SBQ], writes=[PB[bz]])
                if diag:
                    S.op("pe", lambda e, c0=c0, bz=bz: e.matmul(psum[bz][:, c0:c0 + 128], lhsT=penS, rhs=ident,
                                                                 start=False, stop=True), reads=[CBb], writes=[PB[bz]])
                ke = rr("e", NT)
                S.op("act", lambda e, ke=ke, c0=c0, bz=bz: e.activation(out=etile[ke][:, c0:512], in_=psum[bz][:, c0:512],
                                                                        func=AF.Exp, scale=0.125),
                     reads=[PB[bz]], writes=[ET[ke]])
                bput(bz)
                sp = big16[:, kb * 512:(kb + 1) * 512]

                def ln_stage(kb=kb, c0=c0, ke=ke, sp=sp):
                    S.op("act", lambda e: e.activation(out=sp[:, c0:512], in_=etile[ke][:, c0:512], func=AF.Ln, bias=1.0),
                         reads=[ET[ke]], writes=[BIG[kb]])
                    pend.append(lambda: S.op("pe", lambda e: e.matmul(
                        psum[bcs][:, c0:512], lhsT=ekb(kb), rhs=sp[:, c0:512], start=(kb == 0), stop=(kb == nkb - 1)),
                        reads=[CBb, BIG[kb]], writes=[PB[bcs]]))
                pendL.append(ln_stage)
                if len(pendL) > 1:
                    pendL.pop(0)()
                if len(pend) > SKEW:
                    pend.pop(0)()
                yield
            while pendL:
                pendL.pop(0)()
            while pend:
                pend.pop(0)()
            S.op("dve", lambda e: e.tensor_copy(out=cs48[0:32, :], in_=psum[bcs][0:32, :]), reads=[PB[bcs]], writes=[CS])
            S.op("dve", lambda e: e.tensor_tensor(out=cslo[:, :], in0=psum[bcs][0:32, :], in1=cs48[0:32, :], op=ALU.subtract),
                 reads=[PB[bcs], CS], writes=[CS])
            S.op("dve", lambda e: e.tensor_copy(out=cs48[32:64, :], in_=cslo[:, :]), reads=[CS], writes=[CS])
            bput(bcs)
            bo = bget()
            pend = []
            for kb in range(nkb):
                c0 = max(0, kb - 4 * tq) * 128
                diag = kb >= 4 * tq
                bi = bget()
                sp = big16[:, kb * 512:(kb + 1) * 512]
                S.op("pe", lambda e, kb=kb, c0=c0, bi=bi: e.matmul(
                    psum[bi][:, c0:512], lhsT=sbkT[:, ch, kb * 128:(kb + 1) * 128], rhs=sbq[:, h, c0:512],
                    start=True, stop=False), reads=KS(SBK) + [SBQ], writes=[PB[bi]])
                S.op("pe", lambda e, c0=c0, bi=bi, sp=sp: e.matmul(psum[bi][:, c0:512], lhsT=triM8, rhs=sp[:, c0:512],
                                                                   start=False, stop=False),
                     reads=[CBb, BIG[kb]], writes=[PB[bi]])
                S.op("pe", lambda e, kb=kb, c0=c0, bi=bi, diag=diag: e.matmul(psum[bi][:, c0:512], lhsT=selM8(kb),
                                                                              rhs=cs48[:, c0:512], start=False, stop=not diag),
                     reads=[CBb, CS], writes=[PB[bi]])
                if diag:
                    S.op("pe", lambda e, c0=c0, bi=bi: e.matmul(psum[bi][:, c0:c0 + 128], lhsT=penS, rhs=ident,
                                                                 start=False, stop=True), reads=[CBb], writes=[PB[bi]])
                ka = rr("a", NT)
                S.op("act", lambda e, ka=ka, c0=c0, bi=bi: e.activation(out=atile[ka][:, c0:512], in_=psum[bi][:, c0:512],
                                                                        func=AF.Exp, scale=0.125),
                     reads=[PB[bi]], writes=[AT[ka]])
                bput(bi)
                pend.append(lambda kb=kb, c0=c0, ka=ka: S.op("pe", lambda e: e.matmul(
                    psum[bo][:, c0:512], lhsT=sbv[:, kb, ch * 128:(ch + 1) * 128], rhs=atile[ka][:, c0:512],
                    start=(kb == 0), stop=(kb == nkb - 1)), reads=KS(SBV) + [AT[ka]], writes=[PB[bo]]))
                if len(pend) > SKEW:
                    pend.pop(0)()
                yield
            while pend:
                pend.pop(0)()
            S.op("dve", lambda e: e.tensor_tensor(out=mix[hp:hp + 64, ch, :], in0=psum[bo][hp:hp + 64, :],
                                                  in1=sbg[hp:hp + 64, ch, :], op=ALU.mult),
                 reads=[PB[bo], SBG], writes=[HNMIX])
            bput(bo)

        def mem_head(hm):
            ch, hp = hm // 2, (hm % 2) * 64
            bo = bget(); bd = bget()
            for mb in range(2):
                bl = bget()
                S.op("pe", lambda e, mb=mb, bl=bl: e.matmul(psum[bl][:, :], lhsT=memk[hp:hp + 64, ch, mb * 128:(mb + 1) * 128],
                                                            rhs=mq[hp:hp + 64, ch, :], start=True, stop=True),
                     reads=[MEMK, MQ], writes=[PB[bl]])
                kp = rr("p", NT)
                S.op("act", lambda e, kp=kp, bl=bl: e.activation(out=ptile[kp][:, :], in_=psum[bl][:, :], func=AF.Exp, scale=0.125),
                     reads=[PB[bl]], writes=[PT[kp]])
                bput(bl)
                S.op("pe", lambda e, mb=mb, kp=kp: e.matmul(psum[bo][:, :], lhsT=memv[:, mb, ch * 128:(ch + 1) * 128],
                                                            rhs=ptile[kp][:, :], start=(mb == 0), stop=(mb == 1)),
                     reads=[MEMV, PT[kp]], writes=[PB[bo]])
                S.op("pe", lambda e, mb=mb, kp=kp: e.matmul(psum[bd][:, :], lhsT=ones, rhs=ptile[kp][:, :],
                                                            start=(mb == 0), stop=(mb == 1)),
                     reads=[CBb, PT[kp]], writes=[PB[bd]])
            S.op("act", lambda e: e.activation(out=recf[hp:hp + 64, :], in_=psum[bd][hp:hp + 64, :], func=AF.Ln), reads=[PB[bd]], writes=[RECF])
            S.op("act", lambda e: e.activation(out=recf[hp:hp + 64, :], in_=recf[hp:hp + 64, :], func=AF.Exp, scale=-1.0), reads=[RECF], writes=[RECF])
            S.op("dve", lambda e: e.tensor_tensor(out=tmpf[hp:hp + 64, 0:512], in0=psum[bo][hp:hp + 64, :],
                                                  in1=recf[hp:hp + 64, :], op=ALU.mult), reads=[PB[bo], RECF], writes=[TMPF])
            S.op("dve", lambda e: e.tensor_tensor(out=mix[hp:hp + 64, 6 + ch, :], in0=tmpf[hp:hp + 64, 0:512],
                                                  in1=mg[hp:hp + 64, ch, :], op=ALU.mult), reads=[TMPF, MG], writes=[HNMIX])
            bput(bo); bput(bd)

        def dsa_head(h):
            ch, hp = h // 2, (h % 2) * 64
            bq = bget()
            S.op("pe", lambda e: e.matmul(psum[bq][:, :], lhsT=wuk[hp:hp + 64, ch * 128:(ch + 1) * 128], rhs=dq[hp:hp + 64, ch, :],
                                          start=True, stop=True), reads=[SMB, DQ], writes=[PB[bq]])
            kq = rr("q", 2)
            evac_copy(qlat[kq][:, :], psum[bq][:, :], [PB[bq]], [QL[kq]])
            bput(bq)
            bo = bget(); bd = bget()
            pend = []
            for kb in range(nkb):
                i0 = max(0, kb - 4 * tq)
                c0 = i0 * 128
                bl = bget()
                mm = []
                mm.append((lambda e, st, sp_, kb=kb, c0=c0, bl=bl: e.matmul(
                    psum[bl][:, c0:512], lhsT=ckvT[:, kb * 128:(kb + 1) * 128], rhs=qlat[kq][:, c0:512], start=st, stop=sp_),
                    KS(CKVT) + [QL[kq]]))
                for i in range(i0, 4):
                    qb = 4 * tq + i
                    cs_ = slice(i * 128, (i + 1) * 128)
                    if qb >= 2:
                        mm.append((lambda e, st, sp_, i=i, kb=kb, cs_=cs_, bl=bl: e.matmul(
                            psum[bl][:, cs_], lhsT=pen[:, i, kb * 128:(kb + 1) * 128], rhs=ident, start=st, stop=sp_),
                            [PEN[i], CBb]))
                    elif qb == kb:
                        mm.append((lambda e, st, sp_, cs_=cs_, bl=bl: e.matmul(psum[bl][:, cs_], lhsT=penD, rhs=ident,
                                                                              start=st, stop=sp_), [CBb]))
                    if qb - kb <= 1:
                        jj = qb - kb
                        mm.append((lambda e, st, sp_, cs_=cs_, jj=jj, bl=bl: e.matmul(
                            psum[bl][:, cs_], lhsT=ident, rhs=tb8[:, (h * 2 + jj) * 128:(h * 2 + jj + 1) * 128],
                            start=st, stop=sp_), [CBb, SMB]))
                for n_, (fn, rds) in enumerate(mm):
                    S.op("pe", lambda e, fn=fn, n_=n_, nm=len(mm): fn(e, n_ == 0, n_ == nm - 1), reads=rds, writes=[PB[bl]])
                kp = rr("p", NT)
                near_hi = min(512, max(c0, (kb + 2 - 4 * tq) * 128))
                if near_hi > c0:
                    S.op("act", lambda e, kp=kp, c0=c0, near_hi=near_hi, bl=bl: e.activation(
                        out=ptile[kp][:, c0:near_hi], in_=psum[bl][:, c0:near_hi], func=AF.Exp, scale=0.125),
                        reads=[PB[bl]], writes=[PT[kp]])
                if near_hi < 512:
                    S.op("act", lambda e, kp=kp, near_hi=near_hi, bl=bl: e.activation(
                        out=ptile[kp][:, near_hi:512], in_=psum[bl][:, near_hi:512], func=AF.Exp, scale=0.125,
                        bias=b31[:, h:h + 1]), reads=[PB[bl], SMF], writes=[PT[kp]])
                bput(bl)
                def tail(kb=kb, c0=c0, kp=kp):
                    S.op("pe", lambda e: e.matmul(psum[bo][:, c0:512], lhsT=ckv[:, kb, :], rhs=ptile[kp][:, c0:512],
                                                  start=(kb == 0), stop=(kb == nkb - 1)),
                         reads=KS(CKV) + [PT[kp]], writes=[PB[bo]])
                    S.op("pe", lambda e: e.matmul(psum[bd][:, c0:512], lhsT=ones, rhs=ptile[kp][:, c0:512],
                                                  start=(kb == 0), stop=(kb == nkb - 1)),
                         reads=[CBb, PT[kp]], writes=[PB[bd]])
                pend.append(tail)
                if len(pend) > SKEW:
                    pend.pop(0)()
                yield
            while pend:
                pend.pop(0)()
            S.op("act", lambda e: e.activation(out=recf[:, :], in_=psum[bd][:, :], func=AF.Ln), reads=[PB[bd]], writes=[RECF])
            S.op("act", lambda e: e.activation(out=recf[:, :], in_=recf[:, :], func=AF.Exp, scale=-1.0), reads=[RECF], writes=[RECF])
            S.op("dve", lambda e: e.tensor_tensor(out=onb[:, :], in0=psum[bo][:, :], in1=recf[:, :], op=ALU.mult),
                 reads=[PB[bo], RECF], writes=[ONB])
            bput(bo); bput(bd)
            bu = bget()
            S.op("pe", lambda e: e.matmul(psum[bu][:, :], lhsT=wuv[:, ch * 128:(ch + 1) * 128], rhs=onb[:, :], start=True, stop=True),
                 reads=[SMB, ONB], writes=[PB[bu]])
            S.op("dve", lambda e: e.tensor_tensor(out=mix[hp:hp + 64, 3 + ch, :], in0=psum[bu][hp:hp + 64, :],
                                                  in1=dg[hp:hp + 64, ch, :], op=ALU.mult), reads=[PB[bu], DG], writes=[HNMIX])
            bput(bu)

        def run_par(*gens):
            gens = list(gens)
            while gens:
                for g in list(gens):
                    try:
                        next(g)
                    except StopIteration:
                        gens.remove(g)

        def seq(*fns):
            for f in fns:
                r = f()
                if r is not None:
                    yield from r
                yield

        run_par(seq(*[lambda i=i: indexer(i) for i in range(4)]),
                seq(*([lambda h=h: sb_head(h) for h in range(5)] + [lambda hm=hm: mem_head(hm) for hm in range(4)])))
        run_par(sb_head(5), seq(lambda: dsa_head(0), lambda: dsa_head(1)))
        wO3 = big16[:].rearrange("p (c n) -> p c n", c=8)
        for c in range(8):
            S.dma(lambda e, c=c: e.dma_start(out=big16[:, c * 1024:(c + 1) * 1024], in_=wsc_d[l, 30 + c, :, :]), sl_big[c],
                  reads=[WSC[l][30 + c]], writes=[BIG[2 * c], BIG[2 * c + 1]])
        for h in range(2, 6):
            run_par(dsa_head(h))

        for i in range(4):
            b0 = bget(); b1 = bget()
            bb = (b0, b1)
            for half in range(2):
                for c in range(8):
                    S.op("pe", lambda e, c=c, half=half, i=i, bb=bb: e.matmul(
                        psum[bb[half]][:, :], lhsT=mix[:, c, i * 128:(i + 1) * 128], rhs=wO3[:, c, half * 512:(half + 1) * 512],
                        start=(c == 0), stop=(c == 7)), reads=[HNMIX, BIG[2 * c + half]], writes=[PB[bb[half]]])
            for half in range(2):
                evac_copy(tmpf[:, half * 512:(half + 1) * 512], psum[bb[half]][:, :], [PB[bb[half]]], [TMPF])
            S.op("act", lambda e: e.activation(out=junk[:, 0:1024], in_=tmpf[:, :], func=AF.Square, accum_out=ssy2),
                 reads=[TMPF], writes=[JUNK, SMS["ssy"]])
            rms_scale(ssy2, rsy, 1, 1.0 / D, SMS["ssy"], SMS["ssy"])
            S.op("dve", lambda e: e.scalar_tensor_tensor(out=tmpf[:, :], in0=tmpf[:, :], scalar=rsy, in1=postg[:, :],
                                                         op0=ALU.mult, op1=ALU.mult),
                 reads=[TMPF, SMS["ssy"], SMF], writes=[TMPF])
            bput(b0); bput(b1)
            S.op("pool", lambda e, i=i: e.tensor_tensor(out=xc3[:, i, :], in0=tmpf[:, :], in1=xc3[:, i, :], op=ALU.add),
                 reads=[TMPF, XC], writes=[XC])
        wr = [OUTB] if dst_x is out_d else [SCRB[b][tq]]
        S.dma(lambda e: e.dma_start(out=dst_x[b, t0:t0 + 512, :].rearrange("(i p) d -> p i d", p=128), in_=xc3),
              sl_o, reads=[XC], writes=wr)

    for u in range(len(layer_of_unit)):
        unit(u)
    S.final_wait("sp", [OUTB, XC])
    S.emit()
    S.close()
    for g in reversed(ctx):
        g.__exit__(None, None, None)
    return nc


def _t5_bucket(rel):
    n = np.maximum(rel, 0)
    max_exact = 16
    nf = np.maximum(n, 1).astype(np.float32)
    large = max_exact + (np.log(nf / max_exact) / math.log(128 / max_exact) * (32 - max_exact)).astype(np.int32)
    large = np.minimum(large, 31)
    return np.where(n < max_exact, n, large)


def _constants():
    bf = ml_dtypes.bfloat16
    cbv = np.zeros((128, NCB), np.float32)
    p = np.arange(128)
    cbv[:, CB_ID:CB_ID + 128] = np.eye(128)
    cbv[:, CB_TRI:CB_TRI + 128] = np.where(p[:, None] >= p[None, :], -8.0, 0.0)
    cbv[:, CB_ONES:CB_ONES + 128] = 1.0
    cbv[:, CB_PENS:CB_PENS + 128] = np.where(p[None, :] < p[:, None], 0.0, NEG)
    cbv[:, CB_PEND:CB_PEND + 128] = np.where(p[None, :] <= p[:, None], 0.0, NEG)
    for kb in range(16):
        cbv[:, CB_EKB + 128] = 1.0
        for jb in range(16):
            if jb > kb:
                cbv[2 * jb, CB_SEL + kb * 128:CB_SEL + (kb + 1) * 128] = -8.0
                cbv[32 + 2 * jb, CB_SEL + kb * 128:CB_SEL + (kb + 1) * 128] = -8.0
    cfv = np.zeros((128, NCF), np.float32)
    cfv[:, CF_NEGB:CF_NEGB + 128] = np.where(p[None, :] <= p[:, None], 0.0, -1e30)
    cfv[:, CF_P2A:CF_P2A + 16] = 2.0 ** -(np.arange(16) + 2.0)
    cfv[:, CF_P2B:CF_P2B + 16] = 2.0 ** -(np.arange(16) + 1.0)
    return cfv, cbv.astype(bf)


def _chunk_cols(w, cols):
    sel = w[:, cols]
    n = sel.shape[1] // 128
    a = sel.reshape(8, 128, n, 128)
    return np.ascontiguousarray(a.transpose(2, 1, 0, 3).reshape(n, 128, 1024))


def _prep_weights(pre_norm_g, post_norm_g, w_in, w_uk, w_uv, kv_norm_g, w_mem_kv, w_out, rel_bias, layers):
    o = np.cumsum([0, 384, 384, 384, 384, 384, 128, 384, 256, 32, 8, 256, 256])
    (o_sbq, o_sbk, o_sbv, o_sbg, o_dq, o_ckv, o_dg, o_iq, o_ik, o_iw, o_mq, o_mg) = o[:12]
    r = lambda a, n: list(range(a, a + n))
    fcols = (r(o_sbq, 384) + r(o_sbk, 384) + r(o_sbg, 384) + r(o_dq, 384) + r(o_dg, 384) + r(o_iq, 256)
             + r(o_ik, 32) * 4 + r(o_mq, 256) + r(o_mg, 256) + r(o_sbv, 384) + r(o_ckv, 128))
    assert len(fcols) == 26 * 128
    s_l = np.arange(128)
    wF, wO, wM, wsm = [], [], [], []
    for l in layers:
        wF.append(_chunk_cols(w_in[l], fcols))
        wO.append(np.ascontiguousarray(w_out[l].reshape(8, 128, 1024)))
        wM.append(_chunk_cols(w_mem_kv[l], list(range(512))))
        sm = np.zeros((128, NSM), np.float32)
        sm[:, SM_GCOL:SM_GCOL + 8] = pre_norm_g[l].reshape(8, 128).T
        sm[:, SM_IW:SM_IW + 64] = w_in[l][:, o_iw:o_iw + 8].reshape(8, 128, 8).transpose(1, 0, 2).reshape(128, 64)
        uk = w_uk[l]
        t = uk.reshape(128, 3, 2, 64).transpose(2, 3, 1, 0).reshape(128, 3 * 128)
        sm[:, SM_WUK:SM_WUK + 384] = t
        sm[:, SM_WUV:SM_WUV + 384] = w_uv[l].reshape(128, 384)
        sm[:, SM_KVG:SM_KVG + 128] = kv_norm_g[l][None, :]
        sm[:, SM_B31:SM_B31 + 6] = rel_bias[31][None, :]
        sm[:, SM_POSTG:SM_POSTG + 1024] = post_norm_g[l][None, :]
        for h in range(6):
            for j in range(2):
                rel = s_l[None, :] - s_l[:, None] + 128 * j
                sm[:, SM_TB + (h * 2 + j) * 128:SM_TB + (h * 2 + j + 1) * 128] = rel_bias[_t5_bucket(rel), h]
        wsm.append(sm)
    return (np.stack(wF), np.stack(wO), np.stack(wM), np.stack(wsm))


_PROG_CACHE = {}


def _get_prog(key, *args):
    if key not in _PROG_CACHE:
        _PROG_CACHE[key] = build_program(*args)
    return _PROG_CACHE[key]


FUSED = True


def kernel(x, mem, pre_norm_g, post_norm_g, w_in, w_uk, w_uv, kv_norm_g, w_mem_kv, w_out, rel_bias):
    x = np.asarray(x, np.float32)
    mem = np.asarray(mem, np.float32)
    args = [np.asarray(a, np.float32) for a in (pre_norm_g, post_norm_g, w_in, w_uk, w_uv, kv_norm_g, w_mem_kv, w_out, rel_bias)]
    B, S_TOK, _ = x.shape
    depth = w_in.shape[0]
    per = B // N_CORES
    cfv, cbv = _constants()
    if FUSED:
        units_l = []; units_b = []; chain = []
        for b in range(per):
            for l in range(depth):
                units_l.append(l); units_b.append(b); chain.append("in" if l == 0 else "scr")
        nc = _get_prog(("fused", S_TOK, per, depth), S_TOK, per, units_l, units_b, chain)
        wF, wO, wM, wsm = _prep_weights(*args, layers=list(range(depth)))
        in_maps = [{"x": np.ascontiguousarray(x[c * per:(c + 1) * per]), "mem": np.ascontiguousarray(mem[c * per:(c + 1) * per]),
                    "wF": wF, "wO": wO, "wM": wM, "wsm": wsm, "cf32": cfv, "cbf": cbv} for c in range(N_CORES)]
        res = run_bass_kernel_spmd(nc, in_maps, core_ids=list(range(N_CORES)))
        return np.concatenate([r["out"] for r in res.results], axis=0)
    cur = x
    nc = _get_prog(("unit", S_TOK), S_TOK, 1, [0], [0], ["in"])
    for l in range(depth):
        wF, wO, wM, wsm = _prep_weights(*args, layers=[l])
        nxt = np.empty_like(cur)
        for b in range(per):
            in_maps = [{"x": np.ascontiguousarray(cur[c * per + b:c * per + b + 1]),
                        "mem": np.ascontiguousarray(mem[c * per + b:c * per + b + 1]),
                        "wF": wF, "wO": wO, "wM": wM, "wsm": wsm, "cf32": cfv, "cbf": cbv} for c in range(N_CORES)]
            res = run_bass_kernel_spmd(nc, in_maps, core_ids=list(range(N_CORES)))
            for c in range(N_CORES):
                nxt[c * per + b] = res.results[c]["out"][0]
        cur = nxt
    return cur
```
